# Optimizing a Trainium2 kernel written in Bass

```python
import jax, jax.numpy as jnp
from jax import lax
import numpy as np

D_MODEL = 1024
BATCH = 4
SEQ = 8192
DEPTH = 2

D_MIX = D_MODEL
CONV_DIM = D_MIX // 4
CONV_K = 3
MLA_HEADS = 8
MLA_NOPE = 64
MLA_ROPE = 32
MLA_V = 64
MLA_Q_RANK = 384
MLA_KV_RANK = 256
LRU_DIM = D_MIX // 4
LRU_BLOCKS = 4
LRU_BW = LRU_DIM // LRU_BLOCKS
LRU_CONV_K = 4
LRU_C = 8.0
ROPE_THETA = 10000.0
Q_BLOCK = 128
D_FF = 3584
N_EXPERTS = 8
TOP_K = 2
N_DENSE = (DEPTH + 1) // 2
N_MOE = DEPTH // 2
DN_ALPHA = (2.0 * DEPTH) ** 0.25
DN_BETA = (8.0 * DEPTH) ** -0.25
LN_EPS = 1e-5
RMS_EPS = 1e-6

IN_SPLITS = (CONV_DIM, CONV_DIM, CONV_DIM, MLA_Q_RANK, MLA_KV_RANK, MLA_ROPE, LRU_DIM, LRU_DIM)
D_IN = sum(IN_SPLITS)
MIX_SPLITS = (CONV_DIM, MLA_HEADS * MLA_V, LRU_DIM)

kernel_name = "hymba_style_conv_mla_rglru_moe_encoder"


def _offsets(sizes):
    return np.cumsum(sizes)[:-1].tolist()


def layer_norm(x, g, b):
    xf = x.astype(jnp.float32)
    mu = jnp.mean(xf, axis=-1, keepdims=True)
    xc = xf - mu
    var = jnp.mean(xc * xc, axis=-1, keepdims=True)
    return (xc * lax.rsqrt(var + LN_EPS) * g + b).astype(x.dtype)


def rms_norm(x, g):
    xf = x.astype(jnp.float32)
    ms = jnp.mean(xf * xf, axis=-1, keepdims=True)
    return (xf * lax.rsqrt(ms + RMS_EPS) * g).astype(x.dtype)


def depthwise_conv(x, w, pad):
    c = x.shape[-1]
    return lax.conv_general_dilated(
        x, w[:, None, :].astype(x.dtype), window_strides=(1,), padding=[pad],
        dimension_numbers=("NWC", "WIO", "NWC"), feature_group_count=c)


def rope_tables(seq):
    pos = jnp.arange(seq, dtype=jnp.float32)
    inv = ROPE_THETA ** (-jnp.arange(0, MLA_ROPE, 2, dtype=jnp.float32) / MLA_ROPE)
    ang = pos[:, None] * inv[None, :]
    return jnp.cos(ang), jnp.sin(ang)


def apply_rope(x, cos, sin):
    xf = x.astype(jnp.float32)
    x1, x2 = jnp.split(xf, 2, axis=-1)
    return jnp.concatenate([x1 * cos - x2 * sin, x2 * cos + x1 * sin], axis=-1).astype(x.dtype)


def short_conv_mixer(b_gate, c_gate, h, w_conv):
    return b_gate * depthwise_conv(c_gate * h, w_conv, (1, 1))


def mla_mixer(c_q, c_kv, k_r, q_norm_g, w_uq, kv_norm_g, w_ukv, cos, sin):
    b, s, _ = c_q.shape
    q = jnp.einsum('bsr,re->bse', rms_norm(c_q, q_norm_g), w_uq).reshape(b, s, MLA_HEADS, MLA_NOPE + MLA_ROPE)
    kv = jnp.einsum('bsr,re->bse', rms_norm(c_kv, kv_norm_g), w_ukv).reshape(b, s, MLA_HEADS, MLA_NOPE + MLA_V)
    scale = (MLA_NOPE + MLA_ROPE) ** -0.5
    q_nope = q[..., :MLA_NOPE] * scale
    q_rope = apply_rope(q[..., MLA_NOPE:], cos[:, None, :], sin[:, None, :]) * scale
    k_nope, v = kv[..., :MLA_NOPE], kv[..., MLA_NOPE:]
    k_rope = apply_rope(k_r, cos, sin)
    nb = s // Q_BLOCK
    qn = q_nope.reshape(b, nb, Q_BLOCK, MLA_HEADS, MLA_NOPE).transpose(1, 0, 2, 3, 4)
    qr = q_rope.reshape(b, nb, Q_BLOCK, MLA_HEADS, MLA_ROPE).transpose(1, 0, 2, 3, 4)

    def attend(blk):
        qn_b, qr_b = blk
        sc = (jnp.einsum('bqhd,bkhd->bhqk', qn_b, k_nope, preferred_element_type=jnp.float32)
              + jnp.einsum('bqhr,bkr->bhqk', qr_b, k_rope, preferred_element_type=jnp.float32))
        p = jax.nn.softmax(sc, axis=-1).astype(v.dtype)
        return jnp.einsum('bhqk,bkhd->bqhd', p, v)

    o = lax.map(attend, (qn, qr))
    return o.transpose(1, 0, 2, 3, 4).reshape(b, s, MLA_HEADS * MLA_V)


def rglru_scan(x, w_a, b_a, w_i, b_i, lam, reverse):
    b, s, _ = x.shape
    xb = x.reshape(b, s, LRU_BLOCKS, LRU_BW)
    ga = jnp.einsum('bsnc,ncd->bsnd', xb, w_a).reshape(b, s, LRU_DIM) + b_a
    gi = jnp.einsum('bsnc,ncd->bsnd', xb, w_i).reshape(b, s, LRU_DIM) + b_i
    rec = jax.nn.sigmoid(ga.astype(jnp.float32))
    inp = jax.nn.sigmoid(gi.astype(jnp.float32))
    log_a = -LRU_C * rec * jax.nn.softplus(-lam.astype(jnp.float32))
    a = jnp.exp(log_a)
    u = jnp.sqrt(-jnp.expm1(2.0 * log_a)) * (inp * x.astype(jnp.float32))

    def combine(left, right):
        a1, b1 = left
        a2, b2 = right
        return a1 * a2, a2 * b1 + b2

    _, h = lax.associative_scan(combine, (a, u), axis=1, reverse=reverse)
    return h


def griffin_recurrent(g_in, x_in, conv_w, conv_b, w_a, b_a, w_i, b_i, lam):
    xc = depthwise_conv(x_in, conv_w, (1, 2)) + conv_b
    h = (rglru_scan(xc, w_a[0], b_a[0], w_i[0], b_i[0], lam[0], False)
         + rglru_scan(xc, w_a[1], b_a[1], w_i[1], b_i[1], lam[1], True))
    return (jax.nn.gelu(g_in.astype(jnp.float32), approximate=True) * h).astype(g_in.dtype)


def hybrid_mixer(x, w_in, conv_w, q_norm_g, w_uq, kv_norm_g, w_ukv, lru_conv_w, lru_conv_b,
                 lru_wa, lru_ba, lru_wi, lru_bi, lru_lam, mix_norm_g, w_out, cos, sin):
    z = jnp.einsum('bsd,de->bse', x, w_in)
    cb, cc, ch, c_q, c_kv, k_r, lg, lx = jnp.split(z, _offsets(IN_SPLITS), axis=-1)
    y_conv = short_conv_mixer(cb, cc, ch, conv_w)
    y_mla = mla_mixer(c_q, c_kv, k_r, q_norm_g, w_uq, kv_norm_g, w_ukv, cos, sin)
    y_lru = griffin_recurrent(lg, lx, lru_conv_w, lru_conv_b, lru_wa, lru_ba, lru_wi, lru_bi, lru_lam)
    g_conv, g_mla, g_lru = jnp.split(mix_norm_g, _offsets(MIX_SPLITS))
    y = jnp.concatenate([rms_norm(y_conv, g_conv), rms_norm(y_mla, g_mla), rms_norm(y_lru, g_lru)], axis=-1)
    return jnp.einsum('bse,ed->bsd', y, w_out)


def swiglu(x, w_gate, w_up, w_down):
    h = jax.nn.silu(x @ w_gate) * (x @ w_up)
    return h @ w_down


def moe_swiglu(x, w_router, w_gate, w_up, w_down):
    b, s, d = x.shape
    xt = x.reshape(b * s, d)
    logits = (xt @ w_router).astype(jnp.float32)
    top_v, top_i = lax.top_k(logits, TOP_K)
    gates = jax.nn.softmax(top_v, axis=-1)
    combine = jnp.einsum('nk,nke->ne', gates, jax.nn.one_hot(top_i, N_EXPERTS, dtype=jnp.float32))
    out = jnp.zeros((b * s, d), jnp.float32)
    for e in range(N_EXPERTS):
        out = out + combine[:, e:e + 1] * swiglu(xt, w_gate[e], w_up[e], w_down[e]).astype(jnp.float32)
    return out.astype(x.dtype).reshape(b, s, d)


def setup_inputs(seed: int = 0) -> dict:
    key = jax.random.key(seed)
    ks = iter(jax.random.split(key, 32))

    def nrm(shape, scale):
        return scale * jax.random.normal(next(ks), shape, jnp.float32)

    def gain(shape):
        return 1.0 + nrm(shape, 0.02)

    x = nrm((BATCH, SEQ, D_MODEL), 1.0)
    ln_in_g = gain((D_MODEL,))
    ln_in_b = nrm((D_MODEL,), 0.02)
    w_in = nrm((DEPTH, D_MODEL, D_IN), D_MODEL ** -0.5)
    conv_w = nrm((DEPTH, CONV_K, CONV_DIM), CONV_K ** -0.5)
    q_norm_g = gain((DEPTH, MLA_Q_RANK))
    w_uq = nrm((DEPTH, MLA_Q_RANK, MLA_HEADS * (MLA_NOPE + MLA_ROPE)), MLA_Q_RANK ** -0.5)
    kv_norm_g = gain((DEPTH, MLA_KV_RANK))
    w_ukv = nrm((DEPTH, MLA_KV_RANK, MLA_HEADS * (MLA_NOPE + MLA_V)), MLA_KV_RANK ** -0.5)
    lru_conv_w = nrm((DEPTH, LRU_CONV_K, LRU_DIM), LRU_CONV_K ** -0.5)
    lru_conv_b = nrm((DEPTH, LRU_DIM), 0.02)
    lru_wa = nrm((DEPTH, 2, LRU_BLOCKS, LRU_BW, LRU_BW), LRU_BW ** -0.5)
    lru_ba = nrm((DEPTH, 2, LRU_DIM), 0.02)
    lru_wi = nrm((DEPTH, 2, LRU_BLOCKS, LRU_BW, LRU_BW), LRU_BW ** -0.5)
    lru_bi = nrm((DEPTH, 2, LRU_DIM), 0.02)
    u = jax.random.uniform(next(ks), (DEPTH, 2, LRU_DIM), jnp.float32, minval=0.9, maxval=0.999)
    a0 = u ** (1.0 / LRU_C)
    lru_lam = jnp.log(a0) - jnp.log1p(-a0)
    mix_norm_g = gain((DEPTH, D_MIX))
    w_out = nrm((DEPTH, D_MIX, D_MODEL), DN_BETA * D_MIX ** -0.5)
    ln1_g = gain((DEPTH, D_MODEL))
    ln1_b = nrm((DEPTH, D_MODEL), 0.02)
    dense_w_gate = nrm((N_DENSE, D_MODEL, D_FF), D_MODEL ** -0.5)
    dense_w_up = nrm((N_DENSE, D_MODEL, D_FF), D_MODEL ** -0.5)
    dense_w_down = nrm((N_DENSE, D_FF, D_MODEL), DN_BETA * D_FF ** -0.5)
    moe_w_router = nrm((N_MOE, D_MODEL, N_EXPERTS), D_MODEL ** -0.5)
    moe_w_gate = nrm((N_MOE, N_EXPERTS, D_MODEL, D_FF), D_MODEL ** -0.5)
    moe_w_up = nrm((N_MOE, N_EXPERTS, D_MODEL, D_FF), D_MODEL ** -0.5)
    moe_w_down = nrm((N_MOE, N_EXPERTS, D_FF, D_MODEL), DN_BETA * D_FF ** -0.5)
    ln2_g = gain((DEPTH, D_MODEL))
    ln2_b = nrm((DEPTH, D_MODEL), 0.02)
    return {"x": x, "ln_in_g": ln_in_g, "ln_in_b": ln_in_b, "w_in": w_in, "conv_w": conv_w,
            "q_norm_g": q_norm_g, "w_uq": w_uq, "kv_norm_g": kv_norm_g, "w_ukv": w_ukv,
            "lru_conv_w": lru_conv_w, "lru_conv_b": lru_conv_b, "lru_wa": lru_wa, "lru_ba": lru_ba,
            "lru_wi": lru_wi, "lru_bi": lru_bi, "lru_lam": lru_lam, "mix_norm_g": mix_norm_g,
            "w_out": w_out, "ln1_g": ln1_g, "ln1_b": ln1_b, "dense_w_gate": dense_w_gate,
            "dense_w_up": dense_w_up, "dense_w_down": dense_w_down, "moe_w_router": moe_w_router,
            "moe_w_gate": moe_w_gate, "moe_w_up": moe_w_up, "moe_w_down": moe_w_down,
            "ln2_g": ln2_g, "ln2_b": ln2_b}


def reference(x, ln_in_g, ln_in_b, w_in, conv_w, q_norm_g, w_uq, kv_norm_g, w_ukv,
              lru_conv_w, lru_conv_b, lru_wa, lru_ba, lru_wi, lru_bi, lru_lam, mix_norm_g,
              w_out, ln1_g, ln1_b, dense_w_gate, dense_w_up, dense_w_down, moe_w_router,
              moe_w_gate, moe_w_up, moe_w_down, ln2_g, ln2_b):
    cos, sin = rope_tables(x.shape[1])
    x = layer_norm(x, ln_in_g, ln_in_b)
    for l in range(DEPTH):
        mix = hybrid_mixer(x, w_in[l], conv_w[l], q_norm_g[l], w_uq[l], kv_norm_g[l], w_ukv[l],
                           lru_conv_w[l], lru_conv_b[l], lru_wa[l], lru_ba[l], lru_wi[l], lru_bi[l],
                           lru_lam[l], mix_norm_g[l], w_out[l], cos, sin)
        x = layer_norm(DN_ALPHA * x + mix, ln1_g[l], ln1_b[l])
        if l % 2 == 0:
            j = l // 2
            f = swiglu(x, dense_w_gate[j], dense_w_up[j], dense_w_down[j])
        else:
            j = l // 2
            f = moe_swiglu(x, moe_w_router[j], moe_w_gate[j], moe_w_up[j], moe_w_down[j])
        x = layer_norm(DN_ALPHA * x + f, ln2_g[l], ln2_b[l])
    return x
```

```python
import numpy as np
from contextlib import ExitStack
import concourse.bass as bass
import concourse.mybir as mybir
from concourse.bass_utils import run_bass_kernel_spmd

F32 = mybir.dt.float32
BF16 = mybir.dt.bfloat16
AF = mybir.ActivationFunctionType
ALU = mybir.AluOpType

D = 1024
DIN = 1952
NH = 8
DFF = 3584
NE = 8
ALPHA = 4.0 ** 0.25
LN_EPS = 1e-5
RMS_EPS = 1e-6
QSCALE = 96.0 ** -0.5
import os as _os
LNSTOP = int(_os.environ.get("MK_LNSTOP", "99"))
ASTOP = int(_os.environ.get("MK_ASTOP", "99"))


class Dep:
    __slots__ = ("name", "w", "r", "dsem", "dcnt", "excl")

    def __init__(self, name=""):
        self.name = name
        self.excl = any(t in name for t in ("_ps", "_pS", "_pO", "_pD", "_prr", "_pg", "_pu", "_pd", "_pc", "pT"))
        self.w = None
        self.r = []
        self.dsem = None
        self.dcnt = 0


class _Rec:
    def __init__(self):
        self.call = None

    def __getattr__(self, name):
        def f(*a, **k):
            self.call = (name, a, k)
            return self
        return f


class Prog:
    ENGS = ("pe", "act", "dve", "pool", "sp")

    def __init__(self, nc):
        self.nc = nc
        self.streams = {e: [] for e in self.ENGS}
        self.sem = {e: nc.alloc_semaphore("es_" + e) for e in self.ENGS}
        self.cnt = {e: 0 for e in self.ENGS}
        self.seen = {e: {} for e in self.ENGS}
        self.deps = {}
        self.ndsem = 0
        self.store_q = "act"

    def dep(self, name):
        d = self.deps.get(name)
        if d is None:
            d = Dep(name)
            self.deps[name] = d
        return d

    def _dsem(self, d):
        if d.dsem is None:
            d.dsem = self.nc.alloc_semaphore("ds_%d" % self.ndsem)
            self.ndsem += 1
        return d.dsem

    def _collect(self, eng, reads, writes):
        need = {}

        def add(t):
            if t is None:
                return
            sem, val = t
            k = id(sem)
            if k not in need or need[k][1] < val:
                need[k] = (sem, val)
        own_sem = self.sem[eng]
        for d in reads:
            add(d.w)
            if d.excl:
                for t in d.r:
                    if t[0] is not own_sem:
                        add(t)
        for d in writes:
            add(d.w)
            for t in d.r:
                add(t)
        out = []
        seen = self.seen[eng]
        own = self.sem[eng]
        for k, (sem, val) in need.items():
            if sem is own and (eng == "pe" or val > self.cnt[eng]):
                continue
            if seen.get(k, 0) >= val:
                continue
            seen[k] = val
            out.append((sem, val))
        return out

    def _update(self, tk, reads, writes):
        for d in reads:
            d.r.append(tk)
            if len(d.r) > 64:
                d.r = d.r[-48:]
        for d in writes:
            d.w = tk
            d.r = []

    def op(self, eng, fn, reads=(), writes=(), signal=True):
        waits = self._collect(eng, reads, writes)
        sem = self.sem[eng]
        if signal:
            self.cnt[eng] += 1
            tk = (sem, self.cnt[eng])
        else:
            tk = (sem, self.cnt[eng] + 1)

        rec = _Rec()
        fn(rec)
        call = rec.call

        def emit(e, call=call, waits=waits, signal=signal, sem=sem):
            for s, v in waits:
                e.wait_ge(s, v)
            ins = getattr(e, call[0])(*call[1], **call[2])
            if signal:
                ins.then_inc(sem, 1)
        self.streams[eng].append(emit)
        self._update(tk, reads, writes)
        return tk

    def dma(self, q, out, in_, reads=(), writes=(), **kw):
        if q == "sp" and self.store_q is not None and type(out.tensor).__name__.startswith("DRam") \
                and not type(in_.tensor).__name__.startswith("DRam"):
            q = self.store_q
        waits = self._collect(q, reads, writes)
        d0 = writes[0]
        sem = self._dsem(d0)
        d0.dcnt += 16
        tk = (sem, d0.dcnt)

        def emit(e, waits=waits, sem=sem, out=out, in_=in_, kw=kw):
            for s, v in waits:
                e.wait_ge(s, v)
            e.dma_start(out=out, in_=in_, **kw).then_inc(sem, 16)
        self.streams[q].append(emit)
        self._update(tk, reads, writes)
        return tk

    def idma(self, out, out_idx, in_, in_idx, reads=(), writes=(), **kw):
        q = "pool"
        waits = self._collect(q, reads, writes)
        d0 = writes[0]
        sem = self._dsem(d0)
        d0.dcnt += 16
        tk = (sem, d0.dcnt)

        def emit(e, waits=waits, sem=sem):
            for s_, v in waits:
                e.wait_ge(s_, v)
            oo = bass.IndirectOffsetOnAxis(ap=out_idx, axis=0) if out_idx is not None else None
            io = bass.IndirectOffsetOnAxis(ap=in_idx, axis=0) if in_idx is not None else None
            e.indirect_dma_start(out=out, out_offset=oo, in_=in_, in_offset=io, **kw).then_inc(sem, 16)
        self.streams[q].append(emit)
        self._update(tk, reads, writes)
        return tk

    def barrier(self):
        tks = [(self.sem[e], self.cnt[e]) for e in self.ENGS if self.cnt[e] > 0]
        for d in self.deps.values():
            if d.dsem is not None and d.dcnt > 0:
                tks.append((d.dsem, d.dcnt))
        for e in self.ENGS:
            seen = self.seen[e]
            waits = []
            for sem, val in tks:
                if sem is self.sem[e]:
                    continue
                if seen.get(id(sem), 0) >= val:
                    continue
                seen[id(sem)] = val
                waits.append((sem, val))

            def emit(en, waits=waits):
                for s, v in waits:
                    en.wait_ge(s, v)
            self.streams[e].append(emit)
        for d in self.deps.values():
            d.w = None
            d.r = []

    def finish(self):
        nc = self.nc
        st = self.streams
        with nc.Block() as block:
            @block.tensor
            def _(e):
                for f in st["pe"]:
                    f(e)

            @block.scalar
            def _(e):
                for f in st["act"]:
                    f(e)

            @block.vector
            def _(e):
                for f in st["dve"]:
                    f(e)

            @block.gpsimd
            def _(e):
                for f in st["pool"]:
                    f(e)

            @block.sync
            def _(e):
                for f in st["sp"]:
                    f(e)
        return nc


class Builder:
    def __init__(self, S, layers, do_ln_in, final_out, dbg=()):
        self.S = S
        self.T = S // 2
        self.NB = self.T // 512
        self.layers = layers
        self.dbg = set(dbg)
        nc = bass.Bass("TRN2", target_bir_lowering=False)
        self.nc = nc
        self.P = Prog(nc)
        self.do_ln_in = do_ln_in
        self.final_out = final_out
        self.cnt = 0
        self.ps_rr = 0
        self.has_other = True

    def din(self, name, shape, dt=F32):
        return self.nc.dram_tensor(name, list(shape), dt, kind="ExternalInput").ap()

    def dout(self, name, shape, dt=F32):
        return self.nc.dram_tensor(name, list(shape), dt, kind="ExternalOutput").ap()

    def dscr(self, name, shape, dt):
        kind = "ExternalOutput" if name in self.dbg else "Internal"
        return self.nc.dram_tensor(name, list(shape), dt, kind=kind).ap()

    def sb(self, st, name, shape, dt):
        self.cnt += 1
        return st.enter_context(self.nc.sbuf_tensor("%s_%d" % (name, self.cnt), list(shape), dt))

    def psum(self, st, name, shape, dt=F32):
        self.cnt += 1
        return st.enter_context(self.nc.psum_tensor("%s_%d" % (name, self.cnt), list(shape), dt))

    def load_cols(self, st, name, vec_ap, n, q="sp"):
        P = self.P
        nchunk = n // 128
        t = self.sb(st, name, [128, nchunk], F32)
        d = P.dep(name)
        v2 = vec_ap.rearrange("(c p o) -> c p o", p=128, o=1)
        for c in range(nchunk):
            P.dma(q, t[:, c:c + 1], v2[c], writes=[d])
        return t, d

    def build(self):
        nc, P, T, NB = self.nc, self.P, self.T, self.NB
        S2 = 2 * T
        I = {}
        if self.do_ln_in:
            I["x_own"] = self.din("x_own", [T, D])
            I["x_oth"] = self.din("x_oth", [T, D])
            I["ln_in_g"] = self.din("ln_in_g", [D])
            I["ln_in_b"] = self.din("ln_in_b", [D])
        else:
            I["xres_in"] = self.din("xres_in", [T, D])
            I["xT_in"] = self.din("xT_in", [D, S2], BF16)
        I["flags"] = self.din("flags", [128, 2])
        I["ropeC"] = self.din("ropeC", [32, S2])
        I["ropeS"] = self.din("ropeS", [32, S2])
        nl = 2
        shapes = dict(w_in=[nl, D, DIN], conv_w=[nl, 3, 256], q_norm_g=[nl, 384], w_uq=[nl, 384, 768],
                      kv_norm_g=[nl, 256], w_ukv=[nl, 256, 1024], lru_conv_w=[nl, 4, 256],
                      lru_conv_b=[nl, 256], lru_wa=[nl, 2, 4, 64, 64], lru_ba=[nl, 2, 256],
                      lru_wi=[nl, 2, 4, 64, 64], lru_bi=[nl, 2, 256], lru_lam=[nl, 2, 256],
                      mix_norm_g=[nl, D], w_out=[nl, D, D], ln1_g=[nl, D], ln1_b=[nl, D],
                      dense_w_gate=[1, D, DFF], dense_w_up=[1, D, DFF], dense_w_down=[1, DFF, D],
                      moe_w_router=[1, D, NE], moe_w_gate=[1, NE, D, DFF], moe_w_up=[1, NE, D, DFF],
                      moe_w_down=[1, NE, DFF, D], ln2_g=[nl, D], ln2_b=[nl, D])
        need_dense = 0 in self.layers
        need_moe = 1 in self.layers
        for k, shp in shapes.items():
            if k.startswith("dense") and not need_dense:
                continue
            if k.startswith("moe") and not need_moe:
                continue
            I[k] = self.din(k, shp)
        self.I = I
        if self.final_out:
            self.y_out = self.dout("y_out", [T, D])
        else:
            self.y_out = self.dout("xres_out", [T, D])
            self.xT_out = self.dout("xT_out", [D, T], BF16)
        self.xres = self.dscr("s_xres", [T, D], F32)
        self.xT = self.dscr("s_xT", [D, S2], BF16)
        self.QT = self.dscr("s_QT", [NH, 96, T], BF16)
        self.KT = self.dscr("s_KT", [NH, 96, S2], BF16)
        self.Vd = self.dscr("s_V", [S2, NH * 65], BF16)
        self.CB = self.dscr("s_CB", [256, T], F32)
        self.PP = self.dscr("s_PP", [256, S2], F32)
        self.LG = self.dscr("s_LG", [256, T], F32)
        self.LX = self.dscr("s_LX", [256, S2], F32)
        self.Y = self.dscr("s_Y", [D, T], F32)
        self.x1res = self.dscr("s_x1res", [T, D], F32)
        self.x1T = self.dscr("s_x1T", [D, T], BF16)
        self.comb = self.dscr("s_comb", [T, NE], F32)
        self.wb = {}
        for l in self.layers:
            self.wb[("w_in", l)] = self.dscr("b_w_in%d" % l, [D, DIN], BF16)
            self.wb[("w_out", l)] = self.dscr("b_w_out%d" % l, [D, D], BF16)
        if need_dense:
            self.wb["dg"] = self.dscr("b_dg", [D, DFF], BF16)
            self.wb["du"] = self.dscr("b_du", [D, DFF], BF16)
            self.wb["dd"] = self.dscr("b_dd", [DFF, D], BF16)
        if need_moe:
            self.wb["mg"] = self.dscr("b_mg", [NE, D, DFF], BF16)
            self.wb["mu"] = self.dscr("b_mu", [NE, D, DFF], BF16)
            self.wb["md"] = self.dscr("b_md", [NE, DFF, D], BF16)

        with ExitStack() as gst:
            import os
            stop = int(os.environ.get("MK_STOP", "99"))
            self.consts(gst)
            if stop >= 1:
                self.cast_weights()
            if self.do_ln_in:
                self.ln_srcs = ((I["x_own"], 0, True), (I["x_oth"], T, False))
                self.ropeC, self.ropeS = I["ropeC"], I["ropeS"]
                if stop >= 2:
                    self.phase_ln_in()
                xres, xT = self.xres, self.xT
            else:
                self.ropeC, self.ropeS = I["ropeC"], I["ropeS"]
                xres, xT = I["xres_in"], I["xT_in"]
            for li, l in enumerate(self.layers):
                last = li == len(self.layers) - 1
                if stop >= 3:
                    self.phase_a(l, xT)
                if stop >= 4:
                    self.phase_b(l)
                if stop >= 5:
                    self.phase_c(l)
                if stop >= 6:
                    self.phase_d(l, xres)
                if stop < 7:
                    continue
                if last:
                    out_res = self.y_out
                    out_T = None if self.final_out else self.xT_out
                else:
                    out_res, out_T = self.xres, self.xT
                self.phase_e(l, out_res, out_T)
                xres, xT = self.xres, self.xT
            P.barrier()
        P.finish()
        return nc

    def build_fused(self):
        nc, P, S = self.nc, self.P, self.S
        Th = S // 2
        I = {}
        I["x_full"] = self.din("x_full", [S, D])
        I["ln_in_g"] = self.din("ln_in_g", [D])
        I["ln_in_b"] = self.din("ln_in_b", [D])
        I["flags"] = self.din("flags", [128, 2])
        for k in ("ropeC0", "ropeS0", "ropeC1", "ropeS1"):
            I[k] = self.din(k, [32, S])
        nl = 2
        shapes = dict(w_in=[nl, D, DIN], conv_w=[nl, 3, 256], q_norm_g=[nl, 384], w_uq=[nl, 384, 768],
                      kv_norm_g=[nl, 256], w_ukv=[nl, 256, 1024], lru_conv_w=[nl, 4, 256],
                      lru_conv_b=[nl, 256], lru_wa=[nl, 2, 4, 64, 64], lru_ba=[nl, 2, 256],
                      lru_wi=[nl, 2, 4, 64, 64], lru_bi=[nl, 2, 256], lru_lam=[nl, 2, 256],
                      mix_norm_g=[nl, D], w_out=[nl, D, D], ln1_g=[nl, D], ln1_b=[nl, D],
                      dense_w_gate=[1, D, DFF], dense_w_up=[1, D, DFF], dense_w_down=[1, DFF, D],
                      moe_w_router=[1, D, NE], moe_w_gate=[1, NE, D, DFF], moe_w_up=[1, NE, D, DFF],
                      moe_w_down=[1, NE, DFF, D], ln2_g=[nl, D], ln2_b=[nl, D])
        for k, shp in shapes.items():
            I[k] = self.din(k, shp)
        self.I = I
        self.y_out = self.dout("y_out", [Th, D])
        self.xres = self.dscr("s_xres", [S, D], F32)
        self.xT = self.dscr("s_xT", [D, S], BF16)
        self.QT = self.dscr("s_QT", [NH, 96, S], BF16)
        self.KT = self.dscr("s_KT", [NH, 96, S], BF16)
        self.Vd = self.dscr("s_V", [S, NH * 65], BF16)
        self.CB = self.dscr("s_CB", [256, S], F32)
        self.PP = self.dscr("s_PP", [256, S], F32)
        self.LG = self.dscr("s_LG", [256, S], F32)
        self.LX = self.dscr("s_LX", [256, S], F32)
        self.Y = self.dscr("s_Y", [D, S], F32)
        self.x1res = self.dscr("s_x1res", [S, D], F32)
        self.x1T = self.dscr("s_x1T", [D, S], BF16)
        self.comb = self.dscr("s_comb", [S, NE], F32)
        o0res = self.dscr("s_o0res", [S, D], F32)
        o0T = self.dscr("s_o0T", [D, S], BF16)
        self.wb = {}
        for l in (0, 1):
            self.wb[("w_in", l)] = self.dscr("b_w_in%d" % l, [D, DIN], BF16)
            self.wb[("w_out", l)] = self.dscr("b_w_out%d" % l, [D, D], BF16)
        self.wb["dg"] = self.dscr("b_dg", [7 * 128, 8 * 512], BF16)
        self.wb["du"] = self.dscr("b_du", [7 * 128, 8 * 512], BF16)
        self.wb["dd"] = self.dscr("b_dd", [DFF, D], BF16)
        self.WGt = self.dscr("b_WGt", [NE * 7 * 128, 8 * 512], BF16)
        self.WUt = self.dscr("b_WUt", [NE * 7 * 128, 8 * 512], BF16)
        self.WDt = self.dscr("b_WDt", [NE * 4 * 128, 7 * D], BF16)
        self.X1B = self.dscr("s_X1B", [Th, D], BF16)
        ntile = (2 * Th) // 512 + NE
        self.XS = self.dscr("s_XS", [ntile * 512, D], BF16)
        self.YS = self.dscr("s_YS", [ntile * 512, D], F32)
        self.moe_tables = True
        self.layers = [0, 1]
        with ExitStack() as gst:
            self.consts(gst)
            self.cast_weights(part=0)
            import os
            fstop = int(os.environ.get("MK_FUSED_STOP", "99"))
            self.mstop = 99
            steps = []
            def set0():
                self.T, self.NB, self.has_other = S, S // 512, False
                self.ropeC, self.ropeS = I["ropeC0"], I["ropeS0"]
                self.ln_srcs = ((I["x_full"], 0, True),)
            def set1():
                self.T, self.NB, self.has_other = Th, Th // 512, True
            def set1b():
                self.ropeC, self.ropeS = I["ropeC1"], I["ropeS1"]
            steps = [lambda: (set0(), self.phase_ln_in()),
                     lambda: (self.phase_a(0, self.xT), self.cast_weights(part=1)),
                     lambda: self.phase_b(0),
                     lambda: self.phase_c(0),
                     lambda: self.phase_d(0, self.xres),
                     lambda: self.phase_e(0, o0res, o0T),
                     lambda: (set1(), self.phase_sel(o0res, o0T), set1b()),
                     lambda: self.phase_a(1, self.xT),
                     lambda: self.phase_b(1),
                     lambda: self.phase_c(1),
                     lambda: (self.route_setup(gst), self.phase_d(1, self.xres)),
                     lambda: self.phase_r(),
                     lambda: self.phase_e_moe(1, self.y_out)]
            for k, f in enumerate(steps):
                if k < fstop:
                    f()
            P.barrier()
        P.finish()
        return nc

    def phase_sel(self, o0res, o0T):
        P, T, NB = self.P, self.T, self.NB
        P.barrier()
        f0 = self.flags[:, 0:1]
        f1 = self.flags[:, 1:2]
        with ExitStack() as st:
            sb = lambda n, s, d: self.sb(st, n, s, d)
            ra = [sb("ra", [128, D], F32) for _ in range(2)]
            rb = [sb("rb", [128, D], F32) for _ in range(2)]
            dres = P.dep("d_xres")
            for i in range(T // 128):
                a_, da_ = ra[i % 2], P.dep("s_ra%d" % (i % 2))
                b_, db_ = rb[i % 2], P.dep("s_rb%d" % (i % 2))
                P.dma("sp", a_[:], o0res[i * 128:(i + 1) * 128, :], reads=[P.dep("d_outres")], writes=[da_])
                P.dma("sp", b_[:], o0res[T + i * 128:T + (i + 1) * 128, :], reads=[P.dep("d_outres")], writes=[db_])
                P.op("dve", lambda e, a_=a_: e.tensor_scalar(out=a_[:], in0=a_[:], scalar1=f0, scalar2=None, op0=ALU.mult), reads=[da_, self.dconst], writes=[da_])
                P.op("dve", lambda e, a_=a_, b_=b_: e.scalar_tensor_tensor(out=a_[:], in0=b_[:], scalar=f1, in1=a_[:], op0=ALU.mult, op1=ALU.add),
                     reads=[da_, db_, self.dconst], writes=[da_])
                P.dma("sp", self.xres[i * 128:(i + 1) * 128, :], a_[:], reads=[da_], writes=[dres])
            ta = [sb("ta", [128, 8, 512], BF16) for _ in range(2)]
            tb = [sb("tb", [128, 8, 512], BF16) for _ in range(2)]
            to = [sb("to", [128, 8, 512], BF16) for _ in range(2)]
            tt = [sb("tt", [128, 8, 512], BF16) for _ in range(2)]
            ov = o0T.rearrange("(c p) t -> p c t", p=128)
            xv = self.xT.rearrange("(c p) t -> p c t", p=128)
            dxT = P.dep("d_xT")
            for j in range(NB):
                i2 = j % 2
                a_, b_, o_, t_ = ta[i2], tb[i2], to[i2], tt[i2]
                da_, db_, do_, dt_ = (P.dep("s_%s%d" % (n, i2)) for n in ("ta", "tb", "to", "tt"))
                P.dma("sp", a_[:], ov[:, :, j * 512:(j + 1) * 512], reads=[P.dep("d_xT")], writes=[da_])
                P.dma("sp", b_[:], ov[:, :, T + j * 512:T + (j + 1) * 512], reads=[P.dep("d_xT")], writes=[db_])
                P.op("dve", lambda e, o_=o_, a_=a_: e.tensor_scalar(out=o_[:], in0=a_[:], scalar1=f0, scalar2=None, op0=ALU.mult), reads=[da_, self.dconst], writes=[do_])
                P.op("dve", lambda e, o_=o_, b_=b_: e.scalar_tensor_tensor(out=o_[:], in0=b_[:], scalar=f1, in1=o_[:], op0=ALU.mult, op1=ALU.add),
                     reads=[db_, do_, self.dconst], writes=[do_])
                P.op("pool", lambda e, t_=t_, a_=a_: e.tensor_scalar(out=t_[:], in0=a_[:], scalar1=f1, scalar2=None, op0=ALU.mult), reads=[da_, self.dconst], writes=[dt_])
                P.op("dve", lambda e, t_=t_, b_=b_: e.scalar_tensor_tensor(out=t_[:], in0=b_[:], scalar=f0, in1=t_[:], op0=ALU.mult, op1=ALU.add),
                     reads=[db_, dt_, self.dconst], writes=[dt_])
                P.dma("sp", xv[:, :, j * 512:(j + 1) * 512], o_[:], reads=[do_], writes=[dxT])
                P.dma("sp", xv[:, :, T + j * 512:T + (j + 1) * 512], t_[:], reads=[dt_], writes=[dxT])

    def consts(self, st):
        P = self.P
        self.ident = self.sb(st, "ident", [128, 128], BF16)
        self.identf = self.sb(st, "identf", [128, 128], F32)
        self.ones = self.sb(st, "ones", [128, 128], BF16)
        self.eps_ln = self.sb(st, "epsln", [128, 1], F32)
        self.eps_rms = self.sb(st, "epsrms", [128, 1], F32)
        self.flags = self.sb(st, "flags", [128, 2], F32)
        d = P.dep("consts")
        self.dconst = d
        P.op("pool", lambda e: e.memset(self.identf[:], 0.0), writes=[d])
        P.op("pool", lambda e: e.affine_select(out=self.identf[:], in_=self.identf[:], pattern=[[-1, 128]],
                                               compare_op=ALU.not_equal, fill=1.0, base=0,
                                               channel_multiplier=1), reads=[d], writes=[d])
        P.op("dve", lambda e: e.tensor_copy(self.ident[:], self.identf[:]), reads=[d], writes=[d])
        P.op("dve", lambda e: e.memset(self.ones[:], 1.0), writes=[d])
        P.op("dve", lambda e: e.memset(self.eps_ln[:], LN_EPS), writes=[d])
        P.op("dve", lambda e: e.memset(self.eps_rms[:], RMS_EPS), writes=[d])
        P.dma("sp", self.flags[:], self.I["flags"], writes=[d])

    def cast_weights(self, part=None):
        P, I = self.P, self.I
        if not hasattr(self, "dwc"):
            self.dwc = {}
        layers_sel = self.layers if part is None else [part]

        def cast2d(dst, src, rows, cols, key):
            d = P.dep("wc_" + key)
            self.dwc[key] = d
            cstep = cols
            while cstep > 2048:
                cstep //= 2
            rstep = max(1, min(rows, 4096 // (cols // cstep)))
            for r0 in range(0, rows, rstep):
                for c0 in range(0, cols, cstep):
                    P.dma("pool", dst[r0:r0 + rstep, c0:c0 + cstep], src[r0:r0 + rstep, c0:c0 + cstep],
                          writes=[d])
        for l in layers_sel:
            cast2d(self.wb[("w_in", l)], I["w_in"][l], D, DIN, "w_in%d" % l)
            cast2d(self.wb[("w_out", l)], I["w_out"][l], D, D, "w_out%d" % l)
        if 0 in layers_sel and getattr(self, "moe_tables", False):
            for key, tab, src in (("g", self.wb["dg"], I["dense_w_gate"]), ("u", self.wb["du"], I["dense_w_up"])):
                d = P.dep("wc_" + key)
                self.dwc[key] = d
                for g in range(7):
                    P.dma("pool", tab[g * 128:(g + 1) * 128, :].rearrange("p (c n) -> p c n", c=8),
                          src[0][:, g * 512:(g + 1) * 512].rearrange("(c p) n -> p c n", p=128), writes=[d])
        elif 0 in layers_sel:
            cast2d(self.wb["dg"], I["dense_w_gate"][0], D, DFF, "g")
            cast2d(self.wb["du"], I["dense_w_up"][0], D, DFF, "u")
        if 0 in layers_sel:
            cast2d(self.wb["dd"], I["dense_w_down"][0], DFF, D, "d")
        if 1 in layers_sel and getattr(self, "moe_tables", False):
            for key, tab, src in (("mg", self.WGt, I["moe_w_gate"]), ("mu", self.WUt, I["moe_w_up"])):
                d = P.dep("wc_" + key)
                self.dwc[key] = d
                for e in range(NE):
                    for g in range(7):
                        r0 = (e * 7 + g) * 128
                        P.dma("pool", tab[r0:r0 + 128, :].rearrange("p (c n) -> p c n", c=8),
                              src[0, e][:, g * 512:(g + 1) * 512].rearrange("(c p) n -> p c n", p=128), writes=[d])
            d = P.dep("wc_md")
            self.dwc["md"] = d
            for e in range(NE):
                for q in range(4):
                    r0 = (e * 4 + q) * 128
                    P.dma("pool", self.WDt[r0:r0 + 128, :].rearrange("p (f n) -> p f n", f=7),
                          I["moe_w_down"][0, e][q * 896:(q + 1) * 896, :].rearrange("(f p) n -> p f n", p=128), writes=[d])
        elif 1 in layers_sel:
            for e in range(NE):
                cast2d(self.wb["mg"][e], I["moe_w_gate"][0, e], D, DFF, "mg")
                cast2d(self.wb["mu"][e], I["moe_w_up"][0, e], D, DFF, "mu")
                cast2d(self.wb["md"][e], I["moe_w_down"][0, e], DFF, D, "md")

    def ln_setup(self, st, g_ap, b_ap, tag, nbuf=2):
        P = self.P
        L = {}
        L["G"] = self.sb(st, "lnG", [128, D], F32)
        L["B"] = self.sb(st, "lnB", [128, D], F32)
        L["dgb"] = P.dep("lnGB")
        P.dma("sp", L["G"][:], g_ap.partition_broadcast(128), writes=[L["dgb"]])
        P.dma("sp", L["B"][:], b_ap.partition_broadcast(128), writes=[L["dgb"]])
        L["n"] = 0
        L["nbuf"] = nbuf
        for i in range(nbuf):
            L["st%d" % i] = self.sb(st, "lnst", [128, 2, 6], F32)
            L["mv%d" % i] = self.sb(st, "lnmv", [128, 2], F32)
            L["sd%d" % i] = self.sb(st, "lnsd", [128, 1], F32)
            L["rs%d" % i] = self.sb(st, "lnrs", [128, 1], F32)
            L["xn%d" % i] = self.sb(st, "lnxn", [128, D], F32)
            L["xo%d" % i] = self.sb(st, "lnxo", [128, D], F32)
            L["xb%d" % i] = self.sb(st, "lnxb", [128, D], BF16)
            L["pT%d" % i] = self.psum(st, "lnpT", [128, D], BF16)
        return L

    def ln_tile(self, L, xt, dxt, xTs, dxTs, sub, res_dst=None, dres=None, xb_dst=None, dxbd=None):
        P = self.P
        i = L["n"] % L["nbuf"]
        L["n"] += 1
        tg = "ln%d" % i
        st_, mv, sd, rs, xn, xo, xb, pT = (L[k + str(i)] for k in ("st", "mv", "sd", "rs", "xn", "xo", "xb", "pT"))
        dst_, dmv, dsd, drs, dxn, dxo, dxb, dpT = (P.dep(tg + k) for k in ("st", "mv", "sd", "rs", "xn", "xo", "xb", "pT"))
        for h in range(2):
            P.op("dve", lambda e, h=h: e.bn_stats(out=st_[:, h, :], in_=xt[:, h * 512:(h + 1) * 512]),
                 reads=[dxt], writes=[dst_])
        if LNSTOP < 1:
            return
        P.op("dve", lambda e: e.bn_aggr(out=mv[:], in_=st_[:].rearrange("p a b -> p (a b)")), reads=[dst_], writes=[dmv])
        if LNSTOP < 2:
            return
        P.op("act", lambda e: e.activation(out=sd[:], in_=mv[:, 1:2], func=AF.Sqrt, bias=self.eps_ln[:], scale=1.0),
             reads=[dmv, self.dconst], writes=[dsd])
        if LNSTOP < 3:
            return
        P.op("dve", lambda e: e.reciprocal(out=rs[:], in_=sd[:]), reads=[dsd], writes=[drs])
        if LNSTOP < 4:
            return
        P.op("dve", lambda e: e.tensor_scalar(out=xn[:], in0=xt[:], scalar1=mv[:, 0:1], scalar2=rs[:],
                                               op0=ALU.subtract, op1=ALU.mult), reads=[dxt, dmv, drs], writes=[dxn])
        if LNSTOP < 5:
            return
        P.op("pool", lambda e: e.tensor_tensor(out=xn[:], in0=xn[:], in1=L["G"][:], op=ALU.mult),
             reads=[dxn, L["dgb"]], writes=[dxn])
        P.op("pool", lambda e: e.tensor_tensor(out=xo[:], in0=xn[:], in1=L["B"][:], op=ALU.add),
             reads=[dxn, L["dgb"]], writes=[dxo])
        if LNSTOP < 6:
            return
        if res_dst is not None:
            P.dma("sp", res_dst, xo[:], reads=[dxo], writes=[dres])
        if LNSTOP < 7:
            return
        if xTs is None:
            return
        P.op("act", lambda e: e.activation(out=xb[:], in_=xo[:], func=AF.Copy), reads=[dxo], writes=[dxb])
        if xb_dst is not None:
            P.dma("sp", xb_dst, xb[:], reads=[dxb], writes=[dxbd])
        if LNSTOP < 8:
            return
        for c in range(8):
            P.op("pe", lambda e, c=c: e.transpose(pT[:, c * 128:(c + 1) * 128], xb[:, c * 128:(c + 1) * 128], self.ident[:]),
                 reads=[dxb, self.dconst], writes=[dpT], signal=(c == 7))
        if LNSTOP < 9:
            return
        P.op("act", lambda e: e.activation(out=xTs[:, :, sub * 128:(sub + 1) * 128],
                                           in_=pT[:].rearrange("p (c t) -> p c t", c=8), func=AF.Copy),
             reads=[dpT], writes=[dxTs])

    def phase_ln_in(self):
        P, T, NB, I = self.P, self.T, self.NB, self.I
        P.barrier()
        with ExitStack() as st:
            L = self.ln_setup(st, I["ln_in_g"], I["ln_in_b"], "in")
            xt = [self.sb(st, "xt", [128, D], F32) for _ in range(2)]
            xTs = [self.sb(st, "xTs", [128, 8, 512], BF16) for _ in range(2)]
            dres = P.dep("d_xres")
            dxT = P.dep("d_xT")
            k = 0
            for src, base, own in self.ln_srcs:
                for b in range(NB):
                    xs, dxs = xTs[b % 2], P.dep("xTs%d" % (b % 2))
                    for sub in range(4):
                        r0 = b * 512 + sub * 128
                        xtt, dx = xt[k % 2], P.dep("xt%d" % (k % 2))
                        k += 1
                        P.dma("sp", xtt[:], src[r0:r0 + 128, :], writes=[dx])
                        self.ln_tile(L, xtt, dx, xs, dxs, sub,
                                     res_dst=self.xres[r0:r0 + 128, :] if own else None, dres=dres)
                    P.dma("sp", self.xT.rearrange("(c p) t -> p c t", p=128)[:, :, base + b * 512: base + (b + 1) * 512],
                          xs[:], reads=[dxs], writes=[dxT])

    def rms_rstd(self, sq, dsq, nchunk, n, pst, dpst, rstd, drstd, sdt, dsdt):
        P = self.P
        for c in range(nchunk):
            P.op("pe", lambda e, c=c: e.matmul(pst[:], lhsT=self.ones[:], rhs=sq[:, c, :], start=(c == 0), stop=(c == nchunk - 1)),
                 reads=[dsq, self.dconst], writes=[dpst], signal=(c == nchunk - 1))
        P.op("act", lambda e: e.activation(out=sdt[:], in_=pst[:], func=AF.Sqrt, bias=self.eps_rms[:], scale=1.0 / n),
             reads=[dpst, self.dconst], writes=[dsdt])
        P.op("dve", lambda e: e.reciprocal(out=rstd[:], in_=sdt[:]), reads=[dsdt], writes=[drstd])

    def phase_a(self, l, xT):
        P, T, NB, I = self.P, self.T, self.NB, self.I
        NBT = (2 * NB) if self.has_other else NB
        P.barrier()
        with ExitStack() as st:
            sb = lambda n, s, d: self.sb(st, n, s, d)
            Win = sb("Win", [128, 8, DIN], BF16)
            dW = P.dep("a_W")
            wv = self.wb[("w_in", l)].rearrange("(c p) n -> p c n", p=128)
            for c in range(8):
                P.dma("sp", Win[:, c, :], wv[:, c, :], reads=[self.dwc["w_in%d" % l]], writes=[dW])
            Wkr = sb("Wkr", [128, 8, 96], BF16)
            Wkrs = sb("Wkrs", [128, 8, 96], BF16)
            dW2 = P.dep("a_W2")
            P.op("dve", lambda e: e.memset(Wkr[:], 0.0), writes=[dW2])
            P.op("dve", lambda e: e.memset(Wkrs[:], 0.0), writes=[dW2])
            P.op("dve", lambda e: e.tensor_copy(Wkr[:, :, 64:96], Win[:, :, 1408:1440]), reads=[dW], writes=[dW2])
            P.op("dve", lambda e: e.tensor_scalar(out=Wkrs[:, :, 64:80], in0=Win[:, :, 1424:1440], scalar1=-1.0, scalar2=None,
                                                   op0=ALU.mult), reads=[dW], writes=[dW2])
            P.op("dve", lambda e: e.tensor_copy(Wkrs[:, :, 80:96], Win[:, :, 1408:1424]), reads=[dW], writes=[dW2])
            gq, dgq = self.load_cols(st, "gq", I["q_norm_g"][l], 384)
            gkv, dgkv = self.load_cols(st, "gkv", I["kv_norm_g"][l], 256)
            wqf = sb("wqf", [128, 3, 768], F32)
            dwqf = P.dep("a_wqf")
            P.dma("sp", wqf[:], I["w_uq"][l].rearrange("(c p) n -> p c n", p=128), writes=[dwqf])
            Wq = sb("Wq", [128, 3, 768], BF16)
            Wqs = sb("Wqs", [128, 3, 768], BF16)
            dWq = P.dep("a_Wq")
            P.op("pool", lambda e: e.memset(Wqs[:], 0.0), writes=[dWq])
            for c in range(3):
                P.op("dve", lambda e, c=c: e.tensor_scalar(out=Wq[:, c, :], in0=wqf[:, c, :], scalar1=gq[:, c:c + 1],
                                                            scalar2=QSCALE, op0=ALU.mult, op1=ALU.mult),
                     reads=[dwqf, dgq], writes=[dWq])
                wq4 = Wq[:, c, :].rearrange("p (h r) -> p h r", h=NH)
                ws4 = Wqs[:, c, :].rearrange("p (h r) -> p h r", h=NH)
                P.op("dve", lambda e, wq4=wq4, ws4=ws4: e.tensor_scalar(out=ws4[:, :, 64:80], in0=wq4[:, :, 80:96], scalar1=-1.0,
                                                                        scalar2=None, op0=ALU.mult), reads=[dWq], writes=[dWq])
                P.op("dve", lambda e, wq4=wq4, ws4=ws4: e.tensor_copy(ws4[:, :, 80:96], wq4[:, :, 64:80]), reads=[dWq], writes=[dWq])
            wkvf = sb("wkvf", [128, 2, 1024], F32)
            dwkvf = P.dep("a_wkvf")
            P.dma("sp", wkvf[:], I["w_ukv"][l].rearrange("(c p) n -> p c n", p=128), writes=[dwkvf])
            Wkn = sb("Wkn", [128, 2, 512], BF16)
            Wv = sb("Wv", [128, 2, 512], BF16)
            dWkv = P.dep("a_Wkv")
            for c in range(2):
                w4 = wkvf[:, c, :].rearrange("p (h r) -> p h r", h=NH)
                P.op("dve", lambda e, c=c, w4=w4: e.tensor_scalar(out=Wkn[:, c, :].rearrange("p (h r) -> p h r", h=NH), in0=w4[:, :, 0:64],
                                                                  scalar1=gkv[:, c:c + 1], scalar2=None, op0=ALU.mult),
                     reads=[dwkvf, dgkv], writes=[dWkv])
                P.op("dve", lambda e, c=c, w4=w4: e.tensor_scalar(out=Wv[:, c, :].rearrange("p (h r) -> p h r", h=NH), in0=w4[:, :, 64:128],
                                                                  scalar1=gkv[:, c:c + 1], scalar2=None, op0=ALU.mult),
                     reads=[dwkvf, dgkv], writes=[dWkv])
            NPS = 8
            ps = [self.psum(st, "aps", [128, 512], F32) for _ in range(NPS)]
            dps = [P.dep("a_ps%d" % i) for i in range(NPS)]

            def nps():
                i = self.ps_rr % NPS
                self.ps_rr += 1
                return ps[i], dps[i]
            xTb = [sb("xTb", [128, 8, 512], BF16) for _ in range(2)]
            NSTG = 6
            stg = [sb("stg", [128, 512], F32) for _ in range(NSTG)]
            self.stg_rr = 0

            def nstg():
                i = self.stg_rr % NSTG
                self.stg_rr += 1
                return stg[i], P.dep("a_stg%d" % i)
            cctmp = [sb("cctmp", [128, 512], F32) for _ in range(2)]
            cq = sb("cq", [128, 3, 512], F32)
            sq = sb("sq", [128, 3, 512], BF16)
            cqn = sb("cqn", [128, 3, 512], BF16)
            ckv = sb("ckv", [128, 2, 512], F32)
            sk = sb("sk", [128, 2, 512], BF16)
            ckvn = sb("ckvn", [128, 2, 512], BF16)
            rstd = sb("rstd", [128, 512], F32)
            sdt = sb("sdt", [128, 512], F32)
            rstd2 = sb("rstd2", [128, 512], F32)
            sdt2 = sb("sdt2", [128, 512], F32)
            ropeC = [sb("ropeC", [128, 512], F32) for _ in range(2)]
            ropeS = [sb("ropeS", [128, 512], F32) for _ in range(2)]
            t1 = [sb("t1", [128, 512], F32) for _ in range(2)]
            t2 = [sb("t2", [128, 512], F32) for _ in range(2)]
            QTs = [sb("QTs", [128, 512], BF16) for _ in range(3)]
            KTs = [sb("KTs", [128, 512], BF16) for _ in range(3)]
            kro = sb("kro", [128, 512], BF16)
            Vs = [sb("Vs", [128, NH, 65], BF16) for _ in range(2)]
            dVs = [P.dep("a_Vs%d" % i) for i in range(2)]
            for i in range(2):
                P.op("pool", lambda e, i=i: e.memset(Vs[i][:], 1.0), writes=[dVs[i]])
            dcq, dsq, dcqn, dckv, dsk, dckvn = (P.dep("a_" + n) for n in ("cq", "sq", "cqn", "ckv", "sk", "ckvn"))
            drstd, dsdt, drstd2, dsdt2, dkro = (P.dep("a_" + n) for n in ("rstd", "sdt", "rstd2", "sdt2", "kro"))
            dQT, dKT, dV, dCB, dPP, dLG, dLX = (P.dep("d_" + n) for n in ("QT", "KT", "V", "CB", "PP", "LG", "LX"))
            xTv = xT.rearrange("(c p) t -> p c t", p=128)
            nq = 0

            def grp(xb, dxb, col0, m, lhs=None, dl=None):
                pt, dpt = nps()
                for c in range(8):
                    if lhs is None:
                        l_ap, dd = Win[:, c, col0:col0 + m], dW
                    else:
                        l_ap, dd = lhs[:, c, :], dl
                    P.op("pe", lambda e, c=c, l_ap=l_ap, pt=pt: e.matmul(pt[0:m, :], lhsT=l_ap, rhs=xb[:, c, :], start=(c == 0), stop=(c == 7)),
                         reads=[dd, dxb], writes=[dpt], signal=(c == 7))
                return pt, dpt

            def store_fm(dst, ddst, row0, col0, pt, dpt, func=AF.Copy):
                s, ds_ = nstg()
                P.op("act", lambda e: e.activation(out=s[:], in_=pt[:], func=func), reads=[dpt], writes=[ds_])
                P.dma("sp", dst[row0:row0 + 128, col0:col0 + 512], s[:], reads=[ds_], writes=[ddst])

            if ASTOP < 1:
                return
            for blk in range(NBT):
                own = blk < NB
                t0 = blk * 512
                xb, dxb = xTb[blk % 2], P.dep("a_xTb%d" % (blk % 2))
                P.dma("sp", xb[:], xTv[:, :, t0:t0 + 512], reads=[P.dep("d_xT")], writes=[dxb])
                rc, rs_, drope = ropeC[blk % 2], ropeS[blk % 2], P.dep("a_rope%d" % (blk % 2))
                P.dma("sp", rc[64:96, :], self.ropeC[:, t0:t0 + 512], writes=[drope])
                P.dma("sp", rs_[64:96, :], self.ropeS[:, t0:t0 + 512], writes=[drope])
                edge = (not own) and (blk == NB or blk == 2 * NB - 1)
                if own or edge:
                    for c in range(2):
                        pcc, dpcc = grp(xb, dxb, 256 + c * 128, 128)
                        pch, dpch = grp(xb, dxb, 512 + c * 128, 128)
                        ct, dct = cctmp[c], P.dep("a_cct%d" % c)
                        P.op("act", lambda e, ct=ct, pcc=pcc: e.activation(out=ct[:], in_=pcc[:], func=AF.Copy), reads=[dpcc], writes=[dct])
                        s, ds_ = nstg()
                        P.op("dve", lambda e, s=s, pch=pch, ct=ct: e.tensor_tensor(out=s[:], in0=pch[:], in1=ct[:], op=ALU.mult),
                             reads=[dpch, dct], writes=[ds_])
                        P.dma("sp", self.PP[c * 128:(c + 1) * 128, t0:t0 + 512], s[:], reads=[ds_], writes=[dPP])
                if ASTOP < 2:
                    continue
                if own:
                    for c in range(2):
                        pt, dpt = grp(xb, dxb, c * 128, 128)
                        store_fm(self.CB, dCB, c * 128, t0, pt, dpt)
                        pt, dpt = grp(xb, dxb, 1440 + c * 128, 128)
                        store_fm(self.LG, dLG, c * 128, t0, pt, dpt, func=AF.Gelu_apprx_tanh)
                    if ASTOP < 3:
                        continue
                    for c in range(3):
                        pt, dpt = grp(xb, dxb, 768 + c * 128, 128)
                        P.op("act", lambda e, c=c, pt=pt: e.activation(out=sq[:, c, :], in_=pt[:], func=AF.Square), reads=[dpt], writes=[dsq])
                        P.op("dve", lambda e, c=c, pt=pt: e.tensor_copy(cq[:, c, :], pt[:]), reads=[dpt], writes=[dcq])
                    pst, dpst = nps()
                    self.rms_rstd(sq, dsq, 3, 384.0, pst, dpst, rstd, drstd, sdt, dsdt)
                    for c in range(3):
                        P.op("dve", lambda e, c=c: e.tensor_tensor(out=cqn[:, c, :], in0=cq[:, c, :], in1=rstd[:], op=ALU.mult),
                             reads=[dcq, drstd], writes=[dcqn])
                    for h in range(NH):
                        pa, dpa = nps()
                        pb, dpb = nps()
                        for c in range(3):
                            P.op("pe", lambda e, c=c, h=h, pa=pa: e.matmul(pa[0:96, :], lhsT=Wq[:, c, h * 96:(h + 1) * 96], rhs=cqn[:, c, :],
                                                                      start=(c == 0), stop=(c == 2)), reads=[dWq, dcqn], writes=[dpa], signal=(c == 2))
                        for c in range(3):
                            P.op("pe", lambda e, c=c, h=h, pb=pb: e.matmul(pb[0:96, :], lhsT=Wqs[:, c, h * 96:(h + 1) * 96], rhs=cqn[:, c, :],
                                                                      start=(c == 0), stop=(c == 2)), reads=[dWq, dcqn], writes=[dpb], signal=(c == 2))
                        qs, dqs = QTs[nq % 3], P.dep("a_QTs%d" % (nq % 3))
                        ta, dta = t1[nq % 2], P.dep("a_t1%d" % (nq % 2))
                        tb, dtb = t2[nq % 2], P.dep("a_t2%d" % (nq % 2))
                        nq += 1
                        P.op("act", lambda e, qs=qs, pa=pa: e.activation(out=qs[0:64, :], in_=pa[0:64, :], func=AF.Copy), reads=[dpa], writes=[dqs])
                        P.op("dve", lambda e, ta=ta, pa=pa: e.tensor_tensor(out=ta[64:96, :], in0=pa[64:96, :], in1=rc[64:96, :], op=ALU.mult),
                             reads=[dpa, drope], writes=[dta])
                        P.op("dve", lambda e, tb=tb, pb=pb: e.tensor_tensor(out=tb[64:96, :], in0=pb[64:96, :], in1=rs_[64:96, :], op=ALU.mult),
                             reads=[dpb, drope], writes=[dtb])
                        P.op("dve", lambda e, qs=qs, ta=ta, tb=tb: e.tensor_tensor(out=qs[64:96, :], in0=ta[64:96, :], in1=tb[64:96, :], op=ALU.add),
                             reads=[dta, dtb], writes=[dqs])
                        P.dma("sp", self.QT[h, :, t0:t0 + 512], qs[0:96, :], reads=[dqs], writes=[dQT])
                if ASTOP < 4:
                    continue
                for c in range(2):
                    pt, dpt = grp(xb, dxb, 1696 + c * 128, 128)
                    store_fm(self.LX, dLX, c * 128, t0, pt, dpt)
                for c in range(2):
                    pt, dpt = grp(xb, dxb, 1152 + c * 128, 128)
                    P.op("act", lambda e, c=c, pt=pt: e.activation(out=sk[:, c, :], in_=pt[:], func=AF.Square), reads=[dpt], writes=[dsk])
                    P.op("dve", lambda e, c=c, pt=pt: e.tensor_copy(ckv[:, c, :], pt[:]), reads=[dpt], writes=[dckv])
                pst, dpst = nps()
                self.rms_rstd(sk, dsk, 2, 256.0, pst, dpst, rstd2, drstd2, sdt2, dsdt2)
                for c in range(2):
                    P.op("dve", lambda e, c=c: e.tensor_tensor(out=ckvn[:, c, :], in0=ckv[:, c, :], in1=rstd2[:], op=ALU.mult),
                         reads=[dckv, drstd2], writes=[dckvn])
                if ASTOP < 5:
                    continue
                pa, dpa = grp(xb, dxb, 0, 96, lhs=Wkr, dl=dW2)
                pb, dpb = grp(xb, dxb, 0, 96, lhs=Wkrs, dl=dW2)
                ta, dta = t1[nq % 2], P.dep("a_t1%d" % (nq % 2))
                tb, dtb = t2[nq % 2], P.dep("a_t2%d" % (nq % 2))
                nq += 1
                P.op("dve", lambda e, ta=ta, pa=pa: e.tensor_tensor(out=ta[64:96, :], in0=pa[64:96, :], in1=rc[64:96, :], op=ALU.mult),
                     reads=[dpa, drope], writes=[dta])
                P.op("dve", lambda e, tb=tb, pb=pb: e.tensor_tensor(out=tb[64:96, :], in0=pb[64:96, :], in1=rs_[64:96, :], op=ALU.mult),
                     reads=[dpb, drope], writes=[dtb])
                P.op("dve", lambda e, ta=ta, tb=tb: e.tensor_tensor(out=kro[64:96, :], in0=ta[64:96, :], in1=tb[64:96, :], op=ALU.add),
                     reads=[dta, dtb], writes=[dkro])
                if ASTOP < 6:
                    continue
                for h in range(NH):
                    pk, dpk = nps()
                    for c in range(2):
                        P.op("pe", lambda e, c=c, h=h, pk=pk: e.matmul(pk[0:64, :], lhsT=Wkn[:, c, h * 64:(h + 1) * 64], rhs=ckvn[:, c, :],
                                                                  start=(c == 0), stop=(c == 1)), reads=[dWkv, dckvn], writes=[dpk], signal=(c == 1))
                    ks, dks = KTs[h % 3], P.dep("a_KTs%d" % (h % 3))
                    P.op("act", lambda e, ks=ks, pk=pk: e.activation(out=ks[0:64, :], in_=pk[0:64, :], func=AF.Copy), reads=[dpk], writes=[dks])
                    P.dma("sp", self.KT[h, 0:64, t0:t0 + 512], ks[0:64, :], reads=[dks], writes=[dKT])
                    P.dma("sp", self.KT[h, 64:96, t0:t0 + 512], kro[64:96, :], reads=[dkro], writes=[dKT])
                if ASTOP < 7:
                    continue
                for sub in range(4):
                    pv, dpv = nps()
                    for c in range(2):
                        P.op("pe", lambda e, c=c, sub=sub, pv=pv: e.matmul(pv[:], lhsT=ckvn[:, c, sub * 128:(sub + 1) * 128], rhs=Wv[:, c, :],
                                                                      start=(c == 0), stop=(c == 1)), reads=[dWkv, dckvn], writes=[dpv], signal=(c == 1))
                    vs, dvs = Vs[sub % 2], dVs[sub % 2]
                    P.op("act", lambda e, vs=vs, pv=pv: e.activation(out=vs[:, :, 0:64], in_=pv[:].rearrange("p (h r) -> p h r", h=NH), func=AF.Copy),
                         reads=[dpv], writes=[dvs])
                    P.dma("sp", self.Vd[t0 + sub * 128:t0 + (sub + 1) * 128, :], vs[:].rearrange("p h r -> p (h r)"), reads=[dvs], writes=[dV])

    def phase_b(self, l):
        P, T, NB, I = self.P, self.T, self.NB, self.I
        P.barrier()
        dY = P.dep("d_Y")
        f0 = self.flags[:, 0:1]
        f1 = self.flags[:, 1:2]
        with ExitStack() as st:
            sb = lambda n, s, d: self.sb(st, n, s, d)
            cw = sb("cw", [128, 2, 3], F32)
            dcw = P.dep("b_cw")
            cv_t = {"pp": sb("pp", [128, T + 2], F32), "cb": sb("cb", [128, T], F32), "acc": sb("acc", [128, T], F32)}
            for k in range(3):
                for c in range(2):
                    P.dma("sp", cw[:, c, k:k + 1], I["conv_w"][l, k, c * 128:(c + 1) * 128].rearrange("(p o) -> p o", o=1), writes=[dcw])
            for c in range(2):
                pp = cv_t["pp"]
                cb = cv_t["cb"]
                acc = cv_t["acc"]
                edge = sb("edge", [128, 2], F32)
                dpp, dcb, dacc = (P.dep("b_%s" % n) for n in ("pp", "cb", "acc"))
                dedge = P.dep("b_edge%d" % c)
                rows = slice(c * 128, (c + 1) * 128)
                P.dma("sp", pp[:, 1:T + 1], self.PP[rows, 0:T], reads=[P.dep("d_PP")], writes=[dpp])
                P.dma("sp", cb[:], self.CB[rows, 0:T], reads=[P.dep("d_CB")], writes=[dcb])
                if self.has_other:
                    P.dma("sp", edge[:, 0:1], self.PP[rows, 2 * T - 1:2 * T], reads=[P.dep("d_PP")], writes=[dedge], allow_slow_non_contiguous=True)
                    P.dma("sp", edge[:, 1:2], self.PP[rows, T:T + 1], reads=[P.dep("d_PP")], writes=[dedge], allow_slow_non_contiguous=True)
                else:
                    P.op("dve", lambda e, edge=edge: e.memset(edge[:], 0.0), writes=[dedge])
                P.op("dve", lambda e, pp=pp, edge=edge: e.tensor_tensor(out=pp[:, 0:1], in0=edge[:, 0:1], in1=f1, op=ALU.mult),
                     reads=[dedge, self.dconst, dpp], writes=[dpp])
                P.op("dve", lambda e, pp=pp, edge=edge: e.tensor_tensor(out=pp[:, T + 1:T + 2], in0=edge[:, 1:2], in1=f0, op=ALU.mult),
                     reads=[dedge, self.dconst, dpp], writes=[dpp])
                P.op("dve", lambda e, acc=acc, pp=pp, c=c: e.tensor_scalar(out=acc[:], in0=pp[:, 0:T], scalar1=cw[:, c, 0:1], scalar2=None, op0=ALU.mult),
                     reads=[dpp, dcw], writes=[dacc])
                for k in (1, 2):
                    P.op("dve", lambda e, acc=acc, pp=pp, c=c, k=k: e.scalar_tensor_tensor(out=acc[:], in0=pp[:, k:k + T], scalar=cw[:, c, k:k + 1], in1=acc[:],
                                                                                     op0=ALU.mult, op1=ALU.add), reads=[dpp, dcw, dacc], writes=[dacc])
                P.op("pool", lambda e, acc=acc, cb=cb: e.tensor_tensor(out=acc[:], in0=acc[:], in1=cb[:], op=ALU.mult), reads=[dacc, dcb], writes=[dacc])
                P.dma("sp", self.Y[rows, 0:T], acc[:], reads=[dacc], writes=[dY])
        P.barrier()
        with ExitStack() as st:
            sb = lambda n, s, d: self.sb(st, n, s, d)
            prm = sb("prm", [128, 2, 16], F32)
            dprm = P.dep("b_prm")

            def col(dst_col, vec):
                for c in range(2):
                    P.dma("sp", prm[:, c, dst_col:dst_col + 1], vec[c * 128:(c + 1) * 128].rearrange("(p o) -> p o", o=1), writes=[dprm])
            for k in range(4):
                col(k, I["lru_conv_w"][l, k])
            col(4, I["lru_conv_b"][l])
            for d_ in range(2):
                col(5 + d_, I["lru_ba"][l, d_])
                col(7 + d_, I["lru_bi"][l, d_])
                col(9 + d_, I["lru_lam"][l, d_])
            sc = sb("sc", [128, 2, 4], F32)
            dsc = P.dep("b_sc")
            for c in range(2):
                P.op("act", lambda e, c=c: e.activation(out=sc[:, c, 0:2], in_=prm[:, c, 9:11], func=AF.Exp, scale=-1.0), reads=[dprm], writes=[dsc])
                P.op("act", lambda e, c=c: e.activation(out=sc[:, c, 0:2], in_=sc[:, c, 0:2], func=AF.Ln, bias=1.0, scale=1.0), reads=[dsc], writes=[dsc])
                P.op("dve", lambda e, c=c: e.tensor_scalar(out=sc[:, c, 2:4], in0=sc[:, c, 0:2], scalar1=-16.0, scalar2=None, op0=ALU.mult), reads=[dsc], writes=[dsc])
                P.op("dve", lambda e, c=c: e.tensor_scalar(out=sc[:, c, 0:2], in0=sc[:, c, 0:2], scalar1=-8.0, scalar2=None, op0=ALU.mult), reads=[dsc], writes=[dsc])
            wgf = sb("wgf", [128, 2, 4, 128], F32)
            wg = sb("wg", [128, 2, 4, 128], BF16)
            dwg = P.dep("b_wg")
            P.op("pool", lambda e: e.memset(wgf[:], 0.0), writes=[dwg])
            for c in range(2):
                for d_ in range(2):
                    for gi, key in enumerate(("lru_wa", "lru_wi")):
                        for bb in range(2):
                            P.dma("sp", wgf[bb * 64:(bb + 1) * 64, c, 2 * d_ + gi, bb * 64:(bb + 1) * 64], I[key][l, d_, 2 * c + bb], writes=[dwg])
            P.op("dve", lambda e: e.tensor_copy(wg[:], wgf[:]), reads=[dwg], writes=[dwg])
            ps = [self.psum(st, "bps", [128, 512], F32) for _ in range(4)]
            dps = [P.dep("b_ps%d" % i) for i in range(4)]
            HO = self.has_other
            lxo = sb("lxo", [128, T + 3], F32)
            lxt = sb("lxt", [128, T + 3], F32) if HO else None
            big = {}
            for nm in (("o", "t") if HO else ("o",)):
                big["xc" + nm] = sb("xc" + nm, [128, T], F32)
            xcbb = [sb("xcbb", [128, 512], BF16) for _ in range(2)]
            a_ = sb("a_", [128, T], F32)
            u_ = sb("u_", [128, T], F32)
            hsum = sb("hsum", [128, T], F32)
            hb = sb("hb", [128, T], F32) if HO else lxo[:, 0:T]
            carry = sb("carry", [128, 2], F32)
            tmp = [sb("tmp", [128, 512], F32) for _ in range(2)]
            tmpi = [sb("tmpi", [128, 512], F32) for _ in range(2)]
            for c in range(2):
                rows = slice(c * 128, (c + 1) * 128)
                dlx = P.dep("b_lx")
                P.dma("sp", lxo[:, 1:T + 1], self.LX[rows, 0:T], reads=[P.dep("d_LX")], writes=[dlx])
                if HO:
                    P.dma("sp", lxt[:, 1:T + 1], self.LX[rows, T:2 * T], reads=[P.dep("d_LX")], writes=[dlx])
                    P.op("dve", lambda e, lxo=lxo, lxt=lxt: e.tensor_scalar(out=lxo[:, 0:1], in0=lxt[:, T:T + 1], scalar1=f1, scalar2=None, op0=ALU.mult), reads=[dlx, self.dconst], writes=[dlx])
                    P.op("dve", lambda e, lxo=lxo, lxt=lxt: e.tensor_scalar(out=lxo[:, T + 1:T + 3], in0=lxt[:, 1:3], scalar1=f0, scalar2=None, op0=ALU.mult), reads=[dlx, self.dconst], writes=[dlx])
                    P.op("dve", lambda e, lxo=lxo, lxt=lxt: e.tensor_scalar(out=lxt[:, 0:1], in0=lxo[:, T:T + 1], scalar1=f0, scalar2=None, op0=ALU.mult), reads=[dlx, self.dconst], writes=[dlx])
                    P.op("dve", lambda e, lxo=lxo, lxt=lxt: e.tensor_scalar(out=lxt[:, T + 1:T + 3], in0=lxo[:, 1:3], scalar1=f1, scalar2=None, op0=ALU.mult), reads=[dlx, self.dconst], writes=[dlx])
                else:
                    P.op("dve", lambda e, lxo=lxo: e.memset(lxo[:, 0:1], 0.0), reads=[dlx], writes=[dlx])
                    P.op("dve", lambda e, lxo=lxo: e.memset(lxo[:, T + 1:T + 3], 0.0), reads=[dlx], writes=[dlx])
                xc = {}
                xcb = {}
                dxc = P.dep("b_xc")
                for nm, src in ((("o", lxo), ("t", lxt)) if HO else (("o", lxo),)):
                    x_ = big["xc" + nm]
                    P.op("dve", lambda e, x_=x_, src=src: e.tensor_scalar(out=x_[:], in0=src[:, 0:T], scalar1=prm[:, c, 0:1], scalar2=prm[:, c, 4:5],
                                                                       op0=ALU.mult, op1=ALU.add), reads=[dlx, dprm], writes=[dxc])
                    for k in (1, 2, 3):
                        P.op("dve", lambda e, x_=x_, src=src, k=k: e.scalar_tensor_tensor(out=x_[:], in0=src[:, k:k + T], scalar=prm[:, c, k:k + 1], in1=x_[:],
                                                                                       op0=ALU.mult, op1=ALU.add), reads=[dlx, dprm, dxc], writes=[dxc])
                    xc[nm] = x_
                da, du, dh, dhb, dcar = (P.dep("b_%s" % n) for n in ("a", "u", "h", "hb", "car"))

                def gates(nm, d_):
                    for b in range(T // 512):
                        cs = slice(b * 512, (b + 1) * 512)
                        pa, dpa = ps[(2 * b) % 4], dps[(2 * b) % 4]
                        pi, dpi = ps[(2 * b + 1) % 4], dps[(2 * b + 1) % 4]
                        xq, dxq = xcbb[b % 2], P.dep("b_xcbb%d" % (b % 2))
                        P.op("pool", lambda e, xq=xq, cs=cs: e.tensor_copy(xq[:], xc[nm][:, cs]), reads=[dxc], writes=[dxq])
                        P.op("pe", lambda e, pa=pa, xq=xq: e.matmul(pa[:], lhsT=wg[:, c, 2 * d_, :], rhs=xq[:], start=True, stop=True),
                             reads=[dwg, dxq], writes=[dpa])
                        P.op("pe", lambda e, pi=pi, xq=xq: e.matmul(pi[:], lhsT=wg[:, c, 2 * d_ + 1, :], rhs=xq[:], start=True, stop=True),
                             reads=[dwg, dxq], writes=[dpi])
                        tr, dtr = tmp[b % 2], P.dep("b_tmp%d" % (b % 2))
                        ti, dti = tmpi[b % 2], P.dep("b_tmpi%d" % (b % 2))
                        P.op("act", lambda e, tr=tr, pa=pa: e.activation(out=tr[:], in_=pa[:], func=AF.Sigmoid, bias=prm[:, c, 5 + d_:6 + d_], scale=1.0),
                             reads=[dpa, dprm], writes=[dtr])
                        P.op("act", lambda e, ti=ti, pi=pi: e.activation(out=ti[:], in_=pi[:], func=AF.Sigmoid, bias=prm[:, c, 7 + d_:8 + d_], scale=1.0),
                             reads=[dpi, dprm], writes=[dti])
                        P.op("act", lambda e, tr=tr, cs=cs: e.activation(out=a_[:, cs], in_=tr[:], func=AF.Exp, scale=sc[:, c, d_:d_ + 1]),
                             reads=[dtr, dsc], writes=[da])
                        P.op("act", lambda e, tr=tr: e.activation(out=tr[:], in_=tr[:], func=AF.Exp, scale=sc[:, c, 2 + d_:3 + d_]),
                             reads=[dtr, dsc], writes=[dtr])
                        P.op("dve", lambda e, tr=tr: e.tensor_scalar(out=tr[:], in0=tr[:], scalar1=-1.0, scalar2=1.0, op0=ALU.mult, op1=ALU.add),
                             reads=[dtr], writes=[dtr])
                        P.op("act", lambda e, tr=tr: e.activation(out=tr[:], in_=tr[:], func=AF.Sqrt), reads=[dtr], writes=[dtr])
                        P.op("dve", lambda e, ti=ti, cs=cs: e.tensor_tensor(out=ti[:], in0=ti[:], in1=xc[nm][:, cs], op=ALU.mult), reads=[dti, dxc], writes=[dti])
                        P.op("dve", lambda e, tr=tr, ti=ti, cs=cs: e.tensor_tensor(out=u_[:, cs], in0=tr[:], in1=ti[:], op=ALU.mult), reads=[dtr, dti], writes=[du])
                if HO:
                    gates("t", 0)
                    P.op("dve", lambda e: e.tensor_tensor_scan(out=hb[:], data0=a_[:], data1=u_[:], initial=0.0, op0=ALU.mult, op1=ALU.add),
                         reads=[da, du], writes=[dhb])
                    P.op("dve", lambda e: e.tensor_scalar(out=carry[:, 0:1], in0=hb[:, T - 1:T], scalar1=f1, scalar2=None, op0=ALU.mult),
                         reads=[dhb, self.dconst], writes=[dcar])
                    gates("t", 1)
                    P.op("dve", lambda e: e.tensor_tensor_scan(out=hb[:, ::-1], data0=a_[:, ::-1], data1=u_[:, ::-1], initial=0.0, op0=ALU.mult, op1=ALU.add),
                         reads=[da, du], writes=[dhb])
                    P.op("dve", lambda e: e.tensor_scalar(out=carry[:, 1:2], in0=hb[:, 0:1], scalar1=f0, scalar2=None, op0=ALU.mult),
                         reads=[dhb, self.dconst], writes=[dcar])
                else:
                    P.op("dve", lambda e: e.memset(carry[:], 0.0), writes=[dcar])
                gates("o", 0)
                P.op("dve", lambda e: e.tensor_tensor_scan(out=hsum[:], data0=a_[:], data1=u_[:], initial=carry[:, 0:1], op0=ALU.mult, op1=ALU.add),
                     reads=[da, du, dcar], writes=[dh])
                gates("o", 1)
                P.op("dve", lambda e: e.tensor_tensor_scan(out=hb[:, ::-1], data0=a_[:, ::-1], data1=u_[:, ::-1], initial=carry[:, 1:2], op0=ALU.mult, op1=ALU.add),
                     reads=[da, du, dcar], writes=[dhb])
                P.op("pool", lambda e: e.tensor_tensor(out=hsum[:], in0=hsum[:], in1=hb[:], op=ALU.add), reads=[dh, dhb], writes=[dh])
                lgt = a_
                P.dma("sp", lgt[:], self.LG[rows, 0:T], reads=[P.dep("d_LG"), da], writes=[da])
                P.op("pool", lambda e: e.tensor_tensor(out=hsum[:], in0=hsum[:], in1=lgt[:], op=ALU.mult), reads=[dh, da], writes=[dh])
                P.dma("sp", self.Y[768 + c * 128:768 + (c + 1) * 128, 0:T], hsum[:], reads=[dh], writes=[dY])
                P.barrier()

    def phase_c(self, l):
        P, T, NB = self.P, self.T, self.NB
        S2 = (2 * T) if self.has_other else T
        NKT = S2 // 128
        NQB = NB
        P.barrier()
        dY = P.dep("d_Y")
        with ExitStack() as st:
            sb = lambda n, s, d: self.sb(st, n, s, d)
            Vall = sb("Vall", [128, NKT, NH * 65], BF16)
            dVall = P.dep("c_Vall")
            vv = self.Vd[0:S2, :].rearrange("(k p) n -> p k n", p=128)
            for k0 in range(0, NKT, 8):
                P.dma("sp", Vall[:, k0:k0 + 8, :], vv[:, k0:k0 + 8, :], reads=[P.dep("d_V")], writes=[dVall])
            sel = sb("sel", [128, 64], F32)
            dsel = P.dep("c_sel")
            P.op("dve", lambda e: e.memset(sel[:], 0.0), writes=[dsel])
            P.op("dve", lambda e: e.memset(sel[64:65, :], 1.0), reads=[dsel], writes=[dsel])
            KTh = [sb("KTh", [96, S2], BF16) for _ in range(2)]
            QTh = [sb("QTh", [96, T], BF16) for _ in range(2)]
            pS = [self.psum(st, "pS", [128, 1024], F32) for _ in range(2)]
            dpS = [P.dep("c_pS%d" % i) for i in range(2)]
            pO = [self.psum(st, "pO", [128, 512], F32) for _ in range(2)]
            dpO = [P.dep("c_pO%d" % i) for i in range(2)]
            pD = self.psum(st, "pD", [128, 512], F32)
            dpD = P.dep("c_pD")
            PT = [sb("PT", [128, 1024], BF16) for _ in range(3)]
            dPT = [P.dep("c_PT%d" % i) for i in range(3)]
            Osb = [sb("Osb", [128, 512], F32) for _ in range(2)]
            rec = [sb("rec", [64, 512], F32) for _ in range(2)]
            yo = [sb("yo", [64, 512], F32) for _ in range(2)]
            it = 0
            nqb = 0
            for h in range(NH):
                kt_, dkt = KTh[h % 2], P.dep("c_KTh%d" % (h % 2))
                qt_, dqt = QTh[h % 2], P.dep("c_QTh%d" % (h % 2))
                P.dma("sp", kt_[:], self.KT[h, :, 0:S2], reads=[P.dep("d_KT")], writes=[dkt])
                P.dma("sp", qt_[:], self.QT[h, :, 0:T], reads=[P.dep("d_QT")], writes=[dqt])
                for qb in range(NQB):
                    qs = slice(qb * 512, (qb + 1) * 512)
                    po, dpo = pO[nqb % 2], dpO[nqb % 2]
                    NKG = NKT // 2

                    def scores(kg, it_):
                        ps_, dps_ = pS[it_ % 2], dpS[it_ % 2]
                        for j in range(2):
                            k = kg * 2 + j
                            P.op("pe", lambda e, k=k, j=j, ps_=ps_: e.matmul(ps_[:, j * 512:(j + 1) * 512], lhsT=kt_[:, k * 128:(k + 1) * 128], rhs=qt_[:, qs],
                                                                        start=True, stop=True), reads=[dkt, dqt], writes=[dps_], signal=(j == 1))

                    def expo(it_):
                        ps_, dps_ = pS[it_ % 2], dpS[it_ % 2]
                        pt_, dpt_ = PT[it_ % 3], dPT[it_ % 3]
                        P.op("act", lambda e, ps_=ps_, pt_=pt_: e.activation(out=pt_[:], in_=ps_[:], func=AF.Exp), reads=[dps_], writes=[dpt_])

                    def pv(kg, it_):
                        pt_, dpt_ = PT[it_ % 3], dPT[it_ % 3]
                        for j in range(2):
                            k = kg * 2 + j
                            P.op("pe", lambda e, k=k, j=j, pt_=pt_, po=po: e.matmul(po[0:65, :], lhsT=Vall[:, k, h * 65:(h + 1) * 65], rhs=pt_[:, j * 512:(j + 1) * 512],
                                                                               start=(k == 0), stop=(k == NKT - 1)), reads=[dVall, dpt_], writes=[dpo],
                                 signal=(j == 1))
                    scores(0, it)
                    for kg in range(NKG):
                        expo(it + kg)
                        if kg + 1 < NKG:
                            scores(kg + 1, it + kg + 1)
                        pv(kg, it + kg)
                    it += NKG
                    ob, dob = Osb[nqb % 2], P.dep("c_Osb%d" % (nqb % 2))
                    rc_, drc = rec[nqb % 2], P.dep("c_rec%d" % (nqb % 2))
                    y_, dy_ = yo[nqb % 2], P.dep("c_yo%d" % (nqb % 2))
                    nqb += 1
                    P.op("dve", lambda e, ob=ob, po=po: e.tensor_copy(ob[0:65, :], po[0:65, :]), reads=[dpo], writes=[dob])
                    P.op("pe", lambda e, ob=ob: e.matmul(pD[0:64, :], lhsT=sel[0:65, :], rhs=ob[0:65, :], start=True, stop=True),
                         reads=[dsel, dob], writes=[dpD])
                    P.op("dve", lambda e, rc_=rc_: e.reciprocal(out=rc_[:], in_=pD[0:64, :]), reads=[dpD], writes=[drc])
                    P.op("dve", lambda e, y_=y_, ob=ob, rc_=rc_: e.tensor_tensor(out=y_[:], in0=ob[0:64, :], in1=rc_[:], op=ALU.mult),
                         reads=[dob, drc], writes=[dy_])
                    P.dma("sp", self.Y[256 + h * 64:256 + (h + 1) * 64, qs], y_[:], reads=[dy_], writes=[dY])

    def phase_d(self, l, xres):
        P, T, NB, I = self.P, self.T, self.NB, self.I
        P.barrier()
        with ExitStack() as st:
            sb = lambda n, s, d: self.sb(st, n, s, d)
            Wo = sb("Wo", [128, 8, D], BF16)
            dWo = P.dep("d_Wo")
            wv = self.wb[("w_out", l)].rearrange("(c p) n -> p c n", p=128)
            for c in range(8):
                P.dma("sp", Wo[:, c, :], wv[:, c, :], reads=[self.dwc["w_out%d" % l]], writes=[dWo])
            gm, dgm = self.load_cols(st, "gm", I["mix_norm_g"][l], D)
            L = self.ln_setup(st, I["ln1_g"][l], I["ln1_b"][l], "1", nbuf=2)
            Yb = [sb("Yb", [128, 8, 512], F32) for _ in range(2)]
            sq = sb("sq", [128, 8, 512], BF16)
            yn = sb("yn", [128, 8, 512], BF16)
            dsq, dyn = P.dep("dd_sq"), P.dep("dd_yn")
            rstd = [sb("rstd", [128, 512], F32) for _ in range(3)]
            sdt = [sb("sdt", [128, 512], F32) for _ in range(3)]
            pst = [self.psum(st, "dpst", [128, 512], F32) for _ in range(2)]
            pso = [self.psum(st, "dpso", [128, 512], F32) for _ in range(3)]
            xr = [sb("xr", [128, D], F32) for _ in range(2)]
            pre = [sb("pre", [128, D], F32) for _ in range(2)]
            xTs = [sb("xTs", [128, 8, 512], BF16) for _ in range(2)]
            Yv = self.Y.rearrange("(c p) t -> p c t", p=128)
            groups = ((0, 2, 256.0), (2, 6, 512.0), (6, 8, 256.0))
            dres = P.dep("d_x1res")
            dx1T = P.dep("d_x1T")
            npo = 0
            ntile = 0
            moe = (l == 1)
            if moe:
                wr = sb("wr", [128, 8, NE], F32)
                wrb = sb("wrb", [128, 8, NE], BF16)
                dwr = P.dep("dd_wr")
                P.dma("sp", wr[:], I["moe_w_router"][0].rearrange("(c p) n -> p c n", p=128), writes=[dwr])
                P.op("dve", lambda e: e.tensor_copy(wrb[:], wr[:]), reads=[dwr], writes=[dwr])
                prr_full = self.psum(st, "prr", [128, 512], F32)
                prr = prr_full[:, 0:NE]
                dprr = P.dep("dd_prr")
                lg_ = [sb("lg_", [128, NE], F32) for _ in range(2)]
                m8 = [sb("m8", [128, 8], F32) for _ in range(2)]
                msk = [sb("msk", [128, NE], F32) for _ in range(2)]
                ex = [sb("ex", [128, NE], F32) for _ in range(2)]
                den = [sb("den", [128, 2], F32) for _ in range(2)]
                dcomb = P.dep("d_comb")
                rtmp = [sb("rtmp", [128, 2 * NE], F32) for _ in range(2)]
            for b in range(NB):
                ts_ = slice(b * 512, (b + 1) * 512)
                yb, dyb = Yb[b % 2], P.dep("dd_Yb%d" % (b % 2))
                P.dma("sp", yb[:], Yv[:, :, ts_], reads=[P.dep("d_Y")], writes=[dyb])
                for c in range(8):
                    P.op("act", lambda e, c=c, yb=yb: e.activation(out=sq[:, c, :], in_=yb[:, c, :], func=AF.Square), reads=[dyb], writes=[dsq])
                for gi, (c0, c1, n) in enumerate(groups):
                    p_, dp_ = pst[gi % 2], P.dep("dd_pst%d" % (gi % 2))
                    for c in range(c0, c1):
                        P.op("pe", lambda e, c=c, p_=p_, c0=c0, c1=c1: e.matmul(p_[:], lhsT=self.ones[:], rhs=sq[:, c, :], start=(c == c0), stop=(c == c1 - 1)),
                             reads=[dsq, self.dconst], writes=[dp_], signal=(c == c1 - 1))
                    dsd, drs = P.dep("dd_sdt%d" % gi), P.dep("dd_rstd%d" % gi)
                    P.op("act", lambda e, gi=gi, p_=p_, n=n: e.activation(out=sdt[gi][:], in_=p_[:], func=AF.Sqrt, bias=self.eps_rms[:], scale=1.0 / n),
                         reads=[dp_, self.dconst], writes=[dsd])
                    P.op("dve", lambda e, gi=gi: e.reciprocal(out=rstd[gi][:], in_=sdt[gi][:]), reads=[dsd], writes=[drs])
                    for c in range(c0, c1):
                        P.op("dve", lambda e, c=c, gi=gi, yb=yb: e.scalar_tensor_tensor(out=yn[:, c, :], in0=yb[:, c, :], scalar=gm[:, c:c + 1], in1=rstd[gi][:],
                                                                                     op0=ALU.mult, op1=ALU.mult), reads=[dyb, dgm, drs], writes=[dyn])
                xs, dxs = xTs[b % 2], P.dep("dd_xTs%d" % (b % 2))
                for sub in range(4):
                    r0 = b * 512 + sub * 128
                    xrt, dxr = xr[ntile % 2], P.dep("dd_xr%d" % (ntile % 2))
                    pr, dpr = pre[ntile % 2], P.dep("dd_pre%d" % (ntile % 2))
                    ntile += 1
                    P.dma("sp", xrt[:], xres[r0:r0 + 128, :], reads=[P.dep("d_xres")], writes=[dxr])
                    for hf in range(2):
                        po, dpo = pso[npo % 3], P.dep("dd_pso%d" % (npo % 3))
                        npo += 1
                        for c in range(8):
                            P.op("pe", lambda e, c=c, hf=hf, po=po, sub=sub: e.matmul(po[:], lhsT=yn[:, c, sub * 128:(sub + 1) * 128], rhs=Wo[:, c, hf * 512:(hf + 1) * 512],
                                                                                 start=(c == 0), stop=(c == 7)), reads=[dyn, dWo], writes=[dpo], signal=(c == 7))
                        P.op("dve", lambda e, hf=hf, po=po, pr=pr, xrt=xrt: e.scalar_tensor_tensor(out=pr[:, hf * 512:(hf + 1) * 512], in0=xrt[:, hf * 512:(hf + 1) * 512],
                                                                                                scalar=ALPHA, in1=po[:], op0=ALU.mult, op1=ALU.add),
                             reads=[dxr, dpo], writes=[dpr])
                    self.ln_tile(L, pr, dpr, xs, dxs, sub, res_dst=self.x1res[r0:r0 + 128, :], dres=dres,
                                 xb_dst=(self.X1B[r0:r0 + 128, :] if moe else None), dxbd=P.dep("d_X1B"))
                    if moe:
                        i2 = ntile % 2
                        for c in range(8):
                            P.op("pe", lambda e, c=c, sub=sub, xs=xs: e.matmul(prr, lhsT=xs[:, c, sub * 128:(sub + 1) * 128], rhs=wrb[:, c, :], start=(c == 0), stop=(c == 7)),
                                 reads=[dxs, dwr], writes=[dprr], signal=(c == 7))
                        dl_, dm8, dmk, dex, dden = (P.dep("dd_%s%d" % (n, i2)) for n in ("lg", "m8", "msk", "ex", "den"))
                        P.op("dve", lambda e, i2=i2: e.tensor_copy(lg_[i2][:], prr), reads=[dprr], writes=[dl_])
                        P.op("dve", lambda e, i2=i2: e.max(out=m8[i2][:], in_=lg_[i2][:]), reads=[dl_], writes=[dm8])
                        P.op("dve", lambda e, i2=i2: e.tensor_scalar(out=msk[i2][:], in0=lg_[i2][:], scalar1=m8[i2][:, 1:2], scalar2=None, op0=ALU.is_ge),
                             reads=[dl_, dm8], writes=[dmk])
                        P.op("dve", lambda e, i2=i2: e.tensor_scalar(out=den[i2][:, 0:1], in0=m8[i2][:, 0:1], scalar1=-1.0, scalar2=None, op0=ALU.mult),
                             reads=[dm8], writes=[dden])
                        P.op("act", lambda e, i2=i2: e.activation(out=ex[i2][:], in_=lg_[i2][:], func=AF.Exp, bias=den[i2][:, 0:1], scale=1.0),
                             reads=[dl_, dden], writes=[dex])
                        P.op("dve", lambda e, i2=i2: e.tensor_tensor(out=ex[i2][:], in0=ex[i2][:], in1=msk[i2][:], op=ALU.mult), reads=[dex, dmk], writes=[dex])
                        P.op("dve", lambda e, i2=i2: e.reduce_sum(out=den[i2][:, 1:2], in_=ex[i2][:], axis=mybir.AxisListType.X), reads=[dex, dden], writes=[dden])
                        P.op("dve", lambda e, i2=i2: e.reciprocal(out=den[i2][:, 1:2], in_=den[i2][:, 1:2]), reads=[dden], writes=[dden])
                        P.op("dve", lambda e, i2=i2: e.tensor_scalar(out=ex[i2][:], in0=ex[i2][:], scalar1=den[i2][:, 1:2], scalar2=None, op0=ALU.mult),
                             reads=[dex, dden], writes=[dex])
                        R = self.R
                        ti_ = r0 // 128
                        drt = P.dep("r_tab")
                        doh = P.dep("dd_oh%d" % i2)
                        oh1, oh2, tmp8 = R["oh1"][:, ti_, :], R["oh2"][:, ti_, :], rtmp[i2]
                        P.op("dve", lambda e, i2=i2, oh1=oh1: e.tensor_scalar(out=oh1, in0=lg_[i2][:], scalar1=m8[i2][:, 0:1], scalar2=None, op0=ALU.is_equal),
                             reads=[dl_, dm8], writes=[drt])
                        P.op("dve", lambda e, i2=i2, oh1=oh1, oh2=oh2: e.tensor_tensor(out=oh2, in0=msk[i2][:], in1=oh1, op=ALU.subtract), reads=[dmk, drt], writes=[drt])
                        pw, dpw = prr_full[:, 16:32], dprr
                        P.op("pe", lambda e, i2=i2: e.matmul(pw[:, 0:NE], lhsT=R["U"][:], rhs=msk[i2][:], start=True, stop=True), reads=[dmk, P.dep("r_const")], writes=[dpw])
                        P.op("pe", lambda e, i2=i2: e.matmul(pw[:, NE:2 * NE], lhsT=R["onesf"][:], rhs=msk[i2][:], start=True, stop=True), reads=[dmk, P.dep("r_const")], writes=[dpw])
                        drun = P.dep("r_run")
                        P.op("dve", lambda e, tmp8=tmp8: e.tensor_tensor(out=tmp8[:, 0:NE], in0=pw[:, 0:NE], in1=R["run"][:], op=ALU.add), reads=[dpw, drun], writes=[doh])
                        P.op("dve", lambda e: e.tensor_tensor(out=R["run"][:], in0=pw[:, NE:2 * NE], in1=R["run"][:], op=ALU.add), reads=[dpw, drun], writes=[drun])
                        for kk, oh in ((0, oh1), (1, oh2)):
                            P.op("dve", lambda e, tmp8=tmp8, oh=oh: e.tensor_tensor(out=tmp8[:, NE:2 * NE], in0=tmp8[:, 0:NE], in1=oh, op=ALU.mult), reads=[doh, drt], writes=[doh])
                            P.op("dve", lambda e, tmp8=tmp8, kk=kk, ti_=ti_: e.reduce_sum(out=R["r12"][:, ti_, kk:kk + 1], in_=tmp8[:, NE:2 * NE], axis=mybir.AxisListType.X),
                                 reads=[doh], writes=[drt])
                            P.op("dve", lambda e, tmp8=tmp8, oh=oh, i2=i2: e.tensor_tensor(out=tmp8[:, NE:2 * NE], in0=ex[i2][:], in1=oh, op=ALU.mult), reads=[dex, drt, doh], writes=[doh])
                            P.op("dve", lambda e, tmp8=tmp8, kk=kk, ti_=ti_: e.reduce_sum(out=R["g12"][:, ti_, kk:kk + 1], in_=tmp8[:, NE:2 * NE], axis=mybir.AxisListType.X),
                                 reads=[doh], writes=[drt])
                P.dma("sp", self.x1T.rearrange("(c p) t -> p c t", p=128)[:, :, ts_], xs[:], reads=[dxs], writes=[dx1T])

    def route_setup(self, st):
        P, T = self.P, self.T
        NT = T // 128
        self.NTILE = (2 * T) // 512 + NE
        P.barrier()
        R = {}
        sb = lambda n, s, d: self.sb(st, n, s, d)
        R["U"] = sb("rU", [128, 128], F32)
        R["onesf"] = sb("ronesf", [128, 128], F32)
        R["run"] = sb("rrun", [128, NE], F32)
        R["oh1"] = sb("roh1", [128, NT, NE], F32)
        R["oh2"] = sb("roh2", [128, NT, NE], F32)
        R["r12"] = sb("rr12", [128, NT, 2], F32)
        R["g12"] = sb("rg12", [128, NT, 2], F32)
        R["slot"] = sb("rslot", [128, NT, 2], F32)
        R["sloti"] = sb("rsloti", [128, NT, 2], mybir.dt.int32)
        R["pidx"] = sb("rpidx", [128, 1], F32)
        R["pidxi"] = sb("rpidxi", [128, 1], mybir.dt.int32)
        R["ej"] = sb("rej", [128, self.NTILE], F32)
        R["idxg"] = sb("ridxg", [128, self.NTILE, 7], F32)
        R["idxgi"] = sb("ridxgi", [128, self.NTILE, 7], mybir.dt.int32)
        R["idxd"] = sb("ridxd", [128, self.NTILE, 4], F32)
        R["idxdi"] = sb("ridxdi", [128, self.NTILE, 4], mybir.dt.int32)
        dc = P.dep("r_const")
        P.op("pool", lambda e: e.memset(R["onesf"][:], 1.0), writes=[dc])
        P.op("pool", lambda e: e.memset(R["U"][:], 1.0), writes=[dc])
        P.op("pool", lambda e: e.affine_select(out=R["U"][:], in_=R["U"][:], pattern=[[1, 128]], compare_op=ALU.is_gt, fill=0.0,
                                               base=0, channel_multiplier=-1), reads=[dc], writes=[dc])
        P.op("pool", lambda e: e.iota(R["pidxi"][:], pattern=[[0, 1]], base=0, channel_multiplier=1), writes=[dc])
        P.op("pool", lambda e: e.tensor_copy(R["pidx"][:], R["pidxi"][:]), reads=[dc], writes=[dc])
        P.op("dve", lambda e: e.memset(R["run"][:], 0.0), writes=[P.dep("r_run")])
        self.R = R

    def phase_r(self):
        P, T = self.P, self.T
        R = self.R
        NT = T // 128
        NTILE = self.NTILE
        P.barrier()
        with ExitStack() as st:
            sb = lambda n, s, d: self.sb(st, n, s, d)
            t8 = sb("t8", [128, 6, NE], F32)
            d8 = P.dep("r_t8")
            drt = P.dep("r_tab")
            P.op("dve", lambda e: e.memset(t8[:, 5, :], 1.0), writes=[d8])
            t8i = sb("t8i", [128, NE], mybir.dt.int32)
            P.op("dve", lambda e: e.tensor_scalar(out=t8[:, 0, :], in0=R["run"][:], scalar1=1.0 / 512.0, scalar2=511.0 / 512.0 - 0.499, op0=ALU.mult, op1=ALU.add),
                 reads=[P.dep("r_run"), d8], writes=[d8])
            P.op("dve", lambda e: e.tensor_copy(t8i[:], t8[:, 0, :]), reads=[d8], writes=[d8])
            P.op("dve", lambda e: e.tensor_copy(t8[:, 1, :], t8i[:]), reads=[d8], writes=[d8])
            P.op("dve", lambda e: e.tensor_scalar(out=t8[:, 2, :], in0=t8[:, 1, :], scalar1=512.0, scalar2=None, op0=ALU.mult), reads=[d8], writes=[d8])
            P.op("dve", lambda e: e.tensor_tensor_scan(out=t8[:, 3, :], data0=t8[:, 5, :], data1=t8[:, 2, :], initial=0.0, op0=ALU.mult, op1=ALU.add), reads=[d8], writes=[d8])
            P.op("dve", lambda e: e.tensor_tensor(out=t8[:, 4, :], in0=t8[:, 3, :], in1=t8[:, 2, :], op=ALU.subtract), reads=[d8], writes=[d8])
            tmp = sb("rtmp2", [128, NE], F32)
            dtmp = P.dep("r_tmp2")
            for i in range(NT):
                for kk, oh in ((0, R["oh1"]), (1, R["oh2"])):
                    P.op("dve", lambda e, i=i, oh=oh: e.tensor_tensor(out=tmp[:], in0=oh[:, i, :], in1=t8[:, 4, :], op=ALU.mult), reads=[drt, d8, dtmp], writes=[dtmp])
                    P.op("dve", lambda e, i=i, kk=kk: e.reduce_sum(out=R["slot"][:, i, kk:kk + 1], in_=tmp[:], axis=mybir.AxisListType.X), reads=[dtmp], writes=[drt])
            P.op("dve", lambda e: e.tensor_tensor(out=R["slot"][:], in0=R["slot"][:], in1=R["r12"][:], op=ALU.add), reads=[drt], writes=[drt])
            P.op("dve", lambda e: e.tensor_copy(R["sloti"][:], R["slot"][:]), reads=[drt], writes=[drt])
            for j in range(NTILE):
                P.op("dve", lambda e, j=j: e.tensor_scalar(out=tmp[:], in0=t8[:, 3, :], scalar1=float(512 * j), scalar2=None, op0=ALU.is_le), reads=[d8, dtmp], writes=[dtmp])
                P.op("dve", lambda e, j=j: e.reduce_sum(out=R["ej"][:, j:j + 1], in_=tmp[:], axis=mybir.AxisListType.X), reads=[dtmp], writes=[drt])
            P.op("dve", lambda e: e.tensor_scalar(out=R["ej"][:], in0=R["ej"][:], scalar1=float(NE - 1), scalar2=None, op0=ALU.min), reads=[drt], writes=[drt])
            dc = P.dep("r_const")
            for g in range(7):
                P.op("dve", lambda e, g=g: e.tensor_scalar(out=R["idxg"][:, :, g], in0=R["ej"][:], scalar1=896.0, scalar2=float(g * 128), op0=ALU.mult, op1=ALU.add),
                     reads=[drt], writes=[drt])
            for q in range(4):
                P.op("dve", lambda e, q=q: e.tensor_scalar(out=R["idxd"][:, :, q], in0=R["ej"][:], scalar1=512.0, scalar2=float(q * 128), op0=ALU.mult, op1=ALU.add),
                     reads=[drt], writes=[drt])
            P.op("dve", lambda e: e.tensor_scalar(out=R["idxg"][:], in0=R["idxg"][:], scalar1=R["pidx"][:, 0:1], scalar2=None, op0=ALU.add), reads=[drt, dc], writes=[drt])
            P.op("dve", lambda e: e.tensor_scalar(out=R["idxd"][:], in0=R["idxd"][:], scalar1=R["pidx"][:, 0:1], scalar2=None, op0=ALU.add), reads=[drt, dc], writes=[drt])
            P.op("dve", lambda e: e.tensor_copy(R["idxgi"][:], R["idxg"][:]), reads=[drt], writes=[drt])
            P.op("dve", lambda e: e.tensor_copy(R["idxdi"][:], R["idxd"][:]), reads=[drt], writes=[drt])
            import os
            if os.environ.get("MK_DBGR"):
                dd = P.dep("dbgr")
                for nm, t_, dt_ in (("idxgi", R["idxgi"], mybir.dt.int32), ("ej", R["ej"], F32), ("sloti", R["sloti"], mybir.dt.int32),
                                    ("t8", t8, F32), ("pidx", R["pidx"], F32), ("r12", R["r12"], F32), ("g12", R["g12"], F32),
                                    ("oh1", R["oh1"], F32), ("oh2", R["oh2"], F32), ("run", R["run"], F32), ("U", R["U"], F32), ("onesf", R["onesf"], F32),
                                    ("pidxi", R["pidxi"], mybir.dt.int32)):
                    shp = list(t_.shape)
                    o = self.nc.dram_tensor("dbg_" + nm, shp, dt_, kind="ExternalOutput").ap()
                    P.dma("sp", o, t_[:], reads=[drt, d8, dc], writes=[dd])

    def phase_e_moe(self, l, out_res):
        P, T, I = self.P, self.T, self.I
        R = self.R
        NT = T // 128
        NTILE = self.NTILE
        NSLOT = NTILE * 512
        drt = P.dep("r_tab")
        P.barrier()
        with ExitStack() as st0:
            cur = [ExitStack()]
            st0.callback(lambda: cur[0].close())
            sb = lambda n, s, d: self.sb(cur[0], n, s, d)

            class _St:
                def enter_context(self_, x):
                    return cur[0].enter_context(x)
            st = _St()
            dXS = P.dep("d_XS")
            dYS = P.dep("d_YS")
            zt = sb("zt", [128, 4096], BF16)
            dzt = P.dep("m_zt")
            P.op("pool", lambda e: e.memset(zt[:], 0.0), writes=[dzt])
            xsv = self.XS.rearrange("(a p f) n -> a p (f n)", p=128, f=4)
            for a in range(NSLOT // 512):
                P.dma("sp", xsv[a], zt[:], reads=[dzt], writes=[dXS])
            xl = [sb("xl", [128, D], BF16) for _ in range(2)]
            for i in range(NT):
                x_, dx_ = xl[i % 2], P.dep("m_xl%d" % (i % 2))
                P.dma("sp", x_[:], self.X1B[i * 128:(i + 1) * 128, :], reads=[P.dep("d_X1B")], writes=[dx_])
                for kk in range(2):
                    P.idma(self.XS, R["sloti"][:, i, kk:kk + 1], x_[:], None, reads=[dx_, drt], writes=[dXS])
            P.barrier()
            cur[0].close()
            cur[0] = ExitStack()
            if self.mstop < 4:
                return
            xtm = sb("xtm", [128, 4, D], BF16)
            dxtm = P.dep("m_xtm")
            xTb = sb("xTb", [128, 8, 512], BF16)
            dxTb = P.dep("m_xTb")
            pT = self.psum(st, "mpT", [128, D], BF16)
            dpT = P.dep("m_pT")
            Wg = [sb("Wg", [128, 8, 512], BF16) for _ in range(3)]
            Wu = [sb("Wu", [128, 8, 512], BF16) for _ in range(3)]
            Wd = sb("Wd", [128, 28, D], BF16)
            dWd = P.dep("e_Wd")
            hT = sb("hT", [128, 28, 512], BF16)
            dhT = P.dep("e_hT")
            pg = [self.psum(st, "pg", [128, 512], F32) for _ in range(2)]
            pu = [self.psum(st, "pu", [128, 512], F32) for _ in range(2)]
            pd = [self.psum(st, "pd", [128, 512], F32) for _ in range(2)]
            sg = [sb("sg", [128, 512], F32) for _ in range(2)]
            ysb = [sb("ysb", [128, D], F32) for _ in range(2)]
            wi = 0
            npd = 0
            nys = 0
            xsr = self.XS.rearrange("(j s p) n -> j p s n", p=128, s=4)
            for j in range(NTILE):
                P.dma("sp", xtm[:], xsr[j], reads=[dXS], writes=[dxtm])
                for sub in range(4):
                    for c in range(8):
                        P.op("pe", lambda e, c=c, sub=sub: e.transpose(pT[:, c * 128:(c + 1) * 128], xtm[:, sub, c * 128:(c + 1) * 128], self.ident[:]),
                             reads=[dxtm, self.dconst], writes=[dpT], signal=(c == 7))
                    P.op("act", lambda e, sub=sub: e.activation(out=xTb[:, :, sub * 128:(sub + 1) * 128], in_=pT[:].rearrange("p (c t) -> p c t", c=8), func=AF.Copy),
                         reads=[dpT], writes=[dxTb])
                import os
                fstop = int(os.environ.get("MK_FSTOP", "99"))
                if fstop < 1:
                    continue
                for g in range(7):
                    wg_, dwg_ = Wg[wi % 3], P.dep("e_Wg%d" % (wi % 3))
                    wu_, dwu_ = Wu[wi % 3], P.dep("e_Wu%d" % (wi % 3))
                    wi += 1
                    P.idma(wg_[:].rearrange("p c n -> p (c n)"), None, self.WGt, R["idxgi"][:, j, g:g + 1], reads=[self.dwc["mg"], drt], writes=[dwg_])
                    P.idma(wu_[:].rearrange("p c n -> p (c n)"), None, self.WUt, R["idxgi"][:, j, g:g + 1], reads=[self.dwc["mu"], drt], writes=[dwu_])
                    for jj in range(4):
                        f = g * 4 + jj
                        pg_, dpg_ = pg[f % 2], P.dep("e_pg%d" % (f % 2))
                        pu_, dpu_ = pu[f % 2], P.dep("e_pu%d" % (f % 2))
                        for c in range(8):
                            P.op("pe", lambda e, c=c, jj=jj, pg_=pg_, wg_=wg_: e.matmul(pg_[:], lhsT=wg_[:, c, jj * 128:(jj + 1) * 128], rhs=xTb[:, c, :], start=(c == 0), stop=(c == 7)),
                                 reads=[dwg_, dxTb], writes=[dpg_], signal=(c == 7))
                        for c in range(8):
                            P.op("pe", lambda e, c=c, jj=jj, pu_=pu_, wu_=wu_: e.matmul(pu_[:], lhsT=wu_[:, c, jj * 128:(jj + 1) * 128], rhs=xTb[:, c, :], start=(c == 0), stop=(c == 7)),
                                 reads=[dwu_, dxTb], writes=[dpu_], signal=(c == 7))
                        s_, ds_ = sg[f % 2], P.dep("e_sg%d" % (f % 2))
                        P.op("act", lambda e, s_=s_, pg_=pg_: e.activation(out=s_[:], in_=pg_[:], func=AF.Silu), reads=[dpg_], writes=[ds_])
                        P.op("dve", lambda e, f=f, s_=s_, pu_=pu_: e.tensor_tensor(out=hT[:, f, :], in0=pu_[:], in1=s_[:], op=ALU.mult),
                             reads=[dpu_, ds_], writes=[dhT])
                if fstop < 2:
                    continue
                for q in range(4):
                    P.idma(Wd[:, q * 7:(q + 1) * 7, :].rearrange("p f n -> p (f n)"), None, self.WDt, R["idxdi"][:, j, q:q + 1], reads=[self.dwc["md"], drt], writes=[dWd])
                for sub in range(4):
                    y_, dy_ = ysb[nys % 2], P.dep("m_ysb%d" % (nys % 2))
                    nys += 1
                    for hf in range(2):
                        pd_, dpd_ = pd[npd % 2], P.dep("e_pd%d" % (npd % 2))
                        npd += 1
                        for f in range(28):
                            P.op("pe", lambda e, f=f, sub=sub, hf=hf, pd_=pd_: e.matmul(pd_[:], lhsT=hT[:, f, sub * 128:(sub + 1) * 128], rhs=Wd[:, f, hf * 512:(hf + 1) * 512],
                                                                                   start=(f == 0), stop=(f == 27)), reads=[dhT, dWd], writes=[dpd_], signal=(f == 27))
                        if hf == 0:
                            P.op("act", lambda e, y_=y_, pd_=pd_: e.activation(out=y_[:, 0:512], in_=pd_[:], func=AF.Copy), reads=[dpd_], writes=[dy_])
                        else:
                            P.op("dve", lambda e, y_=y_, pd_=pd_: e.tensor_copy(y_[:, 512:1024], pd_[:]), reads=[dpd_], writes=[dy_])
                    r0 = j * 512 + sub * 128
                    P.dma("sp", self.YS[r0:r0 + 128, :], y_[:], reads=[dy_], writes=[dYS])
            P.barrier()
            cur[0].close()
            cur[0] = ExitStack()
            if self.mstop < 5:
                return
            L = self.ln_setup(st, I["ln2_g"][l], I["ln2_b"][l], "2", nbuf=1)
            y1 = [sb("y1", [128, D], F32) for _ in range(2)]
            y2 = [sb("y2", [128, D], F32) for _ in range(2)]
            xr = [sb("xr", [128, D], F32) for _ in range(2)]
            pre = [sb("pre", [128, D], F32) for _ in range(2)]
            dres = P.dep("d_outres")
            for i in range(NT):
                i2 = i % 2
                a_, b_, x_, p_ = y1[i2], y2[i2], xr[i2], pre[i2]
                da_, db_, dx_, dp_ = (P.dep("m_%s%d" % (n, i2)) for n in ("y1", "y2", "xr", "pre"))
                P.idma(a_[:], None, self.YS, R["sloti"][:, i, 0:1], reads=[dYS, drt], writes=[da_])
                P.idma(b_[:], None, self.YS, R["sloti"][:, i, 1:2], reads=[dYS, drt], writes=[db_])
                P.dma("sp", x_[:], self.x1res[i * 128:(i + 1) * 128, :], reads=[P.dep("d_x1res")], writes=[dx_])
                P.op("dve", lambda e, a_=a_, i=i: e.tensor_scalar(out=a_[:], in0=a_[:], scalar1=R["g12"][:, i, 0:1], scalar2=None, op0=ALU.mult), reads=[da_, drt], writes=[da_])
                P.op("dve", lambda e, a_=a_, b_=b_, i=i: e.scalar_tensor_tensor(out=a_[:], in0=b_[:], scalar=R["g12"][:, i, 1:2], in1=a_[:], op0=ALU.mult, op1=ALU.add),
                     reads=[da_, db_, drt], writes=[da_])
                P.op("dve", lambda e, a_=a_, x_=x_, p_=p_: e.scalar_tensor_tensor(out=p_[:], in0=x_[:], scalar=ALPHA, in1=a_[:], op0=ALU.mult, op1=ALU.add),
                     reads=[da_, dx_], writes=[dp_])
                self.ln_tile(L, p_, dp_, None, None, 0, res_dst=out_res[i * 128:(i + 1) * 128, :], dres=dres)

    def phase_e(self, l, out_res, out_T):
        P, T, NB, I = self.P, self.T, self.NB, self.I
        P.barrier()
        moe = (l == 1)
        nexp = NE if moe else 1
        with ExitStack() as st:
            sb = lambda n, s, d: self.sb(st, n, s, d)
            L = self.ln_setup(st, I["ln2_g"][l], I["ln2_b"][l], "2", nbuf=1)
            xb_ = [sb("xb", [128, 8, 512], BF16) for _ in range(1)]
            Wg = [sb("Wg", [128, 8, 512], BF16) for _ in range(3)]
            Wu = [sb("Wu", [128, 8, 512], BF16) for _ in range(3)]
            Wd = sb("Wd", [128, 28, D], BF16)
            dWd = P.dep("e_Wd")
            hT = sb("hT", [128, 28, 512], BF16)
            dhT = P.dep("e_hT")
            pg = [self.psum(st, "pg", [128, 512], F32) for _ in range(2)]
            pu = [self.psum(st, "pu", [128, 512], F32) for _ in range(2)]
            pd = [self.psum(st, "pd", [128, 512], F32) for _ in range(2)]
            pc = self.psum(st, "pc", [128, 512], F32)
            sg = [sb("sg", [128, 512], F32) for _ in range(2)]
            xr = [sb("xr", [128, D], F32) for _ in range(1)]
            pre = [sb("pre", [128, D], F32) for _ in range(1)]
            xTs = [sb("xTs", [128, 8, 512], BF16) for _ in range(1)] if out_T is not None else None
            acc = sb("acc", [128, 4, D], F32) if moe else None
            dacc = P.dep("e_acc")
            if moe:
                selm = sb("selm", [NE, NE, 128], F32)
                dselm = P.dep("e_selm")
                P.op("pool", lambda e: e.memset(selm[:], 0.0), writes=[dselm])
                P.op("pool", lambda e: e.affine_select(out=selm[:], in_=selm[:], pattern=[[1, NE], [0, 128]], compare_op=ALU.not_equal, fill=1.0,
                                                       base=0, channel_multiplier=-1), reads=[dselm], writes=[dselm])
                cmb = [sb("cmb", [128, NE], F32) for _ in range(2)]
                cT = sb("cT", [NE, 512], F32)
                dcT = P.dep("e_cT")
                cbe = [sb("cbe", [128, 512], F32) for _ in range(2)]
                pT8 = pc[0:NE, 0:128]
                dpT8 = P.dep("e_pc")
            x1Tv = self.x1T.rearrange("(c p) t -> p c t", p=128)
            wi = 0
            npd = 0
            ntile = 0
            dres = P.dep("d_outres")
            dxT = P.dep("d_xT")
            for b in range(NB):
                ts_ = slice(b * 512, (b + 1) * 512)
                xb, dxb = xb_[0], P.dep("e_xb0")
                P.dma("sp", xb[:], x1Tv[:, :, ts_], reads=[P.dep("d_x1T")], writes=[dxb])
                if moe:
                    for sub in range(4):
                        cm, dcm = cmb[sub % 2], P.dep("e_cmb%d" % (sub % 2))
                        P.dma("sp", cm[:], self.comb[b * 512 + sub * 128:b * 512 + (sub + 1) * 128, :], reads=[P.dep("d_comb")], writes=[dcm])
                        P.op("pe", lambda e, cm=cm: e.transpose(pT8, cm[:], self.identf[:]), reads=[dcm, self.dconst], writes=[dpT8])
                        P.op("dve", lambda e, sub=sub: e.tensor_copy(cT[:, sub * 128:(sub + 1) * 128], pT8), reads=[dpT8], writes=[dcT])
                for ex_ in range(nexp):
                    if moe:
                        wgd, wud, wdd = self.wb["mg"][ex_], self.wb["mu"][ex_], self.wb["md"][ex_]
                        cb_, dcb_ = cbe[ex_ % 2], P.dep("e_cbe%d" % (ex_ % 2))
                        dpc = P.dep("e_pc")
                        P.op("pe", lambda e, ex_=ex_: e.matmul(pc[:], lhsT=selm[:, ex_, :], rhs=cT[:], start=True, stop=True), reads=[dselm, dcT], writes=[dpc])
                        P.op("act", lambda e, cb_=cb_: e.activation(out=cb_[:], in_=pc[:], func=AF.Copy), reads=[dpc], writes=[dcb_])
                    else:
                        wgd, wud, wdd = self.wb["dg"], self.wb["du"], self.wb["dd"]
                    if not (getattr(self, "moe_tables", False) and not moe):
                        wgv = wgd.rearrange("(c p) n -> p c n", p=128)
                        wuv = wud.rearrange("(c p) n -> p c n", p=128)
                    wdv = wdd.rearrange("(c p) n -> p c n", p=128)
                    for g in range(7):
                        wg_, dwg_ = Wg[wi % 3], P.dep("e_Wg%d" % (wi % 3))
                        wu_, dwu_ = Wu[wi % 3], P.dep("e_Wu%d" % (wi % 3))
                        wi += 1
                        if getattr(self, "moe_tables", False) and not moe:
                            P.dma("sp", wg_[:].rearrange("p c n -> p (c n)"), wgd[g * 128:(g + 1) * 128, :], reads=[self.dwc["g"]], writes=[dwg_])
                            P.dma("sp", wu_[:].rearrange("p c n -> p (c n)"), wud[g * 128:(g + 1) * 128, :], reads=[self.dwc["u"]], writes=[dwu_])
                        else:
                            P.dma("sp", wg_[:], wgv[:, :, g * 512:(g + 1) * 512], reads=[self.dwc["mg" if moe else "g"]], writes=[dwg_])
                            P.dma("sp", wu_[:], wuv[:, :, g * 512:(g + 1) * 512], reads=[self.dwc["mu" if moe else "u"]], writes=[dwu_])
                        for j in range(4):
                            f = g * 4 + j
                            pg_, dpg_ = pg[f % 2], P.dep("e_pg%d" % (f % 2))
                            pu_, dpu_ = pu[f % 2], P.dep("e_pu%d" % (f % 2))
                            for c in range(8):
                                P.op("pe", lambda e, c=c, j=j, pg_=pg_, wg_=wg_: e.matmul(pg_[:], lhsT=wg_[:, c, j * 128:(j + 1) * 128], rhs=xb[:, c, :], start=(c == 0), stop=(c == 7)),
                                     reads=[dwg_, dxb], writes=[dpg_], signal=(c == 7))
                            for c in range(8):
                                P.op("pe", lambda e, c=c, j=j, pu_=pu_, wu_=wu_: e.matmul(pu_[:], lhsT=wu_[:, c, j * 128:(j + 1) * 128], rhs=xb[:, c, :], start=(c == 0), stop=(c == 7)),
                                     reads=[dwu_, dxb], writes=[dpu_], signal=(c == 7))
                            s_, ds_ = sg[f % 2], P.dep("e_sg%d" % (f % 2))
                            P.op("act", lambda e, s_=s_, pg_=pg_: e.activation(out=s_[:], in_=pg_[:], func=AF.Silu), reads=[dpg_], writes=[ds_])
                            if moe:
                                P.op("pool", lambda e, s_=s_, cb_=cb_: e.tensor_tensor(out=s_[:], in0=s_[:], in1=cb_[:], op=ALU.mult), reads=[ds_, dcb_], writes=[ds_])
                            P.op("dve", lambda e, f=f, s_=s_, pu_=pu_: e.tensor_tensor(out=hT[:, f, :], in0=pu_[:], in1=s_[:], op=ALU.mult),
                                 reads=[dpu_, ds_], writes=[dhT])
                    for c0 in range(0, 28, 7):
                        P.dma("sp", Wd[:, c0:c0 + 7, :], wdv[:, c0:c0 + 7, :], reads=[self.dwc["md" if moe else "d"]], writes=[dWd])
                    for sub in range(4):
                        r0 = b * 512 + sub * 128
                        if ex_ == nexp - 1:
                            xrt, dxr = xr[0], P.dep("e_xr0")
                            pr, dpr = pre[0], P.dep("e_pre0")
                            ntile += 1
                            P.dma("sp", xrt[:], self.x1res[r0:r0 + 128, :], reads=[P.dep("d_x1res")], writes=[dxr])
                        for hf in range(2):
                            pd_, dpd_ = pd[npd % 2], P.dep("e_pd%d" % (npd % 2))
                            npd += 1
                            for f in range(28):
                                P.op("pe", lambda e, f=f, sub=sub, hf=hf, pd_=pd_: e.matmul(pd_[:], lhsT=hT[:, f, sub * 128:(sub + 1) * 128], rhs=Wd[:, f, hf * 512:(hf + 1) * 512],
                                                                                       start=(f == 0), stop=(f == 27)), reads=[dhT, dWd], writes=[dpd_], signal=(f == 27))
                            hs = slice(hf * 512, (hf + 1) * 512)
                            if moe and ex_ == 0:
                                P.op("dve", lambda e, sub=sub, hs=hs, pd_=pd_: e.tensor_copy(acc[:, sub, hs], pd_[:]), reads=[dpd_], writes=[dacc])
                            elif moe and ex_ < nexp - 1:
                                P.op("dve", lambda e, sub=sub, hs=hs, pd_=pd_: e.tensor_tensor(out=acc[:, sub, hs], in0=pd_[:], in1=acc[:, sub, hs], op=ALU.add),
                                     reads=[dpd_, dacc], writes=[dacc])
                            else:
                                if moe:
                                    P.op("dve", lambda e, sub=sub, hs=hs, pd_=pd_: e.tensor_tensor(out=acc[:, sub, hs], in0=pd_[:], in1=acc[:, sub, hs], op=ALU.add),
                                         reads=[dpd_, dacc], writes=[dacc])
                                    P.op("dve", lambda e, sub=sub, hs=hs, pr=pr, xrt=xrt: e.scalar_tensor_tensor(out=pr[:, hs], in0=xrt[:, hs], scalar=ALPHA, in1=acc[:, sub, hs],
                                                                                                              op0=ALU.mult, op1=ALU.add), reads=[dxr, dacc], writes=[dpr])
                                else:
                                    P.op("dve", lambda e, hs=hs, pr=pr, xrt=xrt, pd_=pd_: e.scalar_tensor_tensor(out=pr[:, hs], in0=xrt[:, hs], scalar=ALPHA, in1=pd_[:],
                                                                                                              op0=ALU.mult, op1=ALU.add), reads=[dxr, dpd_], writes=[dpr])
                        if ex_ == nexp - 1:
                            xs, dxs = (xTs[0] if xTs is not None else None), P.dep("e_xTs0")
                            self.ln_tile(L, pr, dpr, xs, dxs, sub, res_dst=out_res[r0:r0 + 128, :], dres=dres)
                if out_T is not None:
                    xs, dxs = xTs[0], P.dep("e_xTs0")
                    P.dma("sp", out_T.rearrange("(c p) t -> p c t", p=128)[:, :, ts_], xs[:], reads=[dxs], writes=[dxT])


_CACHE = {}


def _rope_tables(S):
    pos = np.arange(S, dtype=np.float32)
    inv = (np.float32(10000.0) ** (-np.arange(0, 32, 2, dtype=np.float32) / np.float32(32))).astype(np.float32)
    ang = pos[:, None] * inv[None, :]
    c = np.cos(ang).astype(np.float32).T
    s = np.sin(ang).astype(np.float32).T
    return np.concatenate([c, c], 0), np.concatenate([s, s], 0)


def _get_prog(S, layers, do_ln_in, final_out, dbg=()):
    key = (S, tuple(layers), do_ln_in, final_out, tuple(dbg))
    if key not in _CACHE:
        _CACHE[key] = Builder(S, list(layers), do_ln_in, final_out, dbg).build()
    return _CACHE[key]


WEIGHT_KEYS = ["w_in", "conv_w", "q_norm_g", "w_uq", "kv_norm_g", "w_ukv", "lru_conv_w", "lru_conv_b", "lru_wa", "lru_ba",
               "lru_wi", "lru_bi", "lru_lam", "mix_norm_g", "w_out", "ln1_g", "ln1_b", "ln2_g", "ln2_b"]


def _core_common(S, inputs, half, layers):
    T = S // 2
    C, Sn = _rope_tables(S)
    order = np.concatenate([np.arange(half * T, (half + 1) * T), np.arange((1 - half) * T, (2 - half) * T)])
    flags = np.zeros((128, 2), np.float32)
    flags[:, half] = 1.0
    m = {"flags": flags, "ropeC": np.ascontiguousarray(C[:, order]), "ropeS": np.ascontiguousarray(Sn[:, order])}
    for k in WEIGHT_KEYS:
        m[k] = np.ascontiguousarray(inputs[k])
    if 0 in layers:
        for k in ("dense_w_gate", "dense_w_up", "dense_w_down"):
            m[k] = np.ascontiguousarray(inputs[k])
    if 1 in layers:
        for k in ("moe_w_router", "moe_w_gate", "moe_w_up", "moe_w_down"):
            m[k] = np.ascontiguousarray(inputs[k])
    return m


def kernel(**inputs):
    x = np.asarray(inputs["x"])
    B, S, _ = x.shape
    T = S // 2
    ncore = 2 * B
    key = ("fused", S)
    if key not in _CACHE:
        _CACHE[key] = Builder(S, [0, 1], True, True).build_fused()
    nc = _CACHE[key]
    C, Sn = _rope_tables(S)
    maps = []
    for core in range(ncore):
        b, half = core // 2, core % 2
        order = np.concatenate([np.arange(half * T, (half + 1) * T), np.arange((1 - half) * T, (2 - half) * T)])
        flags = np.zeros((128, 2), np.float32)
        flags[:, half] = 1.0
        m = {"flags": flags, "ropeC0": C, "ropeS0": Sn,
             "ropeC1": np.ascontiguousarray(C[:, order]), "ropeS1": np.ascontiguousarray(Sn[:, order]),
             "x_full": np.ascontiguousarray(x[b]),
             "ln_in_g": np.ascontiguousarray(inputs["ln_in_g"]), "ln_in_b": np.ascontiguousarray(inputs["ln_in_b"])}
        for k in WEIGHT_KEYS + ["dense_w_gate", "dense_w_up", "dense_w_down", "moe_w_router", "moe_w_gate", "moe_w_up", "moe_w_down"]:
            m[k] = np.ascontiguousarray(inputs[k])
        maps.append(m)
    res = run_bass_kernel_spmd(nc, maps, core_ids=list(range(ncore))).results
    out = np.empty((B, S, D), np.float32)
    for core in range(ncore):
        b, half = core // 2, core % 2
        out[b, half * T:(half + 1) * T] = res[core]["y_out"]
    return out


def kernel_unfused(**inputs):
    x = np.asarray(inputs["x"])
    B, S, _ = x.shape
    T = S // 2
    ncore = 2 * B
    nc0 = _get_prog(S, (0,), True, False)
    maps = []
    for core in range(ncore):
        b, half = core // 2, core % 2
        m = _core_common(S, inputs, half, (0,))
        m["x_own"] = np.ascontiguousarray(x[b, half * T:(half + 1) * T])
        m["x_oth"] = np.ascontiguousarray(x[b, (1 - half) * T:(2 - half) * T])
        m["ln_in_g"] = np.ascontiguousarray(inputs["ln_in_g"])
        m["ln_in_b"] = np.ascontiguousarray(inputs["ln_in_b"])
        maps.append(m)
    r0 = run_bass_kernel_spmd(nc0, maps, core_ids=list(range(ncore))).results
    nc1 = _get_prog(S, (1,), False, True)
    maps = []
    for core in range(ncore):
        half = core % 2
        m = _core_common(S, inputs, half, (1,))
        m["xres_in"] = r0[core]["xres_out"]
        m["xT_in"] = np.ascontiguousarray(np.concatenate([r0[core]["xT_out"], r0[core ^ 1]["xT_out"]], axis=1))
        maps.append(m)
    r1 = run_bass_kernel_spmd(nc1, maps, core_ids=list(range(ncore))).results
    out = np.empty((B, S, D), np.float32)
    for core in range(ncore):
        b, half = core // 2, core % 2
        out[b, half * T:(half + 1) * T] = r1[core]["y_out"]
    return out
```

```python
import numpy as np
from contextlib import ExitStack
import concourse.bass as bass
import concourse.mybir as mybir
from concourse.bass_utils import run_bass_kernel_spmd

F32 = mybir.dt.float32
BF16 = mybir.dt.bfloat16
AF = mybir.ActivationFunctionType
ALU = mybir.AluOpType

D = 1024
DIN = 1952
NH = 8
DFF = 3584
NE = 8
ALPHA = 4.0 ** 0.25
LN_EPS = 1e-5
RMS_EPS = 1e-6
QSCALE = 96.0 ** -0.5
import os as _os
LNSTOP = int(_os.environ.get("MK_LNSTOP", "99"))
ASTOP = int(_os.environ.get("MK_ASTOP", "99"))


class Dep:
    __slots__ = ("name", "w", "r", "dsem", "dcnt", "excl")

    def __init__(self, name=""):
        self.name = name
        self.excl = any(t in name for t in ("_ps", "_pS", "_pO", "_pD", "_prr", "_pg", "_pu", "_pd", "_pc", "pT"))
        self.w = None
        self.r = []
        self.dsem = None
        self.dcnt = 0


class _Rec:
    def __init__(self):
        self.call = None

    def __getattr__(self, name):
        def f(*a, **k):
            self.call = (name, a, k)
            return self
        return f


class Prog:
    ENGS = ("pe", "act", "dve", "pool", "sp")

    def __init__(self, nc):
        self.nc = nc
        self.streams = {e: [] for e in self.ENGS}
        self.sem = {e: nc.alloc_semaphore("es_" + e) for e in self.ENGS}
        self.cnt = {e: 0 for e in self.ENGS}
        self.seen = {e: {} for e in self.ENGS}
        self.deps = {}
        self.ndsem = 0
        self.store_q = "act"

    def dep(self, name):
        d = self.deps.get(name)
        if d is None:
            d = Dep(name)
            self.deps[name] = d
        return d

    def _dsem(self, d):
        if d.dsem is None:
            d.dsem = self.nc.alloc_semaphore("ds_%d" % self.ndsem)
            self.ndsem += 1
        return d.dsem

    def _collect(self, eng, reads, writes):
        need = {}

        def add(t):
            if t is None:
                return
            sem, val = t
            k = id(sem)
            if k not in need or need[k][1] < val:
                need[k] = (sem, val)
        own_sem = self.sem[eng]
        for d in reads:
            add(d.w)
            if d.excl:
                for t in d.r:
                    if t[0] is not own_sem:
                        add(t)
        for d in writes:
            add(d.w)
            for t in d.r:
                add(t)
        out = []
        seen = self.seen[eng]
        own = self.sem[eng]
        for k, (sem, val) in need.items():
            if sem is own and (eng == "pe" or val > self.cnt[eng]):
                continue
            if seen.get(k, 0) >= val:
                continue
            seen[k] = val
            out.append((sem, val))
        return out

    def _update(self, tk, reads, writes):
        for d in reads:
            d.r.append(tk)
            if len(d.r) > 64:
                d.r = d.r[-48:]
        for d in writes:
            d.w = tk
            d.r = []

    def op(self, eng, fn, reads=(), writes=(), signal=True):
        waits = self._collect(eng, reads, writes)
        sem = self.sem[eng]
        if signal:
            self.cnt[eng] += 1
            tk = (sem, self.cnt[eng])
        else:
            tk = (sem, self.cnt[eng] + 1)

        rec = _Rec()
        fn(rec)
        call = rec.call

        def emit(e, call=call, waits=waits, signal=signal, sem=sem):
            for s, v in waits:
                e.wait_ge(s, v)
            ins = getattr(e, call[0])(*call[1], **call[2])
            if signal:
                ins.then_inc(sem, 1)
        self.streams[eng].append(emit)
        self._update(tk, reads, writes)
        return tk

    def dma(self, q, out, in_, reads=(), writes=(), **kw):
        if q == "sp" and self.store_q is not None and type(out.tensor).__name__.startswith("DRam") \
                and not type(in_.tensor).__name__.startswith("DRam"):
            q = self.store_q
        waits = self._collect(q, reads, writes)
        d0 = writes[0]
        sem = self._dsem(d0)
        d0.dcnt += 16
        tk = (sem, d0.dcnt)

        def emit(e, waits=waits, sem=sem, out=out, in_=in_, kw=kw):
            for s, v in waits:
                e.wait_ge(s, v)
            e.dma_start(out=out, in_=in_, **kw).then_inc(sem, 16)
        self.streams[q].append(emit)
        self._update(tk, reads, writes)
        return tk

    def idma(self, out, out_idx, in_, in_idx, reads=(), writes=(), **kw):
        q = "pool"
        waits = self._collect(q, reads, writes)
        d0 = writes[0]
        sem = self._dsem(d0)
        d0.dcnt += 16
        tk = (sem, d0.dcnt)

        def emit(e, waits=waits, sem=sem):
            for s_, v in waits:
                e.wait_ge(s_, v)
            oo = bass.IndirectOffsetOnAxis(ap=out_idx, axis=0) if out_idx is not None else None
            io = bass.IndirectOffsetOnAxis(ap=in_idx, axis=0) if in_idx is not None else None
            e.indirect_dma_start(out=out, out_offset=oo, in_=in_, in_offset=io, **kw).then_inc(sem, 16)
        self.streams[q].append(emit)
        self._update(tk, reads, writes)
        return tk

    def barrier(self):
        tks = [(self.sem[e], self.cnt[e]) for e in self.ENGS if self.cnt[e] > 0]
        for d in self.deps.values():
            if d.dsem is not None and d.dcnt > 0:
                tks.append((d.dsem, d.dcnt))
        for e in self.ENGS:
            seen = self.seen[e]
            waits = []
            for sem, val in tks:
                if sem is self.sem[e]:
                    continue
                if seen.get(id(sem), 0) >= val:
                    continue
                seen[id(sem)] = val
                waits.append((sem, val))

            def emit(en, waits=waits):
                for s, v in waits:
                    en.wait_ge(s, v)
            self.streams[e].append(emit)
        for d in self.deps.values():
            d.w = None
            d.r = []

    def finish(self):
        nc = self.nc
        st = self.streams
        with nc.Block() as block:
            @block.tensor
            def _(e):
                for f in st["pe"]:
                    f(e)

            @block.scalar
            def _(e):
                for f in st["act"]:
                    f(e)

            @block.vector
            def _(e):
                for f in st["dve"]:
                    f(e)

            @block.gpsimd
            def _(e):
                for f in st["pool"]:
                    f(e)

            @block.sync
            def _(e):
                for f in st["sp"]:
                    f(e)
        return nc


class Builder:
    def __init__(self, S, layers, do_ln_in, final_out, dbg=()):
        self.S = S
        self.T = S // 2
        self.NB = self.T // 512
        self.layers = layers
        self.dbg = set(dbg)
        nc = bass.Bass("TRN2", target_bir_lowering=False)
        self.nc = nc
        self.P = Prog(nc)
        self.do_ln_in = do_ln_in
        self.final_out = final_out
        self.cnt = 0
        self.ps_rr = 0
        self.has_other = True

    def din(self, name, shape, dt=F32):
        return self.nc.dram_tensor(name, list(shape), dt, kind="ExternalInput").ap()

    def dout(self, name, shape, dt=F32):
        return self.nc.dram_tensor(name, list(shape), dt, kind="ExternalOutput").ap()

    def dscr(self, name, shape, dt):
        kind = "ExternalOutput" if name in self.dbg else "Internal"
        return self.nc.dram_tensor(name, list(shape), dt, kind=kind).ap()

    def sb(self, st, name, shape, dt):
        self.cnt += 1
        return st.enter_context(self.nc.sbuf_tensor("%s_%d" % (name, self.cnt), list(shape), dt))

    def psum(self, st, name, shape, dt=F32):
        self.cnt += 1
        return st.enter_context(self.nc.psum_tensor("%s_%d" % (name, self.cnt), list(shape), dt))

    def load_cols(self, st, name, vec_ap, n, q="sp"):
        P = self.P
        nchunk = n // 128
        t = self.sb(st, name, [128, nchunk], F32)
        d = P.dep(name)
        v2 = vec_ap.rearrange("(c p o) -> c p o", p=128, o=1)
        for c in range(nchunk):
            P.dma(q, t[:, c:c + 1], v2[c], writes=[d])
        return t, d

    def build(self):
        nc, P, T, NB = self.nc, self.P, self.T, self.NB
        S2 = 2 * T
        I = {}
        if self.do_ln_in:
            I["x_own"] = self.din("x_own", [T, D])
            I["x_oth"] = self.din("x_oth", [T, D])
            I["ln_in_g"] = self.din("ln_in_g", [D])
            I["ln_in_b"] = self.din("ln_in_b", [D])
        else:
            I["xres_in"] = self.din("xres_in", [T, D])
            I["xT_in"] = self.din("xT_in", [D, S2], BF16)
        I["flags"] = self.din("flags", [128, 2])
        I["ropeC"] = self.din("ropeC", [32, S2])
        I["ropeS"] = self.din("ropeS", [32, S2])
        nl = 2
        shapes = dict(w_in=[nl, D, DIN], conv_w=[nl, 3, 256], q_norm_g=[nl, 384], w_uq=[nl, 384, 768],
                      kv_norm_g=[nl, 256], w_ukv=[nl, 256, 1024], lru_conv_w=[nl, 4, 256],
                      lru_conv_b=[nl, 256], lru_wa=[nl, 2, 4, 64, 64], lru_ba=[nl, 2, 256],
                      lru_wi=[nl, 2, 4, 64, 64], lru_bi=[nl, 2, 256], lru_lam=[nl, 2, 256],
                      mix_norm_g=[nl, D], w_out=[nl, D, D], ln1_g=[nl, D], ln1_b=[nl, D],
                      dense_w_gate=[1, D, DFF], dense_w_up=[1, D, DFF], dense_w_down=[1, DFF, D],
                      moe_w_router=[1, D, NE], moe_w_gate=[1, NE, D, DFF], moe_w_up=[1, NE, D, DFF],
                      moe_w_down=[1, NE, DFF, D], ln2_g=[nl, D], ln2_b=[nl, D])
        need_dense = 0 in self.layers
        need_moe = 1 in self.layers
        for k, shp in shapes.items():
            if k.startswith("dense") and not need_dense:
                continue
            if k.startswith("moe") and not need_moe:
                continue
            I[k] = self.din(k, shp)
        self.I = I
        if self.final_out:
            self.y_out = self.dout("y_out", [T, D])
        else:
            self.y_out = self.dout("xres_out", [T, D])
            self.xT_out = self.dout("xT_out", [D, T], BF16)
        self.xres = self.dscr("s_xres", [T, D], F32)
        self.xT = self.dscr("s_xT", [D, S2], BF16)
        self.QT = self.dscr("s_QT", [NH, 96, T], BF16)
        self.KT = self.dscr("s_KT", [NH, 96, S2], BF16)
        self.Vd = self.dscr("s_V", [S2, NH * 65], BF16)
        self.CB = self.dscr("s_CB", [256, T], F32)
        self.PP = self.dscr("s_PP", [256, S2], F32)
        self.LG = self.dscr("s_LG", [256, T], F32)
        self.LX = self.dscr("s_LX", [256, S2], F32)
        self.Y = self.dscr("s_Y", [D, T], F32)
        self.x1res = self.dscr("s_x1res", [T, D], F32)
        self.x1T = self.dscr("s_x1T", [D, T], BF16)
        self.comb = self.dscr("s_comb", [T, NE], F32)
        self.wb = {}
        for l in self.layers:
            self.wb[("w_in", l)] = self.dscr("b_w_in%d" % l, [D, DIN], BF16)
            self.wb[("w_out", l)] = self.dscr("b_w_out%d" % l, [D, D], BF16)
        if need_dense:
            self.wb["dg"] = self.dscr("b_dg", [D, DFF], BF16)
            self.wb["du"] = self.dscr("b_du", [D, DFF], BF16)
            self.wb["dd"] = self.dscr("b_dd", [DFF, D], BF16)
        if need_moe:
            self.wb["mg"] = self.dscr("b_mg", [NE, D, DFF], BF16)
            self.wb["mu"] = self.dscr("b_mu", [NE, D, DFF], BF16)
            self.wb["md"] = self.dscr("b_md", [NE, DFF, D], BF16)

        with ExitStack() as gst:
            import os
            stop = int(os.environ.get("MK_STOP", "99"))
            self.consts(gst)
            if stop >= 1:
                self.cast_weights()
            if self.do_ln_in:
                self.ln_srcs = ((I["x_own"], 0, True), (I["x_oth"], T, False))
                self.ropeC, self.ropeS = I["ropeC"], I["ropeS"]
                if stop >= 2:
                    self.phase_ln_in()
                xres, xT = self.xres, self.xT
            else:
                self.ropeC, self.ropeS = I["ropeC"], I["ropeS"]
                xres, xT = I["xres_in"], I["xT_in"]
            for li, l in enumerate(self.layers):
                last = li == len(self.layers) - 1
                if stop >= 3:
                    self.phase_a(l, xT)
                if stop >= 4:
                    self.phase_b(l)
                if stop >= 5:
                    self.phase_c(l)
                if stop >= 6:
                    self.phase_d(l, xres)
                if stop < 7:
                    continue
                if last:
                    out_res = self.y_out
                    out_T = None if self.final_out else self.xT_out
                else:
                    out_res, out_T = self.xres, self.xT
                self.phase_e(l, out_res, out_T)
                xres, xT = self.xres, self.xT
            P.barrier()
        P.finish()
        return nc

    def build_fused(self):
        nc, P, S = self.nc, self.P, self.S
        Th = S // 2
        I = {}
        I["x_full"] = self.din("x_full", [S, D])
        I["ln_in_g"] = self.din("ln_in_g", [D])
        I["ln_in_b"] = self.din("ln_in_b", [D])
        I["flags"] = self.din("flags", [128, 2])
        for k in ("ropeC0", "ropeS0", "ropeC1", "ropeS1"):
            I[k] = self.din(k, [32, S])
        nl = 2
        shapes = dict(w_in=[nl, D, DIN], conv_w=[nl, 3, 256], q_norm_g=[nl, 384], w_uq=[nl, 384, 768],
                      kv_norm_g=[nl, 256], w_ukv=[nl, 256, 1024], lru_conv_w=[nl, 4, 256],
                      lru_conv_b=[nl, 256], lru_wa=[nl, 2, 4, 64, 64], lru_ba=[nl, 2, 256],
                      lru_wi=[nl, 2, 4, 64, 64], lru_bi=[nl, 2, 256], lru_lam=[nl, 2, 256],
                      mix_norm_g=[nl, D], w_out=[nl, D, D], ln1_g=[nl, D], ln1_b=[nl, D],
                      dense_w_gate=[1, D, DFF], dense_w_up=[1, D, DFF], dense_w_down=[1, DFF, D],
                      moe_w_router=[1, D, NE], moe_w_gate=[1, NE, D, DFF], moe_w_up=[1, NE, D, DFF],
                      moe_w_down=[1, NE, DFF, D], ln2_g=[nl, D], ln2_b=[nl, D])
        for k, shp in shapes.items():
            I[k] = self.din(k, shp)
        self.I = I
        self.y_out = self.dout("y_out", [Th, D])
        self.xres = self.dscr("s_xres", [S, D], F32)
        self.xT = self.dscr("s_xT", [D, S], BF16)
        self.QT = self.dscr("s_QT", [NH, 96, S], BF16)
        self.KT = self.dscr("s_KT", [NH, 96, S], BF16)
        self.Vd = self.dscr("s_V", [S, NH * 65], BF16)
        self.CB = self.dscr("s_CB", [256, S], F32)
        self.PP = self.dscr("s_PP", [256, S], F32)
        self.LG = self.dscr("s_LG", [256, S], F32)
        self.LX = self.dscr("s_LX", [256, S], F32)
        self.Y = self.dscr("s_Y", [D, S], F32)
        self.x1res = self.dscr("s_x1res", [S, D], F32)
        self.x1T = self.dscr("s_x1T", [D, S], BF16)
        self.comb = self.dscr("s_comb", [S, NE], F32)
        o0res = self.dscr("s_o0res", [S, D], F32)
        o0T = self.dscr("s_o0T", [D, S], BF16)
        self.wb = {}
        for l in (0, 1):
            self.wb[("w_in", l)] = self.dscr("b_w_in%d" % l, [D, DIN], BF16)
            self.wb[("w_out", l)] = self.dscr("b_w_out%d" % l, [D, D], BF16)
        self.wb["dg"] = self.dscr("b_dg", [7 * 128, 8 * 512], BF16)
        self.wb["du"] = self.dscr("b_du", [7 * 128, 8 * 512], BF16)
        self.wb["dd"] = self.dscr("b_dd", [DFF, D], BF16)
        self.WGt = self.dscr("b_WGt", [NE * 7 * 128, 8 * 512], BF16)
        self.WUt = self.dscr("b_WUt", [NE * 7 * 128, 8 * 512], BF16)
        self.WDt = self.dscr("b_WDt", [NE * 4 * 128, 7 * D], BF16)
        self.X1B = self.dscr("s_X1B", [Th, D], BF16)
        ntile = (2 * Th + NE * 511) // 512
        self.XS = self.dscr("s_XS", [ntile * 512, D], BF16)
        self.YS = self.dscr("s_YS", [ntile * 512, D], F32)
        self.moe_tables = True
        self.layers = [0, 1]
        with ExitStack() as gst:
            self.consts(gst)
            self.cast_weights(part=0)
            import os
            fstop = int(os.environ.get("MK_FUSED_STOP", "99"))
            self.mstop = 99
            steps = []
            def set0():
                self.T, self.NB, self.has_other = S, S // 512, False
                self.ropeC, self.ropeS = I["ropeC0"], I["ropeS0"]
                self.ln_srcs = ((I["x_full"], 0, True),)
            def set1():
                self.T, self.NB, self.has_other = Th, Th // 512, True
            def set1b():
                self.ropeC, self.ropeS = I["ropeC1"], I["ropeS1"]
            steps = [lambda: (set0(), self.phase_ln_in()),
                     lambda: (self.phase_a(0, self.xT), self.cast_weights(part=1)),
                     lambda: self.phase_b(0),
                     lambda: self.phase_c(0),
                     lambda: self.phase_d(0, self.xres),
                     lambda: self.phase_e(0, o0res, o0T),
                     lambda: (set1(), self.phase_sel(o0res, o0T), set1b()),
                     lambda: self.phase_a(1, self.xT),
                     lambda: self.phase_b(1),
                     lambda: self.phase_c(1),
                     lambda: (self.route_setup(gst), self.phase_d(1, self.xres)),
                     lambda: self.phase_r(),
                     lambda: self.phase_e_moe(1, self.y_out)]
            for k, f in enumerate(steps):
                if k < fstop:
                    f()
            P.barrier()
        P.finish()
        return nc

    def phase_sel(self, o0res, o0T):
        P, T, NB = self.P, self.T, self.NB
        P.barrier()
        f0 = self.flags[:, 0:1]
        f1 = self.flags[:, 1:2]
        with ExitStack() as st:
            sb = lambda n, s, d: self.sb(st, n, s, d)
            ra = [sb("ra", [128, D], F32) for _ in range(2)]
            rb = [sb("rb", [128, D], F32) for _ in range(2)]
            dres = P.dep("d_xres")
            for i in range(T // 128):
                a_, da_ = ra[i % 2], P.dep("s_ra%d" % (i % 2))
                b_, db_ = rb[i % 2], P.dep("s_rb%d" % (i % 2))
                P.dma("sp", a_[:], o0res[i * 128:(i + 1) * 128, :], reads=[P.dep("d_outres")], writes=[da_])
                P.dma("sp", b_[:], o0res[T + i * 128:T + (i + 1) * 128, :], reads=[P.dep("d_outres")], writes=[db_])
                P.op("dve", lambda e, a_=a_: e.tensor_scalar(out=a_[:], in0=a_[:], scalar1=f0, scalar2=None, op0=ALU.mult), reads=[da_, self.dconst], writes=[da_])
                P.op("dve", lambda e, a_=a_, b_=b_: e.scalar_tensor_tensor(out=a_[:], in0=b_[:], scalar=f1, in1=a_[:], op0=ALU.mult, op1=ALU.add),
                     reads=[da_, db_, self.dconst], writes=[da_])
                P.dma("sp", self.xres[i * 128:(i + 1) * 128, :], a_[:], reads=[da_], writes=[dres])
            ta = [sb("ta", [128, 8, 512], BF16) for _ in range(2)]
            tb = [sb("tb", [128, 8, 512], BF16) for _ in range(2)]
            to = [sb("to", [128, 8, 512], BF16) for _ in range(2)]
            tt = [sb("tt", [128, 8, 512], BF16) for _ in range(2)]
            ov = o0T.rearrange("(c p) t -> p c t", p=128)
            xv = self.xT.rearrange("(c p) t -> p c t", p=128)
            dxT = P.dep("d_xT")
            for j in range(NB):
                i2 = j % 2
                a_, b_, o_, t_ = ta[i2], tb[i2], to[i2], tt[i2]
                da_, db_, do_, dt_ = (P.dep("s_%s%d" % (n, i2)) for n in ("ta", "tb", "to", "tt"))
                P.dma("sp", a_[:], ov[:, :, j * 512:(j + 1) * 512], reads=[P.dep("d_xT")], writes=[da_])
                P.dma("sp", b_[:], ov[:, :, T + j * 512:T + (j + 1) * 512], reads=[P.dep("d_xT")], writes=[db_])
                P.op("dve", lambda e, o_=o_, a_=a_: e.tensor_scalar(out=o_[:], in0=a_[:], scalar1=f0, scalar2=None, op0=ALU.mult), reads=[da_, self.dconst], writes=[do_])
                P.op("dve", lambda e, o_=o_, b_=b_: e.scalar_tensor_tensor(out=o_[:], in0=b_[:], scalar=f1, in1=o_[:], op0=ALU.mult, op1=ALU.add),
                     reads=[db_, do_, self.dconst], writes=[do_])
                P.op("pool", lambda e, t_=t_, a_=a_: e.tensor_scalar(out=t_[:], in0=a_[:], scalar1=f1, scalar2=None, op0=ALU.mult), reads=[da_, self.dconst], writes=[dt_])
                P.op("dve", lambda e, t_=t_, b_=b_: e.scalar_tensor_tensor(out=t_[:], in0=b_[:], scalar=f0, in1=t_[:], op0=ALU.mult, op1=ALU.add),
                     reads=[db_, dt_, self.dconst], writes=[dt_])
                P.dma("sp", xv[:, :, j * 512:(j + 1) * 512], o_[:], reads=[do_], writes=[dxT])
                P.dma("sp", xv[:, :, T + j * 512:T + (j + 1) * 512], t_[:], reads=[dt_], writes=[dxT])

    def consts(self, st):
        P = self.P
        self.ident = self.sb(st, "ident", [128, 128], BF16)
        self.identf = self.sb(st, "identf", [128, 128], F32)
        self.ones = self.sb(st, "ones", [128, 128], BF16)
        self.eps_ln = self.sb(st, "epsln", [128, 1], F32)
        self.eps_rms = self.sb(st, "epsrms", [128, 1], F32)
        self.flags = self.sb(st, "flags", [128, 2], F32)
        d = P.dep("consts")
        self.dconst = d
        P.op("pool", lambda e: e.memset(self.identf[:], 0.0), writes=[d])
        P.op("pool", lambda e: e.affine_select(out=self.identf[:], in_=self.identf[:], pattern=[[-1, 128]],
                                               compare_op=ALU.not_equal, fill=1.0, base=0,
                                               channel_multiplier=1), reads=[d], writes=[d])
        P.op("dve", lambda e: e.tensor_copy(self.ident[:], self.identf[:]), reads=[d], writes=[d])
        P.op("dve", lambda e: e.memset(self.ones[:], 1.0), writes=[d])
        P.op("dve", lambda e: e.memset(self.eps_ln[:], LN_EPS), writes=[d])
        P.op("dve", lambda e: e.memset(self.eps_rms[:], RMS_EPS), writes=[d])
        P.dma("sp", self.flags[:], self.I["flags"], writes=[d])

    def cast_weights(self, part=None):
        P, I = self.P, self.I
        if not hasattr(self, "dwc"):
            self.dwc = {}
        layers_sel = self.layers if part is None else [part]

        def cast2d(dst, src, rows, cols, key):
            d = P.dep("wc_" + key)
            self.dwc[key] = d
            cstep = cols
            while cstep > 2048:
                cstep //= 2
            rstep = max(1, min(rows, 4096 // (cols // cstep)))
            for r0 in range(0, rows, rstep):
                for c0 in range(0, cols, cstep):
                    P.dma("pool", dst[r0:r0 + rstep, c0:c0 + cstep], src[r0:r0 + rstep, c0:c0 + cstep],
                          writes=[d])
        for l in layers_sel:
            cast2d(self.wb[("w_in", l)], I["w_in"][l], D, DIN, "w_in%d" % l)
            cast2d(self.wb[("w_out", l)], I["w_out"][l], D, D, "w_out%d" % l)
        if 0 in layers_sel and getattr(self, "moe_tables", False):
            for key, tab, src in (("g", self.wb["dg"], I["dense_w_gate"]), ("u", self.wb["du"], I["dense_w_up"])):
                d = P.dep("wc_" + key)
                self.dwc[key] = d
                for g in range(7):
                    P.dma("pool", tab[g * 128:(g + 1) * 128, :].rearrange("p (c n) -> p c n", c=8),
                          src[0][:, g * 512:(g + 1) * 512].rearrange("(c p) n -> p c n", p=128), writes=[d])
        elif 0 in layers_sel:
            cast2d(self.wb["dg"], I["dense_w_gate"][0], D, DFF, "g")
            cast2d(self.wb["du"], I["dense_w_up"][0], D, DFF, "u")
        if 0 in layers_sel:
            cast2d(self.wb["dd"], I["dense_w_down"][0], DFF, D, "d")
        if 1 in layers_sel and getattr(self, "moe_tables", False):
            for key, tab, src in (("mg", self.WGt, I["moe_w_gate"]), ("mu", self.WUt, I["moe_w_up"])):
                d = P.dep("wc_" + key)
                self.dwc[key] = d
                for e in range(NE):
                    for g in range(7):
                        r0 = (e * 7 + g) * 128
                        P.dma("pool", tab[r0:r0 + 128, :].rearrange("p (c n) -> p c n", c=8),
                              src[0, e][:, g * 512:(g + 1) * 512].rearrange("(c p) n -> p c n", p=128), writes=[d])
            d = P.dep("wc_md")
            self.dwc["md"] = d
            for e in range(NE):
                for q in range(4):
                    r0 = (e * 4 + q) * 128
                    P.dma("pool", self.WDt[r0:r0 + 128, :].rearrange("p (f n) -> p f n", f=7),
                          I["moe_w_down"][0, e][q * 896:(q + 1) * 896, :].rearrange("(f p) n -> p f n", p=128), writes=[d])
        elif 1 in layers_sel:
            for e in range(NE):
                cast2d(self.wb["mg"][e], I["moe_w_gate"][0, e], D, DFF, "mg")
                cast2d(self.wb["mu"][e], I["moe_w_up"][0, e], D, DFF, "mu")
                cast2d(self.wb["md"][e], I["moe_w_down"][0, e], DFF, D, "md")

    def ln_setup(self, st, g_ap, b_ap, tag, nbuf=2):
        P = self.P
        L = {}
        L["G"] = self.sb(st, "lnG", [128, D], F32)
        L["B"] = self.sb(st, "lnB", [128, D], F32)
        L["dgb"] = P.dep("lnGB")
        P.dma("sp", L["G"][:], g_ap.partition_broadcast(128), writes=[L["dgb"]])
        P.dma("sp", L["B"][:], b_ap.partition_broadcast(128), writes=[L["dgb"]])
        L["n"] = 0
        L["nbuf"] = nbuf
        for i in range(nbuf):
            L["st%d" % i] = self.sb(st, "lnst", [128, 2, 6], F32)
            L["mv%d" % i] = self.sb(st, "lnmv", [128, 2], F32)
            L["sd%d" % i] = self.sb(st, "lnsd", [128, 1], F32)
            L["rs%d" % i] = self.sb(st, "lnrs", [128, 1], F32)
            L["xn%d" % i] = self.sb(st, "lnxn", [128, D], F32)
            L["xo%d" % i] = self.sb(st, "lnxo", [128, D], F32)
            L["xb%d" % i] = self.sb(st, "lnxb", [128, D], BF16)
            L["pT%d" % i] = self.psum(st, "lnpT", [128, D], BF16)
        return L

    def ln_tile(self, L, xt, dxt, xTs, dxTs, sub, res_dst=None, dres=None, xb_dst=None, dxbd=None):
        P = self.P
        i = L["n"] % L["nbuf"]
        L["n"] += 1
        tg = "ln%d" % i
        st_, mv, sd, rs, xn, xo, xb, pT = (L[k + str(i)] for k in ("st", "mv", "sd", "rs", "xn", "xo", "xb", "pT"))
        dst_, dmv, dsd, drs, dxn, dxo, dxb, dpT = (P.dep(tg + k) for k in ("st", "mv", "sd", "rs", "xn", "xo", "xb", "pT"))
        for h in range(2):
            P.op("dve", lambda e, h=h: e.bn_stats(out=st_[:, h, :], in_=xt[:, h * 512:(h + 1) * 512]),
                 reads=[dxt], writes=[dst_])
        if LNSTOP < 1:
            return
        P.op("dve", lambda e: e.bn_aggr(out=mv[:], in_=st_[:].rearrange("p a b -> p (a b)")), reads=[dst_], writes=[dmv])
        if LNSTOP < 2:
            return
        P.op("act", lambda e: e.activation(out=sd[:], in_=mv[:, 1:2], func=AF.Sqrt, bias=self.eps_ln[:], scale=1.0),
             reads=[dmv, self.dconst], writes=[dsd])
        if LNSTOP < 3:
            return
        P.op("dve", lambda e: e.reciprocal(out=rs[:], in_=sd[:]), reads=[dsd], writes=[drs])
        if LNSTOP < 4:
            return
        P.op("dve", lambda e: e.tensor_scalar(out=xn[:], in0=xt[:], scalar1=mv[:, 0:1], scalar2=rs[:],
                                               op0=ALU.subtract, op1=ALU.mult), reads=[dxt, dmv, drs], writes=[dxn])
        if LNSTOP < 5:
            return
        P.op("pool", lambda e: e.tensor_tensor(out=xn[:], in0=xn[:], in1=L["G"][:], op=ALU.mult),
             reads=[dxn, L["dgb"]], writes=[dxn])
        P.op("pool", lambda e: e.tensor_tensor(out=xo[:], in0=xn[:], in1=L["B"][:], op=ALU.add),
             reads=[dxn, L["dgb"]], writes=[dxo])
        if LNSTOP < 6:
            return
        if res_dst is not None:
            P.dma("sp", res_dst, xo[:], reads=[dxo], writes=[dres])
        if LNSTOP < 7:
            return
        if xTs is None:
            return
        P.op("act", lambda e: e.activation(out=xb[:], in_=xo[:], func=AF.Copy), reads=[dxo], writes=[dxb])
        if xb_dst is not None:
            P.dma("sp", xb_dst, xb[:], reads=[dxb], writes=[dxbd])
        if LNSTOP < 8:
            return
        for c in range(8):
            P.op("pe", lambda e, c=c: e.transpose(pT[:, c * 128:(c + 1) * 128], xb[:, c * 128:(c + 1) * 128], self.ident[:]),
                 reads=[dxb, self.dconst], writes=[dpT], signal=(c == 7))
        if LNSTOP < 9:
            return
        P.op("act", lambda e: e.activation(out=xTs[:, :, sub * 128:(sub + 1) * 128],
                                           in_=pT[:].rearrange("p (c t) -> p c t", c=8), func=AF.Copy),
             reads=[dpT], writes=[dxTs])

    def phase_ln_in(self):
        P, T, NB, I = self.P, self.T, self.NB, self.I
        P.barrier()
        with ExitStack() as st:
            L = self.ln_setup(st, I["ln_in_g"], I["ln_in_b"], "in")
            xt = [self.sb(st, "xt", [128, D], F32) for _ in range(2)]
            xTs = [self.sb(st, "xTs", [128, 8, 512], BF16) for _ in range(2)]
            dres = P.dep("d_xres")
            dxT = P.dep("d_xT")
            k = 0
            for src, base, own in self.ln_srcs:
                for b in range(NB):
                    xs, dxs = xTs[b % 2], P.dep("xTs%d" % (b % 2))
                    for sub in range(4):
                        r0 = b * 512 + sub * 128
                        xtt, dx = xt[k % 2], P.dep("xt%d" % (k % 2))
                        k += 1
                        P.dma("sp", xtt[:], src[r0:r0 + 128, :], writes=[dx])
                        self.ln_tile(L, xtt, dx, xs, dxs, sub,
                                     res_dst=self.xres[r0:r0 + 128, :] if own else None, dres=dres)
                    P.dma("sp", self.xT.rearrange("(c p) t -> p c t", p=128)[:, :, base + b * 512: base + (b + 1) * 512],
                          xs[:], reads=[dxs], writes=[dxT])

    def rms_rstd(self, sq, dsq, nchunk, n, pst, dpst, rstd, drstd, sdt, dsdt):
        P = self.P
        for c in range(nchunk):
            P.op("pe", lambda e, c=c: e.matmul(pst[:], lhsT=self.ones[:], rhs=sq[:, c, :], start=(c == 0), stop=(c == nchunk - 1)),
                 reads=[dsq, self.dconst], writes=[dpst], signal=(c == nchunk - 1))
        P.op("act", lambda e: e.activation(out=sdt[:], in_=pst[:], func=AF.Sqrt, bias=self.eps_rms[:], scale=1.0 / n),
             reads=[dpst, self.dconst], writes=[dsdt])
        P.op("dve", lambda e: e.reciprocal(out=rstd[:], in_=sdt[:]), reads=[dsdt], writes=[drstd])

    def phase_a(self, l, xT):
        P, T, NB, I = self.P, self.T, self.NB, self.I
        NBT = (2 * NB) if self.has_other else NB
        P.barrier()
        with ExitStack() as st:
            sb = lambda n, s, d: self.sb(st, n, s, d)
            Win = sb("Win", [128, 8, DIN], BF16)
            dW = P.dep("a_W")
            wv = self.wb[("w_in", l)].rearrange("(c p) n -> p c n", p=128)
            for c in range(8):
                P.dma("sp", Win[:, c, :], wv[:, c, :], reads=[self.dwc["w_in%d" % l]], writes=[dW])
            Wkr = sb("Wkr", [128, 8, 96], BF16)
            Wkrs = sb("Wkrs", [128, 8, 96], BF16)
            dW2 = P.dep("a_W2")
            P.op("dve", lambda e: e.memset(Wkr[:], 0.0), writes=[dW2])
            P.op("dve", lambda e: e.memset(Wkrs[:], 0.0), writes=[dW2])
            P.op("dve", lambda e: e.tensor_copy(Wkr[:, :, 64:96], Win[:, :, 1408:1440]), reads=[dW], writes=[dW2])
            P.op("dve", lambda e: e.tensor_scalar(out=Wkrs[:, :, 64:80], in0=Win[:, :, 1424:1440], scalar1=-1.0, scalar2=None,
                                                   op0=ALU.mult), reads=[dW], writes=[dW2])
            P.op("dve", lambda e: e.tensor_copy(Wkrs[:, :, 80:96], Win[:, :, 1408:1424]), reads=[dW], writes=[dW2])
            gq, dgq = self.load_cols(st, "gq", I["q_norm_g"][l], 384)
            gkv, dgkv = self.load_cols(st, "gkv", I["kv_norm_g"][l], 256)
            wqf = sb("wqf", [128, 3, 768], F32)
            dwqf = P.dep("a_wqf")
            P.dma("sp", wqf[:], I["w_uq"][l].rearrange("(c p) n -> p c n", p=128), writes=[dwqf])
            Wq = sb("Wq", [128, 3, 768], BF16)
            Wqs = sb("Wqs", [128, 3, 768], BF16)
            dWq = P.dep("a_Wq")
            P.op("pool", lambda e: e.memset(Wqs[:], 0.0), writes=[dWq])
            for c in range(3):
                P.op("dve", lambda e, c=c: e.tensor_scalar(out=Wq[:, c, :], in0=wqf[:, c, :], scalar1=gq[:, c:c + 1],
                                                            scalar2=QSCALE, op0=ALU.mult, op1=ALU.mult),
                     reads=[dwqf, dgq], writes=[dWq])
                wq4 = Wq[:, c, :].rearrange("p (h r) -> p h r", h=NH)
                ws4 = Wqs[:, c, :].rearrange("p (h r) -> p h r", h=NH)
                P.op("dve", lambda e, wq4=wq4, ws4=ws4: e.tensor_scalar(out=ws4[:, :, 64:80], in0=wq4[:, :, 80:96], scalar1=-1.0,
                                                                        scalar2=None, op0=ALU.mult), reads=[dWq], writes=[dWq])
                P.op("dve", lambda e, wq4=wq4, ws4=ws4: e.tensor_copy(ws4[:, :, 80:96], wq4[:, :, 64:80]), reads=[dWq], writes=[dWq])
            wkvf = sb("wkvf", [128, 2, 1024], F32)
            dwkvf = P.dep("a_wkvf")
            P.dma("sp", wkvf[:], I["w_ukv"][l].rearrange("(c p) n -> p c n", p=128), writes=[dwkvf])
            Wkn = sb("Wkn", [128, 2, 512], BF16)
            Wv = sb("Wv", [128, 2, 512], BF16)
            dWkv = P.dep("a_Wkv")
            for c in range(2):
                w4 = wkvf[:, c, :].rearrange("p (h r) -> p h r", h=NH)
                P.op("dve", lambda e, c=c, w4=w4: e.tensor_scalar(out=Wkn[:, c, :].rearrange("p (h r) -> p h r", h=NH), in0=w4[:, :, 0:64],
                                                                  scalar1=gkv[:, c:c + 1], scalar2=None, op0=ALU.mult),
                     reads=[dwkvf, dgkv], writes=[dWkv])
                P.op("dve", lambda e, c=c, w4=w4: e.tensor_scalar(out=Wv[:, c, :].rearrange("p (h r) -> p h r", h=NH), in0=w4[:, :, 64:128],
                                                                  scalar1=gkv[:, c:c + 1], scalar2=None, op0=ALU.mult),
                     reads=[dwkvf, dgkv], writes=[dWkv])
            NPS = 8
            ps = [self.psum(st, "aps", [128, 512], F32) for _ in range(NPS)]
            dps = [P.dep("a_ps%d" % i) for i in range(NPS)]

            def nps():
                i = self.ps_rr % NPS
                self.ps_rr += 1
                return ps[i], dps[i]
            xTb = [sb("xTb", [128, 8, 512], BF16) for _ in range(2)]
            NSTG = 6
            stg = [sb("stg", [128, 512], F32) for _ in range(NSTG)]
            self.stg_rr = 0

            def nstg():
                i = self.stg_rr % NSTG
                self.stg_rr += 1
                return stg[i], P.dep("a_stg%d" % i)
            cctmp = [sb("cctmp", [128, 512], F32) for _ in range(2)]
            cq = sb("cq", [128, 3, 512], F32)
            sq = sb("sq", [128, 3, 512], BF16)
            cqn = sb("cqn", [128, 3, 512], BF16)
            ckv = sb("ckv", [128, 2, 512], F32)
            sk = sb("sk", [128, 2, 512], BF16)
            ckvn = sb("ckvn", [128, 2, 512], BF16)
            rstd = sb("rstd", [128, 512], F32)
            sdt = sb("sdt", [128, 512], F32)
            rstd2 = sb("rstd2", [128, 512], F32)
            sdt2 = sb("sdt2", [128, 512], F32)
            ropeC = [sb("ropeC", [128, 512], F32) for _ in range(2)]
            ropeS = [sb("ropeS", [128, 512], F32) for _ in range(2)]
            t1 = [sb("t1", [128, 512], F32) for _ in range(2)]
            t2 = [sb("t2", [128, 512], F32) for _ in range(2)]
            QTs = [sb("QTs", [128, 512], BF16) for _ in range(3)]
            KTs = [sb("KTs", [128, 512], BF16) for _ in range(3)]
            kro = sb("kro", [128, 512], BF16)
            Vs = [sb("Vs", [128, NH, 65], BF16) for _ in range(2)]
            dVs = [P.dep("a_Vs%d" % i) for i in range(2)]
            for i in range(2):
                P.op("pool", lambda e, i=i: e.memset(Vs[i][:], 1.0), writes=[dVs[i]])
            dcq, dsq, dcqn, dckv, dsk, dckvn = (P.dep("a_" + n) for n in ("cq", "sq", "cqn", "ckv", "sk", "ckvn"))
            drstd, dsdt, drstd2, dsdt2, dkro = (P.dep("a_" + n) for n in ("rstd", "sdt", "rstd2", "sdt2", "kro"))
            dQT, dKT, dV, dCB, dPP, dLG, dLX = (P.dep("d_" + n) for n in ("QT", "KT", "V", "CB", "PP", "LG", "LX"))
            xTv = xT.rearrange("(c p) t -> p c t", p=128)
            nq = 0

            def grp(xb, dxb, col0, m, lhs=None, dl=None):
                pt, dpt = nps()
                for c in range(8):
                    if lhs is None:
                        l_ap, dd = Win[:, c, col0:col0 + m], dW
                    else:
                        l_ap, dd = lhs[:, c, :], dl
                    P.op("pe", lambda e, c=c, l_ap=l_ap, pt=pt: e.matmul(pt[0:m, :], lhsT=l_ap, rhs=xb[:, c, :], start=(c == 0), stop=(c == 7)),
                         reads=[dd, dxb], writes=[dpt], signal=(c == 7))
                return pt, dpt

            def store_fm(dst, ddst, row0, col0, pt, dpt, func=AF.Copy):
                s, ds_ = nstg()
                P.op("act", lambda e: e.activation(out=s[:], in_=pt[:], func=func), reads=[dpt], writes=[ds_])
                P.dma("sp", dst[row0:row0 + 128, col0:col0 + 512], s[:], reads=[ds_], writes=[ddst])

            if ASTOP < 1:
                return
            for blk in range(NBT):
                own = blk < NB
                t0 = blk * 512
                xb, dxb = xTb[blk % 2], P.dep("a_xTb%d" % (blk % 2))
                P.dma("sp", xb[:], xTv[:, :, t0:t0 + 512], reads=[P.dep("d_xT")], writes=[dxb])
                rc, rs_, drope = ropeC[blk % 2], ropeS[blk % 2], P.dep("a_rope%d" % (blk % 2))
                P.dma("sp", rc[64:96, :], self.ropeC[:, t0:t0 + 512], writes=[drope])
                P.dma("sp", rs_[64:96, :], self.ropeS[:, t0:t0 + 512], writes=[drope])
                edge = (not own) and (blk == NB or blk == 2 * NB - 1)
                if own or edge:
                    for c in range(2):
                        pcc, dpcc = grp(xb, dxb, 256 + c * 128, 128)
                        pch, dpch = grp(xb, dxb, 512 + c * 128, 128)
                        ct, dct = cctmp[c], P.dep("a_cct%d" % c)
                        P.op("act", lambda e, ct=ct, pcc=pcc: e.activation(out=ct[:], in_=pcc[:], func=AF.Copy), reads=[dpcc], writes=[dct])
                        s, ds_ = nstg()
                        P.op("dve", lambda e, s=s, pch=pch, ct=ct: e.tensor_tensor(out=s[:], in0=pch[:], in1=ct[:], op=ALU.mult),
                             reads=[dpch, dct], writes=[ds_])
                        P.dma("sp", self.PP[c * 128:(c + 1) * 128, t0:t0 + 512], s[:], reads=[ds_], writes=[dPP])
                if ASTOP < 2:
                    continue
                if own:
                    for c in range(2):
                        pt, dpt = grp(xb, dxb, c * 128, 128)
                        store_fm(self.CB, dCB, c * 128, t0, pt, dpt)
                        pt, dpt = grp(xb, dxb, 1440 + c * 128, 128)
                        store_fm(self.LG, dLG, c * 128, t0, pt, dpt, func=AF.Gelu_apprx_tanh)
                    if ASTOP < 3:
                        continue
                    for c in range(3):
                        pt, dpt = grp(xb, dxb, 768 + c * 128, 128)
                        P.op("act", lambda e, c=c, pt=pt: e.activation(out=sq[:, c, :], in_=pt[:], func=AF.Square), reads=[dpt], writes=[dsq])
                        P.op("dve", lambda e, c=c, pt=pt: e.tensor_copy(cq[:, c, :], pt[:]), reads=[dpt], writes=[dcq])
                    pst, dpst = nps()
                    self.rms_rstd(sq, dsq, 3, 384.0, pst, dpst, rstd, drstd, sdt, dsdt)
                    for c in range(3):
                        P.op("dve", lambda e, c=c: e.tensor_tensor(out=cqn[:, c, :], in0=cq[:, c, :], in1=rstd[:], op=ALU.mult),
                             reads=[dcq, drstd], writes=[dcqn])
                    for h in range(NH):
                        pa, dpa = nps()
                        pb, dpb = nps()
                        for c in range(3):
                            P.op("pe", lambda e, c=c, h=h, pa=pa: e.matmul(pa[0:96, :], lhsT=Wq[:, c, h * 96:(h + 1) * 96], rhs=cqn[:, c, :],
                                                                      start=(c == 0), stop=(c == 2)), reads=[dWq, dcqn], writes=[dpa], signal=(c == 2))
                        for c in range(3):
                            P.op("pe", lambda e, c=c, h=h, pb=pb: e.matmul(pb[0:96, :], lhsT=Wqs[:, c, h * 96:(h + 1) * 96], rhs=cqn[:, c, :],
                                                                      start=(c == 0), stop=(c == 2)), reads=[dWq, dcqn], writes=[dpb], signal=(c == 2))
                        qs, dqs = QTs[nq % 3], P.dep("a_QTs%d" % (nq % 3))
                        ta, dta = t1[nq % 2], P.dep("a_t1%d" % (nq % 2))
                        tb, dtb = t2[nq % 2], P.dep("a_t2%d" % (nq % 2))
                        nq += 1
                        P.op("act", lambda e, qs=qs, pa=pa: e.activation(out=qs[0:64, :], in_=pa[0:64, :], func=AF.Copy), reads=[dpa], writes=[dqs])
                        P.op("dve", lambda e, ta=ta, pa=pa: e.tensor_tensor(out=ta[64:96, :], in0=pa[64:96, :], in1=rc[64:96, :], op=ALU.mult),
                             reads=[dpa, drope], writes=[dta])
                        P.op("dve", lambda e, tb=tb, pb=pb: e.tensor_tensor(out=tb[64:96, :], in0=pb[64:96, :], in1=rs_[64:96, :], op=ALU.mult),
                             reads=[dpb, drope], writes=[dtb])
                        P.op("dve", lambda e, qs=qs, ta=ta, tb=tb: e.tensor_tensor(out=qs[64:96, :], in0=ta[64:96, :], in1=tb[64:96, :], op=ALU.add),
                             reads=[dta, dtb], writes=[dqs])
                        P.dma("sp", self.QT[h, :, t0:t0 + 512], qs[0:96, :], reads=[dqs], writes=[dQT])
                if ASTOP < 4:
                    continue
                for c in range(2):
                    pt, dpt = grp(xb, dxb, 1696 + c * 128, 128)
                    store_fm(self.LX, dLX, c * 128, t0, pt, dpt)
                for c in range(2):
                    pt, dpt = grp(xb, dxb, 1152 + c * 128, 128)
                    P.op("act", lambda e, c=c, pt=pt: e.activation(out=sk[:, c, :], in_=pt[:], func=AF.Square), reads=[dpt], writes=[dsk])
                    P.op("dve", lambda e, c=c, pt=pt: e.tensor_copy(ckv[:, c, :], pt[:]), reads=[dpt], writes=[dckv])
                pst, dpst = nps()
                self.rms_rstd(sk, dsk, 2, 256.0, pst, dpst, rstd2, drstd2, sdt2, dsdt2)
                for c in range(2):
                    P.op("dve", lambda e, c=c: e.tensor_tensor(out=ckvn[:, c, :], in0=ckv[:, c, :], in1=rstd2[:], op=ALU.mult),
                         reads=[dckv, drstd2], writes=[dckvn])
                if ASTOP < 5:
                    continue
                pa, dpa = grp(xb, dxb, 0, 96, lhs=Wkr, dl=dW2)
                pb, dpb = grp(xb, dxb, 0, 96, lhs=Wkrs, dl=dW2)
                ta, dta = t1[nq % 2], P.dep("a_t1%d" % (nq % 2))
                tb, dtb = t2[nq % 2], P.dep("a_t2%d" % (nq % 2))
                nq += 1
                P.op("dve", lambda e, ta=ta, pa=pa: e.tensor_tensor(out=ta[64:96, :], in0=pa[64:96, :], in1=rc[64:96, :], op=ALU.mult),
                     reads=[dpa, drope], writes=[dta])
                P.op("dve", lambda e, tb=tb, pb=pb: e.tensor_tensor(out=tb[64:96, :], in0=pb[64:96, :], in1=rs_[64:96, :], op=ALU.mult),
                     reads=[dpb, drope], writes=[dtb])
                P.op("dve", lambda e, ta=ta, tb=tb: e.tensor_tensor(out=kro[64:96, :], in0=ta[64:96, :], in1=tb[64:96, :], op=ALU.add),
                     reads=[dta, dtb], writes=[dkro])
                if ASTOP < 6:
                    continue
                for h in range(NH):
                    pk, dpk = nps()
                    for c in range(2):
                        P.op("pe", lambda e, c=c, h=h, pk=pk: e.matmul(pk[0:64, :], lhsT=Wkn[:, c, h * 64:(h + 1) * 64], rhs=ckvn[:, c, :],
                                                                  start=(c == 0), stop=(c == 1)), reads=[dWkv, dckvn], writes=[dpk], signal=(c == 1))
                    ks, dks = KTs[h % 3], P.dep("a_KTs%d" % (h % 3))
                    P.op("act", lambda e, ks=ks, pk=pk: e.activation(out=ks[0:64, :], in_=pk[0:64, :], func=AF.Copy), reads=[dpk], writes=[dks])
                    P.dma("sp", self.KT[h, 0:64, t0:t0 + 512], ks[0:64, :], reads=[dks], writes=[dKT])
                    P.dma("sp", self.KT[h, 64:96, t0:t0 + 512], kro[64:96, :], reads=[dkro], writes=[dKT])
                if ASTOP < 7:
                    continue
                for sub in range(4):
                    pv, dpv = nps()
                    for c in range(2):
                        P.op("pe", lambda e, c=c, sub=sub, pv=pv: e.matmul(pv[:], lhsT=ckvn[:, c, sub * 128:(sub + 1) * 128], rhs=Wv[:, c, :],
                                                                      start=(c == 0), stop=(c == 1)), reads=[dWkv, dckvn], writes=[dpv], signal=(c == 1))
                    vs, dvs = Vs[sub % 2], dVs[sub % 2]
                    P.op("act", lambda e, vs=vs, pv=pv: e.activation(out=vs[:, :, 0:64], in_=pv[:].rearrange("p (h r) -> p h r", h=NH), func=AF.Copy),
                         reads=[dpv], writes=[dvs])
                    P.dma("sp", self.Vd[t0 + sub * 128:t0 + (sub + 1) * 128, :], vs[:].rearrange("p h r -> p (h r)"), reads=[dvs], writes=[dV])

    def phase_b(self, l):
        P, T, NB, I = self.P, self.T, self.NB, self.I
        P.barrier()
        dY = P.dep("d_Y")
        f0 = self.flags[:, 0:1]
        f1 = self.flags[:, 1:2]
        with ExitStack() as st:
            sb = lambda n, s, d: self.sb(st, n, s, d)
            cw = sb("cw", [128, 2, 3], F32)
            dcw = P.dep("b_cw")
            cv_t = {"pp": sb("pp", [128, T + 2], F32), "cb": sb("cb", [128, T], F32), "acc": sb("acc", [128, T], F32)}
            for k in range(3):
                for c in range(2):
                    P.dma("sp", cw[:, c, k:k + 1], I["conv_w"][l, k, c * 128:(c + 1) * 128].rearrange("(p o) -> p o", o=1), writes=[dcw])
            for c in range(2):
                pp = cv_t["pp"]
                cb = cv_t["cb"]
                acc = cv_t["acc"]
                edge = sb("edge", [128, 2], F32)
                dpp, dcb, dacc = (P.dep("b_%s" % n) for n in ("pp", "cb", "acc"))
                dedge = P.dep("b_edge%d" % c)
                rows = slice(c * 128, (c + 1) * 128)
                P.dma("sp", pp[:, 1:T + 1], self.PP[rows, 0:T], reads=[P.dep("d_PP")], writes=[dpp])
                P.dma("sp", cb[:], self.CB[rows, 0:T], reads=[P.dep("d_CB")], writes=[dcb])
                if self.has_other:
                    P.dma("sp", edge[:, 0:1], self.PP[rows, 2 * T - 1:2 * T], reads=[P.dep("d_PP")], writes=[dedge], allow_slow_non_contiguous=True)
                    P.dma("sp", edge[:, 1:2], self.PP[rows, T:T + 1], reads=[P.dep("d_PP")], writes=[dedge], allow_slow_non_contiguous=True)
                else:
                    P.op("dve", lambda e, edge=edge: e.memset(edge[:], 0.0), writes=[dedge])
                P.op("dve", lambda e, pp=pp, edge=edge: e.tensor_tensor(out=pp[:, 0:1], in0=edge[:, 0:1], in1=f1, op=ALU.mult),
                     reads=[dedge, self.dconst, dpp], writes=[dpp])
                P.op("dve", lambda e, pp=pp, edge=edge: e.tensor_tensor(out=pp[:, T + 1:T + 2], in0=edge[:, 1:2], in1=f0, op=ALU.mult),
                     reads=[dedge, self.dconst, dpp], writes=[dpp])
                P.op("dve", lambda e, acc=acc, pp=pp, c=c: e.tensor_scalar(out=acc[:], in0=pp[:, 0:T], scalar1=cw[:, c, 0:1], scalar2=None, op0=ALU.mult),
                     reads=[dpp, dcw], writes=[dacc])
                for k in (1, 2):
                    P.op("dve", lambda e, acc=acc, pp=pp, c=c, k=k: e.scalar_tensor_tensor(out=acc[:], in0=pp[:, k:k + T], scalar=cw[:, c, k:k + 1], in1=acc[:],
                                                                                     op0=ALU.mult, op1=ALU.add), reads=[dpp, dcw, dacc], writes=[dacc])
                P.op("pool", lambda e, acc=acc, cb=cb: e.tensor_tensor(out=acc[:], in0=acc[:], in1=cb[:], op=ALU.mult), reads=[dacc, dcb], writes=[dacc])
                P.dma("sp", self.Y[rows, 0:T], acc[:], reads=[dacc], writes=[dY])
        P.barrier()
        with ExitStack() as st:
            sb = lambda n, s, d: self.sb(st, n, s, d)
            prm = sb("prm", [128, 2, 16], F32)
            dprm = P.dep("b_prm")

            def col(dst_col, vec):
                for c in range(2):
                    P.dma("sp", prm[:, c, dst_col:dst_col + 1], vec[c * 128:(c + 1) * 128].rearrange("(p o) -> p o", o=1), writes=[dprm])
            for k in range(4):
                col(k, I["lru_conv_w"][l, k])
            col(4, I["lru_conv_b"][l])
            for d_ in range(2):
                col(5 + d_, I["lru_ba"][l, d_])
                col(7 + d_, I["lru_bi"][l, d_])
                col(9 + d_, I["lru_lam"][l, d_])
            sc = sb("sc", [128, 2, 4], F32)
            dsc = P.dep("b_sc")
            for c in range(2):
                P.op("act", lambda e, c=c: e.activation(out=sc[:, c, 0:2], in_=prm[:, c, 9:11], func=AF.Exp, scale=-1.0), reads=[dprm], writes=[dsc])
                P.op("act", lambda e, c=c: e.activation(out=sc[:, c, 0:2], in_=sc[:, c, 0:2], func=AF.Ln, bias=1.0, scale=1.0), reads=[dsc], writes=[dsc])
                P.op("dve", lambda e, c=c: e.tensor_scalar(out=sc[:, c, 2:4], in0=sc[:, c, 0:2], scalar1=-16.0, scalar2=None, op0=ALU.mult), reads=[dsc], writes=[dsc])
                P.op("dve", lambda e, c=c: e.tensor_scalar(out=sc[:, c, 0:2], in0=sc[:, c, 0:2], scalar1=-8.0, scalar2=None, op0=ALU.mult), reads=[dsc], writes=[dsc])
            wgf = sb("wgf", [128, 2, 4, 128], F32)
            wg = sb("wg", [128, 2, 4, 128], BF16)
            dwg = P.dep("b_wg")
            P.op("pool", lambda e: e.memset(wgf[:], 0.0), writes=[dwg])
            for c in range(2):
                for d_ in range(2):
                    for gi, key in enumerate(("lru_wa", "lru_wi")):
                        for bb in range(2):
                            P.dma("sp", wgf[bb * 64:(bb + 1) * 64, c, 2 * d_ + gi, bb * 64:(bb + 1) * 64], I[key][l, d_, 2 * c + bb], writes=[dwg])
            P.op("dve", lambda e: e.tensor_copy(wg[:], wgf[:]), reads=[dwg], writes=[dwg])
            ps = [self.psum(st, "bps", [128, 512], F32) for _ in range(4)]
            dps = [P.dep("b_ps%d" % i) for i in range(4)]
            HO = self.has_other
            lxo = sb("lxo", [128, T + 3], F32)
            lxt = sb("lxt", [128, T + 3], F32) if HO else None
            big = {}
            for nm in (("o", "t") if HO else ("o",)):
                big["xc" + nm] = sb("xc" + nm, [128, T], F32)
            xcbb = [sb("xcbb", [128, 512], BF16) for _ in range(2)]
            a_ = sb("a_", [128, T], F32)
            u_ = sb("u_", [128, T], F32)
            hsum = sb("hsum", [128, T], F32)
            hb = sb("hb", [128, T], F32) if HO else lxo[:, 0:T]
            carry = sb("carry", [128, 2], F32)
            tmp = [sb("tmp", [128, 512], F32) for _ in range(2)]
            tmpi = [sb("tmpi", [128, 512], F32) for _ in range(2)]
            for c in range(2):
                rows = slice(c * 128, (c + 1) * 128)
                dlx = P.dep("b_lx")
                P.dma("sp", lxo[:, 1:T + 1], self.LX[rows, 0:T], reads=[P.dep("d_LX")], writes=[dlx])
                if HO:
                    P.dma("sp", lxt[:, 1:T + 1], self.LX[rows, T:2 * T], reads=[P.dep("d_LX")], writes=[dlx])
                    P.op("dve", lambda e, lxo=lxo, lxt=lxt: e.tensor_scalar(out=lxo[:, 0:1], in0=lxt[:, T:T + 1], scalar1=f1, scalar2=None, op0=ALU.mult), reads=[dlx, self.dconst], writes=[dlx])
                    P.op("dve", lambda e, lxo=lxo, lxt=lxt: e.tensor_scalar(out=lxo[:, T + 1:T + 3], in0=lxt[:, 1:3], scalar1=f0, scalar2=None, op0=ALU.mult), reads=[dlx, self.dconst], writes=[dlx])
                    P.op("dve", lambda e, lxo=lxo, lxt=lxt: e.tensor_scalar(out=lxt[:, 0:1], in0=lxo[:, T:T + 1], scalar1=f0, scalar2=None, op0=ALU.mult), reads=[dlx, self.dconst], writes=[dlx])
                    P.op("dve", lambda e, lxo=lxo, lxt=lxt: e.tensor_scalar(out=lxt[:, T + 1:T + 3], in0=lxo[:, 1:3], scalar1=f1, scalar2=None, op0=ALU.mult), reads=[dlx, self.dconst], writes=[dlx])
                else:
                    P.op("dve", lambda e, lxo=lxo: e.memset(lxo[:, 0:1], 0.0), reads=[dlx], writes=[dlx])
                    P.op("dve", lambda e, lxo=lxo: e.memset(lxo[:, T + 1:T + 3], 0.0), reads=[dlx], writes=[dlx])
                xc = {}
                xcb = {}
                dxc = P.dep("b_xc")
                for nm, src in ((("o", lxo), ("t", lxt)) if HO else (("o", lxo),)):
                    x_ = big["xc" + nm]
                    P.op("dve", lambda e, x_=x_, src=src: e.tensor_scalar(out=x_[:], in0=src[:, 0:T], scalar1=prm[:, c, 0:1], scalar2=prm[:, c, 4:5],
                                                                       op0=ALU.mult, op1=ALU.add), reads=[dlx, dprm], writes=[dxc])
                    for k in (1, 2, 3):
                        P.op("dve", lambda e, x_=x_, src=src, k=k: e.scalar_tensor_tensor(out=x_[:], in0=src[:, k:k + T], scalar=prm[:, c, k:k + 1], in1=x_[:],
                                                                                       op0=ALU.mult, op1=ALU.add), reads=[dlx, dprm, dxc], writes=[dxc])
                    xc[nm] = x_
                da, du, dh, dhb, dcar = (P.dep("b_%s" % n) for n in ("a", "u", "h", "hb", "car"))

                def gates(nm, d_):
                    for b in range(T // 512):
                        cs = slice(b * 512, (b + 1) * 512)
                        pa, dpa = ps[(2 * b) % 4], dps[(2 * b) % 4]
                        pi, dpi = ps[(2 * b + 1) % 4], dps[(2 * b + 1) % 4]
                        xq, dxq = xcbb[b % 2], P.dep("b_xcbb%d" % (b % 2))
                        P.op("pool", lambda e, xq=xq, cs=cs: e.tensor_copy(xq[:], xc[nm][:, cs]), reads=[dxc], writes=[dxq])
                        P.op("pe", lambda e, pa=pa, xq=xq: e.matmul(pa[:], lhsT=wg[:, c, 2 * d_, :], rhs=xq[:], start=True, stop=True),
                             reads=[dwg, dxq], writes=[dpa])
                        P.op("pe", lambda e, pi=pi, xq=xq: e.matmul(pi[:], lhsT=wg[:, c, 2 * d_ + 1, :], rhs=xq[:], start=True, stop=True),
                             reads=[dwg, dxq], writes=[dpi])
                        tr, dtr = tmp[b % 2], P.dep("b_tmp%d" % (b % 2))
                        ti, dti = tmpi[b % 2], P.dep("b_tmpi%d" % (b % 2))
                        P.op("act", lambda e, tr=tr, pa=pa: e.activation(out=tr[:], in_=pa[:], func=AF.Sigmoid, bias=prm[:, c, 5 + d_:6 + d_], scale=1.0),
                             reads=[dpa, dprm], writes=[dtr])
                        P.op("act", lambda e, ti=ti, pi=pi: e.activation(out=ti[:], in_=pi[:], func=AF.Sigmoid, bias=prm[:, c, 7 + d_:8 + d_], scale=1.0),
                             reads=[dpi, dprm], writes=[dti])
                        P.op("act", lambda e, tr=tr, cs=cs: e.activation(out=a_[:, cs], in_=tr[:], func=AF.Exp, scale=sc[:, c, d_:d_ + 1]),
                             reads=[dtr, dsc], writes=[da])
                        P.op("act", lambda e, tr=tr: e.activation(out=tr[:], in_=tr[:], func=AF.Exp, scale=sc[:, c, 2 + d_:3 + d_]),
                             reads=[dtr, dsc], writes=[dtr])
                        P.op("dve", lambda e, tr=tr: e.tensor_scalar(out=tr[:], in0=tr[:], scalar1=-1.0, scalar2=1.0, op0=ALU.mult, op1=ALU.add),
                             reads=[dtr], writes=[dtr])
                        P.op("act", lambda e, tr=tr: e.activation(out=tr[:], in_=tr[:], func=AF.Sqrt), reads=[dtr], writes=[dtr])
                        P.op("dve", lambda e, ti=ti, cs=cs: e.tensor_tensor(out=ti[:], in0=ti[:], in1=xc[nm][:, cs], op=ALU.mult), reads=[dti, dxc], writes=[dti])
                        P.op("dve", lambda e, tr=tr, ti=ti, cs=cs: e.tensor_tensor(out=u_[:, cs], in0=tr[:], in1=ti[:], op=ALU.mult), reads=[dtr, dti], writes=[du])
                if HO:
                    gates("t", 0)
                    P.op("dve", lambda e: e.tensor_tensor_scan(out=hb[:], data0=a_[:], data1=u_[:], initial=0.0, op0=ALU.mult, op1=ALU.add),
                         reads=[da, du], writes=[dhb])
                    P.op("dve", lambda e: e.tensor_scalar(out=carry[:, 0:1], in0=hb[:, T - 1:T], scalar1=f1, scalar2=None, op0=ALU.mult),
                         reads=[dhb, self.dconst], writes=[dcar])
                    gates("t", 1)
                    P.op("dve", lambda e: e.tensor_tensor_scan(out=hb[:, ::-1], data0=a_[:, ::-1], data1=u_[:, ::-1], initial=0.0, op0=ALU.mult, op1=ALU.add),
                         reads=[da, du], writes=[dhb])
                    P.op("dve", lambda e: e.tensor_scalar(out=carry[:, 1:2], in0=hb[:, 0:1], scalar1=f0, scalar2=None, op0=ALU.mult),
                         reads=[dhb, self.dconst], writes=[dcar])
                else:
                    P.op("dve", lambda e: e.memset(carry[:], 0.0), writes=[dcar])
                gates("o", 0)
                P.op("dve", lambda e: e.tensor_tensor_scan(out=hsum[:], data0=a_[:], data1=u_[:], initial=carry[:, 0:1], op0=ALU.mult, op1=ALU.add),
                     reads=[da, du, dcar], writes=[dh])
                gates("o", 1)
                P.op("dve", lambda e: e.tensor_tensor_scan(out=hb[:, ::-1], data0=a_[:, ::-1], data1=u_[:, ::-1], initial=carry[:, 1:2], op0=ALU.mult, op1=ALU.add),
                     reads=[da, du, dcar], writes=[dhb])
                P.op("pool", lambda e: e.tensor_tensor(out=hsum[:], in0=hsum[:], in1=hb[:], op=ALU.add), reads=[dh, dhb], writes=[dh])
                lgt = a_
                P.dma("sp", lgt[:], self.LG[rows, 0:T], reads=[P.dep("d_LG"), da], writes=[da])
                P.op("pool", lambda e: e.tensor_tensor(out=hsum[:], in0=hsum[:], in1=lgt[:], op=ALU.mult), reads=[dh, da], writes=[dh])
                P.dma("sp", self.Y[768 + c * 128:768 + (c + 1) * 128, 0:T], hsum[:], reads=[dh], writes=[dY])
                P.barrier()

    def phase_c(self, l):
        P, T, NB = self.P, self.T, self.NB
        S2 = (2 * T) if self.has_other else T
        NKT = S2 // 128
        NQB = NB
        P.barrier()
        dY = P.dep("d_Y")
        with ExitStack() as st:
            sb = lambda n, s, d: self.sb(st, n, s, d)
            Vall = sb("Vall", [128, NKT, NH * 65], BF16)
            dVall = P.dep("c_Vall")
            vv = self.Vd[0:S2, :].rearrange("(k p) n -> p k n", p=128)
            for k0 in range(0, NKT, 8):
                P.dma("sp", Vall[:, k0:k0 + 8, :], vv[:, k0:k0 + 8, :], reads=[P.dep("d_V")], writes=[dVall])
            sel = sb("sel", [128, 64], F32)
            dsel = P.dep("c_sel")
            P.op("dve", lambda e: e.memset(sel[:], 0.0), writes=[dsel])
            P.op("dve", lambda e: e.memset(sel[64:65, :], 1.0), reads=[dsel], writes=[dsel])
            KTh = [sb("KTh", [96, S2], BF16) for _ in range(2)]
            QTh = [sb("QTh", [96, T], BF16) for _ in range(2)]
            pS = [self.psum(st, "pS", [128, 1536], F32) for _ in range(2)]
            dpS = [P.dep("c_pS%d" % i) for i in range(2)]
            pO = [self.psum(st, "pO", [128, 512], F32) for _ in range(2)]
            dpO = [P.dep("c_pO%d" % i) for i in range(2)]
            PT = [sb("PT", [128, 1536], BF16) for _ in range(3)]
            dPT = [P.dep("c_PT%d" % i) for i in range(3)]
            Osb = [sb("Osb", [128, 512], F32) for _ in range(2)]
            rec = [sb("rec", [64, 512], F32) for _ in range(2)]
            yo = [sb("yo", [64, 512], F32) for _ in range(2)]
            it = 0
            nqb = 0
            for h in range(NH):
                kt_, dkt = KTh[h % 2], P.dep("c_KTh%d" % (h % 2))
                qt_, dqt = QTh[h % 2], P.dep("c_QTh%d" % (h % 2))
                P.dma("sp", kt_[:], self.KT[h, :, 0:S2], reads=[P.dep("d_KT")], writes=[dkt])
                P.dma("sp", qt_[:], self.QT[h, :, 0:T], reads=[P.dep("d_QT")], writes=[dqt])
                for qb in range(NQB):
                    qs = slice(qb * 512, (qb + 1) * 512)
                    po, dpo = pO[nqb % 2], dpO[nqb % 2]
                    GS = 3
                    groups = [list(range(k0, min(k0 + GS, NKT))) for k0 in range(0, NKT, GS)]
                    NKG = len(groups)

                    def scores(kg, it_):
                        ps_, dps_ = pS[it_ % 2], dpS[it_ % 2]
                        ks = groups[kg]
                        for j, k in enumerate(ks):
                            P.op("pe", lambda e, k=k, j=j, ps_=ps_: e.matmul(ps_[:, j * 512:(j + 1) * 512], lhsT=kt_[:, k * 128:(k + 1) * 128], rhs=qt_[:, qs],
                                                                        start=True, stop=True), reads=[dkt, dqt], writes=[dps_], signal=(j == len(ks) - 1))

                    def expo(kg, it_):
                        ps_, dps_ = pS[it_ % 2], dpS[it_ % 2]
                        pt_, dpt_ = PT[it_ % 3], dPT[it_ % 3]
                        w = len(groups[kg]) * 512
                        P.op("act", lambda e, ps_=ps_, pt_=pt_, w=w: e.activation(out=pt_[:, 0:w], in_=ps_[:, 0:w], func=AF.Exp), reads=[dps_], writes=[dpt_])

                    def pv(kg, it_):
                        pt_, dpt_ = PT[it_ % 3], dPT[it_ % 3]
                        ks = groups[kg]
                        for j, k in enumerate(ks):
                            P.op("pe", lambda e, k=k, j=j, pt_=pt_, po=po: e.matmul(po[0:65, :], lhsT=Vall[:, k, h * 65:(h + 1) * 65], rhs=pt_[:, j * 512:(j + 1) * 512],
                                                                               start=(k == 0), stop=(k == NKT - 1)), reads=[dVall, dpt_], writes=[dpo],
                                 signal=(j == len(ks) - 1))
                    scores(0, it)
                    for kg in range(NKG):
                        expo(kg, it + kg)
                        if kg + 1 < NKG:
                            scores(kg + 1, it + kg + 1)
                        pv(kg, it + kg)
                    it += NKG
                    pD, dpD = pS[it % 2], dpS[it % 2]
                    ob, dob = Osb[nqb % 2], P.dep("c_Osb%d" % (nqb % 2))
                    rc_, drc = rec[nqb % 2], P.dep("c_rec%d" % (nqb % 2))
                    y_, dy_ = yo[nqb % 2], P.dep("c_yo%d" % (nqb % 2))
                    nqb += 1
                    P.op("dve", lambda e, ob=ob, po=po: e.tensor_copy(ob[0:65, :], po[0:65, :]), reads=[dpo], writes=[dob])
                    P.op("pe", lambda e, ob=ob: e.matmul(pD[0:64, 0:512], lhsT=sel[0:65, :], rhs=ob[0:65, :], start=True, stop=True),
                         reads=[dsel, dob], writes=[dpD])
                    P.op("dve", lambda e, rc_=rc_: e.reciprocal(out=rc_[:], in_=pD[0:64, 0:512]), reads=[dpD], writes=[drc])
                    P.op("dve", lambda e, y_=y_, ob=ob, rc_=rc_: e.tensor_tensor(out=y_[:], in0=ob[0:64, :], in1=rc_[:], op=ALU.mult),
                         reads=[dob, drc], writes=[dy_])
                    P.dma("sp", self.Y[256 + h * 64:256 + (h + 1) * 64, qs], y_[:], reads=[dy_], writes=[dY])

    def phase_d(self, l, xres):
        P, T, NB, I = self.P, self.T, self.NB, self.I
        P.barrier()
        with ExitStack() as st:
            sb = lambda n, s, d: self.sb(st, n, s, d)
            Wo = sb("Wo", [128, 8, D], BF16)
            dWo = P.dep("d_Wo")
            wv = self.wb[("w_out", l)].rearrange("(c p) n -> p c n", p=128)
            for c in range(8):
                P.dma("sp", Wo[:, c, :], wv[:, c, :], reads=[self.dwc["w_out%d" % l]], writes=[dWo])
            gm, dgm = self.load_cols(st, "gm", I["mix_norm_g"][l], D)
            L = self.ln_setup(st, I["ln1_g"][l], I["ln1_b"][l], "1", nbuf=1)
            Yb = [sb("Yb", [128, 8, 512], F32) for _ in range(2)]
            sq = sb("sq", [128, 8, 512], BF16)
            yn = sb("yn", [128, 8, 512], BF16)
            dsq, dyn = P.dep("dd_sq"), P.dep("dd_yn")
            rstd = [sb("rstd", [128, 512], F32) for _ in range(3)]
            sdt = [sb("sdt", [128, 512], F32) for _ in range(3)]
            pst = [self.psum(st, "dpst", [128, 512], F32) for _ in range(2)]
            pso = [self.psum(st, "dpso", [128, 512], F32) for _ in range(4)]
            xr = [sb("xr", [128, D], F32) for _ in range(2)]
            pre = [sb("pre", [128, D], F32) for _ in range(2)]
            xTs = [sb("xTs", [128, 8, 512], BF16) for _ in range(2)]
            Yv = self.Y.rearrange("(c p) t -> p c t", p=128)
            groups = ((0, 2, 256.0), (2, 6, 512.0), (6, 8, 256.0))
            dres = P.dep("d_x1res")
            dx1T = P.dep("d_x1T")
            npo = 0
            ntile = 0
            moe = (l == 1)
            if moe:
                wr = sb("wr", [128, 8, NE], F32)
                wrb = sb("wrb", [128, 8, NE], BF16)
                dwr = P.dep("dd_wr")
                P.dma("sp", wr[:], I["moe_w_router"][0].rearrange("(c p) n -> p c n", p=128), writes=[dwr])
                P.op("dve", lambda e: e.tensor_copy(wrb[:], wr[:]), reads=[dwr], writes=[dwr])
                prr_full = self.psum(st, "prr", [128, 512], F32)
                prr = prr_full[:, 0:NE]
                dprr = P.dep("dd_prr")
                lg_ = [sb("lg_", [128, NE], F32) for _ in range(2)]
                m8 = [sb("m8", [128, 8], F32) for _ in range(2)]
                msk = [sb("msk", [128, NE], F32) for _ in range(2)]
                ex = [sb("ex", [128, NE], F32) for _ in range(2)]
                den = [sb("den", [128, 2], F32) for _ in range(2)]
                dcomb = P.dep("d_comb")
                rtmp = [sb("rtmp", [128, 2 * NE], F32) for _ in range(2)]
            for b in range(NB):
                ts_ = slice(b * 512, (b + 1) * 512)
                yb, dyb = Yb[b % 2], P.dep("dd_Yb%d" % (b % 2))
                P.dma("sp", yb[:], Yv[:, :, ts_], reads=[P.dep("d_Y")], writes=[dyb])
                for c in range(8):
                    P.op("act", lambda e, c=c, yb=yb: e.activation(out=sq[:, c, :], in_=yb[:, c, :], func=AF.Square), reads=[dyb], writes=[dsq])
                for gi, (c0, c1, n) in enumerate(groups):
                    p_, dp_ = pst[gi % 2], P.dep("dd_pst%d" % (gi % 2))
                    for c in range(c0, c1):
                        P.op("pe", lambda e, c=c, p_=p_, c0=c0, c1=c1: e.matmul(p_[:], lhsT=self.ones[:], rhs=sq[:, c, :], start=(c == c0), stop=(c == c1 - 1)),
                             reads=[dsq, self.dconst], writes=[dp_], signal=(c == c1 - 1))
                    dsd, drs = P.dep("dd_sdt%d" % gi), P.dep("dd_rstd%d" % gi)
                    P.op("act", lambda e, gi=gi, p_=p_, n=n: e.activation(out=sdt[gi][:], in_=p_[:], func=AF.Sqrt, bias=self.eps_rms[:], scale=1.0 / n),
                         reads=[dp_, self.dconst], writes=[dsd])
                    P.op("dve", lambda e, gi=gi: e.reciprocal(out=rstd[gi][:], in_=sdt[gi][:]), reads=[dsd], writes=[drs])
                    for c in range(c0, c1):
                        P.op("dve", lambda e, c=c, gi=gi, yb=yb: e.scalar_tensor_tensor(out=yn[:, c, :], in0=yb[:, c, :], scalar=gm[:, c:c + 1], in1=rstd[gi][:],
                                                                                     op0=ALU.mult, op1=ALU.mult), reads=[dyb, dgm, drs], writes=[dyn])
                xs, dxs = xTs[b % 2], P.dep("dd_xTs%d" % (b % 2))
                for sub in range(4):
                    r0 = b * 512 + sub * 128
                    xrt, dxr = xr[ntile % 2], P.dep("dd_xr%d" % (ntile % 2))
                    pr, dpr = pre[ntile % 2], P.dep("dd_pre%d" % (ntile % 2))
                    ntile += 1
                    P.dma("sp", xrt[:], xres[r0:r0 + 128, :], reads=[P.dep("d_xres")], writes=[dxr])
                    for hf in range(2):
                        po, dpo = pso[npo % 4], P.dep("dd_pso%d" % (npo % 4))
                        npo += 1
                        for c in range(8):
                            P.op("pe", lambda e, c=c, hf=hf, po=po, sub=sub: e.matmul(po[:], lhsT=yn[:, c, sub * 128:(sub + 1) * 128], rhs=Wo[:, c, hf * 512:(hf + 1) * 512],
                                                                                 start=(c == 0), stop=(c == 7)), reads=[dyn, dWo], writes=[dpo], signal=(c == 7))
                        P.op("dve", lambda e, hf=hf, po=po, pr=pr, xrt=xrt: e.scalar_tensor_tensor(out=pr[:, hf * 512:(hf + 1) * 512], in0=xrt[:, hf * 512:(hf + 1) * 512],
                                                                                                scalar=ALPHA, in1=po[:], op0=ALU.mult, op1=ALU.add),
                             reads=[dxr, dpo], writes=[dpr])
                    self.ln_tile(L, pr, dpr, xs, dxs, sub, res_dst=self.x1res[r0:r0 + 128, :], dres=dres,
                                 xb_dst=(self.X1B[r0:r0 + 128, :] if moe else None), dxbd=P.dep("d_X1B"))
                    if moe:
                        i2 = ntile % 2
                        for c in range(8):
                            P.op("pe", lambda e, c=c, sub=sub, xs=xs: e.matmul(prr, lhsT=xs[:, c, sub * 128:(sub + 1) * 128], rhs=wrb[:, c, :], start=(c == 0), stop=(c == 7)),
                                 reads=[dxs, dwr], writes=[dprr], signal=(c == 7))
                        dl_, dm8, dmk, dex, dden = (P.dep("dd_%s%d" % (n, i2)) for n in ("lg", "m8", "msk", "ex", "den"))
                        P.op("dve", lambda e, i2=i2: e.tensor_copy(lg_[i2][:], prr), reads=[dprr], writes=[dl_])
                        P.op("dve", lambda e, i2=i2: e.max(out=m8[i2][:], in_=lg_[i2][:]), reads=[dl_], writes=[dm8])
                        P.op("dve", lambda e, i2=i2: e.tensor_scalar(out=msk[i2][:], in0=lg_[i2][:], scalar1=m8[i2][:, 1:2], scalar2=None, op0=ALU.is_ge),
                             reads=[dl_, dm8], writes=[dmk])
                        P.op("dve", lambda e, i2=i2: e.tensor_scalar(out=den[i2][:, 0:1], in0=m8[i2][:, 0:1], scalar1=-1.0, scalar2=None, op0=ALU.mult),
                             reads=[dm8], writes=[dden])
                        P.op("act", lambda e, i2=i2: e.activation(out=ex[i2][:], in_=lg_[i2][:], func=AF.Exp, bias=den[i2][:, 0:1], scale=1.0),
                             reads=[dl_, dden], writes=[dex])
                        P.op("dve", lambda e, i2=i2: e.tensor_tensor(out=ex[i2][:], in0=ex[i2][:], in1=msk[i2][:], op=ALU.mult), reads=[dex, dmk], writes=[dex])
                        P.op("dve", lambda e, i2=i2: e.reduce_sum(out=den[i2][:, 1:2], in_=ex[i2][:], axis=mybir.AxisListType.X), reads=[dex, dden], writes=[dden])
                        P.op("dve", lambda e, i2=i2: e.reciprocal(out=den[i2][:, 1:2], in_=den[i2][:, 1:2]), reads=[dden], writes=[dden])
                        P.op("dve", lambda e, i2=i2: e.tensor_scalar(out=ex[i2][:], in0=ex[i2][:], scalar1=den[i2][:, 1:2], scalar2=None, op0=ALU.mult),
                             reads=[dex, dden], writes=[dex])
                        R = self.R
                        ti_ = r0 // 128
                        drt = P.dep("r_tab")
                        doh = P.dep("dd_oh%d" % i2)
                        oh1, oh2, tmp8 = R["oh1"][:, ti_, :], R["oh2"][:, ti_, :], rtmp[i2]
                        P.op("dve", lambda e, i2=i2, oh1=oh1: e.tensor_scalar(out=oh1, in0=lg_[i2][:], scalar1=m8[i2][:, 0:1], scalar2=None, op0=ALU.is_equal),
                             reads=[dl_, dm8], writes=[drt])
                        P.op("dve", lambda e, i2=i2, oh1=oh1, oh2=oh2: e.tensor_tensor(out=oh2, in0=msk[i2][:], in1=oh1, op=ALU.subtract), reads=[dmk, drt], writes=[drt])
                        pw, dpw = prr_full[:, 16:32], dprr
                        P.op("pe", lambda e, i2=i2: e.matmul(pw[:, 0:NE], lhsT=R["U"][:], rhs=msk[i2][:], start=True, stop=True), reads=[dmk, P.dep("r_const")], writes=[dpw])
                        P.op("pe", lambda e, i2=i2: e.matmul(pw[:, NE:2 * NE], lhsT=R["onesf"][:], rhs=msk[i2][:], start=True, stop=True), reads=[dmk, P.dep("r_const")], writes=[dpw])
                        drun = P.dep("r_run")
                        P.op("dve", lambda e, tmp8=tmp8: e.tensor_tensor(out=tmp8[:, 0:NE], in0=pw[:, 0:NE], in1=R["run"][:], op=ALU.add), reads=[dpw, drun], writes=[doh])
                        P.op("dve", lambda e: e.tensor_tensor(out=R["run"][:], in0=pw[:, NE:2 * NE], in1=R["run"][:], op=ALU.add), reads=[dpw, drun], writes=[drun])
                        for kk, oh in ((0, oh1), (1, oh2)):
                            P.op("dve", lambda e, tmp8=tmp8, oh=oh: e.tensor_tensor(out=tmp8[:, NE:2 * NE], in0=tmp8[:, 0:NE], in1=oh, op=ALU.mult), reads=[doh, drt], writes=[doh])
                            P.op("dve", lambda e, tmp8=tmp8, kk=kk, ti_=ti_: e.reduce_sum(out=R["r12"][:, ti_, kk:kk + 1], in_=tmp8[:, NE:2 * NE], axis=mybir.AxisListType.X),
                                 reads=[doh], writes=[drt])
                            P.op("dve", lambda e, tmp8=tmp8, oh=oh, i2=i2: e.tensor_tensor(out=tmp8[:, NE:2 * NE], in0=ex[i2][:], in1=oh, op=ALU.mult), reads=[dex, drt, doh], writes=[doh])
                            P.op("dve", lambda e, tmp8=tmp8, kk=kk, ti_=ti_: e.reduce_sum(out=R["g12"][:, ti_, kk:kk + 1], in_=tmp8[:, NE:2 * NE], axis=mybir.AxisListType.X),
                                 reads=[doh], writes=[drt])
                P.dma("sp", self.x1T.rearrange("(c p) t -> p c t", p=128)[:, :, ts_], xs[:], reads=[dxs], writes=[dx1T])

    def route_setup(self, st):
        P, T = self.P, self.T
        NT = T // 128
        self.NTILE = (2 * T + NE * 511) // 512
        P.barrier()
        R = {}
        sb = lambda n, s, d: self.sb(st, n, s, d)
        R["U"] = sb("rU", [128, 128], F32)
        R["onesf"] = sb("ronesf", [128, 128], F32)
        R["run"] = sb("rrun", [128, NE], F32)
        R["oh1"] = sb("roh1", [128, NT, NE], F32)
        R["oh2"] = sb("roh2", [128, NT, NE], F32)
        R["r12"] = sb("rr12", [128, NT, 2], F32)
        R["g12"] = sb("rg12", [128, NT, 2], F32)
        R["slot"] = sb("rslot", [128, NT, 2], F32)
        R["sloti"] = sb("rsloti", [128, NT, 2], mybir.dt.int32)
        R["pidx"] = sb("rpidx", [128, 1], F32)
        R["pidxi"] = sb("rpidxi", [128, 1], mybir.dt.int32)
        R["ej"] = sb("rej", [128, self.NTILE], F32)
        R["idxg"] = sb("ridxg", [128, self.NTILE, 7], F32)
        R["idxgi"] = sb("ridxgi", [128, self.NTILE, 7], mybir.dt.int32)
        R["idxd"] = sb("ridxd", [128, self.NTILE, 4], F32)
        R["idxdi"] = sb("ridxdi", [128, self.NTILE, 4], mybir.dt.int32)
        dc = P.dep("r_const")
        P.op("pool", lambda e: e.memset(R["onesf"][:], 1.0), writes=[dc])
        P.op("pool", lambda e: e.memset(R["U"][:], 1.0), writes=[dc])
        P.op("pool", lambda e: e.affine_select(out=R["U"][:], in_=R["U"][:], pattern=[[1, 128]], compare_op=ALU.is_gt, fill=0.0,
                                               base=0, channel_multiplier=-1), reads=[dc], writes=[dc])
        P.op("pool", lambda e: e.iota(R["pidxi"][:], pattern=[[0, 1]], base=0, channel_multiplier=1), writes=[dc])
        P.op("pool", lambda e: e.tensor_copy(R["pidx"][:], R["pidxi"][:]), reads=[dc], writes=[dc])
        P.op("dve", lambda e: e.memset(R["run"][:], 0.0), writes=[P.dep("r_run")])
        self.R = R

    def phase_r(self):
        P, T = self.P, self.T
        R = self.R
        NT = T // 128
        NTILE = self.NTILE
        P.barrier()
        with ExitStack() as st:
            sb = lambda n, s, d: self.sb(st, n, s, d)
            t8 = sb("t8", [128, 6, NE], F32)
            d8 = P.dep("r_t8")
            drt = P.dep("r_tab")
            P.op("dve", lambda e: e.memset(t8[:, 5, :], 1.0), writes=[d8])
            t8i = sb("t8i", [128, NE], mybir.dt.int32)
            P.op("dve", lambda e: e.tensor_scalar(out=t8[:, 0, :], in0=R["run"][:], scalar1=1.0 / 512.0, scalar2=511.0 / 512.0 - 0.499, op0=ALU.mult, op1=ALU.add),
                 reads=[P.dep("r_run"), d8], writes=[d8])
            P.op("dve", lambda e: e.tensor_copy(t8i[:], t8[:, 0, :]), reads=[d8], writes=[d8])
            P.op("dve", lambda e: e.tensor_copy(t8[:, 1, :], t8i[:]), reads=[d8], writes=[d8])
            P.op("dve", lambda e: e.tensor_scalar(out=t8[:, 2, :], in0=t8[:, 1, :], scalar1=512.0, scalar2=None, op0=ALU.mult), reads=[d8], writes=[d8])
            P.op("dve", lambda e: e.tensor_tensor_scan(out=t8[:, 3, :], data0=t8[:, 5, :], data1=t8[:, 2, :], initial=0.0, op0=ALU.mult, op1=ALU.add), reads=[d8], writes=[d8])
            P.op("dve", lambda e: e.tensor_tensor(out=t8[:, 4, :], in0=t8[:, 3, :], in1=t8[:, 2, :], op=ALU.subtract), reads=[d8], writes=[d8])
            tmp = sb("rtmp2", [128, NE], F32)
            dtmp = P.dep("r_tmp2")
            for i in range(NT):
                for kk, oh in ((0, R["oh1"]), (1, R["oh2"])):
                    P.op("dve", lambda e, i=i, oh=oh: e.tensor_tensor(out=tmp[:], in0=oh[:, i, :], in1=t8[:, 4, :], op=ALU.mult), reads=[drt, d8, dtmp], writes=[dtmp])
                    P.op("dve", lambda e, i=i, kk=kk: e.reduce_sum(out=R["slot"][:, i, kk:kk + 1], in_=tmp[:], axis=mybir.AxisListType.X), reads=[dtmp], writes=[drt])
            P.op("dve", lambda e: e.tensor_tensor(out=R["slot"][:], in0=R["slot"][:], in1=R["r12"][:], op=ALU.add), reads=[drt], writes=[drt])
            P.op("dve", lambda e: e.tensor_copy(R["sloti"][:], R["slot"][:]), reads=[drt], writes=[drt])
            for j in range(NTILE):
                P.op("dve", lambda e, j=j: e.tensor_scalar(out=tmp[:], in0=t8[:, 3, :], scalar1=float(512 * j), scalar2=None, op0=ALU.is_le), reads=[d8, dtmp], writes=[dtmp])
                P.op("dve", lambda e, j=j: e.reduce_sum(out=R["ej"][:, j:j + 1], in_=tmp[:], axis=mybir.AxisListType.X), reads=[dtmp], writes=[drt])
            P.op("dve", lambda e: e.tensor_scalar(out=R["ej"][:], in0=R["ej"][:], scalar1=float(NE - 1), scalar2=None, op0=ALU.min), reads=[drt], writes=[drt])
            dc = P.dep("r_const")
            for g in range(7):
                P.op("dve", lambda e, g=g: e.tensor_scalar(out=R["idxg"][:, :, g], in0=R["ej"][:], scalar1=896.0, scalar2=float(g * 128), op0=ALU.mult, op1=ALU.add),
                     reads=[drt], writes=[drt])
            for q in range(4):
                P.op("dve", lambda e, q=q: e.tensor_scalar(out=R["idxd"][:, :, q], in0=R["ej"][:], scalar1=512.0, scalar2=float(q * 128), op0=ALU.mult, op1=ALU.add),
                     reads=[drt], writes=[drt])
            P.op("dve", lambda e: e.tensor_scalar(out=R["idxg"][:], in0=R["idxg"][:], scalar1=R["pidx"][:, 0:1], scalar2=None, op0=ALU.add), reads=[drt, dc], writes=[drt])
            P.op("dve", lambda e: e.tensor_scalar(out=R["idxd"][:], in0=R["idxd"][:], scalar1=R["pidx"][:, 0:1], scalar2=None, op0=ALU.add), reads=[drt, dc], writes=[drt])
            P.op("dve", lambda e: e.tensor_copy(R["idxgi"][:], R["idxg"][:]), reads=[drt], writes=[drt])
            P.op("dve", lambda e: e.tensor_copy(R["idxdi"][:], R["idxd"][:]), reads=[drt], writes=[drt])
            import os
            if os.environ.get("MK_DBGR"):
                dd = P.dep("dbgr")
                for nm, t_, dt_ in (("idxgi", R["idxgi"], mybir.dt.int32), ("ej", R["ej"], F32), ("sloti", R["sloti"], mybir.dt.int32),
                                    ("t8", t8, F32), ("pidx", R["pidx"], F32), ("r12", R["r12"], F32), ("g12", R["g12"], F32),
                                    ("oh1", R["oh1"], F32), ("oh2", R["oh2"], F32), ("run", R["run"], F32), ("U", R["U"], F32), ("onesf", R["onesf"], F32),
                                    ("pidxi", R["pidxi"], mybir.dt.int32)):
                    shp = list(t_.shape)
                    o = self.nc.dram_tensor("dbg_" + nm, shp, dt_, kind="ExternalOutput").ap()
                    P.dma("sp", o, t_[:], reads=[drt, d8, dc], writes=[dd])

    def phase_e_moe(self, l, out_res):
        P, T, I = self.P, self.T, self.I
        R = self.R
        NT = T // 128
        NTILE = self.NTILE
        NSLOT = NTILE * 512
        drt = P.dep("r_tab")
        P.barrier()
        with ExitStack() as st0:
            cur = [ExitStack()]
            st0.callback(lambda: cur[0].close())
            sb = lambda n, s, d: self.sb(cur[0], n, s, d)

            class _St:
                def enter_context(self_, x):
                    return cur[0].enter_context(x)
            st = _St()
            dXS = P.dep("d_XS")
            dYS = P.dep("d_YS")
            zt = sb("zt", [128, 4096], BF16)
            dzt = P.dep("m_zt")
            P.op("pool", lambda e: e.memset(zt[:], 0.0), writes=[dzt])
            xsv = self.XS.rearrange("(a p f) n -> a p (f n)", p=128, f=4)
            for a in range(NSLOT // 512):
                P.dma("sp", xsv[a], zt[:], reads=[dzt], writes=[dXS])
            xl = [sb("xl", [128, D], BF16) for _ in range(2)]
            for i in range(NT):
                x_, dx_ = xl[i % 2], P.dep("m_xl%d" % (i % 2))
                P.dma("sp", x_[:], self.X1B[i * 128:(i + 1) * 128, :], reads=[P.dep("d_X1B")], writes=[dx_])
                for kk in range(2):
                    P.idma(self.XS, R["sloti"][:, i, kk:kk + 1], x_[:], None, reads=[dx_, drt], writes=[dXS])
            P.barrier()
            cur[0].close()
            cur[0] = ExitStack()
            if self.mstop < 4:
                return
            xtm = sb("xtm", [128, 4, D], BF16)
            dxtm = P.dep("m_xtm")
            xTb = sb("xTb", [128, 8, 512], BF16)
            dxTb = P.dep("m_xTb")
            pT = self.psum(st, "mpT", [128, D], BF16)
            dpT = P.dep("m_pT")
            Wg = [sb("Wg", [128, 8, 512], BF16) for _ in range(3)]
            Wu = [sb("Wu", [128, 8, 512], BF16) for _ in range(3)]
            Wd = sb("Wd", [128, 28, D], BF16)
            dWd = P.dep("e_Wd")
            hT = sb("hT", [128, 28, 512], BF16)
            dhT = P.dep("e_hT")
            pg = [self.psum(st, "pg", [128, 512], F32) for _ in range(2)]
            pu = [self.psum(st, "pu", [128, 512], F32) for _ in range(2)]
            pd = [self.psum(st, "pd", [128, 512], F32) for _ in range(2)]
            sg = [sb("sg", [128, 512], F32) for _ in range(2)]
            ysb = [sb("ysb", [128, D], F32) for _ in range(2)]
            wi = 0
            npd = 0
            nys = 0
            xsr = self.XS.rearrange("(j s p) n -> j p s n", p=128, s=4)
            for j in range(NTILE):
                P.dma("sp", xtm[:], xsr[j], reads=[dXS], writes=[dxtm])
                for sub in range(4):
                    for c in range(8):
                        P.op("pe", lambda e, c=c, sub=sub: e.transpose(pT[:, c * 128:(c + 1) * 128], xtm[:, sub, c * 128:(c + 1) * 128], self.ident[:]),
                             reads=[dxtm, self.dconst], writes=[dpT], signal=(c == 7))
                    P.op("act", lambda e, sub=sub: e.activation(out=xTb[:, :, sub * 128:(sub + 1) * 128], in_=pT[:].rearrange("p (c t) -> p c t", c=8), func=AF.Copy),
                         reads=[dpT], writes=[dxTb])
                import os
                fstop = int(os.environ.get("MK_FSTOP", "99"))
                if fstop < 1:
                    continue
                for g in range(7):
                    wg_, dwg_ = Wg[wi % 3], P.dep("e_Wg%d" % (wi % 3))
                    wu_, dwu_ = Wu[wi % 3], P.dep("e_Wu%d" % (wi % 3))
                    wi += 1
                    P.idma(wg_[:].rearrange("p c n -> p (c n)"), None, self.WGt, R["idxgi"][:, j, g:g + 1], reads=[self.dwc["mg"], drt], writes=[dwg_])
                    P.idma(wu_[:].rearrange("p c n -> p (c n)"), None, self.WUt, R["idxgi"][:, j, g:g + 1], reads=[self.dwc["mu"], drt], writes=[dwu_])
                    for jj in range(4):
                        f = g * 4 + jj
                        pg_, dpg_ = pg[f % 2], P.dep("e_pg%d" % (f % 2))
                        pu_, dpu_ = pu[f % 2], P.dep("e_pu%d" % (f % 2))
                        for c in range(8):
                            P.op("pe", lambda e, c=c, jj=jj, pg_=pg_, wg_=wg_: e.matmul(pg_[:], lhsT=wg_[:, c, jj * 128:(jj + 1) * 128], rhs=xTb[:, c, :], start=(c == 0), stop=(c == 7)),
                                 reads=[dwg_, dxTb], writes=[dpg_], signal=(c == 7))
                        for c in range(8):
                            P.op("pe", lambda e, c=c, jj=jj, pu_=pu_, wu_=wu_: e.matmul(pu_[:], lhsT=wu_[:, c, jj * 128:(jj + 1) * 128], rhs=xTb[:, c, :], start=(c == 0), stop=(c == 7)),
                                 reads=[dwu_, dxTb], writes=[dpu_], signal=(c == 7))
                        s_, ds_ = sg[f % 2], P.dep("e_sg%d" % (f % 2))
                        P.op("act", lambda e, s_=s_, pg_=pg_: e.activation(out=s_[:], in_=pg_[:], func=AF.Silu), reads=[dpg_], writes=[ds_])
                        P.op("dve", lambda e, f=f, s_=s_, pu_=pu_: e.tensor_tensor(out=hT[:, f, :], in0=pu_[:], in1=s_[:], op=ALU.mult),
                             reads=[dpu_, ds_], writes=[dhT])
                if fstop < 2:
                    continue
                for q in range(4):
                    P.idma(Wd[:, q * 7:(q + 1) * 7, :].rearrange("p f n -> p (f n)"), None, self.WDt, R["idxdi"][:, j, q:q + 1], reads=[self.dwc["md"], drt], writes=[dWd])
                for sub in range(4):
                    y_, dy_ = ysb[nys % 2], P.dep("m_ysb%d" % (nys % 2))
                    nys += 1
                    for hf in range(2):
                        pd_, dpd_ = pd[npd % 2], P.dep("e_pd%d" % (npd % 2))
                        npd += 1
                        for f in range(28):
                            P.op("pe", lambda e, f=f, sub=sub, hf=hf, pd_=pd_: e.matmul(pd_[:], lhsT=hT[:, f, sub * 128:(sub + 1) * 128], rhs=Wd[:, f, hf * 512:(hf + 1) * 512],
                                                                                   start=(f == 0), stop=(f == 27)), reads=[dhT, dWd], writes=[dpd_], signal=(f == 27))
                        if hf == 0:
                            P.op("act", lambda e, y_=y_, pd_=pd_: e.activation(out=y_[:, 0:512], in_=pd_[:], func=AF.Copy), reads=[dpd_], writes=[dy_])
                        else:
                            P.op("dve", lambda e, y_=y_, pd_=pd_: e.tensor_copy(y_[:, 512:1024], pd_[:]), reads=[dpd_], writes=[dy_])
                    r0 = j * 512 + sub * 128
                    P.dma("sp", self.YS[r0:r0 + 128, :], y_[:], reads=[dy_], writes=[dYS])
            P.barrier()
            cur[0].close()
            cur[0] = ExitStack()
            if self.mstop < 5:
                return
            L = self.ln_setup(st, I["ln2_g"][l], I["ln2_b"][l], "2", nbuf=1)
            y1 = [sb("y1", [128, D], F32) for _ in range(2)]
            y2 = [sb("y2", [128, D], F32) for _ in range(2)]
            xr = [sb("xr", [128, D], F32) for _ in range(2)]
            pre = [sb("pre", [128, D], F32) for _ in range(2)]
            dres = P.dep("d_outres")
            for i in range(NT):
                i2 = i % 2
                a_, b_, x_, p_ = y1[i2], y2[i2], xr[i2], pre[i2]
                da_, db_, dx_, dp_ = (P.dep("m_%s%d" % (n, i2)) for n in ("y1", "y2", "xr", "pre"))
                P.idma(a_[:], None, self.YS, R["sloti"][:, i, 0:1], reads=[dYS, drt], writes=[da_])
                P.idma(b_[:], None, self.YS, R["sloti"][:, i, 1:2], reads=[dYS, drt], writes=[db_])
                P.dma("sp", x_[:], self.x1res[i * 128:(i + 1) * 128, :], reads=[P.dep("d_x1res")], writes=[dx_])
                P.op("dve", lambda e, a_=a_, i=i: e.tensor_scalar(out=a_[:], in0=a_[:], scalar1=R["g12"][:, i, 0:1], scalar2=None, op0=ALU.mult), reads=[da_, drt], writes=[da_])
                P.op("dve", lambda e, a_=a_, b_=b_, i=i: e.scalar_tensor_tensor(out=a_[:], in0=b_[:], scalar=R["g12"][:, i, 1:2], in1=a_[:], op0=ALU.mult, op1=ALU.add),
                     reads=[da_, db_, drt], writes=[da_])
                P.op("dve", lambda e, a_=a_, x_=x_, p_=p_: e.scalar_tensor_tensor(out=p_[:], in0=x_[:], scalar=ALPHA, in1=a_[:], op0=ALU.mult, op1=ALU.add),
                     reads=[da_, dx_], writes=[dp_])
                self.ln_tile(L, p_, dp_, None, None, 0, res_dst=out_res[i * 128:(i + 1) * 128, :], dres=dres)

    def phase_e(self, l, out_res, out_T):
        P, T, NB, I = self.P, self.T, self.NB, self.I
        P.barrier()
        moe = (l == 1)
        nexp = NE if moe else 1
        with ExitStack() as st:
            sb = lambda n, s, d: self.sb(st, n, s, d)
            L = self.ln_setup(st, I["ln2_g"][l], I["ln2_b"][l], "2", nbuf=1)
            xb_ = [sb("xb", [128, 8, 512], BF16) for _ in range(1)]
            Wg = [sb("Wg", [128, 8, 512], BF16) for _ in range(3)]
            Wu = [sb("Wu", [128, 8, 512], BF16) for _ in range(3)]
            Wd = sb("Wd", [128, 28, D], BF16)
            dWd = P.dep("e_Wd")
            hT = sb("hT", [128, 28, 512], BF16)
            dhT = P.dep("e_hT")
            pg = [self.psum(st, "pg", [128, 512], F32) for _ in range(2)]
            pu = [self.psum(st, "pu", [128, 512], F32) for _ in range(2)]
            pd = [self.psum(st, "pd", [128, 512], F32) for _ in range(2)]
            pc = self.psum(st, "pc", [128, 512], F32)
            sg = [sb("sg", [128, 512], F32) for _ in range(2)]
            xr = [sb("xr", [128, D], F32) for _ in range(1)]
            pre = [sb("pre", [128, D], F32) for _ in range(1)]
            xTs = [sb("xTs", [128, 8, 512], BF16) for _ in range(1)] if out_T is not None else None
            acc = sb("acc", [128, 4, D], F32) if moe else None
            dacc = P.dep("e_acc")
            if moe:
                selm = sb("selm", [NE, NE, 128], F32)
                dselm = P.dep("e_selm")
                P.op("pool", lambda e: e.memset(selm[:], 0.0), writes=[dselm])
                P.op("pool", lambda e: e.affine_select(out=selm[:], in_=selm[:], pattern=[[1, NE], [0, 128]], compare_op=ALU.not_equal, fill=1.0,
                                                       base=0, channel_multiplier=-1), reads=[dselm], writes=[dselm])
                cmb = [sb("cmb", [128, NE], F32) for _ in range(2)]
                cT = sb("cT", [NE, 512], F32)
                dcT = P.dep("e_cT")
                cbe = [sb("cbe", [128, 512], F32) for _ in range(2)]
                pT8 = pc[0:NE, 0:128]
                dpT8 = P.dep("e_pc")
            x1Tv = self.x1T.rearrange("(c p) t -> p c t", p=128)
            wi = 0
            npd = 0
            ntile = 0
            dres = P.dep("d_outres")
            dxT = P.dep("d_xT")
            for b in range(NB):
                ts_ = slice(b * 512, (b + 1) * 512)
                xb, dxb = xb_[0], P.dep("e_xb0")
                P.dma("sp", xb[:], x1Tv[:, :, ts_], reads=[P.dep("d_x1T")], writes=[dxb])
                if moe:
                    for sub in range(4):
                        cm, dcm = cmb[sub % 2], P.dep("e_cmb%d" % (sub % 2))
                        P.dma("sp", cm[:], self.comb[b * 512 + sub * 128:b * 512 + (sub + 1) * 128, :], reads=[P.dep("d_comb")], writes=[dcm])
                        P.op("pe", lambda e, cm=cm: e.transpose(pT8, cm[:], self.identf[:]), reads=[dcm, self.dconst], writes=[dpT8])
                        P.op("dve", lambda e, sub=sub: e.tensor_copy(cT[:, sub * 128:(sub + 1) * 128], pT8), reads=[dpT8], writes=[dcT])
                for ex_ in range(nexp):
                    if moe:
                        wgd, wud, wdd = self.wb["mg"][ex_], self.wb["mu"][ex_], self.wb["md"][ex_]
                        cb_, dcb_ = cbe[ex_ % 2], P.dep("e_cbe%d" % (ex_ % 2))
                        dpc = P.dep("e_pc")
                        P.op("pe", lambda e, ex_=ex_: e.matmul(pc[:], lhsT=selm[:, ex_, :], rhs=cT[:], start=True, stop=True), reads=[dselm, dcT], writes=[dpc])
                        P.op("act", lambda e, cb_=cb_: e.activation(out=cb_[:], in_=pc[:], func=AF.Copy), reads=[dpc], writes=[dcb_])
                    else:
                        wgd, wud, wdd = self.wb["dg"], self.wb["du"], self.wb["dd"]
                    if not (getattr(self, "moe_tables", False) and not moe):
                        wgv = wgd.rearrange("(c p) n -> p c n", p=128)
                        wuv = wud.rearrange("(c p) n -> p c n", p=128)
                    wdv = wdd.rearrange("(c p) n -> p c n", p=128)
                    for g in range(7):
                        wg_, dwg_ = Wg[wi % 3], P.dep("e_Wg%d" % (wi % 3))
                        wu_, dwu_ = Wu[wi % 3], P.dep("e_Wu%d" % (wi % 3))
                        wi += 1
                        if getattr(self, "moe_tables", False) and not moe:
                            P.dma("sp", wg_[:].rearrange("p c n -> p (c n)"), wgd[g * 128:(g + 1) * 128, :], reads=[self.dwc["g"]], writes=[dwg_])
                            P.dma("sp", wu_[:].rearrange("p c n -> p (c n)"), wud[g * 128:(g + 1) * 128, :], reads=[self.dwc["u"]], writes=[dwu_])
                        else:
                            P.dma("sp", wg_[:], wgv[:, :, g * 512:(g + 1) * 512], reads=[self.dwc["mg" if moe else "g"]], writes=[dwg_])
                            P.dma("sp", wu_[:], wuv[:, :, g * 512:(g + 1) * 512], reads=[self.dwc["mu" if moe else "u"]], writes=[dwu_])
                        for j in range(4):
                            f = g * 4 + j
                            pg_, dpg_ = pg[f % 2], P.dep("e_pg%d" % (f % 2))
                            pu_, dpu_ = pu[f % 2], P.dep("e_pu%d" % (f % 2))
                            for c in range(8):
                                P.op("pe", lambda e, c=c, j=j, pg_=pg_, wg_=wg_: e.matmul(pg_[:], lhsT=wg_[:, c, j * 128:(j + 1) * 128], rhs=xb[:, c, :], start=(c == 0), stop=(c == 7)),
                                     reads=[dwg_, dxb], writes=[dpg_], signal=(c == 7))
                            for c in range(8):
                                P.op("pe", lambda e, c=c, j=j, pu_=pu_, wu_=wu_: e.matmul(pu_[:], lhsT=wu_[:, c, j * 128:(j + 1) * 128], rhs=xb[:, c, :], start=(c == 0), stop=(c == 7)),
                                     reads=[dwu_, dxb], writes=[dpu_], signal=(c == 7))
                            s_, ds_ = sg[f % 2], P.dep("e_sg%d" % (f % 2))
                            P.op("act", lambda e, s_=s_, pg_=pg_: e.activation(out=s_[:], in_=pg_[:], func=AF.Silu), reads=[dpg_], writes=[ds_])
                            if moe:
                                P.op("pool", lambda e, s_=s_, cb_=cb_: e.tensor_tensor(out=s_[:], in0=s_[:], in1=cb_[:], op=ALU.mult), reads=[ds_, dcb_], writes=[ds_])
                            P.op("dve", lambda e, f=f, s_=s_, pu_=pu_: e.tensor_tensor(out=hT[:, f, :], in0=pu_[:], in1=s_[:], op=ALU.mult),
                                 reads=[dpu_, ds_], writes=[dhT])
                    for c0 in range(0, 28, 7):
                        P.dma("sp", Wd[:, c0:c0 + 7, :], wdv[:, c0:c0 + 7, :], reads=[self.dwc["md" if moe else "d"]], writes=[dWd])
                    for sub in range(4):
                        r0 = b * 512 + sub * 128
                        if ex_ == nexp - 1:
                            xrt, dxr = xr[0], P.dep("e_xr0")
                            pr, dpr = pre[0], P.dep("e_pre0")
                            ntile += 1
                            P.dma("sp", xrt[:], self.x1res[r0:r0 + 128, :], reads=[P.dep("d_x1res")], writes=[dxr])
                        for hf in range(2):
                            pd_, dpd_ = pd[npd % 2], P.dep("e_pd%d" % (npd % 2))
                            npd += 1
                            for f in range(28):
                                P.op("pe", lambda e, f=f, sub=sub, hf=hf, pd_=pd_: e.matmul(pd_[:], lhsT=hT[:, f, sub * 128:(sub + 1) * 128], rhs=Wd[:, f, hf * 512:(hf + 1) * 512],
                                                                                       start=(f == 0), stop=(f == 27)), reads=[dhT, dWd], writes=[dpd_], signal=(f == 27))
                            hs = slice(hf * 512, (hf + 1) * 512)
                            if moe and ex_ == 0:
                                P.op("dve", lambda e, sub=sub, hs=hs, pd_=pd_: e.tensor_copy(acc[:, sub, hs], pd_[:]), reads=[dpd_], writes=[dacc])
                            elif moe and ex_ < nexp - 1:
                                P.op("dve", lambda e, sub=sub, hs=hs, pd_=pd_: e.tensor_tensor(out=acc[:, sub, hs], in0=pd_[:], in1=acc[:, sub, hs], op=ALU.add),
                                     reads=[dpd_, dacc], writes=[dacc])
                            else:
                                if moe:
                                    P.op("dve", lambda e, sub=sub, hs=hs, pd_=pd_: e.tensor_tensor(out=acc[:, sub, hs], in0=pd_[:], in1=acc[:, sub, hs], op=ALU.add),
                                         reads=[dpd_, dacc], writes=[dacc])
                                    P.op("dve", lambda e, sub=sub, hs=hs, pr=pr, xrt=xrt: e.scalar_tensor_tensor(out=pr[:, hs], in0=xrt[:, hs], scalar=ALPHA, in1=acc[:, sub, hs],
                                                                                                              op0=ALU.mult, op1=ALU.add), reads=[dxr, dacc], writes=[dpr])
                                else:
                                    P.op("dve", lambda e, hs=hs, pr=pr, xrt=xrt, pd_=pd_: e.scalar_tensor_tensor(out=pr[:, hs], in0=xrt[:, hs], scalar=ALPHA, in1=pd_[:],
                                                                                                              op0=ALU.mult, op1=ALU.add), reads=[dxr, dpd_], writes=[dpr])
                        if ex_ == nexp - 1:
                            xs, dxs = (xTs[0] if xTs is not None else None), P.dep("e_xTs0")
                            self.ln_tile(L, pr, dpr, xs, dxs, sub, res_dst=out_res[r0:r0 + 128, :], dres=dres)
                if out_T is not None:
                    xs, dxs = xTs[0], P.dep("e_xTs0")
                    P.dma("sp", out_T.rearrange("(c p) t -> p c t", p=128)[:, :, ts_], xs[:], reads=[dxs], writes=[dxT])


_CACHE = {}


def _rope_tables(S):
    pos = np.arange(S, dtype=np.float32)
    inv = (np.float32(10000.0) ** (-np.arange(0, 32, 2, dtype=np.float32) / np.float32(32))).astype(np.float32)
    ang = pos[:, None] * inv[None, :]
    c = np.cos(ang).astype(np.float32).T
    s = np.sin(ang).astype(np.float32).T
    return np.concatenate([c, c], 0), np.concatenate([s, s], 0)


def _get_prog(S, layers, do_ln_in, final_out, dbg=()):
    key = (S, tuple(layers), do_ln_in, final_out, tuple(dbg))
    if key not in _CACHE:
        _CACHE[key] = Builder(S, list(layers), do_ln_in, final_out, dbg).build()
    return _CACHE[key]


WEIGHT_KEYS = ["w_in", "conv_w", "q_norm_g", "w_uq", "kv_norm_g", "w_ukv", "lru_conv_w", "lru_conv_b", "lru_wa", "lru_ba",
               "lru_wi", "lru_bi", "lru_lam", "mix_norm_g", "w_out", "ln1_g", "ln1_b", "ln2_g", "ln2_b"]


def _core_common(S, inputs, half, layers):
    T = S // 2
    C, Sn = _rope_tables(S)
    order = np.concatenate([np.arange(half * T, (half + 1) * T), np.arange((1 - half) * T, (2 - half) * T)])
    flags = np.zeros((128, 2), np.float32)
    flags[:, half] = 1.0
    m = {"flags": flags, "ropeC": np.ascontiguousarray(C[:, order]), "ropeS": np.ascontiguousarray(Sn[:, order])}
    for k in WEIGHT_KEYS:
        m[k] = np.ascontiguousarray(inputs[k])
    if 0 in layers:
        for k in ("dense_w_gate", "dense_w_up", "dense_w_down"):
            m[k] = np.ascontiguousarray(inputs[k])
    if 1 in layers:
        for k in ("moe_w_router", "moe_w_gate", "moe_w_up", "moe_w_down"):
            m[k] = np.ascontiguousarray(inputs[k])
    return m


def kernel(**inputs):
    x = np.asarray(inputs["x"])
    B, S, _ = x.shape
    T = S // 2
    ncore = 2 * B
    key = ("fused", S)
    if key not in _CACHE:
        _CACHE[key] = Builder(S, [0, 1], True, True).build_fused()
    nc = _CACHE[key]
    C, Sn = _rope_tables(S)
    maps = []
    for core in range(ncore):
        b, half = core // 2, core % 2
        order = np.concatenate([np.arange(half * T, (half + 1) * T), np.arange((1 - half) * T, (2 - half) * T)])
        flags = np.zeros((128, 2), np.float32)
        flags[:, half] = 1.0
        m = {"flags": flags, "ropeC0": C, "ropeS0": Sn,
             "ropeC1": np.ascontiguousarray(C[:, order]), "ropeS1": np.ascontiguousarray(Sn[:, order]),
             "x_full": np.ascontiguousarray(x[b]),
             "ln_in_g": np.ascontiguousarray(inputs["ln_in_g"]), "ln_in_b": np.ascontiguousarray(inputs["ln_in_b"])}
        for k in WEIGHT_KEYS + ["dense_w_gate", "dense_w_up", "dense_w_down", "moe_w_router", "moe_w_gate", "moe_w_up", "moe_w_down"]:
            m[k] = np.ascontiguousarray(inputs[k])
        maps.append(m)
    res = run_bass_kernel_spmd(nc, maps, core_ids=list(range(ncore))).results
    out = np.empty((B, S, D), np.float32)
    for core in range(ncore):
        b, half = core // 2, core % 2
        out[b, half * T:(half + 1) * T] = res[core]["y_out"]
    return out


def kernel_unfused(**inputs):
    x = np.asarray(inputs["x"])
    B, S, _ = x.shape
    T = S // 2
    ncore = 2 * B
    nc0 = _get_prog(S, (0,), True, False)
    maps = []
    for core in range(ncore):
        b, half = core // 2, core % 2
        m = _core_common(S, inputs, half, (0,))
        m["x_own"] = np.ascontiguousarray(x[b, half * T:(half + 1) * T])
        m["x_oth"] = np.ascontiguousarray(x[b, (1 - half) * T:(2 - half) * T])
        m["ln_in_g"] = np.ascontiguousarray(inputs["ln_in_g"])
        m["ln_in_b"] = np.ascontiguousarray(inputs["ln_in_b"])
        maps.append(m)
    r0 = run_bass_kernel_spmd(nc0, maps, core_ids=list(range(ncore))).results
    nc1 = _get_prog(S, (1,), False, True)
    maps = []
    for core in range(ncore):
        half = core % 2
        m = _core_common(S, inputs, half, (1,))
        m["xres_in"] = r0[core]["xres_out"]
        m["xT_in"] = np.ascontiguousarray(np.concatenate([r0[core]["xT_out"], r0[core ^ 1]["xT_out"]], axis=1))
        maps.append(m)
    r1 = run_bass_kernel_spmd(nc1, maps, core_ids=list(range(ncore))).results
    out = np.empty((B, S, D), np.float32)
    for core in range(ncore):
        b, half = core // 2, core % 2
        out[b, half * T:(half + 1) * T] = r1[core]["y_out"]
    return out
```

```python
import numpy as np
from contextlib import ExitStack
import concourse.bass as bass
import concourse.mybir as mybir
from concourse.bass_utils import run_bass_kernel_spmd

F32 = mybir.dt.float32
BF16 = mybir.dt.bfloat16
AF = mybir.ActivationFunctionType
ALU = mybir.AluOpType

D = 1024
DIN = 1952
NH = 8
DFF = 3584
NE = 8
ALPHA = 4.0 ** 0.25
LN_EPS = 1e-5
RMS_EPS = 1e-6
QSCALE = 96.0 ** -0.5
import os as _os
LNSTOP = int(_os.environ.get("MK_LNSTOP", "99"))
ASTOP = int(_os.environ.get("MK_ASTOP", "99"))


class Dep:
    __slots__ = ("name", "w", "r", "dsem", "dcnt", "excl")

    def __init__(self, name=""):
        self.name = name
        self.excl = any(t in name for t in ("_ps", "_pS", "_pO", "_pD", "_prr", "_pg", "_pu", "_pd", "_pc", "pT"))
        self.w = None
        self.r = []
        self.dsem = None
        self.dcnt = 0


class _Rec:
    def __init__(self):
        self.call = None

    def __getattr__(self, name):
        def f(*a, **k):
            self.call = (name, a, k)
            return self
        return f


class Prog:
    ENGS = ("pe", "act", "dve", "pool", "sp")

    def __init__(self, nc):
        self.nc = nc
        self.streams = {e: [] for e in self.ENGS}
        self.sem = {e: nc.alloc_semaphore("es_" + e) for e in self.ENGS}
        self.cnt = {e: 0 for e in self.ENGS}
        self.seen = {e: {} for e in self.ENGS}
        self.deps = {}
        self.ndsem = 0
        self.store_q = "act"

    def dep(self, name):
        d = self.deps.get(name)
        if d is None:
            d = Dep(name)
            self.deps[name] = d
        return d

    def _dsem(self, d):
        if d.dsem is None:
            d.dsem = self.nc.alloc_semaphore("ds_%d" % self.ndsem)
            self.ndsem += 1
        return d.dsem

    def _collect(self, eng, reads, writes):
        need = {}

        def add(t):
            if t is None:
                return
            sem, val = t
            k = id(sem)
            if k not in need or need[k][1] < val:
                need[k] = (sem, val)
        own_sem = self.sem[eng]
        for d in reads:
            add(d.w)
            if d.excl:
                for t in d.r:
                    if t[0] is not own_sem:
                        add(t)
        for d in writes:
            add(d.w)
            for t in d.r:
                add(t)
        out = []
        seen = self.seen[eng]
        own = self.sem[eng]
        for k, (sem, val) in need.items():
            if sem is own and (eng == "pe" or val > self.cnt[eng]):
                continue
            if seen.get(k, 0) >= val:
                continue
            seen[k] = val
            out.append((sem, val))
        return out

    def _update(self, tk, reads, writes):
        for d in reads:
            d.r.append(tk)
            if len(d.r) > 64:
                d.r = d.r[-48:]
        for d in writes:
            d.w = tk
            d.r = []

    def op(self, eng, fn, reads=(), writes=(), signal=True):
        waits = self._collect(eng, reads, writes)
        sem = self.sem[eng]
        if signal:
            self.cnt[eng] += 1
            tk = (sem, self.cnt[eng])
        else:
            tk = (sem, self.cnt[eng] + 1)

        rec = _Rec()
        fn(rec)
        call = rec.call

        def emit(e, call=call, waits=waits, signal=signal, sem=sem):
            for s, v in waits:
                e.wait_ge(s, v)
            ins = getattr(e, call[0])(*call[1], **call[2])
            if signal:
                ins.then_inc(sem, 1)
        self.streams[eng].append(emit)
        self._update(tk, reads, writes)
        return tk

    def dma(self, q, out, in_, reads=(), writes=(), **kw):
        if q == "sp" and self.store_q is not None and type(out.tensor).__name__.startswith("DRam") \
                and not type(in_.tensor).__name__.startswith("DRam"):
            q = self.store_q
        waits = self._collect(q, reads, writes)
        d0 = writes[0]
        sem = self._dsem(d0)
        d0.dcnt += 16
        tk = (sem, d0.dcnt)

        def emit(e, waits=waits, sem=sem, out=out, in_=in_, kw=kw):
            for s, v in waits:
                e.wait_ge(s, v)
            e.dma_start(out=out, in_=in_, **kw).then_inc(sem, 16)
        self.streams[q].append(emit)
        self._update(tk, reads, writes)
        return tk

    def idma(self, out, out_idx, in_, in_idx, reads=(), writes=(), **kw):
        q = "pool"
        waits = self._collect(q, reads, writes)
        d0 = writes[0]
        sem = self._dsem(d0)
        d0.dcnt += 16
        tk = (sem, d0.dcnt)

        def emit(e, waits=waits, sem=sem):
            for s_, v in waits:
                e.wait_ge(s_, v)
            oo = bass.IndirectOffsetOnAxis(ap=out_idx, axis=0) if out_idx is not None else None
            io = bass.IndirectOffsetOnAxis(ap=in_idx, axis=0) if in_idx is not None else None
            e.indirect_dma_start(out=out, out_offset=oo, in_=in_, in_offset=io, **kw).then_inc(sem, 16)
        self.streams[q].append(emit)
        self._update(tk, reads, writes)
        return tk

    def barrier(self):
        tks = [(self.sem[e], self.cnt[e]) for e in self.ENGS if self.cnt[e] > 0]
        for d in self.deps.values():
            if d.dsem is not None and d.dcnt > 0:
                tks.append((d.dsem, d.dcnt))
        for e in self.ENGS:
            seen = self.seen[e]
            waits = []
            for sem, val in tks:
                if sem is self.sem[e]:
                    continue
                if seen.get(id(sem), 0) >= val:
                    continue
                seen[id(sem)] = val
                waits.append((sem, val))

            def emit(en, waits=waits):
                for s, v in waits:
                    en.wait_ge(s, v)
            self.streams[e].append(emit)
        for d in self.deps.values():
            d.w = None
            d.r = []

    def finish(self):
        nc = self.nc
        st = self.streams
        with nc.Block() as block:
            @block.tensor
            def _(e):
                for f in st["pe"]:
                    f(e)

            @block.scalar
            def _(e):
                for f in st["act"]:
                    f(e)

            @block.vector
            def _(e):
                for f in st["dve"]:
                    f(e)

            @block.gpsimd
            def _(e):
                for f in st["pool"]:
                    f(e)

            @block.sync
            def _(e):
                for f in st["sp"]:
                    f(e)
        return nc


class Builder:
    def __init__(self, S, layers, do_ln_in, final_out, dbg=()):
        self.S = S
        self.T = S // 2
        self.NB = self.T // 512
        self.layers = layers
        self.dbg = set(dbg)
        nc = bass.Bass("TRN2", target_bir_lowering=False)
        self.nc = nc
        self.P = Prog(nc)
        self.do_ln_in = do_ln_in
        self.final_out = final_out
        self.cnt = 0
        self.ps_rr = 0
        self.has_other = True

    def din(self, name, shape, dt=F32):
        return self.nc.dram_tensor(name, list(shape), dt, kind="ExternalInput").ap()

    def dout(self, name, shape, dt=F32):
        return self.nc.dram_tensor(name, list(shape), dt, kind="ExternalOutput").ap()

    def dscr(self, name, shape, dt):
        kind = "ExternalOutput" if name in self.dbg else "Internal"
        return self.nc.dram_tensor(name, list(shape), dt, kind=kind).ap()

    def sb(self, st, name, shape, dt):
        self.cnt += 1
        return st.enter_context(self.nc.sbuf_tensor("%s_%d" % (name, self.cnt), list(shape), dt))

    def psum(self, st, name, shape, dt=F32):
        self.cnt += 1
        return st.enter_context(self.nc.psum_tensor("%s_%d" % (name, self.cnt), list(shape), dt))

    def load_cols(self, st, name, vec_ap, n, q="sp"):
        P = self.P
        nchunk = n // 128
        t = self.sb(st, name, [128, nchunk], F32)
        d = P.dep(name)
        v2 = vec_ap.rearrange("(c p o) -> c p o", p=128, o=1)
        for c in range(nchunk):
            P.dma(q, t[:, c:c + 1], v2[c], writes=[d])
        return t, d

    def build(self):
        nc, P, T, NB = self.nc, self.P, self.T, self.NB
        S2 = 2 * T
        I = {}
        if self.do_ln_in:
            I["x_own"] = self.din("x_own", [T, D])
            I["x_oth"] = self.din("x_oth", [T, D])
            I["ln_in_g"] = self.din("ln_in_g", [D])
            I["ln_in_b"] = self.din("ln_in_b", [D])
        else:
            I["xres_in"] = self.din("xres_in", [T, D])
            I["xT_in"] = self.din("xT_in", [D, S2], BF16)
        I["flags"] = self.din("flags", [128, 2])
        I["ropeC"] = self.din("ropeC", [32, S2])
        I["ropeS"] = self.din("ropeS", [32, S2])
        nl = 2
        shapes = dict(w_in=[nl, D, DIN], conv_w=[nl, 3, 256], q_norm_g=[nl, 384], w_uq=[nl, 384, 768],
                      kv_norm_g=[nl, 256], w_ukv=[nl, 256, 1024], lru_conv_w=[nl, 4, 256],
                      lru_conv_b=[nl, 256], lru_wa=[nl, 2, 4, 64, 64], lru_ba=[nl, 2, 256],
                      lru_wi=[nl, 2, 4, 64, 64], lru_bi=[nl, 2, 256], lru_lam=[nl, 2, 256],
                      mix_norm_g=[nl, D], w_out=[nl, D, D], ln1_g=[nl, D], ln1_b=[nl, D],
                      dense_w_gate=[1, D, DFF], dense_w_up=[1, D, DFF], dense_w_down=[1, DFF, D],
                      moe_w_router=[1, D, NE], moe_w_gate=[1, NE, D, DFF], moe_w_up=[1, NE, D, DFF],
                      moe_w_down=[1, NE, DFF, D], ln2_g=[nl, D], ln2_b=[nl, D])
        need_dense = 0 in self.layers
        need_moe = 1 in self.layers
        for k, shp in shapes.items():
            if k.startswith("dense") and not need_dense:
                continue
            if k.startswith("moe") and not need_moe:
                continue
            I[k] = self.din(k, shp)
        self.I = I
        if self.final_out:
            self.y_out = self.dout("y_out", [T, D])
        else:
            self.y_out = self.dout("xres_out", [T, D])
            self.xT_out = self.dout("xT_out", [D, T], BF16)
        self.xres = self.dscr("s_xres", [T, D], F32)
        self.xT = self.dscr("s_xT", [D, S2], BF16)
        self.QT = self.dscr("s_QT", [NH, 96, T], BF16)
        self.KT = self.dscr("s_KT", [NH, 96, S2], BF16)
        self.Vd = self.dscr("s_V", [S2, NH * 65], BF16)
        self.CB = self.dscr("s_CB", [256, T], F32)
        self.PP = self.dscr("s_PP", [256, S2], F32)
        self.LG = self.dscr("s_LG", [256, T], F32)
        self.LX = self.dscr("s_LX", [256, S2], F32)
        self.Y = self.dscr("s_Y", [D, T], F32)
        self.x1res = self.dscr("s_x1res", [T, D], F32)
        self.x1T = self.dscr("s_x1T", [D, T], BF16)
        self.comb = self.dscr("s_comb", [T, NE], F32)
        self.wb = {}
        for l in self.layers:
            self.wb[("w_in", l)] = self.dscr("b_w_in%d" % l, [D, DIN], BF16)
            self.wb[("w_out", l)] = self.dscr("b_w_out%d" % l, [D, D], BF16)
        if need_dense:
            self.wb["dg"] = self.dscr("b_dg", [D, DFF], BF16)
            self.wb["du"] = self.dscr("b_du", [D, DFF], BF16)
            self.wb["dd"] = self.dscr("b_dd", [DFF, D], BF16)
        if need_moe:
            self.wb["mg"] = self.dscr("b_mg", [NE, D, DFF], BF16)
            self.wb["mu"] = self.dscr("b_mu", [NE, D, DFF], BF16)
            self.wb["md"] = self.dscr("b_md", [NE, DFF, D], BF16)

        with ExitStack() as gst:
            import os
            stop = int(os.environ.get("MK_STOP", "99"))
            self.consts(gst)
            if stop >= 1:
                self.cast_weights()
            if self.do_ln_in:
                self.ln_srcs = ((I["x_own"], 0, True), (I["x_oth"], T, False))
                self.ropeC, self.ropeS = I["ropeC"], I["ropeS"]
                if stop >= 2:
                    self.phase_ln_in()
                xres, xT = self.xres, self.xT
            else:
                self.ropeC, self.ropeS = I["ropeC"], I["ropeS"]
                xres, xT = I["xres_in"], I["xT_in"]
            for li, l in enumerate(self.layers):
                last = li == len(self.layers) - 1
                if stop >= 3:
                    self.phase_a(l, xT)
                if stop >= 4:
                    self.phase_b(l)
                if stop >= 5:
                    self.phase_c(l)
                if stop >= 6:
                    self.phase_d(l, xres)
                if stop < 7:
                    continue
                if last:
                    out_res = self.y_out
                    out_T = None if self.final_out else self.xT_out
                else:
                    out_res, out_T = self.xres, self.xT
                self.phase_e(l, out_res, out_T)
                xres, xT = self.xres, self.xT
            P.barrier()
        P.finish()
        return nc

    def build_fused(self):
        nc, P, S = self.nc, self.P, self.S
        Th = S // 2
        I = {}
        I["x_full"] = self.din("x_full", [S, D])
        I["ln_in_g"] = self.din("ln_in_g", [D])
        I["ln_in_b"] = self.din("ln_in_b", [D])
        I["flags"] = self.din("flags", [128, 2])
        for k in ("ropeC0", "ropeS0", "ropeC1", "ropeS1"):
            I[k] = self.din(k, [32, S])
        nl = 2
        shapes = dict(w_in=[nl, D, DIN], conv_w=[nl, 3, 256], q_norm_g=[nl, 384], w_uq=[nl, 384, 768],
                      kv_norm_g=[nl, 256], w_ukv=[nl, 256, 1024], lru_conv_w=[nl, 4, 256],
                      lru_conv_b=[nl, 256], lru_wa=[nl, 2, 4, 64, 64], lru_ba=[nl, 2, 256],
                      lru_wi=[nl, 2, 4, 64, 64], lru_bi=[nl, 2, 256], lru_lam=[nl, 2, 256],
                      mix_norm_g=[nl, D], w_out=[nl, D, D], ln1_g=[nl, D], ln1_b=[nl, D],
                      dense_w_gate=[1, D, DFF], dense_w_up=[1, D, DFF], dense_w_down=[1, DFF, D],
                      moe_w_router=[1, D, NE], moe_w_gate=[1, NE, D, DFF], moe_w_up=[1, NE, D, DFF],
                      moe_w_down=[1, NE, DFF, D], ln2_g=[nl, D], ln2_b=[nl, D])
        for k, shp in shapes.items():
            I[k] = self.din(k, shp)
        self.I = I
        self.y_out = self.dout("y_out", [Th, D])
        self.xres = self.dscr("s_xres", [S, D], F32)
        self.xT = self.dscr("s_xT", [D, S], BF16)
        self.QT = self.dscr("s_QT", [NH, 96, S], BF16)
        self.KT = self.dscr("s_KT", [NH, 96, S], BF16)
        self.Vd = self.dscr("s_V", [S, NH * 65], BF16)
        self.CB = self.dscr("s_CB", [256, S], F32)
        self.PP = self.dscr("s_PP", [256, S], F32)
        self.LG = self.dscr("s_LG", [256, S], F32)
        self.LX = self.dscr("s_LX", [256, S], F32)
        self.Y = self.dscr("s_Y", [D, S], F32)
        self.x1res = self.dscr("s_x1res", [S, D], F32)
        self.x1T = self.dscr("s_x1T", [D, S], BF16)
        self.comb = self.dscr("s_comb", [S, NE], F32)
        o0res = self.dscr("s_o0res", [S, D], F32)
        o0T = self.dscr("s_o0T", [D, S], BF16)
        self.wb = {}
        for l in (0, 1):
            self.wb[("w_in", l)] = self.dscr("b_w_in%d" % l, [D, DIN], BF16)
            self.wb[("w_out", l)] = self.dscr("b_w_out%d" % l, [D, D], BF16)
        self.wb["dg"] = self.dscr("b_dg", [7 * 128, 8 * 512], BF16)
        self.wb["du"] = self.dscr("b_du", [7 * 128, 8 * 512], BF16)
        self.wb["dd"] = self.dscr("b_dd", [DFF, D], BF16)
        self.WGt = self.dscr("b_WGt", [NE * 7 * 128, 8 * 512], BF16)
        self.WUt = self.dscr("b_WUt", [NE * 7 * 128, 8 * 512], BF16)
        self.WDt = self.dscr("b_WDt", [NE * 4 * 128, 7 * D], BF16)
        self.X1B = self.dscr("s_X1B", [Th, D], BF16)
        ntile = (2 * Th + NE * 511) // 512
        self.XS = self.dscr("s_XS", [ntile * 512, D], BF16)
        self.YS = self.dscr("s_YS", [ntile * 512, D], F32)
        self.moe_tables = True
        self.layers = [0, 1]
        with ExitStack() as gst:
            self.consts(gst)
            self.cast_weights(part=0)
            import os
            fstop = int(os.environ.get("MK_FUSED_STOP", "99"))
            self.mstop = 99
            steps = []
            def set0():
                self.T, self.NB, self.has_other = S, S // 512, False
                self.ropeC, self.ropeS = I["ropeC0"], I["ropeS0"]
                self.ln_srcs = ((I["x_full"], 0, True),)
            def set1():
                self.T, self.NB, self.has_other = Th, Th // 512, True
            def set1b():
                self.ropeC, self.ropeS = I["ropeC1"], I["ropeS1"]
            steps = [lambda: (set0(), self.phase_ln_in()),
                     lambda: (self.phase_a(0, self.xT), self.cast_weights(part=1)),
                     lambda: self.phase_b(0),
                     lambda: self.phase_c(0),
                     lambda: self.phase_d(0, self.xres),
                     lambda: self.phase_e(0, o0res, o0T),
                     lambda: (set1(), self.phase_sel(o0res, o0T), set1b()),
                     lambda: self.phase_a(1, self.xT),
                     lambda: self.phase_b(1),
                     lambda: self.phase_c(1),
                     lambda: (self.route_setup(gst), self.phase_d(1, self.xres)),
                     lambda: self.phase_r(),
                     lambda: self.phase_e_moe(1, self.y_out)]
            for k, f in enumerate(steps):
                if k < fstop:
                    f()
            P.barrier()
        P.finish()
        return nc

    def phase_sel(self, o0res, o0T):
        P, T, NB = self.P, self.T, self.NB
        P.barrier()
        f0 = self.flags[:, 0:1]
        f1 = self.flags[:, 1:2]
        with ExitStack() as st:
            sb = lambda n, s, d: self.sb(st, n, s, d)
            ra = [sb("ra", [128, D], F32) for _ in range(2)]
            rb = [sb("rb", [128, D], F32) for _ in range(2)]
            dres = P.dep("d_xres")
            for i in range(T // 128):
                a_, da_ = ra[i % 2], P.dep("s_ra%d" % (i % 2))
                b_, db_ = rb[i % 2], P.dep("s_rb%d" % (i % 2))
                P.dma("sp", a_[:], o0res[i * 128:(i + 1) * 128, :], reads=[P.dep("d_outres")], writes=[da_])
                P.dma("sp", b_[:], o0res[T + i * 128:T + (i + 1) * 128, :], reads=[P.dep("d_outres")], writes=[db_])
                P.op("dve", lambda e, a_=a_: e.tensor_scalar(out=a_[:], in0=a_[:], scalar1=f0, scalar2=None, op0=ALU.mult), reads=[da_, self.dconst], writes=[da_])
                P.op("dve", lambda e, a_=a_, b_=b_: e.scalar_tensor_tensor(out=a_[:], in0=b_[:], scalar=f1, in1=a_[:], op0=ALU.mult, op1=ALU.add),
                     reads=[da_, db_, self.dconst], writes=[da_])
                P.dma("sp", self.xres[i * 128:(i + 1) * 128, :], a_[:], reads=[da_], writes=[dres])
            ta = [sb("ta", [128, 8, 512], BF16) for _ in range(2)]
            tb = [sb("tb", [128, 8, 512], BF16) for _ in range(2)]
            to = [sb("to", [128, 8, 512], BF16) for _ in range(2)]
            tt = [sb("tt", [128, 8, 512], BF16) for _ in range(2)]
            ov = o0T.rearrange("(c p) t -> p c t", p=128)
            xv = self.xT.rearrange("(c p) t -> p c t", p=128)
            dxT = P.dep("d_xT")
            for j in range(NB):
                i2 = j % 2
                a_, b_, o_, t_ = ta[i2], tb[i2], to[i2], tt[i2]
                da_, db_, do_, dt_ = (P.dep("s_%s%d" % (n, i2)) for n in ("ta", "tb", "to", "tt"))
                P.dma("sp", a_[:], ov[:, :, j * 512:(j + 1) * 512], reads=[P.dep("d_xT")], writes=[da_])
                P.dma("sp", b_[:], ov[:, :, T + j * 512:T + (j + 1) * 512], reads=[P.dep("d_xT")], writes=[db_])
                P.op("dve", lambda e, o_=o_, a_=a_: e.tensor_scalar(out=o_[:], in0=a_[:], scalar1=f0, scalar2=None, op0=ALU.mult), reads=[da_, self.dconst], writes=[do_])
                P.op("dve", lambda e, o_=o_, b_=b_: e.scalar_tensor_tensor(out=o_[:], in0=b_[:], scalar=f1, in1=o_[:], op0=ALU.mult, op1=ALU.add),
                     reads=[db_, do_, self.dconst], writes=[do_])
                P.op("pool", lambda e, t_=t_, a_=a_: e.tensor_scalar(out=t_[:], in0=a_[:], scalar1=f1, scalar2=None, op0=ALU.mult), reads=[da_, self.dconst], writes=[dt_])
                P.op("dve", lambda e, t_=t_, b_=b_: e.scalar_tensor_tensor(out=t_[:], in0=b_[:], scalar=f0, in1=t_[:], op0=ALU.mult, op1=ALU.add),
                     reads=[db_, dt_, self.dconst], writes=[dt_])
                P.dma("sp", xv[:, :, j * 512:(j + 1) * 512], o_[:], reads=[do_], writes=[dxT])
                P.dma("sp", xv[:, :, T + j * 512:T + (j + 1) * 512], t_[:], reads=[dt_], writes=[dxT])

    def consts(self, st):
        P = self.P
        self.ident = self.sb(st, "ident", [128, 128], BF16)
        self.identf = self.sb(st, "identf", [128, 128], F32)
        self.ones = self.sb(st, "ones", [128, 128], BF16)
        self.eps_ln = self.sb(st, "epsln", [128, 1], F32)
        self.eps_rms = self.sb(st, "epsrms", [128, 1], F32)
        self.flags = self.sb(st, "flags", [128, 2], F32)
        d = P.dep("consts")
        self.dconst = d
        P.op("pool", lambda e: e.memset(self.identf[:], 0.0), writes=[d])
        P.op("pool", lambda e: e.affine_select(out=self.identf[:], in_=self.identf[:], pattern=[[-1, 128]],
                                               compare_op=ALU.not_equal, fill=1.0, base=0,
                                               channel_multiplier=1), reads=[d], writes=[d])
        P.op("dve", lambda e: e.tensor_copy(self.ident[:], self.identf[:]), reads=[d], writes=[d])
        P.op("dve", lambda e: e.memset(self.ones[:], 1.0), writes=[d])
        P.op("dve", lambda e: e.memset(self.eps_ln[:], LN_EPS), writes=[d])
        P.op("dve", lambda e: e.memset(self.eps_rms[:], RMS_EPS), writes=[d])
        P.dma("sp", self.flags[:], self.I["flags"], writes=[d])

    def cast_weights(self, part=None):
        P, I = self.P, self.I
        if not hasattr(self, "dwc"):
            self.dwc = {}
        layers_sel = self.layers if part is None else [part]

        def cast2d(dst, src, rows, cols, key):
            d = P.dep("wc_" + key)
            self.dwc[key] = d
            cstep = cols
            while cstep > 2048:
                cstep //= 2
            rstep = max(1, min(rows, 4096 // (cols // cstep)))
            for r0 in range(0, rows, rstep):
                for c0 in range(0, cols, cstep):
                    P.dma("pool", dst[r0:r0 + rstep, c0:c0 + cstep], src[r0:r0 + rstep, c0:c0 + cstep],
                          writes=[d])
        for l in layers_sel:
            cast2d(self.wb[("w_in", l)], I["w_in"][l], D, DIN, "w_in%d" % l)
            cast2d(self.wb[("w_out", l)], I["w_out"][l], D, D, "w_out%d" % l)
        if 0 in layers_sel and getattr(self, "moe_tables", False):
            for key, tab, src in (("g", self.wb["dg"], I["dense_w_gate"]), ("u", self.wb["du"], I["dense_w_up"])):
                d = P.dep("wc_" + key)
                self.dwc[key] = d
                for g in range(7):
                    P.dma("pool", tab[g * 128:(g + 1) * 128, :].rearrange("p (c n) -> p c n", c=8),
                          src[0][:, g * 512:(g + 1) * 512].rearrange("(c p) n -> p c n", p=128), writes=[d])
        elif 0 in layers_sel:
            cast2d(self.wb["dg"], I["dense_w_gate"][0], D, DFF, "g")
            cast2d(self.wb["du"], I["dense_w_up"][0], D, DFF, "u")
        if 0 in layers_sel:
            cast2d(self.wb["dd"], I["dense_w_down"][0], DFF, D, "d")
        if 1 in layers_sel and getattr(self, "moe_tables", False):
            for key, tab, src in (("mg", self.WGt, I["moe_w_gate"]), ("mu", self.WUt, I["moe_w_up"])):
                d = P.dep("wc_" + key)
                self.dwc[key] = d
                for e in range(NE):
                    for g in range(7):
                        r0 = (e * 7 + g) * 128
                        P.dma("pool", tab[r0:r0 + 128, :].rearrange("p (c n) -> p c n", c=8),
                              src[0, e][:, g * 512:(g + 1) * 512].rearrange("(c p) n -> p c n", p=128), writes=[d])
            d = P.dep("wc_md")
            self.dwc["md"] = d
            for e in range(NE):
                for q in range(4):
                    r0 = (e * 4 + q) * 128
                    P.dma("pool", self.WDt[r0:r0 + 128, :].rearrange("p (f n) -> p f n", f=7),
                          I["moe_w_down"][0, e][q * 896:(q + 1) * 896, :].rearrange("(f p) n -> p f n", p=128), writes=[d])
        elif 1 in layers_sel:
            for e in range(NE):
                cast2d(self.wb["mg"][e], I["moe_w_gate"][0, e], D, DFF, "mg")
                cast2d(self.wb["mu"][e], I["moe_w_up"][0, e], D, DFF, "mu")
                cast2d(self.wb["md"][e], I["moe_w_down"][0, e], DFF, D, "md")

    def ln_setup(self, st, g_ap, b_ap, tag, nbuf=2):
        P = self.P
        L = {}
        L["G"] = self.sb(st, "lnG", [128, D], F32)
        L["B"] = self.sb(st, "lnB", [128, D], F32)
        L["dgb"] = P.dep("lnGB")
        P.dma("sp", L["G"][:], g_ap.partition_broadcast(128), writes=[L["dgb"]])
        P.dma("sp", L["B"][:], b_ap.partition_broadcast(128), writes=[L["dgb"]])
        L["n"] = 0
        L["nbuf"] = nbuf
        for i in range(nbuf):
            L["st%d" % i] = self.sb(st, "lnst", [128, 2, 6], F32)
            L["mv%d" % i] = self.sb(st, "lnmv", [128, 2], F32)
            L["sd%d" % i] = self.sb(st, "lnsd", [128, 1], F32)
            L["rs%d" % i] = self.sb(st, "lnrs", [128, 1], F32)
            L["xn%d" % i] = self.sb(st, "lnxn", [128, D], F32)
            L["xo%d" % i] = self.sb(st, "lnxo", [128, D], F32)
            L["xb%d" % i] = self.sb(st, "lnxb", [128, D], BF16)
            L["pT%d" % i] = self.psum(st, "lnpT", [128, D], BF16)
        return L

    def ln_tile(self, L, xt, dxt, xTs, dxTs, sub, res_dst=None, dres=None, xb_dst=None, dxbd=None):
        P = self.P
        i = L["n"] % L["nbuf"]
        L["n"] += 1
        tg = "ln%d" % i
        st_, mv, sd, rs, xn, xo, xb, pT = (L[k + str(i)] for k in ("st", "mv", "sd", "rs", "xn", "xo", "xb", "pT"))
        dst_, dmv, dsd, drs, dxn, dxo, dxb, dpT = (P.dep(tg + k) for k in ("st", "mv", "sd", "rs", "xn", "xo", "xb", "pT"))
        for h in range(2):
            P.op("dve", lambda e, h=h: e.bn_stats(out=st_[:, h, :], in_=xt[:, h * 512:(h + 1) * 512]),
                 reads=[dxt], writes=[dst_])
        if LNSTOP < 1:
            return
        P.op("dve", lambda e: e.bn_aggr(out=mv[:], in_=st_[:].rearrange("p a b -> p (a b)")), reads=[dst_], writes=[dmv])
        if LNSTOP < 2:
            return
        P.op("act", lambda e: e.activation(out=sd[:], in_=mv[:, 1:2], func=AF.Sqrt, bias=self.eps_ln[:], scale=1.0),
             reads=[dmv, self.dconst], writes=[dsd])
        if LNSTOP < 3:
            return
        P.op("dve", lambda e: e.reciprocal(out=rs[:], in_=sd[:]), reads=[dsd], writes=[drs])
        if LNSTOP < 4:
            return
        P.op("dve", lambda e: e.tensor_scalar(out=xn[:], in0=xt[:], scalar1=mv[:, 0:1], scalar2=rs[:],
                                               op0=ALU.subtract, op1=ALU.mult), reads=[dxt, dmv, drs], writes=[dxn])
        if LNSTOP < 5:
            return
        P.op("pool", lambda e: e.tensor_tensor(out=xn[:], in0=xn[:], in1=L["G"][:], op=ALU.mult),
             reads=[dxn, L["dgb"]], writes=[dxn])
        P.op("pool", lambda e: e.tensor_tensor(out=xo[:], in0=xn[:], in1=L["B"][:], op=ALU.add),
             reads=[dxn, L["dgb"]], writes=[dxo])
        if LNSTOP < 6:
            return
        if res_dst is not None:
            P.dma("sp", res_dst, xo[:], reads=[dxo], writes=[dres])
        if LNSTOP < 7:
            return
        if xTs is None:
            return
        P.op("act", lambda e: e.activation(out=xb[:], in_=xo[:], func=AF.Copy), reads=[dxo], writes=[dxb])
        if xb_dst is not None:
            P.dma("sp", xb_dst, xb[:], reads=[dxb], writes=[dxbd])
        if LNSTOP < 8:
            return
        for c in range(8):
            P.op("pe", lambda e, c=c: e.transpose(pT[:, c * 128:(c + 1) * 128], xb[:, c * 128:(c + 1) * 128], self.ident[:]),
                 reads=[dxb, self.dconst], writes=[dpT], signal=(c == 7))
        if LNSTOP < 9:
            return
        P.op("act", lambda e: e.activation(out=xTs[:, :, sub * 128:(sub + 1) * 128],
                                           in_=pT[:].rearrange("p (c t) -> p c t", c=8), func=AF.Copy),
             reads=[dpT], writes=[dxTs])

    def phase_ln_in(self):
        P, T, NB, I = self.P, self.T, self.NB, self.I
        P.barrier()
        with ExitStack() as st:
            L = self.ln_setup(st, I["ln_in_g"], I["ln_in_b"], "in")
            xt = [self.sb(st, "xt", [128, D], F32) for _ in range(2)]
            xTs = [self.sb(st, "xTs", [128, 8, 512], BF16) for _ in range(2)]
            dres = P.dep("d_xres")
            dxT = P.dep("d_xT")
            k = 0
            for src, base, own in self.ln_srcs:
                for b in range(NB):
                    xs, dxs = xTs[b % 2], P.dep("xTs%d" % (b % 2))
                    for sub in range(4):
                        r0 = b * 512 + sub * 128
                        xtt, dx = xt[k % 2], P.dep("xt%d" % (k % 2))
                        k += 1
                        P.dma("sp", xtt[:], src[r0:r0 + 128, :], writes=[dx])
                        self.ln_tile(L, xtt, dx, xs, dxs, sub,
                                     res_dst=self.xres[r0:r0 + 128, :] if own else None, dres=dres)
                    P.dma("sp", self.xT.rearrange("(c p) t -> p c t", p=128)[:, :, base + b * 512: base + (b + 1) * 512],
                          xs[:], reads=[dxs], writes=[dxT])

    def rms_rstd(self, sq, dsq, nchunk, n, pst, dpst, rstd, drstd, sdt, dsdt):
        P = self.P
        for c in range(nchunk):
            P.op("pe", lambda e, c=c: e.matmul(pst[:], lhsT=self.ones[:], rhs=sq[:, c, :], start=(c == 0), stop=(c == nchunk - 1)),
                 reads=[dsq, self.dconst], writes=[dpst], signal=(c == nchunk - 1))
        P.op("act", lambda e: e.activation(out=sdt[:], in_=pst[:], func=AF.Sqrt, bias=self.eps_rms[:], scale=1.0 / n),
             reads=[dpst, self.dconst], writes=[dsdt])
        P.op("dve", lambda e: e.reciprocal(out=rstd[:], in_=sdt[:]), reads=[dsdt], writes=[drstd])

    def phase_a(self, l, xT):
        P, T, NB, I = self.P, self.T, self.NB, self.I
        NBT = (2 * NB) if self.has_other else NB
        P.barrier()
        with ExitStack() as st:
            sb = lambda n, s, d: self.sb(st, n, s, d)
            Win = sb("Win", [128, 8, DIN], BF16)
            dW = P.dep("a_W")
            wv = self.wb[("w_in", l)].rearrange("(c p) n -> p c n", p=128)
            for c in range(8):
                P.dma("sp", Win[:, c, :], wv[:, c, :], reads=[self.dwc["w_in%d" % l]], writes=[dW])
            Wkr = sb("Wkr", [128, 8, 96], BF16)
            Wkrs = sb("Wkrs", [128, 8, 96], BF16)
            dW2 = P.dep("a_W2")
            P.op("dve", lambda e: e.memset(Wkr[:], 0.0), writes=[dW2])
            P.op("dve", lambda e: e.memset(Wkrs[:], 0.0), writes=[dW2])
            P.op("dve", lambda e: e.tensor_copy(Wkr[:, :, 64:96], Win[:, :, 1408:1440]), reads=[dW], writes=[dW2])
            P.op("dve", lambda e: e.tensor_scalar(out=Wkrs[:, :, 64:80], in0=Win[:, :, 1424:1440], scalar1=-1.0, scalar2=None,
                                                   op0=ALU.mult), reads=[dW], writes=[dW2])
            P.op("dve", lambda e: e.tensor_copy(Wkrs[:, :, 80:96], Win[:, :, 1408:1424]), reads=[dW], writes=[dW2])
            gq, dgq = self.load_cols(st, "gq", I["q_norm_g"][l], 384)
            gkv, dgkv = self.load_cols(st, "gkv", I["kv_norm_g"][l], 256)
            wqf = sb("wqf", [128, 3, 768], F32)
            dwqf = P.dep("a_wqf")
            P.dma("sp", wqf[:], I["w_uq"][l].rearrange("(c p) n -> p c n", p=128), writes=[dwqf])
            Wq = sb("Wq", [128, 3, 768], BF16)
            Wqs = sb("Wqs", [128, 3, 768], BF16)
            dWq = P.dep("a_Wq")
            P.op("pool", lambda e: e.memset(Wqs[:], 0.0), writes=[dWq])
            for c in range(3):
                P.op("dve", lambda e, c=c: e.tensor_scalar(out=Wq[:, c, :], in0=wqf[:, c, :], scalar1=gq[:, c:c + 1],
                                                            scalar2=QSCALE, op0=ALU.mult, op1=ALU.mult),
                     reads=[dwqf, dgq], writes=[dWq])
                wq4 = Wq[:, c, :].rearrange("p (h r) -> p h r", h=NH)
                ws4 = Wqs[:, c, :].rearrange("p (h r) -> p h r", h=NH)
                P.op("dve", lambda e, wq4=wq4, ws4=ws4: e.tensor_scalar(out=ws4[:, :, 64:80], in0=wq4[:, :, 80:96], scalar1=-1.0,
                                                                        scalar2=None, op0=ALU.mult), reads=[dWq], writes=[dWq])
                P.op("dve", lambda e, wq4=wq4, ws4=ws4: e.tensor_copy(ws4[:, :, 80:96], wq4[:, :, 64:80]), reads=[dWq], writes=[dWq])
            wkvf = sb("wkvf", [128, 2, 1024], F32)
            dwkvf = P.dep("a_wkvf")
            P.dma("sp", wkvf[:], I["w_ukv"][l].rearrange("(c p) n -> p c n", p=128), writes=[dwkvf])
            Wkn = sb("Wkn", [128, 2, 512], BF16)
            Wv = sb("Wv", [128, 2, 512], BF16)
            dWkv = P.dep("a_Wkv")
            for c in range(2):
                w4 = wkvf[:, c, :].rearrange("p (h r) -> p h r", h=NH)
                P.op("dve", lambda e, c=c, w4=w4: e.tensor_scalar(out=Wkn[:, c, :].rearrange("p (h r) -> p h r", h=NH), in0=w4[:, :, 0:64],
                                                                  scalar1=gkv[:, c:c + 1], scalar2=None, op0=ALU.mult),
                     reads=[dwkvf, dgkv], writes=[dWkv])
                P.op("dve", lambda e, c=c, w4=w4: e.tensor_scalar(out=Wv[:, c, :].rearrange("p (h r) -> p h r", h=NH), in0=w4[:, :, 64:128],
                                                                  scalar1=gkv[:, c:c + 1], scalar2=None, op0=ALU.mult),
                     reads=[dwkvf, dgkv], writes=[dWkv])
            NPS = 8
            ps = [self.psum(st, "aps", [128, 512], F32) for _ in range(NPS)]
            dps = [P.dep("a_ps%d" % i) for i in range(NPS)]

            def nps():
                i = self.ps_rr % NPS
                self.ps_rr += 1
                return ps[i], dps[i]
            xTb = [sb("xTb", [128, 8, 512], BF16) for _ in range(2)]
            NSTG = 6
            stg = [sb("stg", [128, 512], F32) for _ in range(NSTG)]
            self.stg_rr = 0

            def nstg():
                i = self.stg_rr % NSTG
                self.stg_rr += 1
                return stg[i], P.dep("a_stg%d" % i)
            cctmp = [sb("cctmp", [128, 512], F32) for _ in range(2)]
            cq = sb("cq", [128, 3, 512], F32)
            sq = sb("sq", [128, 3, 512], BF16)
            cqn = sb("cqn", [128, 3, 512], BF16)
            ckv = sb("ckv", [128, 2, 512], F32)
            sk = sb("sk", [128, 2, 512], BF16)
            ckvn = sb("ckvn", [128, 2, 512], BF16)
            rstd = sb("rstd", [128, 512], F32)
            sdt = sb("sdt", [128, 512], F32)
            rstd2 = sb("rstd2", [128, 512], F32)
            sdt2 = sb("sdt2", [128, 512], F32)
            ropeC = [sb("ropeC", [128, 512], F32) for _ in range(2)]
            ropeS = [sb("ropeS", [128, 512], F32) for _ in range(2)]
            t1 = [sb("t1", [128, 512], F32) for _ in range(2)]
            t2 = [sb("t2", [128, 512], F32) for _ in range(2)]
            QTs = [sb("QTs", [128, 512], BF16) for _ in range(3)]
            KTs = [sb("KTs", [128, 512], BF16) for _ in range(3)]
            kro = sb("kro", [128, 512], BF16)
            Vs = [sb("Vs", [128, NH, 65], BF16) for _ in range(2)]
            dVs = [P.dep("a_Vs%d" % i) for i in range(2)]
            for i in range(2):
                P.op("pool", lambda e, i=i: e.memset(Vs[i][:], 1.0), writes=[dVs[i]])
            dcq, dsq, dcqn, dckv, dsk, dckvn = (P.dep("a_" + n) for n in ("cq", "sq", "cqn", "ckv", "sk", "ckvn"))
            drstd, dsdt, drstd2, dsdt2, dkro = (P.dep("a_" + n) for n in ("rstd", "sdt", "rstd2", "sdt2", "kro"))
            dQT, dKT, dV, dCB, dPP, dLG, dLX = (P.dep("d_" + n) for n in ("QT", "KT", "V", "CB", "PP", "LG", "LX"))
            xTv = xT.rearrange("(c p) t -> p c t", p=128)
            nq = 0

            def grp(xb, dxb, col0, m, lhs=None, dl=None):
                pt, dpt = nps()
                for c in range(8):
                    if lhs is None:
                        l_ap, dd = Win[:, c, col0:col0 + m], dW
                    else:
                        l_ap, dd = lhs[:, c, :], dl
                    P.op("pe", lambda e, c=c, l_ap=l_ap, pt=pt: e.matmul(pt[0:m, :], lhsT=l_ap, rhs=xb[:, c, :], start=(c == 0), stop=(c == 7)),
                         reads=[dd, dxb], writes=[dpt], signal=(c == 7))
                return pt, dpt

            def store_fm(dst, ddst, row0, col0, pt, dpt, func=AF.Copy):
                s, ds_ = nstg()
                P.op("act", lambda e: e.activation(out=s[:], in_=pt[:], func=func), reads=[dpt], writes=[ds_])
                P.dma("sp", dst[row0:row0 + 128, col0:col0 + 512], s[:], reads=[ds_], writes=[ddst])

            if ASTOP < 1:
                return
            for blk in range(NBT):
                own = blk < NB
                t0 = blk * 512
                xb, dxb = xTb[blk % 2], P.dep("a_xTb%d" % (blk % 2))
                P.dma("sp", xb[:], xTv[:, :, t0:t0 + 512], reads=[P.dep("d_xT")], writes=[dxb])
                rc, rs_, drope = ropeC[blk % 2], ropeS[blk % 2], P.dep("a_rope%d" % (blk % 2))
                P.dma("sp", rc[64:96, :], self.ropeC[:, t0:t0 + 512], writes=[drope])
                P.dma("sp", rs_[64:96, :], self.ropeS[:, t0:t0 + 512], writes=[drope])
                edge = (not own) and (blk == NB or blk == 2 * NB - 1)
                if own or edge:
                    for c in range(2):
                        pcc, dpcc = grp(xb, dxb, 256 + c * 128, 128)
                        pch, dpch = grp(xb, dxb, 512 + c * 128, 128)
                        ct, dct = cctmp[c], P.dep("a_cct%d" % c)
                        P.op("act", lambda e, ct=ct, pcc=pcc: e.activation(out=ct[:], in_=pcc[:], func=AF.Copy), reads=[dpcc], writes=[dct])
                        s, ds_ = nstg()
                        P.op("dve", lambda e, s=s, pch=pch, ct=ct: e.tensor_tensor(out=s[:], in0=pch[:], in1=ct[:], op=ALU.mult),
                             reads=[dpch, dct], writes=[ds_])
                        P.dma("sp", self.PP[c * 128:(c + 1) * 128, t0:t0 + 512], s[:], reads=[ds_], writes=[dPP])
                if ASTOP < 2:
                    continue
                if own:
                    for c in range(2):
                        pt, dpt = grp(xb, dxb, c * 128, 128)
                        store_fm(self.CB, dCB, c * 128, t0, pt, dpt)
                        pt, dpt = grp(xb, dxb, 1440 + c * 128, 128)
                        store_fm(self.LG, dLG, c * 128, t0, pt, dpt, func=AF.Gelu_apprx_tanh)
                    if ASTOP < 3:
                        continue
                    for c in range(3):
                        pt, dpt = grp(xb, dxb, 768 + c * 128, 128)
                        P.op("act", lambda e, c=c, pt=pt: e.activation(out=sq[:, c, :], in_=pt[:], func=AF.Square), reads=[dpt], writes=[dsq])
                        P.op("dve", lambda e, c=c, pt=pt: e.tensor_copy(cq[:, c, :], pt[:]), reads=[dpt], writes=[dcq])
                    pst, dpst = nps()
                    self.rms_rstd(sq, dsq, 3, 384.0, pst, dpst, rstd, drstd, sdt, dsdt)
                    for c in range(3):
                        P.op("dve", lambda e, c=c: e.tensor_tensor(out=cqn[:, c, :], in0=cq[:, c, :], in1=rstd[:], op=ALU.mult),
                             reads=[dcq, drstd], writes=[dcqn])
                    for h in range(NH):
                        pa, dpa = nps()
                        pb, dpb = nps()
                        for c in range(3):
                            P.op("pe", lambda e, c=c, h=h, pa=pa: e.matmul(pa[0:96, :], lhsT=Wq[:, c, h * 96:(h + 1) * 96], rhs=cqn[:, c, :],
                                                                      start=(c == 0), stop=(c == 2)), reads=[dWq, dcqn], writes=[dpa], signal=(c == 2))
                        for c in range(3):
                            P.op("pe", lambda e, c=c, h=h, pb=pb: e.matmul(pb[0:96, :], lhsT=Wqs[:, c, h * 96:(h + 1) * 96], rhs=cqn[:, c, :],
                                                                      start=(c == 0), stop=(c == 2)), reads=[dWq, dcqn], writes=[dpb], signal=(c == 2))
                        qs, dqs = QTs[nq % 3], P.dep("a_QTs%d" % (nq % 3))
                        ta, dta = t1[nq % 2], P.dep("a_t1%d" % (nq % 2))
                        tb, dtb = t2[nq % 2], P.dep("a_t2%d" % (nq % 2))
                        nq += 1
                        P.op("act", lambda e, qs=qs, pa=pa: e.activation(out=qs[0:64, :], in_=pa[0:64, :], func=AF.Copy), reads=[dpa], writes=[dqs])
                        P.op("dve", lambda e, ta=ta, pa=pa: e.tensor_tensor(out=ta[64:96, :], in0=pa[64:96, :], in1=rc[64:96, :], op=ALU.mult),
                             reads=[dpa, drope], writes=[dta])
                        P.op("dve", lambda e, tb=tb, pb=pb: e.tensor_tensor(out=tb[64:96, :], in0=pb[64:96, :], in1=rs_[64:96, :], op=ALU.mult),
                             reads=[dpb, drope], writes=[dtb])
                        P.op("dve", lambda e, qs=qs, ta=ta, tb=tb: e.tensor_tensor(out=qs[64:96, :], in0=ta[64:96, :], in1=tb[64:96, :], op=ALU.add),
                             reads=[dta, dtb], writes=[dqs])
                        P.dma("sp", self.QT[h, :, t0:t0 + 512], qs[0:96, :], reads=[dqs], writes=[dQT])
                if ASTOP < 4:
                    continue
                for c in range(2):
                    pt, dpt = grp(xb, dxb, 1696 + c * 128, 128)
                    store_fm(self.LX, dLX, c * 128, t0, pt, dpt)
                for c in range(2):
                    pt, dpt = grp(xb, dxb, 1152 + c * 128, 128)
                    P.op("act", lambda e, c=c, pt=pt: e.activation(out=sk[:, c, :], in_=pt[:], func=AF.Square), reads=[dpt], writes=[dsk])
                    P.op("dve", lambda e, c=c, pt=pt: e.tensor_copy(ckv[:, c, :], pt[:]), reads=[dpt], writes=[dckv])
                pst, dpst = nps()
                self.rms_rstd(sk, dsk, 2, 256.0, pst, dpst, rstd2, drstd2, sdt2, dsdt2)
                for c in range(2):
                    P.op("dve", lambda e, c=c: e.tensor_tensor(out=ckvn[:, c, :], in0=ckv[:, c, :], in1=rstd2[:], op=ALU.mult),
                         reads=[dckv, drstd2], writes=[dckvn])
                if ASTOP < 5:
                    continue
                pa, dpa = grp(xb, dxb, 0, 96, lhs=Wkr, dl=dW2)
                pb, dpb = grp(xb, dxb, 0, 96, lhs=Wkrs, dl=dW2)
                ta, dta = t1[nq % 2], P.dep("a_t1%d" % (nq % 2))
                tb, dtb = t2[nq % 2], P.dep("a_t2%d" % (nq % 2))
                nq += 1
                P.op("dve", lambda e, ta=ta, pa=pa: e.tensor_tensor(out=ta[64:96, :], in0=pa[64:96, :], in1=rc[64:96, :], op=ALU.mult),
                     reads=[dpa, drope], writes=[dta])
                P.op("dve", lambda e, tb=tb, pb=pb: e.tensor_tensor(out=tb[64:96, :], in0=pb[64:96, :], in1=rs_[64:96, :], op=ALU.mult),
                     reads=[dpb, drope], writes=[dtb])
                P.op("dve", lambda e, ta=ta, tb=tb: e.tensor_tensor(out=kro[64:96, :], in0=ta[64:96, :], in1=tb[64:96, :], op=ALU.add),
                     reads=[dta, dtb], writes=[dkro])
                if ASTOP < 6:
                    continue
                for h in range(NH):
                    pk, dpk = nps()
                    for c in range(2):
                        P.op("pe", lambda e, c=c, h=h, pk=pk: e.matmul(pk[0:64, :], lhsT=Wkn[:, c, h * 64:(h + 1) * 64], rhs=ckvn[:, c, :],
                                                                  start=(c == 0), stop=(c == 1)), reads=[dWkv, dckvn], writes=[dpk], signal=(c == 1))
                    ks, dks = KTs[h % 3], P.dep("a_KTs%d" % (h % 3))
                    P.op("act", lambda e, ks=ks, pk=pk: e.activation(out=ks[0:64, :], in_=pk[0:64, :], func=AF.Copy), reads=[dpk], writes=[dks])
                    P.dma("sp", self.KT[h, 0:64, t0:t0 + 512], ks[0:64, :], reads=[dks], writes=[dKT])
                    P.dma("sp", self.KT[h, 64:96, t0:t0 + 512], kro[64:96, :], reads=[dkro], writes=[dKT])
                if ASTOP < 7:
                    continue
                for sub in range(4):
                    pv, dpv = nps()
                    for c in range(2):
                        P.op("pe", lambda e, c=c, sub=sub, pv=pv: e.matmul(pv[:], lhsT=ckvn[:, c, sub * 128:(sub + 1) * 128], rhs=Wv[:, c, :],
                                                                      start=(c == 0), stop=(c == 1)), reads=[dWkv, dckvn], writes=[dpv], signal=(c == 1))
                    vs, dvs = Vs[sub % 2], dVs[sub % 2]
                    P.op("act", lambda e, vs=vs, pv=pv: e.activation(out=vs[:, :, 0:64], in_=pv[:].rearrange("p (h r) -> p h r", h=NH), func=AF.Copy),
                         reads=[dpv], writes=[dvs])
                    P.dma("sp", self.Vd[t0 + sub * 128:t0 + (sub + 1) * 128, :], vs[:].rearrange("p h r -> p (h r)"), reads=[dvs], writes=[dV])

    def phase_b(self, l):
        P, T, NB, I = self.P, self.T, self.NB, self.I
        P.barrier()
        dY = P.dep("d_Y")
        f0 = self.flags[:, 0:1]
        f1 = self.flags[:, 1:2]
        with ExitStack() as st:
            sb = lambda n, s, d: self.sb(st, n, s, d)
            cw = sb("cw", [128, 2, 3], F32)
            dcw = P.dep("b_cw")
            cv_t = {"pp": sb("pp", [128, T + 2], F32), "cb": sb("cb", [128, T], F32), "acc": sb("acc", [128, T], F32)}
            for k in range(3):
                for c in range(2):
                    P.dma("sp", cw[:, c, k:k + 1], I["conv_w"][l, k, c * 128:(c + 1) * 128].rearrange("(p o) -> p o", o=1), writes=[dcw])
            for c in range(2):
                pp = cv_t["pp"]
                cb = cv_t["cb"]
                acc = cv_t["acc"]
                edge = sb("edge", [128, 2], F32)
                dpp, dcb, dacc = (P.dep("b_%s" % n) for n in ("pp", "cb", "acc"))
                dedge = P.dep("b_edge%d" % c)
                rows = slice(c * 128, (c + 1) * 128)
                P.dma("sp", pp[:, 1:T + 1], self.PP[rows, 0:T], reads=[P.dep("d_PP")], writes=[dpp])
                P.dma("sp", cb[:], self.CB[rows, 0:T], reads=[P.dep("d_CB")], writes=[dcb])
                if self.has_other:
                    P.dma("sp", edge[:, 0:1], self.PP[rows, 2 * T - 1:2 * T], reads=[P.dep("d_PP")], writes=[dedge], allow_slow_non_contiguous=True)
                    P.dma("sp", edge[:, 1:2], self.PP[rows, T:T + 1], reads=[P.dep("d_PP")], writes=[dedge], allow_slow_non_contiguous=True)
                else:
                    P.op("dve", lambda e, edge=edge: e.memset(edge[:], 0.0), writes=[dedge])
                P.op("dve", lambda e, pp=pp, edge=edge: e.tensor_tensor(out=pp[:, 0:1], in0=edge[:, 0:1], in1=f1, op=ALU.mult),
                     reads=[dedge, self.dconst, dpp], writes=[dpp])
                P.op("dve", lambda e, pp=pp, edge=edge: e.tensor_tensor(out=pp[:, T + 1:T + 2], in0=edge[:, 1:2], in1=f0, op=ALU.mult),
                     reads=[dedge, self.dconst, dpp], writes=[dpp])
                P.op("dve", lambda e, acc=acc, pp=pp, c=c: e.tensor_scalar(out=acc[:], in0=pp[:, 0:T], scalar1=cw[:, c, 0:1], scalar2=None, op0=ALU.mult),
                     reads=[dpp, dcw], writes=[dacc])
                for k in (1, 2):
                    P.op("dve", lambda e, acc=acc, pp=pp, c=c, k=k: e.scalar_tensor_tensor(out=acc[:], in0=pp[:, k:k + T], scalar=cw[:, c, k:k + 1], in1=acc[:],
                                                                                     op0=ALU.mult, op1=ALU.add), reads=[dpp, dcw, dacc], writes=[dacc])
                P.op("pool", lambda e, acc=acc, cb=cb: e.tensor_tensor(out=acc[:], in0=acc[:], in1=cb[:], op=ALU.mult), reads=[dacc, dcb], writes=[dacc])
                P.dma("sp", self.Y[rows, 0:T], acc[:], reads=[dacc], writes=[dY])
        P.barrier()
        with ExitStack() as st:
            sb = lambda n, s, d: self.sb(st, n, s, d)
            prm = sb("prm", [128, 2, 16], F32)
            dprm = P.dep("b_prm")

            def col(dst_col, vec):
                for c in range(2):
                    P.dma("sp", prm[:, c, dst_col:dst_col + 1], vec[c * 128:(c + 1) * 128].rearrange("(p o) -> p o", o=1), writes=[dprm])
            for k in range(4):
                col(k, I["lru_conv_w"][l, k])
            col(4, I["lru_conv_b"][l])
            for d_ in range(2):
                col(5 + d_, I["lru_ba"][l, d_])
                col(7 + d_, I["lru_bi"][l, d_])
                col(9 + d_, I["lru_lam"][l, d_])
            sc = sb("sc", [128, 2, 4], F32)
            dsc = P.dep("b_sc")
            for c in range(2):
                P.op("act", lambda e, c=c: e.activation(out=sc[:, c, 0:2], in_=prm[:, c, 9:11], func=AF.Exp, scale=-1.0), reads=[dprm], writes=[dsc])
                P.op("act", lambda e, c=c: e.activation(out=sc[:, c, 0:2], in_=sc[:, c, 0:2], func=AF.Ln, bias=1.0, scale=1.0), reads=[dsc], writes=[dsc])
                P.op("dve", lambda e, c=c: e.tensor_scalar(out=sc[:, c, 2:4], in0=sc[:, c, 0:2], scalar1=-16.0, scalar2=None, op0=ALU.mult), reads=[dsc], writes=[dsc])
                P.op("dve", lambda e, c=c: e.tensor_scalar(out=sc[:, c, 0:2], in0=sc[:, c, 0:2], scalar1=-8.0, scalar2=None, op0=ALU.mult), reads=[dsc], writes=[dsc])
            wgf = sb("wgf", [128, 2, 4, 128], F32)
            wg = sb("wg", [128, 2, 4, 128], BF16)
            dwg = P.dep("b_wg")
            P.op("pool", lambda e: e.memset(wgf[:], 0.0), writes=[dwg])
            for c in range(2):
                for d_ in range(2):
                    for gi, key in enumerate(("lru_wa", "lru_wi")):
                        for bb in range(2):
                            P.dma("sp", wgf[bb * 64:(bb + 1) * 64, c, 2 * d_ + gi, bb * 64:(bb + 1) * 64], I[key][l, d_, 2 * c + bb], writes=[dwg])
            P.op("dve", lambda e: e.tensor_copy(wg[:], wgf[:]), reads=[dwg], writes=[dwg])
            ps = [self.psum(st, "bps", [128, 512], F32) for _ in range(4)]
            dps = [P.dep("b_ps%d" % i) for i in range(4)]
            HO = self.has_other
            lxo = sb("lxo", [128, T + 3], F32)
            lxt = sb("lxt", [128, T + 3], F32) if HO else None
            big = {}
            for nm in (("o", "t") if HO else ("o",)):
                big["xc" + nm] = sb("xc" + nm, [128, T], F32)
            xcbb = [sb("xcbb", [128, 512], BF16) for _ in range(2)]
            a_ = sb("a_", [128, T], F32)
            u_ = sb("u_", [128, T], F32)
            hsum = sb("hsum", [128, T], F32)
            hb = sb("hb", [128, T], F32) if HO else lxo[:, 0:T]
            carry = sb("carry", [128, 2], F32)
            tmp = [sb("tmp", [128, 512], F32) for _ in range(2)]
            tmpi = [sb("tmpi", [128, 512], F32) for _ in range(2)]
            for c in range(2):
                rows = slice(c * 128, (c + 1) * 128)
                dlx = P.dep("b_lx")
                P.dma("sp", lxo[:, 1:T + 1], self.LX[rows, 0:T], reads=[P.dep("d_LX")], writes=[dlx])
                if HO:
                    P.dma("sp", lxt[:, 1:T + 1], self.LX[rows, T:2 * T], reads=[P.dep("d_LX")], writes=[dlx])
                    P.op("dve", lambda e, lxo=lxo, lxt=lxt: e.tensor_scalar(out=lxo[:, 0:1], in0=lxt[:, T:T + 1], scalar1=f1, scalar2=None, op0=ALU.mult), reads=[dlx, self.dconst], writes=[dlx])
                    P.op("dve", lambda e, lxo=lxo, lxt=lxt: e.tensor_scalar(out=lxo[:, T + 1:T + 3], in0=lxt[:, 1:3], scalar1=f0, scalar2=None, op0=ALU.mult), reads=[dlx, self.dconst], writes=[dlx])
                    P.op("dve", lambda e, lxo=lxo, lxt=lxt: e.tensor_scalar(out=lxt[:, 0:1], in0=lxo[:, T:T + 1], scalar1=f0, scalar2=None, op0=ALU.mult), reads=[dlx, self.dconst], writes=[dlx])
                    P.op("dve", lambda e, lxo=lxo, lxt=lxt: e.tensor_scalar(out=lxt[:, T + 1:T + 3], in0=lxo[:, 1:3], scalar1=f1, scalar2=None, op0=ALU.mult), reads=[dlx, self.dconst], writes=[dlx])
                else:
                    P.op("dve", lambda e, lxo=lxo: e.memset(lxo[:, 0:1], 0.0), reads=[dlx], writes=[dlx])
                    P.op("dve", lambda e, lxo=lxo: e.memset(lxo[:, T + 1:T + 3], 0.0), reads=[dlx], writes=[dlx])
                xc = {}
                xcb = {}
                dxc = P.dep("b_xc")
                for nm, src in ((("o", lxo), ("t", lxt)) if HO else (("o", lxo),)):
                    x_ = big["xc" + nm]
                    P.op("dve", lambda e, x_=x_, src=src: e.tensor_scalar(out=x_[:], in0=src[:, 0:T], scalar1=prm[:, c, 0:1], scalar2=prm[:, c, 4:5],
                                                                       op0=ALU.mult, op1=ALU.add), reads=[dlx, dprm], writes=[dxc])
                    for k in (1, 2, 3):
                        P.op("dve", lambda e, x_=x_, src=src, k=k: e.scalar_tensor_tensor(out=x_[:], in0=src[:, k:k + T], scalar=prm[:, c, k:k + 1], in1=x_[:],
                                                                                       op0=ALU.mult, op1=ALU.add), reads=[dlx, dprm, dxc], writes=[dxc])
                    xc[nm] = x_
                da, du, dh, dhb, dcar = (P.dep("b_%s" % n) for n in ("a", "u", "h", "hb", "car"))

                def gates(nm, d_):
                    for b in range(T // 512):
                        cs = slice(b * 512, (b + 1) * 512)
                        pa, dpa = ps[(2 * b) % 4], dps[(2 * b) % 4]
                        pi, dpi = ps[(2 * b + 1) % 4], dps[(2 * b + 1) % 4]
                        xq, dxq = xcbb[b % 2], P.dep("b_xcbb%d" % (b % 2))
                        P.op("pool", lambda e, xq=xq, cs=cs: e.tensor_copy(xq[:], xc[nm][:, cs]), reads=[dxc], writes=[dxq])
                        P.op("pe", lambda e, pa=pa, xq=xq: e.matmul(pa[:], lhsT=wg[:, c, 2 * d_, :], rhs=xq[:], start=True, stop=True),
                             reads=[dwg, dxq], writes=[dpa])
                        P.op("pe", lambda e, pi=pi, xq=xq: e.matmul(pi[:], lhsT=wg[:, c, 2 * d_ + 1, :], rhs=xq[:], start=True, stop=True),
                             reads=[dwg, dxq], writes=[dpi])
                        tr, dtr = tmp[b % 2], P.dep("b_tmp%d" % (b % 2))
                        ti, dti = tmpi[b % 2], P.dep("b_tmpi%d" % (b % 2))
                        P.op("act", lambda e, tr=tr, pa=pa: e.activation(out=tr[:], in_=pa[:], func=AF.Sigmoid, bias=prm[:, c, 5 + d_:6 + d_], scale=1.0),
                             reads=[dpa, dprm], writes=[dtr])
                        P.op("act", lambda e, ti=ti, pi=pi: e.activation(out=ti[:], in_=pi[:], func=AF.Sigmoid, bias=prm[:, c, 7 + d_:8 + d_], scale=1.0),
                             reads=[dpi, dprm], writes=[dti])
                        P.op("act", lambda e, tr=tr, cs=cs: e.activation(out=a_[:, cs], in_=tr[:], func=AF.Exp, scale=sc[:, c, d_:d_ + 1]),
                             reads=[dtr, dsc], writes=[da])
                        P.op("act", lambda e, tr=tr: e.activation(out=tr[:], in_=tr[:], func=AF.Exp, scale=sc[:, c, 2 + d_:3 + d_]),
                             reads=[dtr, dsc], writes=[dtr])
                        P.op("dve", lambda e, tr=tr: e.tensor_scalar(out=tr[:], in0=tr[:], scalar1=-1.0, scalar2=1.0, op0=ALU.mult, op1=ALU.add),
                             reads=[dtr], writes=[dtr])
                        P.op("act", lambda e, tr=tr: e.activation(out=tr[:], in_=tr[:], func=AF.Sqrt), reads=[dtr], writes=[dtr])
                        P.op("dve", lambda e, ti=ti, cs=cs: e.tensor_tensor(out=ti[:], in0=ti[:], in1=xc[nm][:, cs], op=ALU.mult), reads=[dti, dxc], writes=[dti])
                        P.op("dve", lambda e, tr=tr, ti=ti, cs=cs: e.tensor_tensor(out=u_[:, cs], in0=tr[:], in1=ti[:], op=ALU.mult), reads=[dtr, dti], writes=[du])
                if HO:
                    gates("t", 0)
                    P.op("dve", lambda e: e.tensor_tensor_scan(out=hb[:], data0=a_[:], data1=u_[:], initial=0.0, op0=ALU.mult, op1=ALU.add),
                         reads=[da, du], writes=[dhb])
                    P.op("dve", lambda e: e.tensor_scalar(out=carry[:, 0:1], in0=hb[:, T - 1:T], scalar1=f1, scalar2=None, op0=ALU.mult),
                         reads=[dhb, self.dconst], writes=[dcar])
                    gates("t", 1)
                    P.op("dve", lambda e: e.tensor_tensor_scan(out=hb[:, ::-1], data0=a_[:, ::-1], data1=u_[:, ::-1], initial=0.0, op0=ALU.mult, op1=ALU.add),
                         reads=[da, du], writes=[dhb])
                    P.op("dve", lambda e: e.tensor_scalar(out=carry[:, 1:2], in0=hb[:, 0:1], scalar1=f0, scalar2=None, op0=ALU.mult),
                         reads=[dhb, self.dconst], writes=[dcar])
                else:
                    P.op("dve", lambda e: e.memset(carry[:], 0.0), writes=[dcar])
                gates("o", 0)
                P.op("dve", lambda e: e.tensor_tensor_scan(out=hsum[:], data0=a_[:], data1=u_[:], initial=carry[:, 0:1], op0=ALU.mult, op1=ALU.add),
                     reads=[da, du, dcar], writes=[dh])
                gates("o", 1)
                P.op("dve", lambda e: e.tensor_tensor_scan(out=hb[:, ::-1], data0=a_[:, ::-1], data1=u_[:, ::-1], initial=carry[:, 1:2], op0=ALU.mult, op1=ALU.add),
                     reads=[da, du, dcar], writes=[dhb])
                P.op("pool", lambda e: e.tensor_tensor(out=hsum[:], in0=hsum[:], in1=hb[:], op=ALU.add), reads=[dh, dhb], writes=[dh])
                lgt = a_
                P.dma("sp", lgt[:], self.LG[rows, 0:T], reads=[P.dep("d_LG"), da], writes=[da])
                P.op("pool", lambda e: e.tensor_tensor(out=hsum[:], in0=hsum[:], in1=lgt[:], op=ALU.mult), reads=[dh, da], writes=[dh])
                P.dma("sp", self.Y[768 + c * 128:768 + (c + 1) * 128, 0:T], hsum[:], reads=[dh], writes=[dY])
                P.barrier()

    def phase_c(self, l):
        P, T, NB = self.P, self.T, self.NB
        S2 = (2 * T) if self.has_other else T
        NKT = S2 // 128
        NQB = NB
        P.barrier()
        dY = P.dep("d_Y")
        with ExitStack() as st:
            sb = lambda n, s, d: self.sb(st, n, s, d)
            Vall = sb("Vall", [128, NKT, NH * 65], BF16)
            dVall = P.dep("c_Vall")
            vv = self.Vd[0:S2, :].rearrange("(k p) n -> p k n", p=128)
            for k0 in range(0, NKT, 8):
                P.dma("sp", Vall[:, k0:k0 + 8, :], vv[:, k0:k0 + 8, :], reads=[P.dep("d_V")], writes=[dVall])
            sel = sb("sel", [128, 64], F32)
            dsel = P.dep("c_sel")
            P.op("dve", lambda e: e.memset(sel[:], 0.0), writes=[dsel])
            P.op("dve", lambda e: e.memset(sel[64:65, :], 1.0), reads=[dsel], writes=[dsel])
            KTh = [sb("KTh", [96, S2], BF16) for _ in range(2)]
            QTh = [sb("QTh", [96, T], BF16) for _ in range(2)]
            pS = [self.psum(st, "pS", [128, 1536], F32) for _ in range(2)]
            dpS = [P.dep("c_pS%d" % i) for i in range(2)]
            pO = [self.psum(st, "pO", [128, 512], F32) for _ in range(2)]
            dpO = [P.dep("c_pO%d" % i) for i in range(2)]
            PT = [sb("PT", [128, 1536], BF16) for _ in range(3)]
            dPT = [P.dep("c_PT%d" % i) for i in range(3)]
            Osb = [sb("Osb", [128, 512], F32) for _ in range(2)]
            rec = [sb("rec", [64, 512], F32) for _ in range(2)]
            yo = [sb("yo", [64, 512], F32) for _ in range(2)]
            it = 0
            nqb = 0
            for h in range(NH):
                kt_, dkt = KTh[h % 2], P.dep("c_KTh%d" % (h % 2))
                qt_, dqt = QTh[h % 2], P.dep("c_QTh%d" % (h % 2))
                P.dma("sp", kt_[:], self.KT[h, :, 0:S2], reads=[P.dep("d_KT")], writes=[dkt])
                P.dma("sp", qt_[:], self.QT[h, :, 0:T], reads=[P.dep("d_QT")], writes=[dqt])
                for qb in range(NQB):
                    qs = slice(qb * 512, (qb + 1) * 512)
                    po, dpo = pO[nqb % 2], dpO[nqb % 2]
                    GS = 3
                    groups = [list(range(k0, min(k0 + GS, NKT))) for k0 in range(0, NKT, GS)]
                    NKG = len(groups)

                    def scores(kg, it_):
                        ps_, dps_ = pS[it_ % 2], dpS[it_ % 2]
                        ks = groups[kg]
                        for j, k in enumerate(ks):
                            P.op("pe", lambda e, k=k, j=j, ps_=ps_: e.matmul(ps_[:, j * 512:(j + 1) * 512], lhsT=kt_[:, k * 128:(k + 1) * 128], rhs=qt_[:, qs],
                                                                        start=True, stop=True), reads=[dkt, dqt], writes=[dps_], signal=(j == len(ks) - 1))

                    def expo(kg, it_):
                        ps_, dps_ = pS[it_ % 2], dpS[it_ % 2]
                        pt_, dpt_ = PT[it_ % 3], dPT[it_ % 3]
                        w = len(groups[kg]) * 512
                        P.op("act", lambda e, ps_=ps_, pt_=pt_, w=w: e.activation(out=pt_[:, 0:w], in_=ps_[:, 0:w], func=AF.Exp), reads=[dps_], writes=[dpt_])

                    def pv(kg, it_):
                        pt_, dpt_ = PT[it_ % 3], dPT[it_ % 3]
                        ks = groups[kg]
                        for j, k in enumerate(ks):
                            P.op("pe", lambda e, k=k, j=j, pt_=pt_, po=po: e.matmul(po[0:65, :], lhsT=Vall[:, k, h * 65:(h + 1) * 65], rhs=pt_[:, j * 512:(j + 1) * 512],
                                                                               start=(k == 0), stop=(k == NKT - 1)), reads=[dVall, dpt_], writes=[dpo],
                                 signal=(j == len(ks) - 1))
                    scores(0, it)
                    for kg in range(NKG):
                        expo(kg, it + kg)
                        if kg + 1 < NKG:
                            scores(kg + 1, it + kg + 1)
                        pv(kg, it + kg)
                    it += NKG
                    pD, dpD = pS[it % 2], dpS[it % 2]
                    ob, dob = Osb[nqb % 2], P.dep("c_Osb%d" % (nqb % 2))
                    rc_, drc = rec[nqb % 2], P.dep("c_rec%d" % (nqb % 2))
                    y_, dy_ = yo[nqb % 2], P.dep("c_yo%d" % (nqb % 2))
                    nqb += 1
                    P.op("dve", lambda e, ob=ob, po=po: e.tensor_copy(ob[0:65, :], po[0:65, :]), reads=[dpo], writes=[dob])
                    P.op("pe", lambda e, ob=ob: e.matmul(pD[0:64, 0:512], lhsT=sel[0:65, :], rhs=ob[0:65, :], start=True, stop=True),
                         reads=[dsel, dob], writes=[dpD])
                    P.op("dve", lambda e, rc_=rc_: e.reciprocal(out=rc_[:], in_=pD[0:64, 0:512]), reads=[dpD], writes=[drc])
                    P.op("dve", lambda e, y_=y_, ob=ob, rc_=rc_: e.tensor_tensor(out=y_[:], in0=ob[0:64, :], in1=rc_[:], op=ALU.mult),
                         reads=[dob, drc], writes=[dy_])
                    P.dma("sp", self.Y[256 + h * 64:256 + (h + 1) * 64, qs], y_[:], reads=[dy_], writes=[dY])

    def phase_d(self, l, xres):
        P, T, NB, I = self.P, self.T, self.NB, self.I
        P.barrier()
        with ExitStack() as st:
            sb = lambda n, s, d: self.sb(st, n, s, d)
            Wo = sb("Wo", [128, 8, D], BF16)
            dWo = P.dep("d_Wo")
            wv = self.wb[("w_out", l)].rearrange("(c p) n -> p c n", p=128)
            for c in range(8):
                P.dma("sp", Wo[:, c, :], wv[:, c, :], reads=[self.dwc["w_out%d" % l]], writes=[dWo])
            gm, dgm = self.load_cols(st, "gm", I["mix_norm_g"][l], D)
            L = self.ln_setup(st, I["ln1_g"][l], I["ln1_b"][l], "1", nbuf=2)
            Yb = [sb("Yb", [128, 8, 512], F32) for _ in range(2)]
            sq = sb("sq", [128, 8, 512], BF16)
            yn = sb("yn", [128, 8, 512], BF16)
            dsq, dyn = P.dep("dd_sq"), P.dep("dd_yn")
            rstd = [sb("rstd", [128, 512], F32) for _ in range(3)]
            sdt = [sb("sdt", [128, 512], F32) for _ in range(3)]
            pst = [self.psum(st, "dpst", [128, 512], F32) for _ in range(2)]
            pso = [self.psum(st, "dpso", [128, 512], F32) for _ in range(3)]
            xr = [sb("xr", [128, D], F32) for _ in range(2)]
            pre = [sb("pre", [128, D], F32) for _ in range(2)]
            xTs = [sb("xTs", [128, 8, 512], BF16) for _ in range(2)]
            Yv = self.Y.rearrange("(c p) t -> p c t", p=128)
            groups = ((0, 2, 256.0), (2, 6, 512.0), (6, 8, 256.0))
            dres = P.dep("d_x1res")
            dx1T = P.dep("d_x1T")
            npo = 0
            ntile = 0
            moe = (l == 1)
            if moe:
                wr = sb("wr", [128, 8, NE], F32)
                wrb = sb("wrb", [128, 8, NE], BF16)
                dwr = P.dep("dd_wr")
                P.dma("sp", wr[:], I["moe_w_router"][0].rearrange("(c p) n -> p c n", p=128), writes=[dwr])
                P.op("dve", lambda e: e.tensor_copy(wrb[:], wr[:]), reads=[dwr], writes=[dwr])
                prr_full = self.psum(st, "prr", [128, 512], F32)
                prr = prr_full[:, 0:NE]
                dprr = P.dep("dd_prr")
                lg_ = [sb("lg_", [128, NE], F32) for _ in range(2)]
                m8 = [sb("m8", [128, 8], F32) for _ in range(2)]
                msk = [sb("msk", [128, NE], F32) for _ in range(2)]
                ex = [sb("ex", [128, NE], F32) for _ in range(2)]
                den = [sb("den", [128, 2], F32) for _ in range(2)]
                dcomb = P.dep("d_comb")
                rtmp = [sb("rtmp", [128, 2 * NE], F32) for _ in range(2)]
            for b in range(NB):
                ts_ = slice(b * 512, (b + 1) * 512)
                yb, dyb = Yb[b % 2], P.dep("dd_Yb%d" % (b % 2))
                P.dma("sp", yb[:], Yv[:, :, ts_], reads=[P.dep("d_Y")], writes=[dyb])
                for c in range(8):
                    P.op("act", lambda e, c=c, yb=yb: e.activation(out=sq[:, c, :], in_=yb[:, c, :], func=AF.Square), reads=[dyb], writes=[dsq])
                for gi, (c0, c1, n) in enumerate(groups):
                    p_, dp_ = pst[gi % 2], P.dep("dd_pst%d" % (gi % 2))
                    for c in range(c0, c1):
                        P.op("pe", lambda e, c=c, p_=p_, c0=c0, c1=c1: e.matmul(p_[:], lhsT=self.ones[:], rhs=sq[:, c, :], start=(c == c0), stop=(c == c1 - 1)),
                             reads=[dsq, self.dconst], writes=[dp_], signal=(c == c1 - 1))
                    dsd, drs = P.dep("dd_sdt%d" % gi), P.dep("dd_rstd%d" % gi)
                    P.op("act", lambda e, gi=gi, p_=p_, n=n: e.activation(out=sdt[gi][:], in_=p_[:], func=AF.Sqrt, bias=self.eps_rms[:], scale=1.0 / n),
                         reads=[dp_, self.dconst], writes=[dsd])
                    P.op("dve", lambda e, gi=gi: e.reciprocal(out=rstd[gi][:], in_=sdt[gi][:]), reads=[dsd], writes=[drs])
                    for c in range(c0, c1):
                        P.op("dve", lambda e, c=c, gi=gi, yb=yb: e.scalar_tensor_tensor(out=yn[:, c, :], in0=yb[:, c, :], scalar=gm[:, c:c + 1], in1=rstd[gi][:],
                                                                                     op0=ALU.mult, op1=ALU.mult), reads=[dyb, dgm, drs], writes=[dyn])
                xs, dxs = xTs[b % 2], P.dep("dd_xTs%d" % (b % 2))
                for sub in range(4):
                    r0 = b * 512 + sub * 128
                    xrt, dxr = xr[ntile % 2], P.dep("dd_xr%d" % (ntile % 2))
                    pr, dpr = pre[ntile % 2], P.dep("dd_pre%d" % (ntile % 2))
                    ntile += 1
                    P.dma("sp", xrt[:], xres[r0:r0 + 128, :], reads=[P.dep("d_xres")], writes=[dxr])
                    for hf in range(2):
                        po, dpo = pso[npo % 3], P.dep("dd_pso%d" % (npo % 3))
                        npo += 1
                        for c in range(8):
                            P.op("pe", lambda e, c=c, hf=hf, po=po, sub=sub: e.matmul(po[:], lhsT=yn[:, c, sub * 128:(sub + 1) * 128], rhs=Wo[:, c, hf * 512:(hf + 1) * 512],
                                                                                 start=(c == 0), stop=(c == 7)), reads=[dyn, dWo], writes=[dpo], signal=(c == 7))
                        P.op("dve", lambda e, hf=hf, po=po, pr=pr, xrt=xrt: e.scalar_tensor_tensor(out=pr[:, hf * 512:(hf + 1) * 512], in0=xrt[:, hf * 512:(hf + 1) * 512],
                                                                                                scalar=ALPHA, in1=po[:], op0=ALU.mult, op1=ALU.add),
                             reads=[dxr, dpo], writes=[dpr])
                    self.ln_tile(L, pr, dpr, xs, dxs, sub, res_dst=self.x1res[r0:r0 + 128, :], dres=dres,
                                 xb_dst=(self.X1B[r0:r0 + 128, :] if moe else None), dxbd=P.dep("d_X1B"))
                    if moe:
                        i2 = ntile % 2
                        for c in range(8):
                            P.op("pe", lambda e, c=c, sub=sub, xs=xs: e.matmul(prr, lhsT=xs[:, c, sub * 128:(sub + 1) * 128], rhs=wrb[:, c, :], start=(c == 0), stop=(c == 7)),
                                 reads=[dxs, dwr], writes=[dprr], signal=(c == 7))
                        dl_, dm8, dmk, dex, dden = (P.dep("dd_%s%d" % (n, i2)) for n in ("lg", "m8", "msk", "ex", "den"))
                        P.op("dve", lambda e, i2=i2: e.tensor_copy(lg_[i2][:], prr), reads=[dprr], writes=[dl_])
                        P.op("dve", lambda e, i2=i2: e.max(out=m8[i2][:], in_=lg_[i2][:]), reads=[dl_], writes=[dm8])
                        P.op("dve", lambda e, i2=i2: e.tensor_scalar(out=msk[i2][:], in0=lg_[i2][:], scalar1=m8[i2][:, 1:2], scalar2=None, op0=ALU.is_ge),
                             reads=[dl_, dm8], writes=[dmk])
                        P.op("dve", lambda e, i2=i2: e.tensor_scalar(out=den[i2][:, 0:1], in0=m8[i2][:, 0:1], scalar1=-1.0, scalar2=None, op0=ALU.mult),
                             reads=[dm8], writes=[dden])
                        P.op("act", lambda e, i2=i2: e.activation(out=ex[i2][:], in_=lg_[i2][:], func=AF.Exp, bias=den[i2][:, 0:1], scale=1.0),
                             reads=[dl_, dden], writes=[dex])
                        P.op("dve", lambda e, i2=i2: e.tensor_tensor(out=ex[i2][:], in0=ex[i2][:], in1=msk[i2][:], op=ALU.mult), reads=[dex, dmk], writes=[dex])
                        P.op("dve", lambda e, i2=i2: e.reduce_sum(out=den[i2][:, 1:2], in_=ex[i2][:], axis=mybir.AxisListType.X), reads=[dex, dden], writes=[dden])
                        P.op("dve", lambda e, i2=i2: e.reciprocal(out=den[i2][:, 1:2], in_=den[i2][:, 1:2]), reads=[dden], writes=[dden])
                        P.op("dve", lambda e, i2=i2: e.tensor_scalar(out=ex[i2][:], in0=ex[i2][:], scalar1=den[i2][:, 1:2], scalar2=None, op0=ALU.mult),
                             reads=[dex, dden], writes=[dex])
                        R = self.R
                        ti_ = r0 // 128
                        drt = P.dep("r_tab")
                        doh = P.dep("dd_oh%d" % i2)
                        oh1, oh2, tmp8 = R["oh1"][:, ti_, :], R["oh2"][:, ti_, :], rtmp[i2]
                        P.op("dve", lambda e, i2=i2, oh1=oh1: e.tensor_scalar(out=oh1, in0=lg_[i2][:], scalar1=m8[i2][:, 0:1], scalar2=None, op0=ALU.is_equal),
                             reads=[dl_, dm8], writes=[drt])
                        P.op("dve", lambda e, i2=i2, oh1=oh1, oh2=oh2: e.tensor_tensor(out=oh2, in0=msk[i2][:], in1=oh1, op=ALU.subtract), reads=[dmk, drt], writes=[drt])
                        pw, dpw = prr_full[:, 16:32], dprr
                        P.op("pe", lambda e, i2=i2: e.matmul(pw[:, 0:NE], lhsT=R["U"][:], rhs=msk[i2][:], start=True, stop=True), reads=[dmk, P.dep("r_const")], writes=[dpw])
                        P.op("pe", lambda e, i2=i2: e.matmul(pw[:, NE:2 * NE], lhsT=R["onesf"][:], rhs=msk[i2][:], start=True, stop=True), reads=[dmk, P.dep("r_const")], writes=[dpw])
                        drun = P.dep("r_run")
                        P.op("dve", lambda e, tmp8=tmp8: e.tensor_tensor(out=tmp8[:, 0:NE], in0=pw[:, 0:NE], in1=R["run"][:], op=ALU.add), reads=[dpw, drun], writes=[doh])
                        P.op("dve", lambda e: e.tensor_tensor(out=R["run"][:], in0=pw[:, NE:2 * NE], in1=R["run"][:], op=ALU.add), reads=[dpw, drun], writes=[drun])
                        for kk, oh in ((0, oh1), (1, oh2)):
                            P.op("dve", lambda e, tmp8=tmp8, oh=oh: e.tensor_tensor(out=tmp8[:, NE:2 * NE], in0=tmp8[:, 0:NE], in1=oh, op=ALU.mult), reads=[doh, drt], writes=[doh])
                            P.op("dve", lambda e, tmp8=tmp8, kk=kk, ti_=ti_: e.reduce_sum(out=R["r12"][:, ti_, kk:kk + 1], in_=tmp8[:, NE:2 * NE], axis=mybir.AxisListType.X),
                                 reads=[doh], writes=[drt])
                            P.op("dve", lambda e, tmp8=tmp8, oh=oh, i2=i2: e.tensor_tensor(out=tmp8[:, NE:2 * NE], in0=ex[i2][:], in1=oh, op=ALU.mult), reads=[dex, drt, doh], writes=[doh])
                            P.op("dve", lambda e, tmp8=tmp8, kk=kk, ti_=ti_: e.reduce_sum(out=R["g12"][:, ti_, kk:kk + 1], in_=tmp8[:, NE:2 * NE], axis=mybir.AxisListType.X),
                                 reads=[doh], writes=[drt])
                P.dma("sp", self.x1T.rearrange("(c p) t -> p c t", p=128)[:, :, ts_], xs[:], reads=[dxs], writes=[dx1T])

    def route_setup(self, st):
        P, T = self.P, self.T
        NT = T // 128
        self.NTILE = (2 * T + NE * 511) // 512
        P.barrier()
        R = {}
        sb = lambda n, s, d: self.sb(st, n, s, d)
        R["U"] = sb("rU", [128, 128], F32)
        R["onesf"] = sb("ronesf", [128, 128], F32)
        R["run"] = sb("rrun", [128, NE], F32)
        R["oh1"] = sb("roh1", [128, NT, NE], F32)
        R["oh2"] = sb("roh2", [128, NT, NE], F32)
        R["r12"] = sb("rr12", [128, NT, 2], F32)
        R["g12"] = sb("rg12", [128, NT, 2], F32)
        R["slot"] = sb("rslot", [128, NT, 2], F32)
        R["sloti"] = sb("rsloti", [128, NT, 2], mybir.dt.int32)
        R["pidx"] = sb("rpidx", [128, 1], F32)
        R["pidxi"] = sb("rpidxi", [128, 1], mybir.dt.int32)
        R["ej"] = sb("rej", [128, self.NTILE], F32)
        R["idxg"] = sb("ridxg", [128, self.NTILE, 7], F32)
        R["idxgi"] = sb("ridxgi", [128, self.NTILE, 7], mybir.dt.int32)
        R["idxd"] = sb("ridxd", [128, self.NTILE, 4], F32)
        R["idxdi"] = sb("ridxdi", [128, self.NTILE, 4], mybir.dt.int32)
        dc = P.dep("r_const")
        P.op("pool", lambda e: e.memset(R["onesf"][:], 1.0), writes=[dc])
        P.op("pool", lambda e: e.memset(R["U"][:], 1.0), writes=[dc])
        P.op("pool", lambda e: e.affine_select(out=R["U"][:], in_=R["U"][:], pattern=[[1, 128]], compare_op=ALU.is_gt, fill=0.0,
                                               base=0, channel_multiplier=-1), reads=[dc], writes=[dc])
        P.op("pool", lambda e: e.iota(R["pidxi"][:], pattern=[[0, 1]], base=0, channel_multiplier=1), writes=[dc])
        P.op("pool", lambda e: e.tensor_copy(R["pidx"][:], R["pidxi"][:]), reads=[dc], writes=[dc])
        P.op("dve", lambda e: e.memset(R["run"][:], 0.0), writes=[P.dep("r_run")])
        self.R = R

    def phase_r(self):
        P, T = self.P, self.T
        R = self.R
        NT = T // 128
        NTILE = self.NTILE
        P.barrier()
        with ExitStack() as st:
            sb = lambda n, s, d: self.sb(st, n, s, d)
            t8 = sb("t8", [128, 6, NE], F32)
            d8 = P.dep("r_t8")
            drt = P.dep("r_tab")
            P.op("dve", lambda e: e.memset(t8[:, 5, :], 1.0), writes=[d8])
            t8i = sb("t8i", [128, NE], mybir.dt.int32)
            P.op("dve", lambda e: e.tensor_scalar(out=t8[:, 0, :], in0=R["run"][:], scalar1=1.0 / 512.0, scalar2=511.0 / 512.0 - 0.499, op0=ALU.mult, op1=ALU.add),
                 reads=[P.dep("r_run"), d8], writes=[d8])
            P.op("dve", lambda e: e.tensor_copy(t8i[:], t8[:, 0, :]), reads=[d8], writes=[d8])
            P.op("dve", lambda e: e.tensor_copy(t8[:, 1, :], t8i[:]), reads=[d8], writes=[d8])
            P.op("dve", lambda e: e.tensor_scalar(out=t8[:, 2, :], in0=t8[:, 1, :], scalar1=512.0, scalar2=None, op0=ALU.mult), reads=[d8], writes=[d8])
            P.op("dve", lambda e: e.tensor_tensor_scan(out=t8[:, 3, :], data0=t8[:, 5, :], data1=t8[:, 2, :], initial=0.0, op0=ALU.mult, op1=ALU.add), reads=[d8], writes=[d8])
            P.op("dve", lambda e: e.tensor_tensor(out=t8[:, 4, :], in0=t8[:, 3, :], in1=t8[:, 2, :], op=ALU.subtract), reads=[d8], writes=[d8])
            tmp = sb("rtmp2", [128, NE], F32)
            dtmp = P.dep("r_tmp2")
            for i in range(NT):
                for kk, oh in ((0, R["oh1"]), (1, R["oh2"])):
                    P.op("dve", lambda e, i=i, oh=oh: e.tensor_tensor(out=tmp[:], in0=oh[:, i, :], in1=t8[:, 4, :], op=ALU.mult), reads=[drt, d8, dtmp], writes=[dtmp])
                    P.op("dve", lambda e, i=i, kk=kk: e.reduce_sum(out=R["slot"][:, i, kk:kk + 1], in_=tmp[:], axis=mybir.AxisListType.X), reads=[dtmp], writes=[drt])
            P.op("dve", lambda e: e.tensor_tensor(out=R["slot"][:], in0=R["slot"][:], in1=R["r12"][:], op=ALU.add), reads=[drt], writes=[drt])
            P.op("dve", lambda e: e.tensor_copy(R["sloti"][:], R["slot"][:]), reads=[drt], writes=[drt])
            for j in range(NTILE):
                P.op("dve", lambda e, j=j: e.tensor_scalar(out=tmp[:], in0=t8[:, 3, :], scalar1=float(512 * j), scalar2=None, op0=ALU.is_le), reads=[d8, dtmp], writes=[dtmp])
                P.op("dve", lambda e, j=j: e.reduce_sum(out=R["ej"][:, j:j + 1], in_=tmp[:], axis=mybir.AxisListType.X), reads=[dtmp], writes=[drt])
            P.op("dve", lambda e: e.tensor_scalar(out=R["ej"][:], in0=R["ej"][:], scalar1=float(NE - 1), scalar2=None, op0=ALU.min), reads=[drt], writes=[drt])
            dc = P.dep("r_const")
            for g in range(7):
                P.op("dve", lambda e, g=g: e.tensor_scalar(out=R["idxg"][:, :, g], in0=R["ej"][:], scalar1=896.0, scalar2=float(g * 128), op0=ALU.mult, op1=ALU.add),
                     reads=[drt], writes=[drt])
            for q in range(4):
                P.op("dve", lambda e, q=q: e.tensor_scalar(out=R["idxd"][:, :, q], in0=R["ej"][:], scalar1=512.0, scalar2=float(q * 128), op0=ALU.mult, op1=ALU.add),
                     reads=[drt], writes=[drt])
            P.op("dve", lambda e: e.tensor_scalar(out=R["idxg"][:], in0=R["idxg"][:], scalar1=R["pidx"][:, 0:1], scalar2=None, op0=ALU.add), reads=[drt, dc], writes=[drt])
            P.op("dve", lambda e: e.tensor_scalar(out=R["idxd"][:], in0=R["idxd"][:], scalar1=R["pidx"][:, 0:1], scalar2=None, op0=ALU.add), reads=[drt, dc], writes=[drt])
            P.op("dve", lambda e: e.tensor_copy(R["idxgi"][:], R["idxg"][:]), reads=[drt], writes=[drt])
            P.op("dve", lambda e: e.tensor_copy(R["idxdi"][:], R["idxd"][:]), reads=[drt], writes=[drt])
            import os
            if os.environ.get("MK_DBGR"):
                dd = P.dep("dbgr")
                for nm, t_, dt_ in (("idxgi", R["idxgi"], mybir.dt.int32), ("ej", R["ej"], F32), ("sloti", R["sloti"], mybir.dt.int32),
                                    ("t8", t8, F32), ("pidx", R["pidx"], F32), ("r12", R["r12"], F32), ("g12", R["g12"], F32),
                                    ("oh1", R["oh1"], F32), ("oh2", R["oh2"], F32), ("run", R["run"], F32), ("U", R["U"], F32), ("onesf", R["onesf"], F32),
                                    ("pidxi", R["pidxi"], mybir.dt.int32)):
                    shp = list(t_.shape)
                    o = self.nc.dram_tensor("dbg_" + nm, shp, dt_, kind="ExternalOutput").ap()
                    P.dma("sp", o, t_[:], reads=[drt, d8, dc], writes=[dd])

    def phase_e_moe(self, l, out_res):
        P, T, I = self.P, self.T, self.I
        R = self.R
        NT = T // 128
        NTILE = self.NTILE
        NSLOT = NTILE * 512
        drt = P.dep("r_tab")
        P.barrier()
        with ExitStack() as st0:
            cur = [ExitStack()]
            st0.callback(lambda: cur[0].close())
            sb = lambda n, s, d: self.sb(cur[0], n, s, d)

            class _St:
                def enter_context(self_, x):
                    return cur[0].enter_context(x)
            st = _St()
            dXS = P.dep("d_XS")
            dYS = P.dep("d_YS")
            zt = sb("zt", [128, 4096], BF16)
            dzt = P.dep("m_zt")
            P.op("pool", lambda e: e.memset(zt[:], 0.0), writes=[dzt])
            xsv = self.XS.rearrange("(a p f) n -> a p (f n)", p=128, f=4)
            for a in range(NSLOT // 512):
                P.dma("sp", xsv[a], zt[:], reads=[dzt], writes=[dXS])
            xl = [sb("xl", [128, D], BF16) for _ in range(2)]
            for i in range(NT):
                x_, dx_ = xl[i % 2], P.dep("m_xl%d" % (i % 2))
                P.dma("sp", x_[:], self.X1B[i * 128:(i + 1) * 128, :], reads=[P.dep("d_X1B")], writes=[dx_])
                for kk in range(2):
                    P.idma(self.XS, R["sloti"][:, i, kk:kk + 1], x_[:], None, reads=[dx_, drt], writes=[dXS])
            P.barrier()
            cur[0].close()
            cur[0] = ExitStack()
            if self.mstop < 4:
                return
            xtm = sb("xtm", [128, 4, D], BF16)
            dxtm = P.dep("m_xtm")
            xTb = sb("xTb", [128, 8, 512], BF16)
            dxTb = P.dep("m_xTb")
            pT = self.psum(st, "mpT", [128, D], BF16)
            dpT = P.dep("m_pT")
            Wg = [sb("Wg", [128, 8, 512], BF16) for _ in range(3)]
            Wu = [sb("Wu", [128, 8, 512], BF16) for _ in range(3)]
            Wd = sb("Wd", [128, 28, D], BF16)
            dWd = P.dep("e_Wd")
            hT = sb("hT", [128, 28, 512], BF16)
            dhT = P.dep("e_hT")
            pg = [self.psum(st, "pg", [128, 512], F32) for _ in range(2)]
            pu = [self.psum(st, "pu", [128, 512], F32) for _ in range(2)]
            pd = [self.psum(st, "pd", [128, 512], F32) for _ in range(2)]
            sg = [sb("sg", [128, 512], F32) for _ in range(2)]
            ysb = [sb("ysb", [128, D], F32) for _ in range(2)]
            wi = 0
            npd = 0
            nys = 0
            xsr = self.XS.rearrange("(j s p) n -> j p s n", p=128, s=4)
            for j in range(NTILE):
                P.dma("sp", xtm[:], xsr[j], reads=[dXS], writes=[dxtm])
                for sub in range(4):
                    for c in range(8):
                        P.op("pe", lambda e, c=c, sub=sub: e.transpose(pT[:, c * 128:(c + 1) * 128], xtm[:, sub, c * 128:(c + 1) * 128], self.ident[:]),
                             reads=[dxtm, self.dconst], writes=[dpT], signal=(c == 7))
                    P.op("act", lambda e, sub=sub: e.activation(out=xTb[:, :, sub * 128:(sub + 1) * 128], in_=pT[:].rearrange("p (c t) -> p c t", c=8), func=AF.Copy),
                         reads=[dpT], writes=[dxTb])
                import os
                fstop = int(os.environ.get("MK_FSTOP", "99"))
                if fstop < 1:
                    continue
                for g in range(7):
                    wg_, dwg_ = Wg[wi % 3], P.dep("e_Wg%d" % (wi % 3))
                    wu_, dwu_ = Wu[wi % 3], P.dep("e_Wu%d" % (wi % 3))
                    wi += 1
                    P.idma(wg_[:].rearrange("p c n -> p (c n)"), None, self.WGt, R["idxgi"][:, j, g:g + 1], reads=[self.dwc["mg"], drt], writes=[dwg_])
                    P.idma(wu_[:].rearrange("p c n -> p (c n)"), None, self.WUt, R["idxgi"][:, j, g:g + 1], reads=[self.dwc["mu"], drt], writes=[dwu_])
                    for jj in range(4):
                        f = g * 4 + jj
                        pg_, dpg_ = pg[f % 2], P.dep("e_pg%d" % (f % 2))
                        pu_, dpu_ = pu[f % 2], P.dep("e_pu%d" % (f % 2))
                        for c in range(8):
                            P.op("pe", lambda e, c=c, jj=jj, pg_=pg_, wg_=wg_: e.matmul(pg_[:], lhsT=wg_[:, c, jj * 128:(jj + 1) * 128], rhs=xTb[:, c, :], start=(c == 0), stop=(c == 7)),
                                 reads=[dwg_, dxTb], writes=[dpg_], signal=(c == 7))
                        for c in range(8):
                            P.op("pe", lambda e, c=c, jj=jj, pu_=pu_, wu_=wu_: e.matmul(pu_[:], lhsT=wu_[:, c, jj * 128:(jj + 1) * 128], rhs=xTb[:, c, :], start=(c == 0), stop=(c == 7)),
                                 reads=[dwu_, dxTb], writes=[dpu_], signal=(c == 7))
                        s_, ds_ = sg[f % 2], P.dep("e_sg%d" % (f % 2))
                        P.op("act", lambda e, s_=s_, pg_=pg_: e.activation(out=s_[:], in_=pg_[:], func=AF.Silu), reads=[dpg_], writes=[ds_])
                        P.op("dve", lambda e, f=f, s_=s_, pu_=pu_: e.tensor_tensor(out=hT[:, f, :], in0=pu_[:], in1=s_[:], op=ALU.mult),
                             reads=[dpu_, ds_], writes=[dhT])
                if fstop < 2:
                    continue
                for q in range(4):
                    P.idma(Wd[:, q * 7:(q + 1) * 7, :].rearrange("p f n -> p (f n)"), None, self.WDt, R["idxdi"][:, j, q:q + 1], reads=[self.dwc["md"], drt], writes=[dWd])
                for sub in range(4):
                    y_, dy_ = ysb[nys % 2], P.dep("m_ysb%d" % (nys % 2))
                    nys += 1
                    for hf in range(2):
                        pd_, dpd_ = pd[npd % 2], P.dep("e_pd%d" % (npd % 2))
                        npd += 1
                        for f in range(28):
                            P.op("pe", lambda e, f=f, sub=sub, hf=hf, pd_=pd_: e.matmul(pd_[:], lhsT=hT[:, f, sub * 128:(sub + 1) * 128], rhs=Wd[:, f, hf * 512:(hf + 1) * 512],
                                                                                   start=(f == 0), stop=(f == 27)), reads=[dhT, dWd], writes=[dpd_], signal=(f == 27))
                        if hf == 0:
                            P.op("act", lambda e, y_=y_, pd_=pd_: e.activation(out=y_[:, 0:512], in_=pd_[:], func=AF.Copy), reads=[dpd_], writes=[dy_])
                        else:
                            P.op("dve", lambda e, y_=y_, pd_=pd_: e.tensor_copy(y_[:, 512:1024], pd_[:]), reads=[dpd_], writes=[dy_])
                    r0 = j * 512 + sub * 128
                    P.dma("sp", self.YS[r0:r0 + 128, :], y_[:], reads=[dy_], writes=[dYS])
            P.barrier()
            cur[0].close()
            cur[0] = ExitStack()
            if self.mstop < 5:
                return
            L = self.ln_setup(st, I["ln2_g"][l], I["ln2_b"][l], "2", nbuf=1)
            y1 = [sb("y1", [128, D], F32) for _ in range(2)]
            y2 = [sb("y2", [128, D], F32) for _ in range(2)]
            xr = [sb("xr", [128, D], F32) for _ in range(2)]
            pre = [sb("pre", [128, D], F32) for _ in range(2)]
            dres = P.dep("d_outres")
            for i in range(NT):
                i2 = i % 2
                a_, b_, x_, p_ = y1[i2], y2[i2], xr[i2], pre[i2]
                da_, db_, dx_, dp_ = (P.dep("m_%s%d" % (n, i2)) for n in ("y1", "y2", "xr", "pre"))
                P.idma(a_[:], None, self.YS, R["sloti"][:, i, 0:1], reads=[dYS, drt], writes=[da_])
                P.idma(b_[:], None, self.YS, R["sloti"][:, i, 1:2], reads=[dYS, drt], writes=[db_])
                P.dma("sp", x_[:], self.x1res[i * 128:(i + 1) * 128, :], reads=[P.dep("d_x1res")], writes=[dx_])
                P.op("dve", lambda e, a_=a_, i=i: e.tensor_scalar(out=a_[:], in0=a_[:], scalar1=R["g12"][:, i, 0:1], scalar2=None, op0=ALU.mult), reads=[da_, drt], writes=[da_])
                P.op("dve", lambda e, a_=a_, b_=b_, i=i: e.scalar_tensor_tensor(out=a_[:], in0=b_[:], scalar=R["g12"][:, i, 1:2], in1=a_[:], op0=ALU.mult, op1=ALU.add),
                     reads=[da_, db_, drt], writes=[da_])
                P.op("dve", lambda e, a_=a_, x_=x_, p_=p_: e.scalar_tensor_tensor(out=p_[:], in0=x_[:], scalar=ALPHA, in1=a_[:], op0=ALU.mult, op1=ALU.add),
                     reads=[da_, dx_], writes=[dp_])
                self.ln_tile(L, p_, dp_, None, None, 0, res_dst=out_res[i * 128:(i + 1) * 128, :], dres=dres)

    def phase_e(self, l, out_res, out_T):
        P, T, NB, I = self.P, self.T, self.NB, self.I
        P.barrier()
        moe = (l == 1)
        nexp = NE if moe else 1
        with ExitStack() as st:
            sb = lambda n, s, d: self.sb(st, n, s, d)
            L = self.ln_setup(st, I["ln2_g"][l], I["ln2_b"][l], "2", nbuf=1)
            xb_ = [sb("xb", [128, 8, 512], BF16) for _ in range(1)]
            Wg = [sb("Wg", [128, 8, 512], BF16) for _ in range(3)]
            Wu = [sb("Wu", [128, 8, 512], BF16) for _ in range(3)]
            Wd = sb("Wd", [128, 28, D], BF16)
            dWd = P.dep("e_Wd")
            hT = sb("hT", [128, 28, 512], BF16)
            dhT = P.dep("e_hT")
            pg = [self.psum(st, "pg", [128, 512], F32) for _ in range(2)]
            pu = [self.psum(st, "pu", [128, 512], F32) for _ in range(2)]
            pd = [self.psum(st, "pd", [128, 512], F32) for _ in range(2)]
            pc = self.psum(st, "pc", [128, 512], F32)
            sg = [sb("sg", [128, 512], F32) for _ in range(2)]
            xr = [sb("xr", [128, D], F32) for _ in range(1)]
            pre = [sb("pre", [128, D], F32) for _ in range(1)]
            xTs = [sb("xTs", [128, 8, 512], BF16) for _ in range(1)] if out_T is not None else None
            acc = sb("acc", [128, 4, D], F32) if moe else None
            dacc = P.dep("e_acc")
            if moe:
                selm = sb("selm", [NE, NE, 128], F32)
                dselm = P.dep("e_selm")
                P.op("pool", lambda e: e.memset(selm[:], 0.0), writes=[dselm])
                P.op("pool", lambda e: e.affine_select(out=selm[:], in_=selm[:], pattern=[[1, NE], [0, 128]], compare_op=ALU.not_equal, fill=1.0,
                                                       base=0, channel_multiplier=-1), reads=[dselm], writes=[dselm])
                cmb = [sb("cmb", [128, NE], F32) for _ in range(2)]
                cT = sb("cT", [NE, 512], F32)
                dcT = P.dep("e_cT")
                cbe = [sb("cbe", [128, 512], F32) for _ in range(2)]
                pT8 = pc[0:NE, 0:128]
                dpT8 = P.dep("e_pc")
            x1Tv = self.x1T.rearrange("(c p) t -> p c t", p=128)
            wi = 0
            npd = 0
            ntile = 0
            dres = P.dep("d_outres")
            dxT = P.dep("d_xT")
            for b in range(NB):
                ts_ = slice(b * 512, (b + 1) * 512)
                xb, dxb = xb_[0], P.dep("e_xb0")
                P.dma("sp", xb[:], x1Tv[:, :, ts_], reads=[P.dep("d_x1T")], writes=[dxb])
                if moe:
                    for sub in range(4):
                        cm, dcm = cmb[sub % 2], P.dep("e_cmb%d" % (sub % 2))
                        P.dma("sp", cm[:], self.comb[b * 512 + sub * 128:b * 512 + (sub + 1) * 128, :], reads=[P.dep("d_comb")], writes=[dcm])
                        P.op("pe", lambda e, cm=cm: e.transpose(pT8, cm[:], self.identf[:]), reads=[dcm, self.dconst], writes=[dpT8])
                        P.op("dve", lambda e, sub=sub: e.tensor_copy(cT[:, sub * 128:(sub + 1) * 128], pT8), reads=[dpT8], writes=[dcT])
                for ex_ in range(nexp):
                    if moe:
                        wgd, wud, wdd = self.wb["mg"][ex_], self.wb["mu"][ex_], self.wb["md"][ex_]
                        cb_, dcb_ = cbe[ex_ % 2], P.dep("e_cbe%d" % (ex_ % 2))
                        dpc = P.dep("e_pc")
                        P.op("pe", lambda e, ex_=ex_: e.matmul(pc[:], lhsT=selm[:, ex_, :], rhs=cT[:], start=True, stop=True), reads=[dselm, dcT], writes=[dpc])
                        P.op("act", lambda e, cb_=cb_: e.activation(out=cb_[:], in_=pc[:], func=AF.Copy), reads=[dpc], writes=[dcb_])
                    else:
                        wgd, wud, wdd = self.wb["dg"], self.wb["du"], self.wb["dd"]
                    if not (getattr(self, "moe_tables", False) and not moe):
                        wgv = wgd.rearrange("(c p) n -> p c n", p=128)
                        wuv = wud.rearrange("(c p) n -> p c n", p=128)
                    wdv = wdd.rearrange("(c p) n -> p c n", p=128)
                    for g in range(7):
                        wg_, dwg_ = Wg[wi % 3], P.dep("e_Wg%d" % (wi % 3))
                        wu_, dwu_ = Wu[wi % 3], P.dep("e_Wu%d" % (wi % 3))
                        wi += 1
                        if getattr(self, "moe_tables", False) and not moe:
                            P.dma("sp", wg_[:].rearrange("p c n -> p (c n)"), wgd[g * 128:(g + 1) * 128, :], reads=[self.dwc["g"]], writes=[dwg_])
                            P.dma("sp", wu_[:].rearrange("p c n -> p (c n)"), wud[g * 128:(g + 1) * 128, :], reads=[self.dwc["u"]], writes=[dwu_])
                        else:
                            P.dma("sp", wg_[:], wgv[:, :, g * 512:(g + 1) * 512], reads=[self.dwc["mg" if moe else "g"]], writes=[dwg_])
                            P.dma("sp", wu_[:], wuv[:, :, g * 512:(g + 1) * 512], reads=[self.dwc["mu" if moe else "u"]], writes=[dwu_])
                        for j in range(4):
                            f = g * 4 + j
                            pg_, dpg_ = pg[f % 2], P.dep("e_pg%d" % (f % 2))
                            pu_, dpu_ = pu[f % 2], P.dep("e_pu%d" % (f % 2))
                            for c in range(8):
                                P.op("pe", lambda e, c=c, j=j, pg_=pg_, wg_=wg_: e.matmul(pg_[:], lhsT=wg_[:, c, j * 128:(j + 1) * 128], rhs=xb[:, c, :], start=(c == 0), stop=(c == 7)),
                                     reads=[dwg_, dxb], writes=[dpg_], signal=(c == 7))
                            for c in range(8):
                                P.op("pe", lambda e, c=c, j=j, pu_=pu_, wu_=wu_: e.matmul(pu_[:], lhsT=wu_[:, c, j * 128:(j + 1) * 128], rhs=xb[:, c, :], start=(c == 0), stop=(c == 7)),
                                     reads=[dwu_, dxb], writes=[dpu_], signal=(c == 7))
                            s_, ds_ = sg[f % 2], P.dep("e_sg%d" % (f % 2))
                            P.op("act", lambda e, s_=s_, pg_=pg_: e.activation(out=s_[:], in_=pg_[:], func=AF.Silu), reads=[dpg_], writes=[ds_])
                            if moe:
                                P.op("pool", lambda e, s_=s_, cb_=cb_: e.tensor_tensor(out=s_[:], in0=s_[:], in1=cb_[:], op=ALU.mult), reads=[ds_, dcb_], writes=[ds_])
                            P.op("dve", lambda e, f=f, s_=s_, pu_=pu_: e.tensor_tensor(out=hT[:, f, :], in0=pu_[:], in1=s_[:], op=ALU.mult),
                                 reads=[dpu_, ds_], writes=[dhT])
                    for c0 in range(0, 28, 7):
                        P.dma("sp", Wd[:, c0:c0 + 7, :], wdv[:, c0:c0 + 7, :], reads=[self.dwc["md" if moe else "d"]], writes=[dWd])
                    for sub in range(4):
                        r0 = b * 512 + sub * 128
                        if ex_ == nexp - 1:
                            xrt, dxr = xr[0], P.dep("e_xr0")
                            pr, dpr = pre[0], P.dep("e_pre0")
                            ntile += 1
                            P.dma("sp", xrt[:], self.x1res[r0:r0 + 128, :], reads=[P.dep("d_x1res")], writes=[dxr])
                        for hf in range(2):
                            pd_, dpd_ = pd[npd % 2], P.dep("e_pd%d" % (npd % 2))
                            npd += 1
                            for f in range(28):
                                P.op("pe", lambda e, f=f, sub=sub, hf=hf, pd_=pd_: e.matmul(pd_[:], lhsT=hT[:, f, sub * 128:(sub + 1) * 128], rhs=Wd[:, f, hf * 512:(hf + 1) * 512],
                                                                                       start=(f == 0), stop=(f == 27)), reads=[dhT, dWd], writes=[dpd_], signal=(f == 27))
                            hs = slice(hf * 512, (hf + 1) * 512)
                            if moe and ex_ == 0:
                                P.op("dve", lambda e, sub=sub, hs=hs, pd_=pd_: e.tensor_copy(acc[:, sub, hs], pd_[:]), reads=[dpd_], writes=[dacc])
                            elif moe and ex_ < nexp - 1:
                                P.op("dve", lambda e, sub=sub, hs=hs, pd_=pd_: e.tensor_tensor(out=acc[:, sub, hs], in0=pd_[:], in1=acc[:, sub, hs], op=ALU.add),
                                     reads=[dpd_, dacc], writes=[dacc])
                            else:
                                if moe:
                                    P.op("dve", lambda e, sub=sub, hs=hs, pd_=pd_: e.tensor_tensor(out=acc[:, sub, hs], in0=pd_[:], in1=acc[:, sub, hs], op=ALU.add),
                                         reads=[dpd_, dacc], writes=[dacc])
                                    P.op("dve", lambda e, sub=sub, hs=hs, pr=pr, xrt=xrt: e.scalar_tensor_tensor(out=pr[:, hs], in0=xrt[:, hs], scalar=ALPHA, in1=acc[:, sub, hs],
                                                                                                              op0=ALU.mult, op1=ALU.add), reads=[dxr, dacc], writes=[dpr])
                                else:
                                    P.op("dve", lambda e, hs=hs, pr=pr, xrt=xrt, pd_=pd_: e.scalar_tensor_tensor(out=pr[:, hs], in0=xrt[:, hs], scalar=ALPHA, in1=pd_[:],
                                                                                                              op0=ALU.mult, op1=ALU.add), reads=[dxr, dpd_], writes=[dpr])
                        if ex_ == nexp - 1:
                            xs, dxs = (xTs[0] if xTs is not None else None), P.dep("e_xTs0")
                            self.ln_tile(L, pr, dpr, xs, dxs, sub, res_dst=out_res[r0:r0 + 128, :], dres=dres)
                if out_T is not None:
                    xs, dxs = xTs[0], P.dep("e_xTs0")
                    P.dma("sp", out_T.rearrange("(c p) t -> p c t", p=128)[:, :, ts_], xs[:], reads=[dxs], writes=[dxT])


_CACHE = {}


def _rope_tables(S):
    pos = np.arange(S, dtype=np.float32)
    inv = (np.float32(10000.0) ** (-np.arange(0, 32, 2, dtype=np.float32) / np.float32(32))).astype(np.float32)
    ang = pos[:, None] * inv[None, :]
    c = np.cos(ang).astype(np.float32).T
    s = np.sin(ang).astype(np.float32).T
    return np.concatenate([c, c], 0), np.concatenate([s, s], 0)


def _get_prog(S, layers, do_ln_in, final_out, dbg=()):
    key = (S, tuple(layers), do_ln_in, final_out, tuple(dbg))
    if key not in _CACHE:
        _CACHE[key] = Builder(S, list(layers), do_ln_in, final_out, dbg).build()
    return _CACHE[key]


WEIGHT_KEYS = ["w_in", "conv_w", "q_norm_g", "w_uq", "kv_norm_g", "w_ukv", "lru_conv_w", "lru_conv_b", "lru_wa", "lru_ba",
               "lru_wi", "lru_bi", "lru_lam", "mix_norm_g", "w_out", "ln1_g", "ln1_b", "ln2_g", "ln2_b"]


def _core_common(S, inputs, half, layers):
    T = S // 2
    C, Sn = _rope_tables(S)
    order = np.concatenate([np.arange(half * T, (half + 1) * T), np.arange((1 - half) * T, (2 - half) * T)])
    flags = np.zeros((128, 2), np.float32)
    flags[:, half] = 1.0
    m = {"flags": flags, "ropeC": np.ascontiguousarray(C[:, order]), "ropeS": np.ascontiguousarray(Sn[:, order])}
    for k in WEIGHT_KEYS:
        m[k] = np.ascontiguousarray(inputs[k])
    if 0 in layers:
        for k in ("dense_w_gate", "dense_w_up", "dense_w_down"):
            m[k] = np.ascontiguousarray(inputs[k])
    if 1 in layers:
        for k in ("moe_w_router", "moe_w_gate", "moe_w_up", "moe_w_down"):
            m[k] = np.ascontiguousarray(inputs[k])
    return m


def kernel(**inputs):
    x = np.asarray(inputs["x"])
    B, S, _ = x.shape
    T = S // 2
    ncore = 2 * B
    key = ("fused", S)
    if key not in _CACHE:
        _CACHE[key] = Builder(S, [0, 1], True, True).build_fused()
    nc = _CACHE[key]
    C, Sn = _rope_tables(S)
    maps = []
    for core in range(ncore):
        b, half = core // 2, core % 2
        order = np.concatenate([np.arange(half * T, (half + 1) * T), np.arange((1 - half) * T, (2 - half) * T)])
        flags = np.zeros((128, 2), np.float32)
        flags[:, half] = 1.0
        m = {"flags": flags, "ropeC0": C, "ropeS0": Sn,
             "ropeC1": np.ascontiguousarray(C[:, order]), "ropeS1": np.ascontiguousarray(Sn[:, order]),
             "x_full": np.ascontiguousarray(x[b]),
             "ln_in_g": np.ascontiguousarray(inputs["ln_in_g"]), "ln_in_b": np.ascontiguousarray(inputs["ln_in_b"])}
        for k in WEIGHT_KEYS + ["dense_w_gate", "dense_w_up", "dense_w_down", "moe_w_router", "moe_w_gate", "moe_w_up", "moe_w_down"]:
            m[k] = np.ascontiguousarray(inputs[k])
        maps.append(m)
    res = run_bass_kernel_spmd(nc, maps, core_ids=list(range(ncore))).results
    out = np.empty((B, S, D), np.float32)
    for core in range(ncore):
        b, half = core // 2, core % 2
        out[b, half * T:(half + 1) * T] = res[core]["y_out"]
    return out


def kernel_unfused(**inputs):
    x = np.asarray(inputs["x"])
    B, S, _ = x.shape
    T = S // 2
    ncore = 2 * B
    nc0 = _get_prog(S, (0,), True, False)
    maps = []
    for core in range(ncore):
        b, half = core // 2, core % 2
        m = _core_common(S, inputs, half, (0,))
        m["x_own"] = np.ascontiguousarray(x[b, half * T:(half + 1) * T])
        m["x_oth"] = np.ascontiguousarray(x[b, (1 - half) * T:(2 - half) * T])
        m["ln_in_g"] = np.ascontiguousarray(inputs["ln_in_g"])
        m["ln_in_b"] = np.ascontiguousarray(inputs["ln_in_b"])
        maps.append(m)
    r0 = run_bass_kernel_spmd(nc0, maps, core_ids=list(range(ncore))).results
    nc1 = _get_prog(S, (1,), False, True)
    maps = []
    for core in range(ncore):
        half = core % 2
        m = _core_common(S, inputs, half, (1,))
        m["xres_in"] = r0[core]["xres_out"]
        m["xT_in"] = np.ascontiguousarray(np.concatenate([r0[core]["xT_out"], r0[core ^ 1]["xT_out"]], axis=1))
        maps.append(m)
    r1 = run_bass_kernel_spmd(nc1, maps, core_ids=list(range(ncore))).results
    out = np.empty((B, S, D), np.float32)
    for core in range(ncore):
        b, half = core // 2, core % 2
        out[b, half * T:(half + 1) * T] = r1[core]["y_out"]
    return out
```

```python
import numpy as np
from contextlib import ExitStack
import concourse.bass as bass
import concourse.mybir as mybir
from concourse.bass_utils import run_bass_kernel_spmd

F32 = mybir.dt.float32
BF16 = mybir.dt.bfloat16
AF = mybir.ActivationFunctionType
ALU = mybir.AluOpType

D = 1024
DIN = 1952
NH = 8
DFF = 3584
NE = 8
ALPHA = 4.0 ** 0.25
LN_EPS = 1e-5
RMS_EPS = 1e-6
QSCALE = 96.0 ** -0.5
import os as _os
LNSTOP = int(_os.environ.get("MK_LNSTOP", "99"))
ASTOP = int(_os.environ.get("MK_ASTOP", "99"))


class Dep:
    __slots__ = ("name", "w", "r", "dsem", "dcnt", "excl")

    def __init__(self, name=""):
        self.name = name
        self.excl = any(t in name for t in ("_ps", "_pS", "_pO", "_pD", "_prr", "_pg", "_pu", "_pd", "_pc", "pT"))
        self.w = None
        self.r = []
        self.dsem = None
        self.dcnt = 0


class _Rec:
    def __init__(self):
        self.call = None

    def __getattr__(self, name):
        def f(*a, **k):
            self.call = (name, a, k)
            return self
        return f


class Prog:
    ENGS = ("pe", "act", "dve", "pool", "sp")

    def __init__(self, nc):
        self.nc = nc
        self.streams = {e: [] for e in self.ENGS}
        self.sem = {e: nc.alloc_semaphore("es_" + e) for e in self.ENGS}
        self.cnt = {e: 0 for e in self.ENGS}
        self.seen = {e: {} for e in self.ENGS}
        self.deps = {}
        self.ndsem = 0
        self.store_q = "act"

    def dep(self, name):
        d = self.deps.get(name)
        if d is None:
            d = Dep(name)
            self.deps[name] = d
        return d

    def _dsem(self, d):
        if d.dsem is None:
            d.dsem = self.nc.alloc_semaphore("ds_%d" % self.ndsem)
            self.ndsem += 1
        return d.dsem

    def _collect(self, eng, reads, writes):
        need = {}

        def add(t):
            if t is None:
                return
            sem, val = t
            k = id(sem)
            if k not in need or need[k][1] < val:
                need[k] = (sem, val)
        own_sem = self.sem[eng]
        for d in reads:
            add(d.w)
            if d.excl:
                for t in d.r:
                    if t[0] is not own_sem:
                        add(t)
        for d in writes:
            add(d.w)
            for t in d.r:
                add(t)
        out = []
        seen = self.seen[eng]
        own = self.sem[eng]
        for k, (sem, val) in need.items():
            if sem is own and (eng == "pe" or val > self.cnt[eng]):
                continue
            if seen.get(k, 0) >= val:
                continue
            seen[k] = val
            out.append((sem, val))
        return out

    def _update(self, tk, reads, writes):
        for d in reads:
            d.r.append(tk)
            if len(d.r) > 64:
                d.r = d.r[-48:]
        for d in writes:
            d.w = tk
            d.r = []

    def op(self, eng, fn, reads=(), writes=(), signal=True):
        waits = self._collect(eng, reads, writes)
        sem = self.sem[eng]
        if signal:
            self.cnt[eng] += 1
            tk = (sem, self.cnt[eng])
        else:
            tk = (sem, self.cnt[eng] + 1)

        rec = _Rec()
        fn(rec)
        call = rec.call

        def emit(e, call=call, waits=waits, signal=signal, sem=sem):
            for s, v in waits:
                e.wait_ge(s, v)
            ins = getattr(e, call[0])(*call[1], **call[2])
            if signal:
                ins.then_inc(sem, 1)
        self.streams[eng].append(emit)
        self._update(tk, reads, writes)
        return tk

    def dma(self, q, out, in_, reads=(), writes=(), **kw):
        if q == "sp" and self.store_q is not None and type(out.tensor).__name__.startswith("DRam") \
                and not type(in_.tensor).__name__.startswith("DRam"):
            q = self.store_q
        waits = self._collect(q, reads, writes)
        d0 = writes[0]
        sem = self._dsem(d0)
        d0.dcnt += 16
        tk = (sem, d0.dcnt)

        def emit(e, waits=waits, sem=sem, out=out, in_=in_, kw=kw):
            for s, v in waits:
                e.wait_ge(s, v)
            e.dma_start(out=out, in_=in_, **kw).then_inc(sem, 16)
        self.streams[q].append(emit)
        self._update(tk, reads, writes)
        return tk

    def idma(self, out, out_idx, in_, in_idx, reads=(), writes=(), **kw):
        q = "pool"
        waits = self._collect(q, reads, writes)
        d0 = writes[0]
        sem = self._dsem(d0)
        d0.dcnt += 16
        tk = (sem, d0.dcnt)

        def emit(e, waits=waits, sem=sem):
            for s_, v in waits:
                e.wait_ge(s_, v)
            oo = bass.IndirectOffsetOnAxis(ap=out_idx, axis=0) if out_idx is not None else None
            io = bass.IndirectOffsetOnAxis(ap=in_idx, axis=0) if in_idx is not None else None
            e.indirect_dma_start(out=out, out_offset=oo, in_=in_, in_offset=io, **kw).then_inc(sem, 16)
        self.streams[q].append(emit)
        self._update(tk, reads, writes)
        return tk

    def barrier(self):
        tks = [(self.sem[e], self.cnt[e]) for e in self.ENGS if self.cnt[e] > 0]
        for d in self.deps.values():
            if d.dsem is not None and d.dcnt > 0:
                tks.append((d.dsem, d.dcnt))
        for e in self.ENGS:
            seen = self.seen[e]
            waits = []
            for sem, val in tks:
                if sem is self.sem[e]:
                    continue
                if seen.get(id(sem), 0) >= val:
                    continue
                seen[id(sem)] = val
                waits.append((sem, val))

            def emit(en, waits=waits):
                for s, v in waits:
                    en.wait_ge(s, v)
            self.streams[e].append(emit)
        for d in self.deps.values():
            d.w = None
            d.r = []

    def finish(self):
        nc = self.nc
        st = self.streams
        with nc.Block() as block:
            @block.tensor
            def _(e):
                for f in st["pe"]:
                    f(e)

            @block.scalar
            def _(e):
                for f in st["act"]:
                    f(e)

            @block.vector
            def _(e):
                for f in st["dve"]:
                    f(e)

            @block.gpsimd
            def _(e):
                for f in st["pool"]:
                    f(e)

            @block.sync
            def _(e):
                for f in st["sp"]:
                    f(e)
        return nc


class Builder:
    def __init__(self, S, layers, do_ln_in, final_out, dbg=()):
        self.S = S
        self.T = S // 2
        self.NB = self.T // 512
        self.layers = layers
        self.dbg = set(dbg)
        nc = bass.Bass("TRN2", target_bir_lowering=False)
        self.nc = nc
        self.P = Prog(nc)
        self.do_ln_in = do_ln_in
        self.final_out = final_out
        self.cnt = 0
        self.ps_rr = 0
        self.has_other = True

    def din(self, name, shape, dt=F32):
        return self.nc.dram_tensor(name, list(shape), dt, kind="ExternalInput").ap()

    def dout(self, name, shape, dt=F32):
        return self.nc.dram_tensor(name, list(shape), dt, kind="ExternalOutput").ap()

    def dscr(self, name, shape, dt):
        kind = "ExternalOutput" if name in self.dbg else "Internal"
        return self.nc.dram_tensor(name, list(shape), dt, kind=kind).ap()

    def sb(self, st, name, shape, dt):
        self.cnt += 1
        return st.enter_context(self.nc.sbuf_tensor("%s_%d" % (name, self.cnt), list(shape), dt))

    def psum(self, st, name, shape, dt=F32):
        self.cnt += 1
        return st.enter_context(self.nc.psum_tensor("%s_%d" % (name, self.cnt), list(shape), dt))

    def load_cols(self, st, name, vec_ap, n, q="sp"):
        P = self.P
        nchunk = n // 128
        t = self.sb(st, name, [128, nchunk], F32)
        d = P.dep(name)
        v2 = vec_ap.rearrange("(c p o) -> c p o", p=128, o=1)
        for c in range(nchunk):
            P.dma(q, t[:, c:c + 1], v2[c], writes=[d])
        return t, d

    def build(self):
        nc, P, T, NB = self.nc, self.P, self.T, self.NB
        S2 = 2 * T
        I = {}
        if self.do_ln_in:
            I["x_own"] = self.din("x_own", [T, D])
            I["x_oth"] = self.din("x_oth", [T, D])
            I["ln_in_g"] = self.din("ln_in_g", [D])
            I["ln_in_b"] = self.din("ln_in_b", [D])
        else:
            I["xres_in"] = self.din("xres_in", [T, D])
            I["xT_in"] = self.din("xT_in", [D, S2], BF16)
        I["flags"] = self.din("flags", [128, 2])
        I["ropeC"] = self.din("ropeC", [32, S2])
        I["ropeS"] = self.din("ropeS", [32, S2])
        nl = 2
        shapes = dict(w_in=[nl, D, DIN], conv_w=[nl, 3, 256], q_norm_g=[nl, 384], w_uq=[nl, 384, 768],
                      kv_norm_g=[nl, 256], w_ukv=[nl, 256, 1024], lru_conv_w=[nl, 4, 256],
                      lru_conv_b=[nl, 256], lru_wa=[nl, 2, 4, 64, 64], lru_ba=[nl, 2, 256],
                      lru_wi=[nl, 2, 4, 64, 64], lru_bi=[nl, 2, 256], lru_lam=[nl, 2, 256],
                      mix_norm_g=[nl, D], w_out=[nl, D, D], ln1_g=[nl, D], ln1_b=[nl, D],
                      dense_w_gate=[1, D, DFF], dense_w_up=[1, D, DFF], dense_w_down=[1, DFF, D],
                      moe_w_router=[1, D, NE], moe_w_gate=[1, NE, D, DFF], moe_w_up=[1, NE, D, DFF],
                      moe_w_down=[1, NE, DFF, D], ln2_g=[nl, D], ln2_b=[nl, D])
        need_dense = 0 in self.layers
        need_moe = 1 in self.layers
        for k, shp in shapes.items():
            if k.startswith("dense") and not need_dense:
                continue
            if k.startswith("moe") and not need_moe:
                continue
            I[k] = self.din(k, shp)
        self.I = I
        if self.final_out:
            self.y_out = self.dout("y_out", [T, D])
        else:
            self.y_out = self.dout("xres_out", [T, D])
            self.xT_out = self.dout("xT_out", [D, T], BF16)
        self.xres = self.dscr("s_xres", [T, D], F32)
        self.xT = self.dscr("s_xT", [D, S2], BF16)
        self.QT = self.dscr("s_QT", [NH, 96, T], BF16)
        self.KT = self.dscr("s_KT", [NH, 96, S2], BF16)
        self.Vd = self.dscr("s_V", [S2, NH * 65], BF16)
        self.CB = self.dscr("s_CB", [256, T], F32)
        self.PP = self.dscr("s_PP", [256, S2], F32)
        self.LG = self.dscr("s_LG", [256, T], F32)
        self.LX = self.dscr("s_LX", [256, S2], F32)
        self.Y = self.dscr("s_Y", [D, T], F32)
        self.x1res = self.dscr("s_x1res", [T, D], F32)
        self.x1T = self.dscr("s_x1T", [D, T], BF16)
        self.comb = self.dscr("s_comb", [T, NE], F32)
        self.wb = {}
        for l in self.layers:
            self.wb[("w_in", l)] = self.dscr("b_w_in%d" % l, [D, DIN], BF16)
            self.wb[("w_out", l)] = self.dscr("b_w_out%d" % l, [D, D], BF16)
        if need_dense:
            self.wb["dg"] = self.dscr("b_dg", [D, DFF], BF16)
            self.wb["du"] = self.dscr("b_du", [D, DFF], BF16)
            self.wb["dd"] = self.dscr("b_dd", [DFF, D], BF16)
        if need_moe:
            self.wb["mg"] = self.dscr("b_mg", [NE, D, DFF], BF16)
            self.wb["mu"] = self.dscr("b_mu", [NE, D, DFF], BF16)
            self.wb["md"] = self.dscr("b_md", [NE, DFF, D], BF16)

        with ExitStack() as gst:
            import os
            stop = int(os.environ.get("MK_STOP", "99"))
            self.consts(gst)
            if stop >= 1:
                self.cast_weights()
            if self.do_ln_in:
                self.ln_srcs = ((I["x_own"], 0, True), (I["x_oth"], T, False))
                self.ropeC, self.ropeS = I["ropeC"], I["ropeS"]
                if stop >= 2:
                    self.phase_ln_in()
                xres, xT = self.xres, self.xT
            else:
                self.ropeC, self.ropeS = I["ropeC"], I["ropeS"]
                xres, xT = I["xres_in"], I["xT_in"]
            for li, l in enumerate(self.layers):
                last = li == len(self.layers) - 1
                if stop >= 3:
                    self.phase_a(l, xT)
                if stop >= 4:
                    self.phase_b(l)
                if stop >= 5:
                    self.phase_c(l)
                if stop >= 6:
                    self.phase_d(l, xres)
                if stop < 7:
                    continue
                if last:
                    out_res = self.y_out
                    out_T = None if self.final_out else self.xT_out
                else:
                    out_res, out_T = self.xres, self.xT
                self.phase_e(l, out_res, out_T)
                xres, xT = self.xres, self.xT
            P.barrier()
        P.finish()
        return nc

    def build_fused(self):
        nc, P, S = self.nc, self.P, self.S
        Th = S // 2
        I = {}
        I["x_full"] = self.din("x_full", [S, D])
        I["ln_in_g"] = self.din("ln_in_g", [D])
        I["ln_in_b"] = self.din("ln_in_b", [D])
        I["flags"] = self.din("flags", [128, 2])
        for k in ("ropeC0", "ropeS0", "ropeC1", "ropeS1"):
            I[k] = self.din(k, [32, S])
        nl = 2
        shapes = dict(w_in=[nl, D, DIN], conv_w=[nl, 3, 256], q_norm_g=[nl, 384], w_uq=[nl, 384, 768],
                      kv_norm_g=[nl, 256], w_ukv=[nl, 256, 1024], lru_conv_w=[nl, 4, 256],
                      lru_conv_b=[nl, 256], lru_wa=[nl, 2, 4, 64, 64], lru_ba=[nl, 2, 256],
                      lru_wi=[nl, 2, 4, 64, 64], lru_bi=[nl, 2, 256], lru_lam=[nl, 2, 256],
                      mix_norm_g=[nl, D], w_out=[nl, D, D], ln1_g=[nl, D], ln1_b=[nl, D],
                      dense_w_gate=[1, D, DFF], dense_w_up=[1, D, DFF], dense_w_down=[1, DFF, D],
                      moe_w_router=[1, D, NE], moe_w_gate=[1, NE, D, DFF], moe_w_up=[1, NE, D, DFF],
                      moe_w_down=[1, NE, DFF, D], ln2_g=[nl, D], ln2_b=[nl, D])
        for k, shp in shapes.items():
            I[k] = self.din(k, shp)
        self.I = I
        self.y_out = self.dout("y_out", [Th, D])
        self.xres = self.dscr("s_xres", [S, D], F32)
        self.xT = self.dscr("s_xT", [D, S], BF16)
        self.QT = self.dscr("s_QT", [NH, 96, S], BF16)
        self.KT = self.dscr("s_KT", [NH, 96, S], BF16)
        self.Vd = self.dscr("s_V", [S, NH * 65], BF16)
        self.CB = self.dscr("s_CB", [256, S], F32)
        self.PP = self.dscr("s_PP", [256, S], F32)
        self.LG = self.dscr("s_LG", [256, S], F32)
        self.LX = self.dscr("s_LX", [256, S], F32)
        self.Y = self.dscr("s_Y", [D, S], F32)
        self.x1res = self.dscr("s_x1res", [S, D], F32)
        self.x1T = self.dscr("s_x1T", [D, S], BF16)
        self.comb = self.dscr("s_comb", [S, NE], F32)
        o0res = self.dscr("s_o0res", [S, D], F32)
        o0T = self.dscr("s_o0T", [D, S], BF16)
        self.wb = {}
        for l in (0, 1):
            self.wb[("w_in", l)] = self.dscr("b_w_in%d" % l, [D, DIN], BF16)
            self.wb[("w_out", l)] = self.dscr("b_w_out%d" % l, [D, D], BF16)
        self.wb["dg"] = self.dscr("b_dg", [7 * 128, 8 * 512], BF16)
        self.wb["du"] = self.dscr("b_du", [7 * 128, 8 * 512], BF16)
        self.wb["dd"] = self.dscr("b_dd", [DFF, D], BF16)
        self.WGt = self.dscr("b_WGt", [NE * 7 * 128, 8 * 512], BF16)
        self.WUt = self.dscr("b_WUt", [NE * 7 * 128, 8 * 512], BF16)
        self.WDt = self.dscr("b_WDt", [NE * 4 * 128, 7 * D], BF16)
        self.X1B = self.dscr("s_X1B", [Th, D], BF16)
        ntile = (2 * Th + NE * 511) // 512
        self.XS = self.dscr("s_XS", [ntile * 512, D], BF16)
        self.YS = self.dscr("s_YS", [ntile * 512, D], F32)
        self.moe_tables = True
        self.layers = [0, 1]
        with ExitStack() as gst:
            self.consts(gst)
            self.cast_weights(part=0)
            import os
            fstop = int(os.environ.get("MK_FUSED_STOP", "99"))
            self.mstop = 99
            steps = []
            def set0():
                self.T, self.NB, self.has_other = S, S // 512, False
                self.ropeC, self.ropeS = I["ropeC0"], I["ropeS0"]
                self.ln_srcs = ((I["x_full"], 0, True),)
            def set1():
                self.T, self.NB, self.has_other = Th, Th // 512, True
            def set1b():
                self.ropeC, self.ropeS = I["ropeC1"], I["ropeS1"]
            steps = [lambda: (set0(), self.phase_ln_in()),
                     lambda: (self.phase_a(0, self.xT), self.cast_weights(part=1)),
                     lambda: self.phase_b(0),
                     lambda: self.phase_c(0),
                     lambda: self.phase_d(0, self.xres),
                     lambda: self.phase_e(0, o0res, o0T),
                     lambda: (set1(), self.phase_sel(o0res, o0T), set1b()),
                     lambda: self.phase_a(1, self.xT),
                     lambda: self.phase_b(1),
                     lambda: self.phase_c(1),
                     lambda: (self.route_setup(gst), self.phase_d(1, self.xres)),
                     lambda: self.phase_r(),
                     lambda: self.phase_e_moe(1, self.y_out)]
            for k, f in enumerate(steps):
                if k < fstop:
                    f()
            P.barrier()
        P.finish()
        return nc

    def phase_sel(self, o0res, o0T):
        P, T, NB = self.P, self.T, self.NB
        P.barrier()
        f0 = self.flags[:, 0:1]
        f1 = self.flags[:, 1:2]
        with ExitStack() as st:
            sb = lambda n, s, d: self.sb(st, n, s, d)
            ra = [sb("ra", [128, D], F32) for _ in range(2)]
            rb = [sb("rb", [128, D], F32) for _ in range(2)]
            dres = P.dep("d_xres")
            for i in range(T // 128):
                a_, da_ = ra[i % 2], P.dep("s_ra%d" % (i % 2))
                b_, db_ = rb[i % 2], P.dep("s_rb%d" % (i % 2))
                P.dma("sp", a_[:], o0res[i * 128:(i + 1) * 128, :], reads=[P.dep("d_outres")], writes=[da_])
                P.dma("sp", b_[:], o0res[T + i * 128:T + (i + 1) * 128, :], reads=[P.dep("d_outres")], writes=[db_])
                P.op("dve", lambda e, a_=a_: e.tensor_scalar(out=a_[:], in0=a_[:], scalar1=f0, scalar2=None, op0=ALU.mult), reads=[da_, self.dconst], writes=[da_])
                P.op("dve", lambda e, a_=a_, b_=b_: e.scalar_tensor_tensor(out=a_[:], in0=b_[:], scalar=f1, in1=a_[:], op0=ALU.mult, op1=ALU.add),
                     reads=[da_, db_, self.dconst], writes=[da_])
                P.dma("sp", self.xres[i * 128:(i + 1) * 128, :], a_[:], reads=[da_], writes=[dres])
            ta = [sb("ta", [128, 8, 512], BF16) for _ in range(2)]
            tb = [sb("tb", [128, 8, 512], BF16) for _ in range(2)]
            to = [sb("to", [128, 8, 512], BF16) for _ in range(2)]
            tt = [sb("tt", [128, 8, 512], BF16) for _ in range(2)]
            ov = o0T.rearrange("(c p) t -> p c t", p=128)
            xv = self.xT.rearrange("(c p) t -> p c t", p=128)
            dxT = P.dep("d_xT")
            for j in range(NB):
                i2 = j % 2
                a_, b_, o_, t_ = ta[i2], tb[i2], to[i2], tt[i2]
                da_, db_, do_, dt_ = (P.dep("s_%s%d" % (n, i2)) for n in ("ta", "tb", "to", "tt"))
                P.dma("sp", a_[:], ov[:, :, j * 512:(j + 1) * 512], reads=[P.dep("d_xT")], writes=[da_])
                P.dma("sp", b_[:], ov[:, :, T + j * 512:T + (j + 1) * 512], reads=[P.dep("d_xT")], writes=[db_])
                P.op("dve", lambda e, o_=o_, a_=a_: e.tensor_scalar(out=o_[:], in0=a_[:], scalar1=f0, scalar2=None, op0=ALU.mult), reads=[da_, self.dconst], writes=[do_])
                P.op("dve", lambda e, o_=o_, b_=b_: e.scalar_tensor_tensor(out=o_[:], in0=b_[:], scalar=f1, in1=o_[:], op0=ALU.mult, op1=ALU.add),
                     reads=[db_, do_, self.dconst], writes=[do_])
                P.op("pool", lambda e, t_=t_, a_=a_: e.tensor_scalar(out=t_[:], in0=a_[:], scalar1=f1, scalar2=None, op0=ALU.mult), reads=[da_, self.dconst], writes=[dt_])
                P.op("dve", lambda e, t_=t_, b_=b_: e.scalar_tensor_tensor(out=t_[:], in0=b_[:], scalar=f0, in1=t_[:], op0=ALU.mult, op1=ALU.add),
                     reads=[db_, dt_, self.dconst], writes=[dt_])
                P.dma("sp", xv[:, :, j * 512:(j + 1) * 512], o_[:], reads=[do_], writes=[dxT])
                P.dma("sp", xv[:, :, T + j * 512:T + (j + 1) * 512], t_[:], reads=[dt_], writes=[dxT])

    def consts(self, st):
        P = self.P
        self.ident = self.sb(st, "ident", [128, 128], BF16)
        self.identf = self.sb(st, "identf", [128, 128], F32)
        self.ones = self.sb(st, "ones", [128, 128], BF16)
        self.eps_ln = self.sb(st, "epsln", [128, 1], F32)
        self.eps_rms = self.sb(st, "epsrms", [128, 1], F32)
        self.flags = self.sb(st, "flags", [128, 2], F32)
        d = P.dep("consts")
        self.dconst = d
        P.op("pool", lambda e: e.memset(self.identf[:], 0.0), writes=[d])
        P.op("pool", lambda e: e.affine_select(out=self.identf[:], in_=self.identf[:], pattern=[[-1, 128]],
                                               compare_op=ALU.not_equal, fill=1.0, base=0,
                                               channel_multiplier=1), reads=[d], writes=[d])
        P.op("dve", lambda e: e.tensor_copy(self.ident[:], self.identf[:]), reads=[d], writes=[d])
        P.op("dve", lambda e: e.memset(self.ones[:], 1.0), writes=[d])
        P.op("dve", lambda e: e.memset(self.eps_ln[:], LN_EPS), writes=[d])
        P.op("dve", lambda e: e.memset(self.eps_rms[:], RMS_EPS), writes=[d])
        P.dma("sp", self.flags[:], self.I["flags"], writes=[d])

    def cast_weights(self, part=None):
        P, I = self.P, self.I
        if not hasattr(self, "dwc"):
            self.dwc = {}
        layers_sel = self.layers if part is None else [part]

        def cast2d(dst, src, rows, cols, key):
            d = P.dep("wc_" + key)
            self.dwc[key] = d
            cstep = cols
            while cstep > 2048:
                cstep //= 2
            rstep = max(1, min(rows, 4096 // (cols // cstep)))
            for r0 in range(0, rows, rstep):
                for c0 in range(0, cols, cstep):
                    P.dma("pool", dst[r0:r0 + rstep, c0:c0 + cstep], src[r0:r0 + rstep, c0:c0 + cstep],
                          writes=[d])
        for l in layers_sel:
            cast2d(self.wb[("w_in", l)], I["w_in"][l], D, DIN, "w_in%d" % l)
            cast2d(self.wb[("w_out", l)], I["w_out"][l], D, D, "w_out%d" % l)
        if 0 in layers_sel and getattr(self, "moe_tables", False):
            for key, tab, src in (("g", self.wb["dg"], I["dense_w_gate"]), ("u", self.wb["du"], I["dense_w_up"])):
                d = P.dep("wc_" + key)
                self.dwc[key] = d
                for g in range(7):
                    P.dma("pool", tab[g * 128:(g + 1) * 128, :].rearrange("p (c n) -> p c n", c=8),
                          src[0][:, g * 512:(g + 1) * 512].rearrange("(c p) n -> p c n", p=128), writes=[d])
        elif 0 in layers_sel:
            cast2d(self.wb["dg"], I["dense_w_gate"][0], D, DFF, "g")
            cast2d(self.wb["du"], I["dense_w_up"][0], D, DFF, "u")
        if 0 in layers_sel:
            cast2d(self.wb["dd"], I["dense_w_down"][0], DFF, D, "d")
        if 1 in layers_sel and getattr(self, "moe_tables", False):
            for key, tab, src in (("mg", self.WGt, I["moe_w_gate"]), ("mu", self.WUt, I["moe_w_up"])):
                d = P.dep("wc_" + key)
                self.dwc[key] = d
                for e in range(NE):
                    for g in range(7):
                        r0 = (e * 7 + g) * 128
                        P.dma("pool", tab[r0:r0 + 128, :].rearrange("p (c n) -> p c n", c=8),
                              src[0, e][:, g * 512:(g + 1) * 512].rearrange("(c p) n -> p c n", p=128), writes=[d])
            d = P.dep("wc_md")
            self.dwc["md"] = d
            for e in range(NE):
                for q in range(4):
                    r0 = (e * 4 + q) * 128
                    P.dma("pool", self.WDt[r0:r0 + 128, :].rearrange("p (f n) -> p f n", f=7),
                          I["moe_w_down"][0, e][q * 896:(q + 1) * 896, :].rearrange("(f p) n -> p f n", p=128), writes=[d])
        elif 1 in layers_sel:
            for e in range(NE):
                cast2d(self.wb["mg"][e], I["moe_w_gate"][0, e], D, DFF, "mg")
                cast2d(self.wb["mu"][e], I["moe_w_up"][0, e], D, DFF, "mu")
                cast2d(self.wb["md"][e], I["moe_w_down"][0, e], DFF, D, "md")

    def ln_setup(self, st, g_ap, b_ap, tag, nbuf=2):
        P = self.P
        L = {}
        L["G"] = self.sb(st, "lnG", [128, D], F32)
        L["B"] = self.sb(st, "lnB", [128, D], F32)
        L["dgb"] = P.dep("lnGB")
        P.dma("sp", L["G"][:], g_ap.partition_broadcast(128), writes=[L["dgb"]])
        P.dma("sp", L["B"][:], b_ap.partition_broadcast(128), writes=[L["dgb"]])
        L["n"] = 0
        L["nbuf"] = nbuf
        for i in range(nbuf):
            L["st%d" % i] = self.sb(st, "lnst", [128, 2, 6], F32)
            L["mv%d" % i] = self.sb(st, "lnmv", [128, 2], F32)
            L["sd%d" % i] = self.sb(st, "lnsd", [128, 1], F32)
            L["rs%d" % i] = self.sb(st, "lnrs", [128, 1], F32)
            L["xn%d" % i] = self.sb(st, "lnxn", [128, D], F32)
            L["xo%d" % i] = self.sb(st, "lnxo", [128, D], F32)
            L["xb%d" % i] = self.sb(st, "lnxb", [128, D], BF16)
            L["pT%d" % i] = self.psum(st, "lnpT", [128, D], BF16)
        return L

    def ln_tile(self, L, xt, dxt, xTs, dxTs, sub, res_dst=None, dres=None, xb_dst=None, dxbd=None):
        P = self.P
        i = L["n"] % L["nbuf"]
        L["n"] += 1
        tg = "ln%d" % i
        st_, mv, sd, rs, xn, xo, xb, pT = (L[k + str(i)] for k in ("st", "mv", "sd", "rs", "xn", "xo", "xb", "pT"))
        dst_, dmv, dsd, drs, dxn, dxo, dxb, dpT = (P.dep(tg + k) for k in ("st", "mv", "sd", "rs", "xn", "xo", "xb", "pT"))
        for h in range(2):
            P.op("dve", lambda e, h=h: e.bn_stats(out=st_[:, h, :], in_=xt[:, h * 512:(h + 1) * 512]),
                 reads=[dxt], writes=[dst_])
        if LNSTOP < 1:
            return
        P.op("dve", lambda e: e.bn_aggr(out=mv[:], in_=st_[:].rearrange("p a b -> p (a b)")), reads=[dst_], writes=[dmv])
        if LNSTOP < 2:
            return
        P.op("act", lambda e: e.activation(out=sd[:], in_=mv[:, 1:2], func=AF.Sqrt, bias=self.eps_ln[:], scale=1.0),
             reads=[dmv, self.dconst], writes=[dsd])
        if LNSTOP < 3:
            return
        P.op("dve", lambda e: e.reciprocal(out=rs[:], in_=sd[:]), reads=[dsd], writes=[drs])
        if LNSTOP < 4:
            return
        P.op("dve", lambda e: e.tensor_scalar(out=xn[:], in0=xt[:], scalar1=mv[:, 0:1], scalar2=rs[:],
                                               op0=ALU.subtract, op1=ALU.mult), reads=[dxt, dmv, drs], writes=[dxn])
        if LNSTOP < 5:
            return
        P.op("pool", lambda e: e.tensor_tensor(out=xn[:], in0=xn[:], in1=L["G"][:], op=ALU.mult),
             reads=[dxn, L["dgb"]], writes=[dxn])
        P.op("pool", lambda e: e.tensor_tensor(out=xo[:], in0=xn[:], in1=L["B"][:], op=ALU.add),
             reads=[dxn, L["dgb"]], writes=[dxo])
        if LNSTOP < 6:
            return
        if res_dst is not None:
            P.dma("sp", res_dst, xo[:], reads=[dxo], writes=[dres])
        if LNSTOP < 7:
            return
        if xTs is None:
            return
        P.op("act", lambda e: e.activation(out=xb[:], in_=xo[:], func=AF.Copy), reads=[dxo], writes=[dxb])
        if xb_dst is not None:
            P.dma("sp", xb_dst, xb[:], reads=[dxb], writes=[dxbd])
        if LNSTOP < 8:
            return
        for c in range(8):
            P.op("pe", lambda e, c=c: e.transpose(pT[:, c * 128:(c + 1) * 128], xb[:, c * 128:(c + 1) * 128], self.ident[:]),
                 reads=[dxb, self.dconst], writes=[dpT], signal=(c == 7))
        if LNSTOP < 9:
            return
        P.op("act", lambda e: e.activation(out=xTs[:, :, sub * 128:(sub + 1) * 128],
                                           in_=pT[:].rearrange("p (c t) -> p c t", c=8), func=AF.Copy),
             reads=[dpT], writes=[dxTs])

    def phase_ln_in(self):
        P, T, NB, I = self.P, self.T, self.NB, self.I
        P.barrier()
        with ExitStack() as st:
            L = self.ln_setup(st, I["ln_in_g"], I["ln_in_b"], "in")
            xt = [self.sb(st, "xt", [128, D], F32) for _ in range(2)]
            xTs = [self.sb(st, "xTs", [128, 8, 512], BF16) for _ in range(2)]
            dres = P.dep("d_xres")
            dxT = P.dep("d_xT")
            k = 0
            for src, base, own in self.ln_srcs:
                for b in range(NB):
                    xs, dxs = xTs[b % 2], P.dep("xTs%d" % (b % 2))
                    for sub in range(4):
                        r0 = b * 512 + sub * 128
                        xtt, dx = xt[k % 2], P.dep("xt%d" % (k % 2))
                        k += 1
                        P.dma("sp", xtt[:], src[r0:r0 + 128, :], writes=[dx])
                        self.ln_tile(L, xtt, dx, xs, dxs, sub,
                                     res_dst=self.xres[r0:r0 + 128, :] if own else None, dres=dres)
                    P.dma("sp", self.xT.rearrange("(c p) t -> p c t", p=128)[:, :, base + b * 512: base + (b + 1) * 512],
                          xs[:], reads=[dxs], writes=[dxT])

    def rms_rstd(self, sq, dsq, nchunk, n, pst, dpst, rstd, drstd, sdt, dsdt):
        P = self.P
        for c in range(nchunk):
            P.op("pe", lambda e, c=c: e.matmul(pst[:], lhsT=self.ones[:], rhs=sq[:, c, :], start=(c == 0), stop=(c == nchunk - 1)),
                 reads=[dsq, self.dconst], writes=[dpst], signal=(c == nchunk - 1))
        P.op("act", lambda e: e.activation(out=sdt[:], in_=pst[:], func=AF.Sqrt, bias=self.eps_rms[:], scale=1.0 / n),
             reads=[dpst, self.dconst], writes=[dsdt])
        P.op("dve", lambda e: e.reciprocal(out=rstd[:], in_=sdt[:]), reads=[dsdt], writes=[drstd])

    def phase_a(self, l, xT):
        P, T, NB, I = self.P, self.T, self.NB, self.I
        NBT = (2 * NB) if self.has_other else NB
        P.barrier()
        with ExitStack() as st:
            sb = lambda n, s, d: self.sb(st, n, s, d)
            Win = sb("Win", [128, 8, DIN], BF16)
            dW = P.dep("a_W")
            wv = self.wb[("w_in", l)].rearrange("(c p) n -> p c n", p=128)
            for c in range(8):
                P.dma("sp", Win[:, c, :], wv[:, c, :], reads=[self.dwc["w_in%d" % l]], writes=[dW])
            Wkr = sb("Wkr", [128, 8, 96], BF16)
            Wkrs = sb("Wkrs", [128, 8, 96], BF16)
            dW2 = P.dep("a_W2")
            P.op("dve", lambda e: e.memset(Wkr[:], 0.0), writes=[dW2])
            P.op("dve", lambda e: e.memset(Wkrs[:], 0.0), writes=[dW2])
            P.op("dve", lambda e: e.tensor_copy(Wkr[:, :, 64:96], Win[:, :, 1408:1440]), reads=[dW], writes=[dW2])
            P.op("dve", lambda e: e.tensor_scalar(out=Wkrs[:, :, 64:80], in0=Win[:, :, 1424:1440], scalar1=-1.0, scalar2=None,
                                                   op0=ALU.mult), reads=[dW], writes=[dW2])
            P.op("dve", lambda e: e.tensor_copy(Wkrs[:, :, 80:96], Win[:, :, 1408:1424]), reads=[dW], writes=[dW2])
            gq, dgq = self.load_cols(st, "gq", I["q_norm_g"][l], 384)
            gkv, dgkv = self.load_cols(st, "gkv", I["kv_norm_g"][l], 256)
            wqf = sb("wqf", [128, 3, 768], F32)
            dwqf = P.dep("a_wqf")
            P.dma("sp", wqf[:], I["w_uq"][l].rearrange("(c p) n -> p c n", p=128), writes=[dwqf])
            Wq = sb("Wq", [128, 3, 768], BF16)
            Wqs = sb("Wqs", [128, 3, 768], BF16)
            dWq = P.dep("a_Wq")
            P.op("pool", lambda e: e.memset(Wqs[:], 0.0), writes=[dWq])
            for c in range(3):
                P.op("dve", lambda e, c=c: e.tensor_scalar(out=Wq[:, c, :], in0=wqf[:, c, :], scalar1=gq[:, c:c + 1],
                                                            scalar2=QSCALE, op0=ALU.mult, op1=ALU.mult),
                     reads=[dwqf, dgq], writes=[dWq])
                wq4 = Wq[:, c, :].rearrange("p (h r) -> p h r", h=NH)
                ws4 = Wqs[:, c, :].rearrange("p (h r) -> p h r", h=NH)
                P.op("dve", lambda e, wq4=wq4, ws4=ws4: e.tensor_scalar(out=ws4[:, :, 64:80], in0=wq4[:, :, 80:96], scalar1=-1.0,
                                                                        scalar2=None, op0=ALU.mult), reads=[dWq], writes=[dWq])
                P.op("dve", lambda e, wq4=wq4, ws4=ws4: e.tensor_copy(ws4[:, :, 80:96], wq4[:, :, 64:80]), reads=[dWq], writes=[dWq])
            wkvf = sb("wkvf", [128, 2, 1024], F32)
            dwkvf = P.dep("a_wkvf")
            P.dma("sp", wkvf[:], I["w_ukv"][l].rearrange("(c p) n -> p c n", p=128), writes=[dwkvf])
            Wkn = sb("Wkn", [128, 2, 512], BF16)
            Wv = sb("Wv", [128, 2, 512], BF16)
            dWkv = P.dep("a_Wkv")
            for c in range(2):
                w4 = wkvf[:, c, :].rearrange("p (h r) -> p h r", h=NH)
                P.op("dve", lambda e, c=c, w4=w4: e.tensor_scalar(out=Wkn[:, c, :].rearrange("p (h r) -> p h r", h=NH), in0=w4[:, :, 0:64],
                                                                  scalar1=gkv[:, c:c + 1], scalar2=None, op0=ALU.mult),
                     reads=[dwkvf, dgkv], writes=[dWkv])
                P.op("dve", lambda e, c=c, w4=w4: e.tensor_scalar(out=Wv[:, c, :].rearrange("p (h r) -> p h r", h=NH), in0=w4[:, :, 64:128],
                                                                  scalar1=gkv[:, c:c + 1], scalar2=None, op0=ALU.mult),
                     reads=[dwkvf, dgkv], writes=[dWkv])
            NPS = 8
            ps = [self.psum(st, "aps", [128, 512], F32) for _ in range(NPS)]
            dps = [P.dep("a_ps%d" % i) for i in range(NPS)]

            def nps():
                i = self.ps_rr % NPS
                self.ps_rr += 1
                return ps[i], dps[i]
            xTb = [sb("xTb", [128, 8, 512], BF16) for _ in range(2)]
            NSTG = 6
            stg = [sb("stg", [128, 512], F32) for _ in range(NSTG)]
            self.stg_rr = 0

            def nstg():
                i = self.stg_rr % NSTG
                self.stg_rr += 1
                return stg[i], P.dep("a_stg%d" % i)
            cctmp = [sb("cctmp", [128, 512], F32) for _ in range(2)]
            cq = sb("cq", [128, 3, 512], F32)
            sq = sb("sq", [128, 3, 512], BF16)
            cqn = sb("cqn", [128, 3, 512], BF16)
            ckv = sb("ckv", [128, 2, 512], F32)
            sk = sb("sk", [128, 2, 512], BF16)
            ckvn = sb("ckvn", [128, 2, 512], BF16)
            rstd = sb("rstd", [128, 512], F32)
            sdt = sb("sdt", [128, 512], F32)
            rstd2 = sb("rstd2", [128, 512], F32)
            sdt2 = sb("sdt2", [128, 512], F32)
            ropeC = [sb("ropeC", [128, 512], F32) for _ in range(2)]
            ropeS = [sb("ropeS", [128, 512], F32) for _ in range(2)]
            t1 = [sb("t1", [128, 512], F32) for _ in range(2)]
            t2 = [sb("t2", [128, 512], F32) for _ in range(2)]
            QTs = [sb("QTs", [128, 512], BF16) for _ in range(3)]
            KTs = [sb("KTs", [128, 512], BF16) for _ in range(3)]
            kro = sb("kro", [128, 512], BF16)
            Vs = [sb("Vs", [128, NH, 65], BF16) for _ in range(2)]
            dVs = [P.dep("a_Vs%d" % i) for i in range(2)]
            for i in range(2):
                P.op("pool", lambda e, i=i: e.memset(Vs[i][:], 1.0), writes=[dVs[i]])
            dcq, dsq, dcqn, dckv, dsk, dckvn = (P.dep("a_" + n) for n in ("cq", "sq", "cqn", "ckv", "sk", "ckvn"))
            drstd, dsdt, drstd2, dsdt2, dkro = (P.dep("a_" + n) for n in ("rstd", "sdt", "rstd2", "sdt2", "kro"))
            dQT, dKT, dV, dCB, dPP, dLG, dLX = (P.dep("d_" + n) for n in ("QT", "KT", "V", "CB", "PP", "LG", "LX"))
            xTv = xT.rearrange("(c p) t -> p c t", p=128)
            nq = 0

            def grp(xb, dxb, col0, m, lhs=None, dl=None):
                pt, dpt = nps()
                for c in range(8):
                    if lhs is None:
                        l_ap, dd = Win[:, c, col0:col0 + m], dW
                    else:
                        l_ap, dd = lhs[:, c, :], dl
                    P.op("pe", lambda e, c=c, l_ap=l_ap, pt=pt: e.matmul(pt[0:m, :], lhsT=l_ap, rhs=xb[:, c, :], start=(c == 0), stop=(c == 7)),
                         reads=[dd, dxb], writes=[dpt], signal=(c == 7))
                return pt, dpt

            def store_fm(dst, ddst, row0, col0, pt, dpt, func=AF.Copy):
                s, ds_ = nstg()
                P.op("act", lambda e: e.activation(out=s[:], in_=pt[:], func=func), reads=[dpt], writes=[ds_])
                P.dma("sp", dst[row0:row0 + 128, col0:col0 + 512], s[:], reads=[ds_], writes=[ddst])

            if ASTOP < 1:
                return
            for blk in range(NBT):
                own = blk < NB
                t0 = blk * 512
                xb, dxb = xTb[blk % 2], P.dep("a_xTb%d" % (blk % 2))
                P.dma("sp", xb[:], xTv[:, :, t0:t0 + 512], reads=[P.dep("d_xT")], writes=[dxb])
                rc, rs_, drope = ropeC[blk % 2], ropeS[blk % 2], P.dep("a_rope%d" % (blk % 2))
                P.dma("sp", rc[64:96, :], self.ropeC[:, t0:t0 + 512], writes=[drope])
                P.dma("sp", rs_[64:96, :], self.ropeS[:, t0:t0 + 512], writes=[drope])
                edge = (not own) and (blk == NB or blk == 2 * NB - 1)
                if own or edge:
                    for c in range(2):
                        pcc, dpcc = grp(xb, dxb, 256 + c * 128, 128)
                        pch, dpch = grp(xb, dxb, 512 + c * 128, 128)
                        ct, dct = cctmp[c], P.dep("a_cct%d" % c)
                        P.op("act", lambda e, ct=ct, pcc=pcc: e.activation(out=ct[:], in_=pcc[:], func=AF.Copy), reads=[dpcc], writes=[dct])
                        s, ds_ = nstg()
                        P.op("dve", lambda e, s=s, pch=pch, ct=ct: e.tensor_tensor(out=s[:], in0=pch[:], in1=ct[:], op=ALU.mult),
                             reads=[dpch, dct], writes=[ds_])
                        P.dma("sp", self.PP[c * 128:(c + 1) * 128, t0:t0 + 512], s[:], reads=[ds_], writes=[dPP])
                if ASTOP < 2:
                    continue
                if own:
                    for c in range(2):
                        pt, dpt = grp(xb, dxb, c * 128, 128)
                        store_fm(self.CB, dCB, c * 128, t0, pt, dpt)
                        pt, dpt = grp(xb, dxb, 1440 + c * 128, 128)
                        store_fm(self.LG, dLG, c * 128, t0, pt, dpt, func=AF.Gelu_apprx_tanh)
                    if ASTOP < 3:
                        continue
                    for c in range(3):
                        pt, dpt = grp(xb, dxb, 768 + c * 128, 128)
                        P.op("act", lambda e, c=c, pt=pt: e.activation(out=sq[:, c, :], in_=pt[:], func=AF.Square), reads=[dpt], writes=[dsq])
                        P.op("dve", lambda e, c=c, pt=pt: e.tensor_copy(cq[:, c, :], pt[:]), reads=[dpt], writes=[dcq])
                    pst, dpst = nps()
                    self.rms_rstd(sq, dsq, 3, 384.0, pst, dpst, rstd, drstd, sdt, dsdt)
                    for c in range(3):
                        P.op("dve", lambda e, c=c: e.tensor_tensor(out=cqn[:, c, :], in0=cq[:, c, :], in1=rstd[:], op=ALU.mult),
                             reads=[dcq, drstd], writes=[dcqn])
                    for h in range(NH):
                        pa, dpa = nps()
                        pb, dpb = nps()
                        for c in range(3):
                            P.op("pe", lambda e, c=c, h=h, pa=pa: e.matmul(pa[0:96, :], lhsT=Wq[:, c, h * 96:(h + 1) * 96], rhs=cqn[:, c, :],
                                                                      start=(c == 0), stop=(c == 2)), reads=[dWq, dcqn], writes=[dpa], signal=(c == 2))
                        for c in range(3):
                            P.op("pe", lambda e, c=c, h=h, pb=pb: e.matmul(pb[0:96, :], lhsT=Wqs[:, c, h * 96:(h + 1) * 96], rhs=cqn[:, c, :],
                                                                      start=(c == 0), stop=(c == 2)), reads=[dWq, dcqn], writes=[dpb], signal=(c == 2))
                        qs, dqs = QTs[nq % 3], P.dep("a_QTs%d" % (nq % 3))
                        ta, dta = t1[nq % 2], P.dep("a_t1%d" % (nq % 2))
                        tb, dtb = t2[nq % 2], P.dep("a_t2%d" % (nq % 2))
                        nq += 1
                        P.op("act", lambda e, qs=qs, pa=pa: e.activation(out=qs[0:64, :], in_=pa[0:64, :], func=AF.Copy), reads=[dpa], writes=[dqs])
                        P.op("dve", lambda e, ta=ta, pa=pa: e.tensor_tensor(out=ta[64:96, :], in0=pa[64:96, :], in1=rc[64:96, :], op=ALU.mult),
                             reads=[dpa, drope], writes=[dta])
                        P.op("dve", lambda e, tb=tb, pb=pb: e.tensor_tensor(out=tb[64:96, :], in0=pb[64:96, :], in1=rs_[64:96, :], op=ALU.mult),
                             reads=[dpb, drope], writes=[dtb])
                        P.op("dve", lambda e, qs=qs, ta=ta, tb=tb: e.tensor_tensor(out=qs[64:96, :], in0=ta[64:96, :], in1=tb[64:96, :], op=ALU.add),
                             reads=[dta, dtb], writes=[dqs])
                        P.dma("sp", self.QT[h, :, t0:t0 + 512], qs[0:96, :], reads=[dqs], writes=[dQT])
                if ASTOP < 4:
                    continue
                for c in range(2):
                    pt, dpt = grp(xb, dxb, 1696 + c * 128, 128)
                    store_fm(self.LX, dLX, c * 128, t0, pt, dpt)
                for c in range(2):
                    pt, dpt = grp(xb, dxb, 1152 + c * 128, 128)
                    P.op("act", lambda e, c=c, pt=pt: e.activation(out=sk[:, c, :], in_=pt[:], func=AF.Square), reads=[dpt], writes=[dsk])
                    P.op("dve", lambda e, c=c, pt=pt: e.tensor_copy(ckv[:, c, :], pt[:]), reads=[dpt], writes=[dckv])
                pst, dpst = nps()
                self.rms_rstd(sk, dsk, 2, 256.0, pst, dpst, rstd2, drstd2, sdt2, dsdt2)
                for c in range(2):
                    P.op("dve", lambda e, c=c: e.tensor_tensor(out=ckvn[:, c, :], in0=ckv[:, c, :], in1=rstd2[:], op=ALU.mult),
                         reads=[dckv, drstd2], writes=[dckvn])
                if ASTOP < 5:
                    continue
                pa, dpa = grp(xb, dxb, 0, 96, lhs=Wkr, dl=dW2)
                pb, dpb = grp(xb, dxb, 0, 96, lhs=Wkrs, dl=dW2)
                ta, dta = t1[nq % 2], P.dep("a_t1%d" % (nq % 2))
                tb, dtb = t2[nq % 2], P.dep("a_t2%d" % (nq % 2))
                nq += 1
                P.op("dve", lambda e, ta=ta, pa=pa: e.tensor_tensor(out=ta[64:96, :], in0=pa[64:96, :], in1=rc[64:96, :], op=ALU.mult),
                     reads=[dpa, drope], writes=[dta])
                P.op("dve", lambda e, tb=tb, pb=pb: e.tensor_tensor(out=tb[64:96, :], in0=pb[64:96, :], in1=rs_[64:96, :], op=ALU.mult),
                     reads=[dpb, drope], writes=[dtb])
                P.op("dve", lambda e, ta=ta, tb=tb: e.tensor_tensor(out=kro[64:96, :], in0=ta[64:96, :], in1=tb[64:96, :], op=ALU.add),
                     reads=[dta, dtb], writes=[dkro])
                if ASTOP < 6:
                    continue
                for h in range(NH):
                    pk, dpk = nps()
                    for c in range(2):
                        P.op("pe", lambda e, c=c, h=h, pk=pk: e.matmul(pk[0:64, :], lhsT=Wkn[:, c, h * 64:(h + 1) * 64], rhs=ckvn[:, c, :],
                                                                  start=(c == 0), stop=(c == 1)), reads=[dWkv, dckvn], writes=[dpk], signal=(c == 1))
                    ks, dks = KTs[h % 3], P.dep("a_KTs%d" % (h % 3))
                    P.op("act", lambda e, ks=ks, pk=pk: e.activation(out=ks[0:64, :], in_=pk[0:64, :], func=AF.Copy), reads=[dpk], writes=[dks])
                    P.dma("sp", self.KT[h, 0:64, t0:t0 + 512], ks[0:64, :], reads=[dks], writes=[dKT])
                    P.dma("sp", self.KT[h, 64:96, t0:t0 + 512], kro[64:96, :], reads=[dkro], writes=[dKT])
                if ASTOP < 7:
                    continue
                for sub in range(4):
                    pv, dpv = nps()
                    for c in range(2):
                        P.op("pe", lambda e, c=c, sub=sub, pv=pv: e.matmul(pv[:], lhsT=ckvn[:, c, sub * 128:(sub + 1) * 128], rhs=Wv[:, c, :],
                                                                      start=(c == 0), stop=(c == 1)), reads=[dWkv, dckvn], writes=[dpv], signal=(c == 1))
                    vs, dvs = Vs[sub % 2], dVs[sub % 2]
                    P.op("act", lambda e, vs=vs, pv=pv: e.activation(out=vs[:, :, 0:64], in_=pv[:].rearrange("p (h r) -> p h r", h=NH), func=AF.Copy),
                         reads=[dpv], writes=[dvs])
                    P.dma("sp", self.Vd[t0 + sub * 128:t0 + (sub + 1) * 128, :], vs[:].rearrange("p h r -> p (h r)"), reads=[dvs], writes=[dV])

    def phase_b(self, l):
        P, T, NB, I = self.P, self.T, self.NB, self.I
        P.barrier()
        dY = P.dep("d_Y")
        f0 = self.flags[:, 0:1]
        f1 = self.flags[:, 1:2]
        with ExitStack() as st:
            sb = lambda n, s, d: self.sb(st, n, s, d)
            cw = sb("cw", [128, 2, 3], F32)
            dcw = P.dep("b_cw")
            cv_t = {"pp": sb("pp", [128, T + 2], F32), "cb": sb("cb", [128, T], F32), "acc": sb("acc", [128, T], F32)}
            for k in range(3):
                for c in range(2):
                    P.dma("sp", cw[:, c, k:k + 1], I["conv_w"][l, k, c * 128:(c + 1) * 128].rearrange("(p o) -> p o", o=1), writes=[dcw])
            for c in range(2):
                pp = cv_t["pp"]
                cb = cv_t["cb"]
                acc = cv_t["acc"]
                edge = sb("edge", [128, 2], F32)
                dpp, dcb, dacc = (P.dep("b_%s" % n) for n in ("pp", "cb", "acc"))
                dedge = P.dep("b_edge%d" % c)
                rows = slice(c * 128, (c + 1) * 128)
                P.dma("sp", pp[:, 1:T + 1], self.PP[rows, 0:T], reads=[P.dep("d_PP")], writes=[dpp])
                P.dma("sp", cb[:], self.CB[rows, 0:T], reads=[P.dep("d_CB")], writes=[dcb])
                if self.has_other:
                    P.dma("sp", edge[:, 0:1], self.PP[rows, 2 * T - 1:2 * T], reads=[P.dep("d_PP")], writes=[dedge], allow_slow_non_contiguous=True)
                    P.dma("sp", edge[:, 1:2], self.PP[rows, T:T + 1], reads=[P.dep("d_PP")], writes=[dedge], allow_slow_non_contiguous=True)
                else:
                    P.op("dve", lambda e, edge=edge: e.memset(edge[:], 0.0), writes=[dedge])
                P.op("dve", lambda e, pp=pp, edge=edge: e.tensor_tensor(out=pp[:, 0:1], in0=edge[:, 0:1], in1=f1, op=ALU.mult),
                     reads=[dedge, self.dconst, dpp], writes=[dpp])
                P.op("dve", lambda e, pp=pp, edge=edge: e.tensor_tensor(out=pp[:, T + 1:T + 2], in0=edge[:, 1:2], in1=f0, op=ALU.mult),
                     reads=[dedge, self.dconst, dpp], writes=[dpp])
                P.op("dve", lambda e, acc=acc, pp=pp, c=c: e.tensor_scalar(out=acc[:], in0=pp[:, 0:T], scalar1=cw[:, c, 0:1], scalar2=None, op0=ALU.mult),
                     reads=[dpp, dcw], writes=[dacc])
                for k in (1, 2):
                    P.op("dve", lambda e, acc=acc, pp=pp, c=c, k=k: e.scalar_tensor_tensor(out=acc[:], in0=pp[:, k:k + T], scalar=cw[:, c, k:k + 1], in1=acc[:],
                                                                                     op0=ALU.mult, op1=ALU.add), reads=[dpp, dcw, dacc], writes=[dacc])
                P.op("pool", lambda e, acc=acc, cb=cb: e.tensor_tensor(out=acc[:], in0=acc[:], in1=cb[:], op=ALU.mult), reads=[dacc, dcb], writes=[dacc])
                P.dma("sp", self.Y[rows, 0:T], acc[:], reads=[dacc], writes=[dY])
        P.barrier()
        with ExitStack() as st:
            sb = lambda n, s, d: self.sb(st, n, s, d)
            prm = sb("prm", [128, 2, 16], F32)
            dprm = P.dep("b_prm")

            def col(dst_col, vec):
                for c in range(2):
                    P.dma("sp", prm[:, c, dst_col:dst_col + 1], vec[c * 128:(c + 1) * 128].rearrange("(p o) -> p o", o=1), writes=[dprm])
            for k in range(4):
                col(k, I["lru_conv_w"][l, k])
            col(4, I["lru_conv_b"][l])
            for d_ in range(2):
                col(5 + d_, I["lru_ba"][l, d_])
                col(7 + d_, I["lru_bi"][l, d_])
                col(9 + d_, I["lru_lam"][l, d_])
            sc = sb("sc", [128, 2, 4], F32)
            dsc = P.dep("b_sc")
            for c in range(2):
                P.op("act", lambda e, c=c: e.activation(out=sc[:, c, 0:2], in_=prm[:, c, 9:11], func=AF.Exp, scale=-1.0), reads=[dprm], writes=[dsc])
                P.op("act", lambda e, c=c: e.activation(out=sc[:, c, 0:2], in_=sc[:, c, 0:2], func=AF.Ln, bias=1.0, scale=1.0), reads=[dsc], writes=[dsc])
                P.op("dve", lambda e, c=c: e.tensor_scalar(out=sc[:, c, 2:4], in0=sc[:, c, 0:2], scalar1=-16.0, scalar2=None, op0=ALU.mult), reads=[dsc], writes=[dsc])
                P.op("dve", lambda e, c=c: e.tensor_scalar(out=sc[:, c, 0:2], in0=sc[:, c, 0:2], scalar1=-8.0, scalar2=None, op0=ALU.mult), reads=[dsc], writes=[dsc])
            wgf = sb("wgf", [128, 2, 4, 128], F32)
            wg = sb("wg", [128, 2, 4, 128], BF16)
            dwg = P.dep("b_wg")
            P.op("pool", lambda e: e.memset(wgf[:], 0.0), writes=[dwg])
            for c in range(2):
                for d_ in range(2):
                    for gi, key in enumerate(("lru_wa", "lru_wi")):
                        for bb in range(2):
                            P.dma("sp", wgf[bb * 64:(bb + 1) * 64, c, 2 * d_ + gi, bb * 64:(bb + 1) * 64], I[key][l, d_, 2 * c + bb], writes=[dwg])
            P.op("dve", lambda e: e.tensor_copy(wg[:], wgf[:]), reads=[dwg], writes=[dwg])
            ps = [self.psum(st, "bps", [128, 512], F32) for _ in range(4)]
            dps = [P.dep("b_ps%d" % i) for i in range(4)]
            HO = self.has_other
            lxo = sb("lxo", [128, T + 3], F32)
            lxt = sb("lxt", [128, T + 3], F32) if HO else None
            big = {}
            for nm in (("o", "t") if HO else ("o",)):
                big["xc" + nm] = sb("xc" + nm, [128, T], F32)
            xcbb = [sb("xcbb", [128, 512], BF16) for _ in range(2)]
            a_ = sb("a_", [128, T], F32)
            u_ = sb("u_", [128, T], F32)
            hsum = sb("hsum", [128, T], F32)
            hb = sb("hb", [128, T], F32) if HO else lxo[:, 0:T]
            carry = sb("carry", [128, 2], F32)
            tmp = [sb("tmp", [128, 512], F32) for _ in range(2)]
            tmpi = [sb("tmpi", [128, 512], F32) for _ in range(2)]
            for c in range(2):
                rows = slice(c * 128, (c + 1) * 128)
                dlx = P.dep("b_lx")
                P.dma("sp", lxo[:, 1:T + 1], self.LX[rows, 0:T], reads=[P.dep("d_LX")], writes=[dlx])
                if HO:
                    P.dma("sp", lxt[:, 1:T + 1], self.LX[rows, T:2 * T], reads=[P.dep("d_LX")], writes=[dlx])
                    P.op("dve", lambda e, lxo=lxo, lxt=lxt: e.tensor_scalar(out=lxo[:, 0:1], in0=lxt[:, T:T + 1], scalar1=f1, scalar2=None, op0=ALU.mult), reads=[dlx, self.dconst], writes=[dlx])
                    P.op("dve", lambda e, lxo=lxo, lxt=lxt: e.tensor_scalar(out=lxo[:, T + 1:T + 3], in0=lxt[:, 1:3], scalar1=f0, scalar2=None, op0=ALU.mult), reads=[dlx, self.dconst], writes=[dlx])
                    P.op("dve", lambda e, lxo=lxo, lxt=lxt: e.tensor_scalar(out=lxt[:, 0:1], in0=lxo[:, T:T + 1], scalar1=f0, scalar2=None, op0=ALU.mult), reads=[dlx, self.dconst], writes=[dlx])
                    P.op("dve", lambda e, lxo=lxo, lxt=lxt: e.tensor_scalar(out=lxt[:, T + 1:T + 3], in0=lxo[:, 1:3], scalar1=f1, scalar2=None, op0=ALU.mult), reads=[dlx, self.dconst], writes=[dlx])
                else:
                    P.op("dve", lambda e, lxo=lxo: e.memset(lxo[:, 0:1], 0.0), reads=[dlx], writes=[dlx])
                    P.op("dve", lambda e, lxo=lxo: e.memset(lxo[:, T + 1:T + 3], 0.0), reads=[dlx], writes=[dlx])
                xc = {}
                xcb = {}
                dxc = P.dep("b_xc")
                for nm, src in ((("o", lxo), ("t", lxt)) if HO else (("o", lxo),)):
                    x_ = big["xc" + nm]
                    P.op("dve", lambda e, x_=x_, src=src: e.tensor_scalar(out=x_[:], in0=src[:, 0:T], scalar1=prm[:, c, 0:1], scalar2=prm[:, c, 4:5],
                                                                       op0=ALU.mult, op1=ALU.add), reads=[dlx, dprm], writes=[dxc])
                    for k in (1, 2, 3):
                        P.op("dve", lambda e, x_=x_, src=src, k=k: e.scalar_tensor_tensor(out=x_[:], in0=src[:, k:k + T], scalar=prm[:, c, k:k + 1], in1=x_[:],
                                                                                       op0=ALU.mult, op1=ALU.add), reads=[dlx, dprm, dxc], writes=[dxc])
                    xc[nm] = x_
                da, du, dh, dhb, dcar = (P.dep("b_%s" % n) for n in ("a", "u", "h", "hb", "car"))

                def gates(nm, d_):
                    for b in range(T // 512):
                        cs = slice(b * 512, (b + 1) * 512)
                        pa, dpa = ps[(2 * b) % 4], dps[(2 * b) % 4]
                        pi, dpi = ps[(2 * b + 1) % 4], dps[(2 * b + 1) % 4]
                        xq, dxq = xcbb[b % 2], P.dep("b_xcbb%d" % (b % 2))
                        P.op("pool", lambda e, xq=xq, cs=cs: e.tensor_copy(xq[:], xc[nm][:, cs]), reads=[dxc], writes=[dxq])
                        P.op("pe", lambda e, pa=pa, xq=xq: e.matmul(pa[:], lhsT=wg[:, c, 2 * d_, :], rhs=xq[:], start=True, stop=True),
                             reads=[dwg, dxq], writes=[dpa])
                        P.op("pe", lambda e, pi=pi, xq=xq: e.matmul(pi[:], lhsT=wg[:, c, 2 * d_ + 1, :], rhs=xq[:], start=True, stop=True),
                             reads=[dwg, dxq], writes=[dpi])
                        tr, dtr = tmp[b % 2], P.dep("b_tmp%d" % (b % 2))
                        ti, dti = tmpi[b % 2], P.dep("b_tmpi%d" % (b % 2))
                        P.op("act", lambda e, tr=tr, pa=pa: e.activation(out=tr[:], in_=pa[:], func=AF.Sigmoid, bias=prm[:, c, 5 + d_:6 + d_], scale=1.0),
                             reads=[dpa, dprm], writes=[dtr])
                        P.op("act", lambda e, ti=ti, pi=pi: e.activation(out=ti[:], in_=pi[:], func=AF.Sigmoid, bias=prm[:, c, 7 + d_:8 + d_], scale=1.0),
                             reads=[dpi, dprm], writes=[dti])
                        P.op("act", lambda e, tr=tr, cs=cs: e.activation(out=a_[:, cs], in_=tr[:], func=AF.Exp, scale=sc[:, c, d_:d_ + 1]),
                             reads=[dtr, dsc], writes=[da])
                        P.op("act", lambda e, tr=tr: e.activation(out=tr[:], in_=tr[:], func=AF.Exp, scale=sc[:, c, 2 + d_:3 + d_]),
                             reads=[dtr, dsc], writes=[dtr])
                        P.op("dve", lambda e, tr=tr: e.tensor_scalar(out=tr[:], in0=tr[:], scalar1=-1.0, scalar2=1.0, op0=ALU.mult, op1=ALU.add),
                             reads=[dtr], writes=[dtr])
                        P.op("act", lambda e, tr=tr: e.activation(out=tr[:], in_=tr[:], func=AF.Sqrt), reads=[dtr], writes=[dtr])
                        P.op("dve", lambda e, ti=ti, cs=cs: e.tensor_tensor(out=ti[:], in0=ti[:], in1=xc[nm][:, cs], op=ALU.mult), reads=[dti, dxc], writes=[dti])
                        P.op("dve", lambda e, tr=tr, ti=ti, cs=cs: e.tensor_tensor(out=u_[:, cs], in0=tr[:], in1=ti[:], op=ALU.mult), reads=[dtr, dti], writes=[du])
                if HO:
                    gates("t", 0)
                    P.op("dve", lambda e: e.tensor_tensor_scan(out=hb[:], data0=a_[:], data1=u_[:], initial=0.0, op0=ALU.mult, op1=ALU.add),
                         reads=[da, du], writes=[dhb])
                    P.op("dve", lambda e: e.tensor_scalar(out=carry[:, 0:1], in0=hb[:, T - 1:T], scalar1=f1, scalar2=None, op0=ALU.mult),
                         reads=[dhb, self.dconst], writes=[dcar])
                    gates("t", 1)
                    P.op("dve", lambda e: e.tensor_tensor_scan(out=hb[:, ::-1], data0=a_[:, ::-1], data1=u_[:, ::-1], initial=0.0, op0=ALU.mult, op1=ALU.add),
                         reads=[da, du], writes=[dhb])
                    P.op("dve", lambda e: e.tensor_scalar(out=carry[:, 1:2], in0=hb[:, 0:1], scalar1=f0, scalar2=None, op0=ALU.mult),
                         reads=[dhb, self.dconst], writes=[dcar])
                else:
                    P.op("dve", lambda e: e.memset(carry[:], 0.0), writes=[dcar])
                gates("o", 0)
                P.op("dve", lambda e: e.tensor_tensor_scan(out=hsum[:], data0=a_[:], data1=u_[:], initial=carry[:, 0:1], op0=ALU.mult, op1=ALU.add),
                     reads=[da, du, dcar], writes=[dh])
                gates("o", 1)
                P.op("dve", lambda e: e.tensor_tensor_scan(out=hb[:, ::-1], data0=a_[:, ::-1], data1=u_[:, ::-1], initial=carry[:, 1:2], op0=ALU.mult, op1=ALU.add),
                     reads=[da, du, dcar], writes=[dhb])
                P.op("pool", lambda e: e.tensor_tensor(out=hsum[:], in0=hsum[:], in1=hb[:], op=ALU.add), reads=[dh, dhb], writes=[dh])
                lgt = a_
                P.dma("sp", lgt[:], self.LG[rows, 0:T], reads=[P.dep("d_LG"), da], writes=[da])
                P.op("pool", lambda e: e.tensor_tensor(out=hsum[:], in0=hsum[:], in1=lgt[:], op=ALU.mult), reads=[dh, da], writes=[dh])
                P.dma("sp", self.Y[768 + c * 128:768 + (c + 1) * 128, 0:T], hsum[:], reads=[dh], writes=[dY])
                P.barrier()

    def phase_c(self, l):
        P, T, NB = self.P, self.T, self.NB
        S2 = (2 * T) if self.has_other else T
        NKT = S2 // 128
        NQB = NB
        P.barrier()
        dY = P.dep("d_Y")
        with ExitStack() as st:
            sb = lambda n, s, d: self.sb(st, n, s, d)
            Vall = sb("Vall", [128, NKT, NH * 65], BF16)
            dVall = P.dep("c_Vall")
            vv = self.Vd[0:S2, :].rearrange("(k p) n -> p k n", p=128)
            for k0 in range(0, NKT, 8):
                P.dma("sp", Vall[:, k0:k0 + 8, :], vv[:, k0:k0 + 8, :], reads=[P.dep("d_V")], writes=[dVall])
            sel = sb("sel", [128, 64], F32)
            dsel = P.dep("c_sel")
            P.op("dve", lambda e: e.memset(sel[:], 0.0), writes=[dsel])
            P.op("dve", lambda e: e.memset(sel[64:65, :], 1.0), reads=[dsel], writes=[dsel])
            KTh = [sb("KTh", [96, S2], BF16) for _ in range(2)]
            QTh = [sb("QTh", [96, T], BF16) for _ in range(2)]
            pS = [self.psum(st, "pS", [128, 1536], F32) for _ in range(2)]
            dpS = [P.dep("c_pS%d" % i) for i in range(2)]
            pO = [self.psum(st, "pO", [128, 512], F32) for _ in range(2)]
            dpO = [P.dep("c_pO%d" % i) for i in range(2)]
            PT = [sb("PT", [128, 1536], BF16) for _ in range(3)]
            dPT = [P.dep("c_PT%d" % i) for i in range(3)]
            Osb = [sb("Osb", [128, 512], F32) for _ in range(2)]
            rec = [sb("rec", [64, 512], F32) for _ in range(2)]
            yo = [sb("yo", [64, 512], F32) for _ in range(2)]
            it = 0
            nqb = 0
            for h in range(NH):
                kt_, dkt = KTh[h % 2], P.dep("c_KTh%d" % (h % 2))
                qt_, dqt = QTh[h % 2], P.dep("c_QTh%d" % (h % 2))
                P.dma("sp", kt_[:], self.KT[h, :, 0:S2], reads=[P.dep("d_KT")], writes=[dkt])
                P.dma("sp", qt_[:], self.QT[h, :, 0:T], reads=[P.dep("d_QT")], writes=[dqt])
                for qb in range(NQB):
                    qs = slice(qb * 512, (qb + 1) * 512)
                    po, dpo = pO[nqb % 2], dpO[nqb % 2]
                    GS = 3
                    groups = [list(range(k0, min(k0 + GS, NKT))) for k0 in range(0, NKT, GS)]
                    NKG = len(groups)

                    def scores(kg, it_):
                        ps_, dps_ = pS[it_ % 2], dpS[it_ % 2]
                        ks = groups[kg]
                        for j, k in enumerate(ks):
                            P.op("pe", lambda e, k=k, j=j, ps_=ps_: e.matmul(ps_[:, j * 512:(j + 1) * 512], lhsT=kt_[:, k * 128:(k + 1) * 128], rhs=qt_[:, qs],
                                                                        start=True, stop=True), reads=[dkt, dqt], writes=[dps_], signal=(j == len(ks) - 1))

                    def expo(kg, it_):
                        ps_, dps_ = pS[it_ % 2], dpS[it_ % 2]
                        pt_, dpt_ = PT[it_ % 3], dPT[it_ % 3]
                        w = len(groups[kg]) * 512
                        P.op("act", lambda e, ps_=ps_, pt_=pt_, w=w: e.activation(out=pt_[:, 0:w], in_=ps_[:, 0:w], func=AF.Exp), reads=[dps_], writes=[dpt_])

                    def pv(kg, it_):
                        pt_, dpt_ = PT[it_ % 3], dPT[it_ % 3]
                        ks = groups[kg]
                        for j, k in enumerate(ks):
                            P.op("pe", lambda e, k=k, j=j, pt_=pt_, po=po: e.matmul(po[0:65, :], lhsT=Vall[:, k, h * 65:(h + 1) * 65], rhs=pt_[:, j * 512:(j + 1) * 512],
                                                                               start=(k == 0), stop=(k == NKT - 1)), reads=[dVall, dpt_], writes=[dpo],
                                 signal=(j == len(ks) - 1))
                    scores(0, it)
                    for kg in range(NKG):
                        expo(kg, it + kg)
                        if kg + 1 < NKG:
                            scores(kg + 1, it + kg + 1)
                        pv(kg, it + kg)
                    it += NKG
                    pD, dpD = pS[it % 2], dpS[it % 2]
                    ob, dob = Osb[nqb % 2], P.dep("c_Osb%d" % (nqb % 2))
                    rc_, drc = rec[nqb % 2], P.dep("c_rec%d" % (nqb % 2))
                    y_, dy_ = yo[nqb % 2], P.dep("c_yo%d" % (nqb % 2))
                    nqb += 1
                    P.op("dve", lambda e, ob=ob, po=po: e.tensor_copy(ob[0:65, :], po[0:65, :]), reads=[dpo], writes=[dob])
                    P.op("pe", lambda e, ob=ob: e.matmul(pD[0:64, 0:512], lhsT=sel[0:65, :], rhs=ob[0:65, :], start=True, stop=True),
                         reads=[dsel, dob], writes=[dpD])
                    P.op("dve", lambda e, rc_=rc_: e.reciprocal(out=rc_[:], in_=pD[0:64, 0:512]), reads=[dpD], writes=[drc])
                    P.op("dve", lambda e, y_=y_, ob=ob, rc_=rc_: e.tensor_tensor(out=y_[:], in0=ob[0:64, :], in1=rc_[:], op=ALU.mult),
                         reads=[dob, drc], writes=[dy_])
                    P.dma("sp", self.Y[256 + h * 64:256 + (h + 1) * 64, qs], y_[:], reads=[dy_], writes=[dY])

    def phase_d(self, l, xres):
        P, T, NB, I = self.P, self.T, self.NB, self.I
        P.barrier()
        with ExitStack() as st:
            sb = lambda n, s, d: self.sb(st, n, s, d)
            Wo = sb("Wo", [128, 8, D], BF16)
            dWo = P.dep("d_Wo")
            wv = self.wb[("w_out", l)].rearrange("(c p) n -> p c n", p=128)
            for c in range(8):
                P.dma("sp", Wo[:, c, :], wv[:, c, :], reads=[self.dwc["w_out%d" % l]], writes=[dWo])
            gm, dgm = self.load_cols(st, "gm", I["mix_norm_g"][l], D)
            L = self.ln_setup(st, I["ln1_g"][l], I["ln1_b"][l], "1", nbuf=2)
            Yb = [sb("Yb", [128, 8, 512], F32) for _ in range(2)]
            sq = sb("sq", [128, 8, 512], BF16)
            yn = sb("yn", [128, 8, 512], BF16)
            dsq, dyn = P.dep("dd_sq"), P.dep("dd_yn")
            rstd = [sb("rstd", [128, 512], F32) for _ in range(3)]
            sdt = [sb("sdt", [128, 512], F32) for _ in range(3)]
            pst = [self.psum(st, "dpst", [128, 512], F32) for _ in range(2)]
            pso = [self.psum(st, "dpso", [128, 512], F32) for _ in range(3)]
            xr = [sb("xr", [128, D], F32) for _ in range(2)]
            pre = [sb("pre", [128, D], F32) for _ in range(2)]
            xTs = [sb("xTs", [128, 8, 512], BF16) for _ in range(2)]
            Yv = self.Y.rearrange("(c p) t -> p c t", p=128)
            groups = ((0, 2, 256.0), (2, 6, 512.0), (6, 8, 256.0))
            dres = P.dep("d_x1res")
            dx1T = P.dep("d_x1T")
            npo = 0
            ntile = 0
            moe = (l == 1)
            if moe:
                wr = sb("wr", [128, 8, NE], F32)
                wrb = sb("wrb", [128, 8, NE], BF16)
                dwr = P.dep("dd_wr")
                P.dma("sp", wr[:], I["moe_w_router"][0].rearrange("(c p) n -> p c n", p=128), writes=[dwr])
                P.op("dve", lambda e: e.tensor_copy(wrb[:], wr[:]), reads=[dwr], writes=[dwr])
                prr_full = self.psum(st, "prr", [128, 512], F32)
                prr = prr_full[:, 0:NE]
                dprr = P.dep("dd_prr")
                lg_ = [sb("lg_", [128, NE], F32) for _ in range(2)]
                m8 = [sb("m8", [128, 8], F32) for _ in range(2)]
                msk = [sb("msk", [128, NE], F32) for _ in range(2)]
                ex = [sb("ex", [128, NE], F32) for _ in range(2)]
                den = [sb("den", [128, 2], F32) for _ in range(2)]
                dcomb = P.dep("d_comb")
                rtmp = [sb("rtmp", [128, 2 * NE], F32) for _ in range(2)]
            for b in range(NB):
                ts_ = slice(b * 512, (b + 1) * 512)
                yb, dyb = Yb[b % 2], P.dep("dd_Yb%d" % (b % 2))
                P.dma("sp", yb[:], Yv[:, :, ts_], reads=[P.dep("d_Y")], writes=[dyb])
                for c in range(8):
                    P.op("act", lambda e, c=c, yb=yb: e.activation(out=sq[:, c, :], in_=yb[:, c, :], func=AF.Square), reads=[dyb], writes=[dsq])
                for gi, (c0, c1, n) in enumerate(groups):
                    p_, dp_ = pst[gi % 2], P.dep("dd_pst%d" % (gi % 2))
                    for c in range(c0, c1):
                        P.op("pe", lambda e, c=c, p_=p_, c0=c0, c1=c1: e.matmul(p_[:], lhsT=self.ones[:], rhs=sq[:, c, :], start=(c == c0), stop=(c == c1 - 1)),
                             reads=[dsq, self.dconst], writes=[dp_], signal=(c == c1 - 1))
                    dsd, drs = P.dep("dd_sdt%d" % gi), P.dep("dd_rstd%d" % gi)
                    P.op("act", lambda e, gi=gi, p_=p_, n=n: e.activation(out=sdt[gi][:], in_=p_[:], func=AF.Sqrt, bias=self.eps_rms[:], scale=1.0 / n),
                         reads=[dp_, self.dconst], writes=[dsd])
                    P.op("dve", lambda e, gi=gi: e.reciprocal(out=rstd[gi][:], in_=sdt[gi][:]), reads=[dsd], writes=[drs])
                    for c in range(c0, c1):
                        P.op("dve", lambda e, c=c, gi=gi, yb=yb: e.scalar_tensor_tensor(out=yn[:, c, :], in0=yb[:, c, :], scalar=gm[:, c:c + 1], in1=rstd[gi][:],
                                                                                     op0=ALU.mult, op1=ALU.mult), reads=[dyb, dgm, drs], writes=[dyn])
                xs, dxs = xTs[b % 2], P.dep("dd_xTs%d" % (b % 2))
                for sub in range(4):
                    r0 = b * 512 + sub * 128
                    xrt, dxr = xr[ntile % 2], P.dep("dd_xr%d" % (ntile % 2))
                    pr, dpr = pre[ntile % 2], P.dep("dd_pre%d" % (ntile % 2))
                    ntile += 1
                    P.dma("sp", xrt[:], xres[r0:r0 + 128, :], reads=[P.dep("d_xres")], writes=[dxr])
                    for hf in range(2):
                        po, dpo = pso[npo % 3], P.dep("dd_pso%d" % (npo % 3))
                        npo += 1
                        for c in range(8):
                            P.op("pe", lambda e, c=c, hf=hf, po=po, sub=sub: e.matmul(po[:], lhsT=yn[:, c, sub * 128:(sub + 1) * 128], rhs=Wo[:, c, hf * 512:(hf + 1) * 512],
                                                                                 start=(c == 0), stop=(c == 7)), reads=[dyn, dWo], writes=[dpo], signal=(c == 7))
                        P.op("dve", lambda e, hf=hf, po=po, pr=pr, xrt=xrt: e.scalar_tensor_tensor(out=pr[:, hf * 512:(hf + 1) * 512], in0=xrt[:, hf * 512:(hf + 1) * 512],
                                                                                                scalar=ALPHA, in1=po[:], op0=ALU.mult, op1=ALU.add),
                             reads=[dxr, dpo], writes=[dpr])
                    self.ln_tile(L, pr, dpr, xs, dxs, sub, res_dst=self.x1res[r0:r0 + 128, :], dres=dres,
                                 xb_dst=(self.X1B[r0:r0 + 128, :] if moe else None), dxbd=P.dep("d_X1B"))
                    if moe:
                        i2 = ntile % 2
                        for c in range(8):
                            P.op("pe", lambda e, c=c, sub=sub, xs=xs: e.matmul(prr, lhsT=xs[:, c, sub * 128:(sub + 1) * 128], rhs=wrb[:, c, :], start=(c == 0), stop=(c == 7)),
                                 reads=[dxs, dwr], writes=[dprr], signal=(c == 7))
                        dl_, dm8, dmk, dex, dden = (P.dep("dd_%s%d" % (n, i2)) for n in ("lg", "m8", "msk", "ex", "den"))
                        P.op("dve", lambda e, i2=i2: e.tensor_copy(lg_[i2][:], prr), reads=[dprr], writes=[dl_])
                        P.op("dve", lambda e, i2=i2: e.max(out=m8[i2][:], in_=lg_[i2][:]), reads=[dl_], writes=[dm8])
                        P.op("dve", lambda e, i2=i2: e.tensor_scalar(out=msk[i2][:], in0=lg_[i2][:], scalar1=m8[i2][:, 1:2], scalar2=None, op0=ALU.is_ge),
                             reads=[dl_, dm8], writes=[dmk])
                        P.op("dve", lambda e, i2=i2: e.tensor_scalar(out=den[i2][:, 0:1], in0=m8[i2][:, 0:1], scalar1=-1.0, scalar2=None, op0=ALU.mult),
                             reads=[dm8], writes=[dden])
                        P.op("act", lambda e, i2=i2: e.activation(out=ex[i2][:], in_=lg_[i2][:], func=AF.Exp, bias=den[i2][:, 0:1], scale=1.0),
                             reads=[dl_, dden], writes=[dex])
                        P.op("dve", lambda e, i2=i2: e.tensor_tensor(out=ex[i2][:], in0=ex[i2][:], in1=msk[i2][:], op=ALU.mult), reads=[dex, dmk], writes=[dex])
                        P.op("dve", lambda e, i2=i2: e.reduce_sum(out=den[i2][:, 1:2], in_=ex[i2][:], axis=mybir.AxisListType.X), reads=[dex, dden], writes=[dden])
                        P.op("dve", lambda e, i2=i2: e.reciprocal(out=den[i2][:, 1:2], in_=den[i2][:, 1:2]), reads=[dden], writes=[dden])
                        P.op("dve", lambda e, i2=i2: e.tensor_scalar(out=ex[i2][:], in0=ex[i2][:], scalar1=den[i2][:, 1:2], scalar2=None, op0=ALU.mult),
                             reads=[dex, dden], writes=[dex])
                        R = self.R
                        ti_ = r0 // 128
                        drt = P.dep("r_tab")
                        doh = P.dep("dd_oh%d" % i2)
                        oh1, oh2, tmp8 = R["oh1"][:, ti_, :], R["oh2"][:, ti_, :], rtmp[i2]
                        P.op("dve", lambda e, i2=i2, oh1=oh1: e.tensor_scalar(out=oh1, in0=lg_[i2][:], scalar1=m8[i2][:, 0:1], scalar2=None, op0=ALU.is_equal),
                             reads=[dl_, dm8], writes=[drt])
                        P.op("dve", lambda e, i2=i2, oh1=oh1, oh2=oh2: e.tensor_tensor(out=oh2, in0=msk[i2][:], in1=oh1, op=ALU.subtract), reads=[dmk, drt], writes=[drt])
                        pw, dpw = prr_full[:, 16:32], dprr
                        P.op("pe", lambda e, i2=i2: e.matmul(pw[:, 0:NE], lhsT=R["U"][:], rhs=msk[i2][:], start=True, stop=True), reads=[dmk, P.dep("r_const")], writes=[dpw])
                        P.op("pe", lambda e, i2=i2: e.matmul(pw[:, NE:2 * NE], lhsT=R["onesf"][:], rhs=msk[i2][:], start=True, stop=True), reads=[dmk, P.dep("r_const")], writes=[dpw])
                        drun = P.dep("r_run")
                        P.op("dve", lambda e, tmp8=tmp8: e.tensor_tensor(out=tmp8[:, 0:NE], in0=pw[:, 0:NE], in1=R["run"][:], op=ALU.add), reads=[dpw, drun], writes=[doh])
                        P.op("dve", lambda e: e.tensor_tensor(out=R["run"][:], in0=pw[:, NE:2 * NE], in1=R["run"][:], op=ALU.add), reads=[dpw, drun], writes=[drun])
                        for kk, oh in ((0, oh1), (1, oh2)):
                            P.op("dve", lambda e, tmp8=tmp8, oh=oh: e.tensor_tensor(out=tmp8[:, NE:2 * NE], in0=tmp8[:, 0:NE], in1=oh, op=ALU.mult), reads=[doh, drt], writes=[doh])
                            P.op("dve", lambda e, tmp8=tmp8, kk=kk, ti_=ti_: e.reduce_sum(out=R["r12"][:, ti_, kk:kk + 1], in_=tmp8[:, NE:2 * NE], axis=mybir.AxisListType.X),
                                 reads=[doh], writes=[drt])
                            P.op("dve", lambda e, tmp8=tmp8, oh=oh, i2=i2: e.tensor_tensor(out=tmp8[:, NE:2 * NE], in0=ex[i2][:], in1=oh, op=ALU.mult), reads=[dex, drt, doh], writes=[doh])
                            P.op("dve", lambda e, tmp8=tmp8, kk=kk, ti_=ti_: e.reduce_sum(out=R["g12"][:, ti_, kk:kk + 1], in_=tmp8[:, NE:2 * NE], axis=mybir.AxisListType.X),
                                 reads=[doh], writes=[drt])
                P.dma("sp", self.x1T.rearrange("(c p) t -> p c t", p=128)[:, :, ts_], xs[:], reads=[dxs], writes=[dx1T])

    def route_setup(self, st):
        P, T = self.P, self.T
        NT = T // 128
        self.NTILE = (2 * T + NE * 511) // 512
        P.barrier()
        R = {}
        sb = lambda n, s, d: self.sb(st, n, s, d)
        R["U"] = sb("rU", [128, 128], F32)
        R["onesf"] = sb("ronesf", [128, 128], F32)
        R["run"] = sb("rrun", [128, NE], F32)
        R["oh1"] = sb("roh1", [128, NT, NE], F32)
        R["oh2"] = sb("roh2", [128, NT, NE], F32)
        R["r12"] = sb("rr12", [128, NT, 2], F32)
        R["g12"] = sb("rg12", [128, NT, 2], F32)
        R["slot"] = sb("rslot", [128, NT, 2], F32)
        R["sloti"] = sb("rsloti", [128, NT, 2], mybir.dt.int32)
        R["pidx"] = sb("rpidx", [128, 1], F32)
        R["pidxi"] = sb("rpidxi", [128, 1], mybir.dt.int32)
        R["ej"] = sb("rej", [128, self.NTILE], F32)
        R["idxg"] = sb("ridxg", [128, self.NTILE, 7], F32)
        R["idxgi"] = sb("ridxgi", [128, self.NTILE, 7], mybir.dt.int32)
        R["idxd"] = sb("ridxd", [128, self.NTILE, 4], F32)
        R["idxdi"] = sb("ridxdi", [128, self.NTILE, 4], mybir.dt.int32)
        dc = P.dep("r_const")
        P.op("pool", lambda e: e.memset(R["onesf"][:], 1.0), writes=[dc])
        P.op("pool", lambda e: e.memset(R["U"][:], 1.0), writes=[dc])
        P.op("pool", lambda e: e.affine_select(out=R["U"][:], in_=R["U"][:], pattern=[[1, 128]], compare_op=ALU.is_gt, fill=0.0,
                                               base=0, channel_multiplier=-1), reads=[dc], writes=[dc])
        P.op("pool", lambda e: e.iota(R["pidxi"][:], pattern=[[0, 1]], base=0, channel_multiplier=1), writes=[dc])
        P.op("pool", lambda e: e.tensor_copy(R["pidx"][:], R["pidxi"][:]), reads=[dc], writes=[dc])
        P.op("dve", lambda e: e.memset(R["run"][:], 0.0), writes=[P.dep("r_run")])
        self.R = R

    def phase_r(self):
        P, T = self.P, self.T
        R = self.R
        NT = T // 128
        NTILE = self.NTILE
        P.barrier()
        with ExitStack() as st:
            sb = lambda n, s, d: self.sb(st, n, s, d)
            t8 = sb("t8", [128, 6, NE], F32)
            d8 = P.dep("r_t8")
            drt = P.dep("r_tab")
            P.op("dve", lambda e: e.memset(t8[:, 5, :], 1.0), writes=[d8])
            t8i = sb("t8i", [128, NE], mybir.dt.int32)
            P.op("dve", lambda e: e.tensor_scalar(out=t8[:, 0, :], in0=R["run"][:], scalar1=1.0 / 512.0, scalar2=511.0 / 512.0 - 0.499, op0=ALU.mult, op1=ALU.add),
                 reads=[P.dep("r_run"), d8], writes=[d8])
            P.op("dve", lambda e: e.tensor_copy(t8i[:], t8[:, 0, :]), reads=[d8], writes=[d8])
            P.op("dve", lambda e: e.tensor_copy(t8[:, 1, :], t8i[:]), reads=[d8], writes=[d8])
            P.op("dve", lambda e: e.tensor_scalar(out=t8[:, 2, :], in0=t8[:, 1, :], scalar1=512.0, scalar2=None, op0=ALU.mult), reads=[d8], writes=[d8])
            P.op("dve", lambda e: e.tensor_tensor_scan(out=t8[:, 3, :], data0=t8[:, 5, :], data1=t8[:, 2, :], initial=0.0, op0=ALU.mult, op1=ALU.add), reads=[d8], writes=[d8])
            P.op("dve", lambda e: e.tensor_tensor(out=t8[:, 4, :], in0=t8[:, 3, :], in1=t8[:, 2, :], op=ALU.subtract), reads=[d8], writes=[d8])
            tmp = sb("rtmp2", [128, NE], F32)
            dtmp = P.dep("r_tmp2")
            for i in range(NT):
                for kk, oh in ((0, R["oh1"]), (1, R["oh2"])):
                    P.op("dve", lambda e, i=i, oh=oh: e.tensor_tensor(out=tmp[:], in0=oh[:, i, :], in1=t8[:, 4, :], op=ALU.mult), reads=[drt, d8, dtmp], writes=[dtmp])
                    P.op("dve", lambda e, i=i, kk=kk: e.reduce_sum(out=R["slot"][:, i, kk:kk + 1], in_=tmp[:], axis=mybir.AxisListType.X), reads=[dtmp], writes=[drt])
            P.op("dve", lambda e: e.tensor_tensor(out=R["slot"][:], in0=R["slot"][:], in1=R["r12"][:], op=ALU.add), reads=[drt], writes=[drt])
            P.op("dve", lambda e: e.tensor_copy(R["sloti"][:], R["slot"][:]), reads=[drt], writes=[drt])
            for j in range(NTILE):
                P.op("dve", lambda e, j=j: e.tensor_scalar(out=tmp[:], in0=t8[:, 3, :], scalar1=float(512 * j), scalar2=None, op0=ALU.is_le), reads=[d8, dtmp], writes=[dtmp])
                P.op("dve", lambda e, j=j: e.reduce_sum(out=R["ej"][:, j:j + 1], in_=tmp[:], axis=mybir.AxisListType.X), reads=[dtmp], writes=[drt])
            P.op("dve", lambda e: e.tensor_scalar(out=R["ej"][:], in0=R["ej"][:], scalar1=float(NE - 1), scalar2=None, op0=ALU.min), reads=[drt], writes=[drt])
            dc = P.dep("r_const")
            for g in range(7):
                P.op("dve", lambda e, g=g: e.tensor_scalar(out=R["idxg"][:, :, g], in0=R["ej"][:], scalar1=896.0, scalar2=float(g * 128), op0=ALU.mult, op1=ALU.add),
                     reads=[drt], writes=[drt])
            for q in range(4):
                P.op("dve", lambda e, q=q: e.tensor_scalar(out=R["idxd"][:, :, q], in0=R["ej"][:], scalar1=512.0, scalar2=float(q * 128), op0=ALU.mult, op1=ALU.add),
                     reads=[drt], writes=[drt])
            P.op("dve", lambda e: e.tensor_scalar(out=R["idxg"][:], in0=R["idxg"][:], scalar1=R["pidx"][:, 0:1], scalar2=None, op0=ALU.add), reads=[drt, dc], writes=[drt])
            P.op("dve", lambda e: e.tensor_scalar(out=R["idxd"][:], in0=R["idxd"][:], scalar1=R["pidx"][:, 0:1], scalar2=None, op0=ALU.add), reads=[drt, dc], writes=[drt])
            P.op("dve", lambda e: e.tensor_copy(R["idxgi"][:], R["idxg"][:]), reads=[drt], writes=[drt])
            P.op("dve", lambda e: e.tensor_copy(R["idxdi"][:], R["idxd"][:]), reads=[drt], writes=[drt])
            import os
            if os.environ.get("MK_DBGR"):
                dd = P.dep("dbgr")
                for nm, t_, dt_ in (("idxgi", R["idxgi"], mybir.dt.int32), ("ej", R["ej"], F32), ("sloti", R["sloti"], mybir.dt.int32),
                                    ("t8", t8, F32), ("pidx", R["pidx"], F32), ("r12", R["r12"], F32), ("g12", R["g12"], F32),
                                    ("oh1", R["oh1"], F32), ("oh2", R["oh2"], F32), ("run", R["run"], F32), ("U", R["U"], F32), ("onesf", R["onesf"], F32),
                                    ("pidxi", R["pidxi"], mybir.dt.int32)):
                    shp = list(t_.shape)
                    o = self.nc.dram_tensor("dbg_" + nm, shp, dt_, kind="ExternalOutput").ap()
                    P.dma("sp", o, t_[:], reads=[drt, d8, dc], writes=[dd])

    def phase_e_moe(self, l, out_res):
        P, T, I = self.P, self.T, self.I
        R = self.R
        NT = T // 128
        NTILE = self.NTILE
        NSLOT = NTILE * 512
        drt = P.dep("r_tab")
        P.barrier()
        with ExitStack() as st0:
            cur = [ExitStack()]
            st0.callback(lambda: cur[0].close())
            sb = lambda n, s, d: self.sb(cur[0], n, s, d)

            class _St:
                def enter_context(self_, x):
                    return cur[0].enter_context(x)
            st = _St()
            dXS = P.dep("d_XS")
            dYS = P.dep("d_YS")
            zt = sb("zt", [128, 4096], BF16)
            dzt = P.dep("m_zt")
            P.op("pool", lambda e: e.memset(zt[:], 0.0), writes=[dzt])
            xsv = self.XS.rearrange("(a p f) n -> a p (f n)", p=128, f=4)
            for a in range(NSLOT // 512):
                P.dma("sp", xsv[a], zt[:], reads=[dzt], writes=[dXS])
            xl = [sb("xl", [128, D], BF16) for _ in range(2)]
            for i in range(NT):
                x_, dx_ = xl[i % 2], P.dep("m_xl%d" % (i % 2))
                P.dma("sp", x_[:], self.X1B[i * 128:(i + 1) * 128, :], reads=[P.dep("d_X1B")], writes=[dx_])
                for kk in range(2):
                    P.idma(self.XS, R["sloti"][:, i, kk:kk + 1], x_[:], None, reads=[dx_, drt], writes=[dXS])
            P.barrier()
            cur[0].close()
            cur[0] = ExitStack()
            if self.mstop < 4:
                return
            xtm = sb("xtm", [128, 4, D], BF16)
            dxtm = P.dep("m_xtm")
            xTb = sb("xTb", [128, 8, 512], BF16)
            dxTb = P.dep("m_xTb")
            pT = self.psum(st, "mpT", [128, D], BF16)
            dpT = P.dep("m_pT")
            Wg = [sb("Wg", [128, 8, 512], BF16) for _ in range(3)]
            Wu = [sb("Wu", [128, 8, 512], BF16) for _ in range(3)]
            Wd = sb("Wd", [128, 28, D], BF16)
            dWd = P.dep("e_Wd")
            hT = sb("hT", [128, 28, 512], BF16)
            dhT = P.dep("e_hT")
            pg = [self.psum(st, "pg", [128, 512], F32) for _ in range(2)]
            pu = [self.psum(st, "pu", [128, 512], F32) for _ in range(2)]
            pd = [self.psum(st, "pd", [128, 512], F32) for _ in range(2)]
            sg = [sb("sg", [128, 512], F32) for _ in range(2)]
            ysb = [sb("ysb", [128, D], F32) for _ in range(2)]
            wi = 0
            npd = 0
            nys = 0
            xsr = self.XS.rearrange("(j s p) n -> j p s n", p=128, s=4)
            for j in range(NTILE):
                P.dma("sp", xtm[:], xsr[j], reads=[dXS], writes=[dxtm])
                for sub in range(4):
                    for c in range(8):
                        P.op("pe", lambda e, c=c, sub=sub: e.transpose(pT[:, c * 128:(c + 1) * 128], xtm[:, sub, c * 128:(c + 1) * 128], self.ident[:]),
                             reads=[dxtm, self.dconst], writes=[dpT], signal=(c == 7))
                    P.op("act", lambda e, sub=sub: e.activation(out=xTb[:, :, sub * 128:(sub + 1) * 128], in_=pT[:].rearrange("p (c t) -> p c t", c=8), func=AF.Copy),
                         reads=[dpT], writes=[dxTb])
                import os
                fstop = int(os.environ.get("MK_FSTOP", "99"))
                if fstop < 1:
                    continue
                for g in range(7):
                    wg_, dwg_ = Wg[wi % 3], P.dep("e_Wg%d" % (wi % 3))
                    wu_, dwu_ = Wu[wi % 3], P.dep("e_Wu%d" % (wi % 3))
                    wi += 1
                    P.idma(wg_[:].rearrange("p c n -> p (c n)"), None, self.WGt, R["idxgi"][:, j, g:g + 1], reads=[self.dwc["mg"], drt], writes=[dwg_])
                    P.idma(wu_[:].rearrange("p c n -> p (c n)"), None, self.WUt, R["idxgi"][:, j, g:g + 1], reads=[self.dwc["mu"], drt], writes=[dwu_])
                    for jj in range(4):
                        f = g * 4 + jj
                        pg_, dpg_ = pg[f % 2], P.dep("e_pg%d" % (f % 2))
                        pu_, dpu_ = pu[f % 2], P.dep("e_pu%d" % (f % 2))
                        for c in range(8):
                            P.op("pe", lambda e, c=c, jj=jj, pg_=pg_, wg_=wg_: e.matmul(pg_[:], lhsT=wg_[:, c, jj * 128:(jj + 1) * 128], rhs=xTb[:, c, :], start=(c == 0), stop=(c == 7)),
                                 reads=[dwg_, dxTb], writes=[dpg_], signal=(c == 7))
                        for c in range(8):
                            P.op("pe", lambda e, c=c, jj=jj, pu_=pu_, wu_=wu_: e.matmul(pu_[:], lhsT=wu_[:, c, jj * 128:(jj + 1) * 128], rhs=xTb[:, c, :], start=(c == 0), stop=(c == 7)),
                                 reads=[dwu_, dxTb], writes=[dpu_], signal=(c == 7))
                        s_, ds_ = sg[f % 2], P.dep("e_sg%d" % (f % 2))
                        P.op("act", lambda e, s_=s_, pg_=pg_: e.activation(out=s_[:], in_=pg_[:], func=AF.Silu), reads=[dpg_], writes=[ds_])
                        P.op("dve", lambda e, f=f, s_=s_, pu_=pu_: e.tensor_tensor(out=hT[:, f, :], in0=pu_[:], in1=s_[:], op=ALU.mult),
                             reads=[dpu_, ds_], writes=[dhT])
                if fstop < 2:
                    continue
                for q in range(4):
                    P.idma(Wd[:, q * 7:(q + 1) * 7, :].rearrange("p f n -> p (f n)"), None, self.WDt, R["idxdi"][:, j, q:q + 1], reads=[self.dwc["md"], drt], writes=[dWd])
                for sub in range(4):
                    y_, dy_ = ysb[nys % 2], P.dep("m_ysb%d" % (nys % 2))
                    nys += 1
                    for hf in range(2):
                        pd_, dpd_ = pd[npd % 2], P.dep("e_pd%d" % (npd % 2))
                        npd += 1
                        for f in range(28):
                            P.op("pe", lambda e, f=f, sub=sub, hf=hf, pd_=pd_: e.matmul(pd_[:], lhsT=hT[:, f, sub * 128:(sub + 1) * 128], rhs=Wd[:, f, hf * 512:(hf + 1) * 512],
                                                                                   start=(f == 0), stop=(f == 27)), reads=[dhT, dWd], writes=[dpd_], signal=(f == 27))
                        if hf == 0:
                            P.op("act", lambda e, y_=y_, pd_=pd_: e.activation(out=y_[:, 0:512], in_=pd_[:], func=AF.Copy), reads=[dpd_], writes=[dy_])
                        else:
                            P.op("dve", lambda e, y_=y_, pd_=pd_: e.tensor_copy(y_[:, 512:1024], pd_[:]), reads=[dpd_], writes=[dy_])
                    r0 = j * 512 + sub * 128
                    P.dma("sp", self.YS[r0:r0 + 128, :], y_[:], reads=[dy_], writes=[dYS])
            P.barrier()
            cur[0].close()
            cur[0] = ExitStack()
            if self.mstop < 5:
                return
            L = self.ln_setup(st, I["ln2_g"][l], I["ln2_b"][l], "2", nbuf=2)
            y1 = [sb("y1", [128, D], F32) for _ in range(2)]
            y2 = [sb("y2", [128, D], F32) for _ in range(2)]
            xr = [sb("xr", [128, D], F32) for _ in range(2)]
            pre = [sb("pre", [128, D], F32) for _ in range(2)]
            dres = P.dep("d_outres")
            for i in range(NT):
                i2 = i % 2
                a_, b_, x_, p_ = y1[i2], y2[i2], xr[i2], pre[i2]
                da_, db_, dx_, dp_ = (P.dep("m_%s%d" % (n, i2)) for n in ("y1", "y2", "xr", "pre"))
                P.idma(a_[:], None, self.YS, R["sloti"][:, i, 0:1], reads=[dYS, drt], writes=[da_])
                P.idma(b_[:], None, self.YS, R["sloti"][:, i, 1:2], reads=[dYS, drt], writes=[db_])
                P.dma("sp", x_[:], self.x1res[i * 128:(i + 1) * 128, :], reads=[P.dep("d_x1res")], writes=[dx_])
                P.op("dve", lambda e, a_=a_, i=i: e.tensor_scalar(out=a_[:], in0=a_[:], scalar1=R["g12"][:, i, 0:1], scalar2=None, op0=ALU.mult), reads=[da_, drt], writes=[da_])
                P.op("dve", lambda e, a_=a_, b_=b_, i=i: e.scalar_tensor_tensor(out=a_[:], in0=b_[:], scalar=R["g12"][:, i, 1:2], in1=a_[:], op0=ALU.mult, op1=ALU.add),
                     reads=[da_, db_, drt], writes=[da_])
                P.op("dve", lambda e, a_=a_, x_=x_, p_=p_: e.scalar_tensor_tensor(out=p_[:], in0=x_[:], scalar=ALPHA, in1=a_[:], op0=ALU.mult, op1=ALU.add),
                     reads=[da_, dx_], writes=[dp_])
                self.ln_tile(L, p_, dp_, None, None, 0, res_dst=out_res[i * 128:(i + 1) * 128, :], dres=dres)

    def phase_e(self, l, out_res, out_T):
        P, T, NB, I = self.P, self.T, self.NB, self.I
        P.barrier()
        moe = (l == 1)
        nexp = NE if moe else 1
        with ExitStack() as st:
            sb = lambda n, s, d: self.sb(st, n, s, d)
            L = self.ln_setup(st, I["ln2_g"][l], I["ln2_b"][l], "2", nbuf=(1 if moe else 2))
            xb_ = [sb("xb", [128, 8, 512], BF16) for _ in range(1)]
            Wg = [sb("Wg", [128, 8, 512], BF16) for _ in range(3)]
            Wu = [sb("Wu", [128, 8, 512], BF16) for _ in range(3)]
            Wd = sb("Wd", [128, 28, D], BF16)
            dWd = P.dep("e_Wd")
            hT = sb("hT", [128, 28, 512], BF16)
            dhT = P.dep("e_hT")
            pg = [self.psum(st, "pg", [128, 512], F32) for _ in range(2)]
            pu = [self.psum(st, "pu", [128, 512], F32) for _ in range(2)]
            pd = [self.psum(st, "pd", [128, 512], F32) for _ in range(2)]
            pc = self.psum(st, "pc", [128, 512], F32) if moe else None
            sg = [sb("sg", [128, 512], F32) for _ in range(2)]
            xr = [sb("xr", [128, D], F32) for _ in range(1)]
            pre = [sb("pre", [128, D], F32) for _ in range(1)]
            xTs = [sb("xTs", [128, 8, 512], BF16) for _ in range(1)] if out_T is not None else None
            acc = sb("acc", [128, 4, D], F32) if moe else None
            dacc = P.dep("e_acc")
            if moe:
                selm = sb("selm", [NE, NE, 128], F32)
                dselm = P.dep("e_selm")
                P.op("pool", lambda e: e.memset(selm[:], 0.0), writes=[dselm])
                P.op("pool", lambda e: e.affine_select(out=selm[:], in_=selm[:], pattern=[[1, NE], [0, 128]], compare_op=ALU.not_equal, fill=1.0,
                                                       base=0, channel_multiplier=-1), reads=[dselm], writes=[dselm])
                cmb = [sb("cmb", [128, NE], F32) for _ in range(2)]
                cT = sb("cT", [NE, 512], F32)
                dcT = P.dep("e_cT")
                cbe = [sb("cbe", [128, 512], F32) for _ in range(2)]
                pT8 = pc[0:NE, 0:128]
                dpT8 = P.dep("e_pc")
            x1Tv = self.x1T.rearrange("(c p) t -> p c t", p=128)
            wi = 0
            npd = 0
            ntile = 0
            dres = P.dep("d_outres")
            dxT = P.dep("d_xT")
            for b in range(NB):
                ts_ = slice(b * 512, (b + 1) * 512)
                xb, dxb = xb_[0], P.dep("e_xb0")
                P.dma("sp", xb[:], x1Tv[:, :, ts_], reads=[P.dep("d_x1T")], writes=[dxb])
                if moe:
                    for sub in range(4):
                        cm, dcm = cmb[sub % 2], P.dep("e_cmb%d" % (sub % 2))
                        P.dma("sp", cm[:], self.comb[b * 512 + sub * 128:b * 512 + (sub + 1) * 128, :], reads=[P.dep("d_comb")], writes=[dcm])
                        P.op("pe", lambda e, cm=cm: e.transpose(pT8, cm[:], self.identf[:]), reads=[dcm, self.dconst], writes=[dpT8])
                        P.op("dve", lambda e, sub=sub: e.tensor_copy(cT[:, sub * 128:(sub + 1) * 128], pT8), reads=[dpT8], writes=[dcT])
                for ex_ in range(nexp):
                    if moe:
                        wgd, wud, wdd = self.wb["mg"][ex_], self.wb["mu"][ex_], self.wb["md"][ex_]
                        cb_, dcb_ = cbe[ex_ % 2], P.dep("e_cbe%d" % (ex_ % 2))
                        dpc = P.dep("e_pc")
                        P.op("pe", lambda e, ex_=ex_: e.matmul(pc[:], lhsT=selm[:, ex_, :], rhs=cT[:], start=True, stop=True), reads=[dselm, dcT], writes=[dpc])
                        P.op("act", lambda e, cb_=cb_: e.activation(out=cb_[:], in_=pc[:], func=AF.Copy), reads=[dpc], writes=[dcb_])
                    else:
                        wgd, wud, wdd = self.wb["dg"], self.wb["du"], self.wb["dd"]
                    if not (getattr(self, "moe_tables", False) and not moe):
                        wgv = wgd.rearrange("(c p) n -> p c n", p=128)
                        wuv = wud.rearrange("(c p) n -> p c n", p=128)
                    wdv = wdd.rearrange("(c p) n -> p c n", p=128)
                    for g in range(7):
                        wg_, dwg_ = Wg[wi % 3], P.dep("e_Wg%d" % (wi % 3))
                        wu_, dwu_ = Wu[wi % 3], P.dep("e_Wu%d" % (wi % 3))
                        wi += 1
                        if getattr(self, "moe_tables", False) and not moe:
                            P.dma("sp", wg_[:].rearrange("p c n -> p (c n)"), wgd[g * 128:(g + 1) * 128, :], reads=[self.dwc["g"]], writes=[dwg_])
                            P.dma("sp", wu_[:].rearrange("p c n -> p (c n)"), wud[g * 128:(g + 1) * 128, :], reads=[self.dwc["u"]], writes=[dwu_])
                        else:
                            P.dma("sp", wg_[:], wgv[:, :, g * 512:(g + 1) * 512], reads=[self.dwc["mg" if moe else "g"]], writes=[dwg_])
                            P.dma("sp", wu_[:], wuv[:, :, g * 512:(g + 1) * 512], reads=[self.dwc["mu" if moe else "u"]], writes=[dwu_])
                        for j in range(4):
                            f = g * 4 + j
                            pg_, dpg_ = pg[f % 2], P.dep("e_pg%d" % (f % 2))
                            pu_, dpu_ = pu[f % 2], P.dep("e_pu%d" % (f % 2))
                            for c in range(8):
                                P.op("pe", lambda e, c=c, j=j, pg_=pg_, wg_=wg_: e.matmul(pg_[:], lhsT=wg_[:, c, j * 128:(j + 1) * 128], rhs=xb[:, c, :], start=(c == 0), stop=(c == 7)),
                                     reads=[dwg_, dxb], writes=[dpg_], signal=(c == 7))
                            for c in range(8):
                                P.op("pe", lambda e, c=c, j=j, pu_=pu_, wu_=wu_: e.matmul(pu_[:], lhsT=wu_[:, c, j * 128:(j + 1) * 128], rhs=xb[:, c, :], start=(c == 0), stop=(c == 7)),
                                     reads=[dwu_, dxb], writes=[dpu_], signal=(c == 7))
                            s_, ds_ = sg[f % 2], P.dep("e_sg%d" % (f % 2))
                            P.op("act", lambda e, s_=s_, pg_=pg_: e.activation(out=s_[:], in_=pg_[:], func=AF.Silu), reads=[dpg_], writes=[ds_])
                            if moe:
                                P.op("pool", lambda e, s_=s_, cb_=cb_: e.tensor_tensor(out=s_[:], in0=s_[:], in1=cb_[:], op=ALU.mult), reads=[ds_, dcb_], writes=[ds_])
                            P.op("dve", lambda e, f=f, s_=s_, pu_=pu_: e.tensor_tensor(out=hT[:, f, :], in0=pu_[:], in1=s_[:], op=ALU.mult),
                                 reads=[dpu_, ds_], writes=[dhT])
                    for c0 in range(0, 28, 7):
                        P.dma("sp", Wd[:, c0:c0 + 7, :], wdv[:, c0:c0 + 7, :], reads=[self.dwc["md" if moe else "d"]], writes=[dWd])
                    for sub in range(4):
                        r0 = b * 512 + sub * 128
                        if ex_ == nexp - 1:
                            xrt, dxr = xr[0], P.dep("e_xr0")
                            pr, dpr = pre[0], P.dep("e_pre0")
                            ntile += 1
                            P.dma("sp", xrt[:], self.x1res[r0:r0 + 128, :], reads=[P.dep("d_x1res")], writes=[dxr])
                        for hf in range(2):
                            pd_, dpd_ = pd[npd % 2], P.dep("e_pd%d" % (npd % 2))
                            npd += 1
                            for f in range(28):
                                P.op("pe", lambda e, f=f, sub=sub, hf=hf, pd_=pd_: e.matmul(pd_[:], lhsT=hT[:, f, sub * 128:(sub + 1) * 128], rhs=Wd[:, f, hf * 512:(hf + 1) * 512],
                                                                                       start=(f == 0), stop=(f == 27)), reads=[dhT, dWd], writes=[dpd_], signal=(f == 27))
                            hs = slice(hf * 512, (hf + 1) * 512)
                            if moe and ex_ == 0:
                                P.op("dve", lambda e, sub=sub, hs=hs, pd_=pd_: e.tensor_copy(acc[:, sub, hs], pd_[:]), reads=[dpd_], writes=[dacc])
                            elif moe and ex_ < nexp - 1:
                                P.op("dve", lambda e, sub=sub, hs=hs, pd_=pd_: e.tensor_tensor(out=acc[:, sub, hs], in0=pd_[:], in1=acc[:, sub, hs], op=ALU.add),
                                     reads=[dpd_, dacc], writes=[dacc])
                            else:
                                if moe:
                                    P.op("dve", lambda e, sub=sub, hs=hs, pd_=pd_: e.tensor_tensor(out=acc[:, sub, hs], in0=pd_[:], in1=acc[:, sub, hs], op=ALU.add),
                                         reads=[dpd_, dacc], writes=[dacc])
                                    P.op("dve", lambda e, sub=sub, hs=hs, pr=pr, xrt=xrt: e.scalar_tensor_tensor(out=pr[:, hs], in0=xrt[:, hs], scalar=ALPHA, in1=acc[:, sub, hs],
                                                                                                              op0=ALU.mult, op1=ALU.add), reads=[dxr, dacc], writes=[dpr])
                                else:
                                    P.op("dve", lambda e, hs=hs, pr=pr, xrt=xrt, pd_=pd_: e.scalar_tensor_tensor(out=pr[:, hs], in0=xrt[:, hs], scalar=ALPHA, in1=pd_[:],
                                                                                                              op0=ALU.mult, op1=ALU.add), reads=[dxr, dpd_], writes=[dpr])
                        if ex_ == nexp - 1:
                            xs, dxs = (xTs[0] if xTs is not None else None), P.dep("e_xTs0")
                            self.ln_tile(L, pr, dpr, xs, dxs, sub, res_dst=out_res[r0:r0 + 128, :], dres=dres)
                if out_T is not None:
                    xs, dxs = xTs[0], P.dep("e_xTs0")
                    P.dma("sp", out_T.rearrange("(c p) t -> p c t", p=128)[:, :, ts_], xs[:], reads=[dxs], writes=[dxT])


_CACHE = {}


def _rope_tables(S):
    pos = np.arange(S, dtype=np.float32)
    inv = (np.float32(10000.0) ** (-np.arange(0, 32, 2, dtype=np.float32) / np.float32(32))).astype(np.float32)
    ang = pos[:, None] * inv[None, :]
    c = np.cos(ang).astype(np.float32).T
    s = np.sin(ang).astype(np.float32).T
    return np.concatenate([c, c], 0), np.concatenate([s, s], 0)


def _get_prog(S, layers, do_ln_in, final_out, dbg=()):
    key = (S, tuple(layers), do_ln_in, final_out, tuple(dbg))
    if key not in _CACHE:
        _CACHE[key] = Builder(S, list(layers), do_ln_in, final_out, dbg).build()
    return _CACHE[key]


WEIGHT_KEYS = ["w_in", "conv_w", "q_norm_g", "w_uq", "kv_norm_g", "w_ukv", "lru_conv_w", "lru_conv_b", "lru_wa", "lru_ba",
               "lru_wi", "lru_bi", "lru_lam", "mix_norm_g", "w_out", "ln1_g", "ln1_b", "ln2_g", "ln2_b"]


def _core_common(S, inputs, half, layers):
    T = S // 2
    C, Sn = _rope_tables(S)
    order = np.concatenate([np.arange(half * T, (half + 1) * T), np.arange((1 - half) * T, (2 - half) * T)])
    flags = np.zeros((128, 2), np.float32)
    flags[:, half] = 1.0
    m = {"flags": flags, "ropeC": np.ascontiguousarray(C[:, order]), "ropeS": np.ascontiguousarray(Sn[:, order])}
    for k in WEIGHT_KEYS:
        m[k] = np.ascontiguousarray(inputs[k])
    if 0 in layers:
        for k in ("dense_w_gate", "dense_w_up", "dense_w_down"):
            m[k] = np.ascontiguousarray(inputs[k])
    if 1 in layers:
        for k in ("moe_w_router", "moe_w_gate", "moe_w_up", "moe_w_down"):
            m[k] = np.ascontiguousarray(inputs[k])
    return m


def kernel(**inputs):
    x = np.asarray(inputs["x"])
    B, S, _ = x.shape
    T = S // 2
    ncore = 2 * B
    key = ("fused", S)
    if key not in _CACHE:
        _CACHE[key] = Builder(S, [0, 1], True, True).build_fused()
    nc = _CACHE[key]
    C, Sn = _rope_tables(S)
    maps = []
    for core in range(ncore):
        b, half = core // 2, core % 2
        order = np.concatenate([np.arange(half * T, (half + 1) * T), np.arange((1 - half) * T, (2 - half) * T)])
        flags = np.zeros((128, 2), np.float32)
        flags[:, half] = 1.0
        m = {"flags": flags, "ropeC0": C, "ropeS0": Sn,
             "ropeC1": np.ascontiguousarray(C[:, order]), "ropeS1": np.ascontiguousarray(Sn[:, order]),
             "x_full": np.ascontiguousarray(x[b]),
             "ln_in_g": np.ascontiguousarray(inputs["ln_in_g"]), "ln_in_b": np.ascontiguousarray(inputs["ln_in_b"])}
        for k in WEIGHT_KEYS + ["dense_w_gate", "dense_w_up", "dense_w_down", "moe_w_router", "moe_w_gate", "moe_w_up", "moe_w_down"]:
            m[k] = np.ascontiguousarray(inputs[k])
        maps.append(m)
    res = run_bass_kernel_spmd(nc, maps, core_ids=list(range(ncore))).results
    out = np.empty((B, S, D), np.float32)
    for core in range(ncore):
        b, half = core // 2, core % 2
        out[b, half * T:(half + 1) * T] = res[core]["y_out"]
    return out


def kernel_unfused(**inputs):
    x = np.asarray(inputs["x"])
    B, S, _ = x.shape
    T = S // 2
    ncore = 2 * B
    nc0 = _get_prog(S, (0,), True, False)
    maps = []
    for core in range(ncore):
        b, half = core // 2, core % 2
        m = _core_common(S, inputs, half, (0,))
        m["x_own"] = np.ascontiguousarray(x[b, half * T:(half + 1) * T])
        m["x_oth"] = np.ascontiguousarray(x[b, (1 - half) * T:(2 - half) * T])
        m["ln_in_g"] = np.ascontiguousarray(inputs["ln_in_g"])
        m["ln_in_b"] = np.ascontiguousarray(inputs["ln_in_b"])
        maps.append(m)
    r0 = run_bass_kernel_spmd(nc0, maps, core_ids=list(range(ncore))).results
    nc1 = _get_prog(S, (1,), False, True)
    maps = []
    for core in range(ncore):
        half = core % 2
        m = _core_common(S, inputs, half, (1,))
        m["xres_in"] = r0[core]["xres_out"]
        m["xT_in"] = np.ascontiguousarray(np.concatenate([r0[core]["xT_out"], r0[core ^ 1]["xT_out"]], axis=1))
        maps.append(m)
    r1 = run_bass_kernel_spmd(nc1, maps, core_ids=list(range(ncore))).results
    out = np.empty((B, S, D), np.float32)
    for core in range(ncore):
        b, half = core // 2, core % 2
        out[b, half * T:(half + 1) * T] = r1[core]["y_out"]
    return out
```
